# Optimizing a Trainium2 kernel written in Bass

```python
import math
import jax
import jax.numpy as jnp
from jax import lax
import numpy as np

D_MODEL = 2048
BATCH = 4
SEQ = 4096
DEPTH = 2

F32 = jnp.float32
N_EVEN = (DEPTH + 1) // 2
N_ODD = DEPTH // 2

DN_ALPHA = (2.0 * DEPTH) ** 0.25
DN_BETA = (8.0 * DEPTH) ** -0.25
LN_EPS = 1e-5
RMS_EPS = 1e-6
NEG_INF = -1e30
MIX_WIDTH = D_MODEL

MLA_HEADS = 8
MLA_NOPE = 128
MLA_ROPE = 64
MLA_V = 128
MLA_QK = MLA_NOPE + MLA_ROPE
MLA_Q_RANK = 512
MLA_KV_RANK = 256
ROPE_THETA = 10000.0
Q_BLOCK = 128

S5_WIDTH = MIX_WIDTH - MLA_HEADS * MLA_V
S5_GROUP = 16
S5_GROUPS = S5_WIDTH // S5_GROUP
S5_STATE = 64
S5_DT_MIN = 1e-3
S5_DT_MAX = 1e-1
EVEN_IN = MLA_Q_RANK + MLA_KV_RANK + MLA_ROPE + S5_WIDTH

DIL_PATTERNS = ((128, 1), (512, 4), (2048, 16))
N_DIL = len(DIL_PATTERNS)
DIL_HEADS = 8
DIL_HEAD_DIM = 64
DIL_WIDTH = DIL_HEADS * DIL_HEAD_DIM
DIL_IN = N_DIL * 3 * DIL_WIDTH

T5_BUCKETS = 32
T5_MAX_DIST = 2048
T5_HEADS = N_DIL * DIL_HEADS

RW_WIDTH = MIX_WIDTH - DIL_WIDTH
RW_HEAD = 64
RW_HEADS = RW_WIDTH // RW_HEAD
RW_LORA_W = 64
RW_LORA_A = 64
RW_LORA_G = 224
RW_GN_EPS = 64e-5
RW_IN = 3 * RW_WIDTH + RW_LORA_W + RW_LORA_A + RW_LORA_G
RW_SPLITS = [RW_WIDTH, 2 * RW_WIDTH, 3 * RW_WIDTH, 3 * RW_WIDTH + RW_LORA_W, 3 * RW_WIDTH + RW_LORA_W + RW_LORA_A]
ODD_IN = DIL_IN + RW_IN

N_EXPERTS = 16
N_EXPERT_GROUPS = 4
EXPERTS_PER_GROUP = N_EXPERTS // N_EXPERT_GROUPS
TOP_K = 2
D_EXPERT = 512

kernel_name = "hybrid_mla_s5_dilated_rwkv7_moe"


def _layernorm(x, g, b):
    xf = x.astype(F32)
    mu = xf.mean(-1, keepdims=True)
    var = jnp.square(xf - mu).mean(-1, keepdims=True)
    return (xf - mu) * lax.rsqrt(var + LN_EPS) * g + b


def _rmsnorm(x, g):
    xf = x.astype(F32)
    return xf * lax.rsqrt(jnp.mean(xf * xf, -1, keepdims=True) + RMS_EPS) * g


def _rope_tables(seq):
    inv = ROPE_THETA ** (-jnp.arange(0, MLA_ROPE, 2, dtype=F32) / MLA_ROPE)
    ang = jnp.arange(seq, dtype=F32)[:, None] * inv[None]
    return jnp.cos(ang), jnp.sin(ang)


def _rope(x, cos, sin):
    x1, x2 = jnp.split(x.astype(F32), 2, axis=-1)
    return jnp.concatenate([x1 * cos - x2 * sin, x1 * sin + x2 * cos], axis=-1)


def _mla(q_c, kv_c, k_r, q_norm, w_uq, kv_norm, w_ukv):
    B_, S_, _ = q_c.shape
    q = (_rmsnorm(q_c, q_norm) @ w_uq).astype(F32).reshape(B_, S_, MLA_HEADS, MLA_QK)
    kv = (_rmsnorm(kv_c, kv_norm) @ w_ukv).astype(F32).reshape(B_, S_, MLA_HEADS, MLA_NOPE + MLA_V)
    cos, sin = _rope_tables(S_)
    q_nope = q[..., :MLA_NOPE]
    q_rope = _rope(q[..., MLA_NOPE:], cos[None, :, None], sin[None, :, None])
    k_rope = _rope(k_r, cos[None], sin[None])
    k_nope, v = kv[..., :MLA_NOPE], kv[..., MLA_NOPE:]
    scale = MLA_QK ** -0.5
    outs = []
    for i in range(S_ // Q_BLOCK):
        q0, q1 = i * Q_BLOCK, (i + 1) * Q_BLOCK
        s = (jnp.einsum('bqhc,bkhc->bhqk', q_nope[:, q0:q1], k_nope[:, :q1])
             + jnp.einsum('bqhr,bkr->bhqk', q_rope[:, q0:q1], k_rope[:, :q1])) * scale
        causal = jnp.arange(q1)[None, :] <= jnp.arange(q0, q1)[:, None]
        p = jax.nn.softmax(jnp.where(causal, s, NEG_INF), axis=-1)
        outs.append(jnp.einsum('bhqk,bkhd->bqhd', p, v[:, :q1]))
    return jnp.concatenate(outs, axis=1).reshape(B_, S_, MLA_HEADS * MLA_V)


def _lin_rec(left, right):
    a_l, b_l = left
    a_r, b_r = right
    return a_l * a_r, a_r * b_l + b_r


def _s5(u, lam_re, lam_im, b_re, b_im, c_re, c_im, d_skip, log_dt, w_glu):
    B_, S_, _ = u.shape
    uf = u.astype(F32).reshape(B_, S_, S5_GROUPS, S5_GROUP)
    lam = lax.complex(lam_re.astype(F32), lam_im.astype(F32))
    dt = jnp.exp(log_dt.astype(F32))[:, None]
    lam_bar = jnp.exp(lam * dt)
    b_bar = ((lam_bar - 1.0) / lam)[..., None] * lax.complex(b_re.astype(F32), b_im.astype(F32))
    bu = jnp.einsum('bsgc,gpc->bsgp', uf.astype(jnp.complex64), b_bar)
    a = jnp.broadcast_to(lam_bar[None, None], (1, S_, S5_GROUPS, S5_STATE))
    _, states = lax.associative_scan(_lin_rec, (a, bu), axis=1)
    c = lax.complex(c_re.astype(F32), c_im.astype(F32))
    y = jnp.einsum('bsgp,gcp->bsgc', states, c).real + d_skip.astype(F32) * uf
    y = jax.nn.gelu(y.reshape(B_, S_, S5_WIDTH))
    return y * jax.nn.sigmoid(y @ w_glu)


def _t5_bucket(dist):
    exact = T5_BUCKETS // 2
    logd = jnp.log(jnp.maximum(dist, 1).astype(F32) / exact) / math.log(T5_MAX_DIST / exact)
    large = jnp.minimum(exact + (logd * (T5_BUCKETS - exact)).astype(jnp.int32), T5_BUCKETS - 1)
    return jnp.where(dist < exact, dist, large)


def _dilated_branch(q, k, v, bias_tab, dil, span):
    B_, S_, H, Dh = q.shape
    L = S_ // dil
    nb = -(-L // span)
    Lp = nb * span

    def streams(t):
        return t.reshape(B_, L, dil, H, Dh).transpose(0, 2, 1, 3, 4)

    qb = jnp.pad(streams(q), ((0, 0), (0, 0), (0, Lp - L), (0, 0), (0, 0))).reshape(B_, dil, nb, span, H, Dh)

    def band_blocks(t):
        t = jnp.pad(streams(t), ((0, 0), (0, 0), (span, Lp - L), (0, 0), (0, 0))).reshape(B_, dil, nb + 1, span, H, Dh)
        return jnp.concatenate([t[:, :, :-1], t[:, :, 1:]], axis=3)

    kb, vb = band_blocks(k), band_blocks(v)
    qi = jnp.arange(span)[:, None]
    ku = jnp.arange(2 * span)[None, :]
    delta = span + qi - ku
    key_pos = (jnp.arange(nb)[:, None, None] - 1) * span + ku[None]
    valid = ((delta >= 0) & (delta <= span))[None] & (key_pos >= 0)
    bias = bias_tab.astype(F32)[_t5_bucket(jnp.clip(delta, 0, span) * dil)]
    s = jnp.einsum('bgnihc,bgnuhc->bgnihu', qb, kb) * (Dh ** -0.5) + bias.transpose(0, 2, 1)[None, None, None]
    s = jnp.where(valid[None, None, :, :, None, :], s, NEG_INF)
    m = s.max(-1)
    p = jnp.exp(s - m[..., None])
    l = p.sum(-1)
    acc = jnp.einsum('bgnihu,bgnuhc->bgnihc', p, vb)

    def unstream(t):
        t = t.reshape(B_, dil, Lp, *t.shape[4:])[:, :, :L]
        return jnp.swapaxes(t, 1, 2).reshape(B_, S_, *t.shape[3:])

    return unstream(acc), unstream(m), unstream(l)


def _dilated_mixture(zc, rel_bias):
    B_, S_, _ = zc.shape
    z = zc.astype(F32).reshape(B_, S_, N_DIL, 3, DIL_HEADS, DIL_HEAD_DIM)
    accs, ms, ls = [], [], []
    for gi, (win, dil) in enumerate(DIL_PATTERNS):
        acc, m, l = _dilated_branch(z[:, :, gi, 0], z[:, :, gi, 1], z[:, :, gi, 2],
                                    rel_bias[:, gi * DIL_HEADS:(gi + 1) * DIL_HEADS], dil, win // dil)
        accs.append(acc)
        ms.append(m)
        ls.append(l)
    m_all = jnp.stack(ms)
    wts = jnp.exp(m_all - m_all.max(0))
    num = sum(wts[i][..., None] * accs[i] for i in range(N_DIL))
    den = (wts * jnp.stack(ls)).sum(0)
    return (num / den[..., None]).reshape(B_, S_, DIL_WIDTH)


def _rwkv7(zr, mu, w0, w2, a0, a2, g2, k_k, k_a, r_k, lnx_g, lnx_b):
    B_, S_, _ = zr.shape
    zf = zr.astype(F32)
    prev = jnp.pad(zf[:, :-1], ((0, 0), (1, 0), (0, 0)))
    xs = zf + (prev - zf) * mu
    r, k, v, wd, ad, gd = jnp.split(xs, RW_SPLITS, axis=-1)
    w_log = -jax.nn.softplus(-(w0 + jnp.tanh(wd) @ w2)) - 0.5
    decay = jnp.exp(-jnp.exp(w_log))
    a = jax.nn.sigmoid(a0 + ad @ a2)
    g = jax.nn.sigmoid(gd) @ g2

    def heads(t):
        return t.reshape(B_, S_, RW_HEADS, RW_HEAD)

    kk = heads(k * k_k)
    kk = kk / jnp.maximum(jnp.sqrt(jnp.sum(kk * kk, -1, keepdims=True)), 1e-12)
    k = k * (1.0 + (a - 1.0) * k_a)
    r_h, k_h, v_h, w_h, a_h = heads(r), heads(k), heads(v), heads(decay), heads(a)
    b_h = kk * a_h

    def step(state, inp):
        rt, wt, kt, vt, at, bt = inp
        sa = jnp.einsum('bhij,bhj->bhi', state, at)
        state = state * wt[:, :, None, :] + sa[..., None] * bt[:, :, None, :] + vt[..., None] * kt[:, :, None, :]
        return state, jnp.einsum('bhij,bhj->bhi', state, rt)

    init = jnp.zeros((B_, RW_HEADS, RW_HEAD, RW_HEAD), F32)
    seq = (r_h, w_h, k_h, v_h, -kk, b_h)
    _, y = lax.scan(step, init, tuple(jnp.moveaxis(t, 1, 0) for t in seq))
    y = jnp.moveaxis(y, 0, 1)
    mean = y.mean(-1, keepdims=True)
    var = jnp.square(y - mean).mean(-1, keepdims=True)
    y = ((y - mean) * lax.rsqrt(var + RW_GN_EPS)).reshape(B_, S_, RW_WIDTH) * lnx_g + lnx_b
    bonus = jnp.sum(r_h * k_h * r_k, -1, keepdims=True) * v_h
    y = y + bonus.reshape(B_, S_, RW_WIDTH)
    return y * g


def _moe(h, router_w, router_b, w_gate_up, w_down):
    B_, S_, D = h.shape
    t = h.reshape(B_ * S_, D)
    s = jax.nn.sigmoid((t @ router_w).astype(F32))
    sb = s + router_b.astype(F32)
    sbg = sb.reshape(-1, N_EXPERT_GROUPS, EXPERTS_PER_GROUP)
    gscore = lax.top_k(sbg, TOP_K)[0].sum(-1)
    gmask = jax.nn.one_hot(jnp.argmax(gscore, -1), N_EXPERT_GROUPS, dtype=F32)
    masked = jnp.where(gmask[..., None] > 0, sbg, NEG_INF).reshape(-1, N_EXPERTS)
    _, eidx = lax.top_k(masked, TOP_K)
    s_sel = jnp.take_along_axis(s, eidx, axis=1)
    gw = s_sel / s_sel.sum(-1, keepdims=True)
    gates = (jax.nn.one_hot(eidx, N_EXPERTS, dtype=F32) * gw[..., None]).sum(1)
    y = jnp.zeros((B_ * S_, D), F32)
    for e in range(N_EXPERTS):
        gt, up = jnp.split(t @ w_gate_up[e], 2, axis=-1)
        y = y + gates[:, e:e + 1] * ((jax.nn.silu(gt) * up) @ w_down[e])
    return y.reshape(B_, S_, D)


def setup_inputs(seed: int = 0) -> dict:
    key = jax.random.key(seed)
    ks = iter(jax.random.split(key, 64))
    D = D_MODEL

    def nrm(shape, scale):
        return jax.random.normal(next(ks), shape, F32) * scale

    def gain(shape):
        return 1.0 + nrm(shape, 0.01)

    def unif(shape, lo, hi):
        return jax.random.uniform(next(ks), shape, F32, minval=lo, maxval=hi)

    return {
        "x": nrm((BATCH, SEQ, D), 1.0),
        "c": nrm((BATCH, D), 1.0),
        "ada_w": nrm((DEPTH, D, 6 * D), 0.1 * D ** -0.5),
        "ada_b": nrm((DEPTH, 6 * D), 0.01),
        "ln_mix_g": gain((DEPTH, D)),
        "ln_mix_b": nrm((DEPTH, D), 0.01),
        "ln_ffn_g": gain((DEPTH, D)),
        "ln_ffn_b": nrm((DEPTH, D), 0.01),
        "router_w": nrm((D, N_EXPERTS), D ** -0.5),
        "router_b": nrm((N_EXPERTS,), 0.01),
        "moe_w_gate_up": nrm((DEPTH, N_EXPERTS, D, 2 * D_EXPERT), D ** -0.5),
        "moe_w_down": nrm((DEPTH, N_EXPERTS, D_EXPERT, D), DN_BETA * D_EXPERT ** -0.5),
        "rel_bias": nrm((T5_BUCKETS, T5_HEADS), 0.5),
        "ev_w_in": nrm((N_EVEN, D, EVEN_IN), D ** -0.5),
        "mla_q_norm": gain((N_EVEN, MLA_Q_RANK)),
        "mla_w_uq": nrm((N_EVEN, MLA_Q_RANK, MLA_HEADS * MLA_QK), MLA_Q_RANK ** -0.5),
        "mla_kv_norm": gain((N_EVEN, MLA_KV_RANK)),
        "mla_w_ukv": nrm((N_EVEN, MLA_KV_RANK, MLA_HEADS * (MLA_NOPE + MLA_V)), MLA_KV_RANK ** -0.5),
        "s5_lambda_re": -0.5 + nrm((N_EVEN, S5_GROUPS, S5_STATE), 0.01),
        "s5_lambda_im": jnp.tile(jnp.pi * jnp.arange(S5_STATE, dtype=F32), (N_EVEN, S5_GROUPS, 1)),
        "s5_b_re": nrm((N_EVEN, S5_GROUPS, S5_STATE, S5_GROUP), (2 * S5_GROUP) ** -0.5),
        "s5_b_im": nrm((N_EVEN, S5_GROUPS, S5_STATE, S5_GROUP), (2 * S5_GROUP) ** -0.5),
        "s5_c_re": nrm((N_EVEN, S5_GROUPS, S5_GROUP, S5_STATE), S5_STATE ** -0.5),
        "s5_c_im": nrm((N_EVEN, S5_GROUPS, S5_GROUP, S5_STATE), S5_STATE ** -0.5),
        "s5_d": nrm((N_EVEN, S5_GROUPS, S5_GROUP), 1.0),
        "s5_log_dt": unif((N_EVEN, S5_GROUPS), math.log(S5_DT_MIN), math.log(S5_DT_MAX)),
        "s5_w_glu": nrm((N_EVEN, S5_WIDTH, S5_WIDTH), S5_WIDTH ** -0.5),
        "ev_w_out": nrm((N_EVEN, MIX_WIDTH, D), DN_BETA * MIX_WIDTH ** -0.5),
        "od_w_in": nrm((N_ODD, D, ODD_IN), D ** -0.5),
        "rw_mu": unif((N_ODD, RW_IN), 0.0, 1.0),
        "rw_w0": unif((N_ODD, RW_WIDTH), -5.0, 1.0),
        "rw_w2": nrm((N_ODD, RW_LORA_W, RW_WIDTH), 0.1 * RW_LORA_W ** -0.5),
        "rw_a0": nrm((N_ODD, RW_WIDTH), 0.1),
        "rw_a2": nrm((N_ODD, RW_LORA_A, RW_WIDTH), 0.1 * RW_LORA_A ** -0.5),
        "rw_g2": nrm((N_ODD, RW_LORA_G, RW_WIDTH), RW_LORA_G ** -0.5),
        "rw_k_k": 0.85 + nrm((N_ODD, RW_WIDTH), 0.05),
        "rw_k_a": gain((N_ODD, RW_WIDTH)),
        "rw_r_k": nrm((N_ODD, RW_HEADS, RW_HEAD), 0.1),
        "rw_lnx_g": gain((N_ODD, RW_WIDTH)),
        "rw_lnx_b": nrm((N_ODD, RW_WIDTH), 0.01),
        "od_w_out": nrm((N_ODD, MIX_WIDTH, D), DN_BETA * MIX_WIDTH ** -0.5),
    }


def reference(x, c, ada_w, ada_b, ln_mix_g, ln_mix_b, ln_ffn_g, ln_ffn_b, router_w, router_b,
              moe_w_gate_up, moe_w_down, rel_bias, ev_w_in, mla_q_norm, mla_w_uq, mla_kv_norm, mla_w_ukv,
              s5_lambda_re, s5_lambda_im, s5_b_re, s5_b_im, s5_c_re, s5_c_im, s5_d, s5_log_dt, s5_w_glu,
              ev_w_out, od_w_in, rw_mu, rw_w0, rw_w2, rw_a0, rw_a2, rw_g2, rw_k_k, rw_k_a, rw_r_k,
              rw_lnx_g, rw_lnx_b, od_w_out):
    dt = x.dtype
    cond = jax.nn.silu(c.astype(F32))
    q_split = [MLA_Q_RANK, MLA_Q_RANK + MLA_KV_RANK, MLA_Q_RANK + MLA_KV_RANK + MLA_ROPE]
    for layer in range(DEPTH):
        mod = (cond @ ada_w[layer] + ada_b[layer])[:, None, :]
        sh_m, sc_m, g_m, sh_f, sc_f, g_f = jnp.split(mod, 6, axis=-1)
        h = (x * (1.0 + sc_m) + sh_m).astype(dt)
        if layer % 2 == 0:
            e = layer // 2
            q_c, kv_c, k_r, u = jnp.split(h @ ev_w_in[e], q_split, axis=-1)
            att = _mla(q_c, kv_c, k_r, mla_q_norm[e], mla_w_uq[e], mla_kv_norm[e], mla_w_ukv[e])
            ssm = _s5(u, s5_lambda_re[e], s5_lambda_im[e], s5_b_re[e], s5_b_im[e], s5_c_re[e], s5_c_im[e],
                      s5_d[e], s5_log_dt[e], s5_w_glu[e])
            y = jnp.concatenate([att, ssm], axis=-1).astype(dt) @ ev_w_out[e]
        else:
            o = layer // 2
            z = h @ od_w_in[o]
            att = _dilated_mixture(z[..., :DIL_IN], rel_bias)
            tm = _rwkv7(z[..., DIL_IN:], rw_mu[o], rw_w0[o], rw_w2[o], rw_a0[o], rw_a2[o], rw_g2[o],
                        rw_k_k[o], rw_k_a[o], rw_r_k[o], rw_lnx_g[o], rw_lnx_b[o])
            y = jnp.concatenate([att, tm], axis=-1).astype(dt) @ od_w_out[o]
        x = _layernorm(DN_ALPHA * x + (1.0 + g_m) * y, ln_mix_g[layer], ln_mix_b[layer]).astype(dt)
        h = (x * (1.0 + sc_f) + sh_f).astype(dt)
        y = _moe(h, router_w, router_b, moe_w_gate_up[layer], moe_w_down[layer])
        x = _layernorm(DN_ALPHA * x + (1.0 + g_f) * y, ln_ffn_g[layer], ln_ffn_b[layer]).astype(dt)
    return x
```

```python
import contextlib
import numpy as np
import concourse.bass as bass
import concourse.mybir as mybir
from concourse.bass_utils import run_bass_kernel_spmd

F32 = mybir.dt.float32
BF16 = mybir.dt.bfloat16
AF = mybir.ActivationFunctionType
ALU = mybir.AluOpType
AX = mybir.AxisListType

D = 2048
NCORES = 8
DN_ALPHA = 4.0 ** 0.25
LN_EPS = 1e-5


class Buf:
    __slots__ = ("w", "r", "dsem", "dval")

    def __init__(self):
        self.w = None
        self.r = {}
        self.dsem = None
        self.dval = 0


def bufs(n):
    return [Buf() for _ in range(n)]


class KB:
    def __init__(self):
        self.nc = bass.Bass("TRN2", target_bir_lowering=False)
        nc = self.nc
        self.eng = {"pe": nc.tensor, "act": nc.scalar, "dve": nc.vector, "pool": nc.gpsimd, "sp": nc.sync}
        self.stack = contextlib.ExitStack()
        self.sem, self.seq, self.seen = {}, {}, {}
        for e in self.eng:
            self.sem[e] = self.stack.enter_context(nc.semaphore("s_" + e))
            self.seq[e] = 0
            self.seen[e] = {}
        self.nsem = 0
        self.finals = []
        self.nm = 0

    def name(self, p):
        self.nm += 1
        return "%s%d" % (p, self.nm)

    def din(self, name, shape, dt=F32):
        return self.nc.dram_tensor(name, list(shape), dt, kind="ExternalInput").ap()

    def dout(self, name, shape, dt=F32):
        return self.nc.dram_tensor(name, list(shape), dt, kind="ExternalOutput").ap()

    def sb(self, shape, dt=F32, name=None):
        return self.stack.enter_context(self.nc.sbuf_tensor(name or self.name("sb"), list(shape), dt))

    def ps(self, shape, dt=F32, name=None):
        return self.stack.enter_context(self.nc.psum_tensor(name or self.name("ps"), list(shape), dt))

    def _wait(self, e, ev):
        sem, val, src = ev
        key = id(sem)
        if self.seen[e].get(key, 0) >= val:
            return
        if src == e and e == "pe":
            return
        self.eng[e].wait_ge(sem, val)
        self.seen[e][key] = val

    def _deps(self, e, r, w):
        for b in r:
            if b.w is not None:
                self._wait(e, b.w)
        for b in w:
            if b.w is not None:
                self._wait(e, b.w)
            for ev in b.r.values():
                self._wait(e, ev)

    def _record(self, ev, r, w):
        for b in r:
            b.r[id(ev[0])] = ev
        for b in w:
            b.w = ev
            b.r = {}

    def op(self, e, fn, r=(), w=()):
        self._deps(e, r, w)
        ins = fn(self.eng[e])
        self.seq[e] += 1
        ins.then_inc(self.sem[e], 1)
        self._record((self.sem[e], self.seq[e], e), r, w)
        return ins

    def dma(self, out, in_, r=(), w=(), q="sp", final=False):
        self._deps(q, r, w)
        ins = self.eng[q].dma_start(out=out, in_=in_)
        owner = w[0] if w else r[0]
        if owner.dsem is None:
            owner.dsem = self.stack.enter_context(self.nc.semaphore(self.name("sd")))
            self.nsem += 1
        owner.dval += 16
        ins.then_inc(owner.dsem, 16)
        ev = (owner.dsem, owner.dval, "dma")
        self._record(ev, r, w)
        if final:
            self.finals.append(ev)
        return ins

    def finish(self):
        for ev in self.finals:
            self._wait("sp", ev)
        self.stack.close()
        return self.nc


def mm(kb, out, lhsT, rhs, start, stop, r, w):
    return kb.op("pe", lambda e: e.matmul(out, lhsT=lhsT, rhs=rhs, start=start, stop=stop), r=r, w=w)


TT = 512


def build_post(layer0, ntok=2048):
    kb = KB()
    nc = kb.nc
    NT = ntok // TT
    mixT = kb.din("mixT", [D, ntok]).rearrange("(c p) t -> p c t", p=128)
    xT = kb.din("xT", [D, ntok]).rearrange("(c p) t -> p c t", p=128)
    wout = kb.din("wout", [D, D]).rearrange("(c p) n -> p c n", p=128)
    if layer0:
        wglu = kb.din("wglu", [1024, 1024]).rearrange("(c p) n -> p c n", p=128)
    pvec_d = kb.din("pvec", [128, 8 * 16])
    rw_d = kb.din("router_w", [D, 16]).rearrange("(c p) n -> p c n", p=128)
    rb_d = kb.din("router_b_bc", [128, 16])
    wgu = kb.din("wgu", [16, D, 1024])
    wd = kb.din("wd", [16, 512, D])
    ident_d = kb.din("ident", [128, 128])
    sel_d = kb.din("sel", [16, 16 * 128])
    xoT = kb.dout("xoT", [D, ntok]).rearrange("(c p) t -> p c t", p=128)

    xt = kb.sb([128, 16, TT]); xt_b = bufs(16)
    yacc = kb.sb([128, 16, TT]); yacc_b = bufs(16)
    mixb = kb.sb([128, 16, TT], BF16); mixb_b = bufs(16)
    hT = kb.sb([128, 16, TT], BF16); hT_b = bufs(16)
    NSTG = 3
    stg = [kb.sb([128, 4096]) for _ in range(NSTG)]; stg_b = bufs(NSTG)
    NWB = 3
    wb = [kb.sb([128, 4096], BF16) for _ in range(NWB)]; wb_b = bufs(NWB)
    aT = kb.sb([128, 4, TT], BF16); aT_b = bufs(4)
    tmp = [kb.sb([128, TT]) for _ in range(4)]; tmp_b = bufs(4)
    h32 = [kb.sb([128, TT]) for _ in range(2)]; h32_b = bufs(2)
    mean = kb.sb([128, TT]); mean_b = Buf()
    rstd = kb.sb([128, TT]); rstd_b = Buf()
    pvec = kb.sb([128, 8 * 16]); pvec_b = Buf()
    pv1 = kb.sb([128, 8 * 16]); pv1_b = Buf()
    rw = kb.sb([128, 16, 16]); rw_b = Buf()
    rb = kb.sb([128, 16]); rb_b = Buf()
    ident = kb.sb([128, 128]); ident_b = Buf()
    ones = kb.sb([128, 128]); ones_b = Buf()
    sel = kb.sb([16, 16 * 128]); sel_b = Buf()
    lgT = kb.sb([16, TT]); lgT_b = Buf()
    gatesT = kb.sb([16, TT]); gatesT_b = Buf()
    R = {n: kb.sb([128, 4, 16], name="r_" + n) for n in ["s", "sb", "masked", "m2", "sel1", "sel2", "ssel", "gates"]}
    R_b = {n: Buf() for n in R}
    S4 = {n: kb.sb([128, 16], name="q_" + n) for n in ["p0", "p1", "gscore", "gmask", "pen"]}
    S4_b = {n: Buf() for n in S4}
    S1 = {n: kb.sb([128, 4], name="o_" + n) for n in ["gmax", "m1", "m2", "den", "rden"]}
    S1_b = {n: Buf() for n in S1}

    pbank = [kb.ps([128, TT]) for _ in range(8)]; pb = bufs(8)

    stg_i = [0]; wb_i = [0]; tmp_i = [0]

    def nxt(ctr, n):
        i = ctr[0] % n
        ctr[0] += 1
        return i

    kb.dma(pvec[:], pvec_d[:, :], w=[pvec_b])
    kb.dma(rw[:], rw_d[:, :, :], w=[rw_b])
    kb.dma(rb[:], rb_d[:, :], w=[rb_b])
    kb.dma(ident[:], ident_d[:, :], w=[ident_b])
    kb.dma(sel[:], sel_d[:, :], w=[sel_b])
    kb.op("dve", lambda e: e.memset(ones[:], 1.0), w=[ones_b])
    kb.op("dve", lambda e: e.tensor_scalar_add(pv1[:], pvec[:], 1.0), r=[pvec_b], w=[pv1_b])

    def pcol(which, c, plus1=False):
        t = pv1 if plus1 else pvec
        return t[:, which * 16 + c: which * 16 + c + 1]

    def load_weight(src_ap_fn, ncols):
        si = nxt(stg_i, NSTG)
        src_ap_fn(stg[si], stg_b[si])
        wi = nxt(wb_i, NWB)
        kb.op("pool", lambda e: e.tensor_copy(out=wb[wi][:, :ncols], in_=stg[si][:, :ncols]), r=[stg_b[si]], w=[wb_b[wi]])
        return wb[wi], wb_b[wi]

    def layernorm(gi, bi, emit_h, out_dma_t0=None):
        ps_sum, ps_sq = pbank[6], pbank[7]
        for c in range(16):
            ti = nxt(tmp_i, 4)
            kb.op("act", lambda e: e.activation(out=tmp[ti][:], in_=xt[:, c, :], func=AF.Square), r=[xt_b[c]], w=[tmp_b[ti]])
            mm(kb, ps_sum[:], ones[:], xt[:, c, :], c == 0, c == 15, r=[ones_b, xt_b[c]], w=[pb[6]])
            mm(kb, ps_sq[:], ones[:], tmp[ti][:], c == 0, c == 15, r=[ones_b, tmp_b[ti]], w=[pb[7]])
        kb.op("act", lambda e: e.mul(mean[:], ps_sum[:], 1.0 / D), r=[pb[6]], w=[mean_b])
        ti = nxt(tmp_i, 4)
        kb.op("dve", lambda e: e.tensor_tensor(out=tmp[ti][:], in0=mean[:], in1=mean[:], op=ALU.mult), r=[mean_b], w=[tmp_b[ti]])
        kb.op("dve", lambda e: e.scalar_tensor_tensor(out=rstd[:], in0=ps_sq[:], scalar=1.0 / D, in1=tmp[ti][:], op0=ALU.mult, op1=ALU.subtract),
              r=[pb[7], tmp_b[ti]], w=[rstd_b])
        kb.op("dve", lambda e: e.tensor_scalar_add(rstd[:], rstd[:], LN_EPS), r=[rstd_b], w=[rstd_b])
        kb.op("act", lambda e: e.sqrt(rstd[:], rstd[:]), r=[rstd_b], w=[rstd_b])
        kb.op("dve", lambda e: e.reciprocal(rstd[:], rstd[:]), r=[rstd_b], w=[rstd_b])
        for c in range(16):
            kb.op("dve", lambda e: e.tensor_tensor(out=xt[:, c, :], in0=xt[:, c, :], in1=mean[:], op=ALU.subtract), r=[xt_b[c], mean_b], w=[xt_b[c]])
            kb.op("dve", lambda e: e.tensor_tensor(out=xt[:, c, :], in0=xt[:, c, :], in1=rstd[:], op=ALU.mult), r=[xt_b[c], rstd_b], w=[xt_b[c]])
            kb.op("act", lambda e: e.activation(out=xt[:, c, :], in_=xt[:, c, :], func=AF.Identity, scale=pcol(gi, c), bias=pcol(bi, c)),
                  r=[xt_b[c], pvec_b], w=[xt_b[c]])
            if emit_h:
                hi = c % 2
                kb.op("dve", lambda e: e.tensor_scalar(out=h32[hi][:], in0=xt[:, c, :], scalar1=pcol(1, c, True), scalar2=pcol(2, c), op0=ALU.mult, op1=ALU.add),
                      r=[xt_b[c], pvec_b, pv1_b], w=[h32_b[hi]])
                kb.op("act", lambda e: e.copy(hT[:, c, :], h32[hi][:]), r=[h32_b[hi]], w=[hT_b[c]])
                mm(kb, pbank[5][0:16, :], rw[:, c, :], h32[hi][:], c == 0, c == 15, r=[rw_b, h32_b[hi]], w=[pb[5]])
            if out_dma_t0 is not None:
                kb.dma(xoT[:, c, out_dma_t0:out_dma_t0 + TT], xt[:, c, :], r=[xt_b[c]], final=True)

    for tt in range(NT):
        t0 = tt * TT
        kb.dma(xt[:], xT[:, :, t0:t0 + TT], w=xt_b)
        for c in range(16):
            kb.op("act", lambda e: e.mul(xt[:, c, :], xt[:, c, :], DN_ALPHA), r=[xt_b[c]], w=[xt_b[c]])
        for q in range(4):
            si = nxt(stg_i, NSTG)
            sv = stg[si][:, :4 * TT].rearrange("p (c t) -> p c t", c=4)
            kb.dma(sv, mixT[:, 4 * q:4 * q + 4, t0:t0 + TT], w=[stg_b[si]])
            for cc in range(4):
                c = 4 * q + cc
                if layer0 and c >= 8:
                    ti = nxt(tmp_i, 4)
                    kb.op("dve", lambda e: e.tensor_tensor(out=tmp[ti][:], in0=sv[:, cc, :], in1=sv[:, cc, :], op=ALU.mult), r=[stg_b[si]], w=[tmp_b[ti]])
                    kb.op("dve", lambda e: e.tensor_scalar(out=tmp[ti][:], in0=tmp[ti][:], scalar1=0.044715, scalar2=1.0, op0=ALU.mult, op1=ALU.add), r=[tmp_b[ti]], w=[tmp_b[ti]])
                    kb.op("dve", lambda e: e.tensor_tensor(out=tmp[ti][:], in0=tmp[ti][:], in1=sv[:, cc, :], op=ALU.mult), r=[tmp_b[ti], stg_b[si]], w=[tmp_b[ti]])
                    kb.op("act", lambda e: e.activation(out=tmp[ti][:], in_=tmp[ti][:], func=AF.Sigmoid, scale=1.5957691216), r=[tmp_b[ti]], w=[tmp_b[ti]])
                    kb.op("dve", lambda e: e.tensor_tensor(out=hT[:, c, :], in0=tmp[ti][:], in1=sv[:, cc, :], op=ALU.mult), r=[tmp_b[ti], stg_b[si]], w=[hT_b[c]])
                else:
                    kb.op("pool", lambda e: e.tensor_copy(out=mixb[:, c, :], in_=sv[:, cc, :]), r=[stg_b[si]], w=[mixb_b[c]])
        if layer0:
            for m in range(8):
                def ld(st, sbuf_, m=m):
                    kb.dma(st[:, :8 * 128].rearrange("p (c n) -> p c n", c=8), wglu[:, :, m * 128:(m + 1) * 128], w=[sbuf_])
                wt, wtb = load_weight(ld, 8 * 128)
                pi = m % 2
                for k in range(8):
                    mm(kb, pbank[pi][:], wt[:, k * 128:(k + 1) * 128], hT[:, 8 + k, :], k == 0, k == 7, r=[wtb, hT_b[8 + k]], w=[pb[pi]])
                ti = nxt(tmp_i, 4)
                kb.op("act", lambda e: e.activation(out=tmp[ti][:], in_=pbank[pi][:], func=AF.Sigmoid), r=[pb[pi]], w=[tmp_b[ti]])
                kb.op("dve", lambda e: e.tensor_tensor(out=mixb[:, 8 + m, :], in0=tmp[ti][:], in1=hT[:, 8 + m, :], op=ALU.mult), r=[tmp_b[ti], hT_b[8 + m]], w=[mixb_b[8 + m]])
        for m in range(16):
            def ld(st, sbuf_, m=m):
                kb.dma(st[:, :16 * 128].rearrange("p (c n) -> p c n", c=16), wout[:, :, m * 128:(m + 1) * 128], w=[sbuf_])
            wt, wtb = load_weight(ld, 16 * 128)
            pi = m % 2
            for k in range(16):
                mm(kb, pbank[pi][:], wt[:, k * 128:(k + 1) * 128], mixb[:, k, :], k == 0, k == 15, r=[wtb, mixb_b[k]], w=[pb[pi]])
            kb.op("dve", lambda e: e.scalar_tensor_tensor(out=xt[:, m, :], in0=pbank[pi][:], scalar=pcol(0, m, True), in1=xt[:, m, :], op0=ALU.mult, op1=ALU.add),
                  r=[pb[pi], pv1_b, xt_b[m]], w=[xt_b[m]])
        layernorm(4, 5, True)
        kb.op("act", lambda e: e.copy(lgT[:], pbank[5][0:16, :]), r=[pb[5]], w=[lgT_b])
        for s in range(4):
            kb.op("pe", lambda e: e.transpose(pbank[4][:, s * 16:(s + 1) * 16], lgT[:, s * 128:(s + 1) * 128], ident[0:16, 0:16]), r=[lgT_b, ident_b], w=[pb[4]])
        lg = pbank[4][:, 0:64].rearrange("p (s e) -> p s e", s=4)
        kb.op("act", lambda e: e.activation(out=R["s"][:], in_=lg, func=AF.Sigmoid), r=[pb[4]], w=[R_b["s"]])
        rb_bc = rb[:].rearrange("p (o e) -> p o e", o=1).to_broadcast([128, 4, 16])
        kb.op("dve", lambda e: e.tensor_tensor(out=R["sb"][:], in0=R["s"][:], in1=rb_bc, op=ALU.add), r=[R_b["s"], rb_b], w=[R_b["sb"]])
        sbg = R["sb"][:].rearrange("p s (g k) -> p (s g) k", k=4)
        first = True
        for (i, j) in [(0, 1), (0, 2), (0, 3), (1, 2), (1, 3), (2, 3)]:
            if first:
                kb.op("dve", lambda e: e.tensor_tensor(out=S4["gscore"][:], in0=sbg[:, :, i], in1=sbg[:, :, j], op=ALU.add), r=[R_b["sb"]], w=[S4_b["gscore"]])
                first = False
            else:
                kb.op("dve", lambda e: e.tensor_tensor(out=S4["p0"][:], in0=sbg[:, :, i], in1=sbg[:, :, j], op=ALU.add), r=[R_b["sb"]], w=[S4_b["p0"]])
                kb.op("dve", lambda e: e.tensor_tensor(out=S4["gscore"][:], in0=S4["gscore"][:], in1=S4["p0"][:], op=ALU.max), r=[S4_b["p0"], S4_b["gscore"]], w=[S4_b["gscore"]])
        gs3 = S4["gscore"][:].rearrange("p (s g) -> p s g", g=4)
        kb.op("dve", lambda e: e.tensor_reduce(out=S1["gmax"][:], in_=gs3, axis=AX.X, op=ALU.max), r=[S4_b["gscore"]], w=[S1_b["gmax"]])
        gmax_bc = S1["gmax"][:].rearrange("p (s o) -> p s o", o=1).to_broadcast([128, 4, 4])
        gm3 = S4["gmask"][:].rearrange("p (s g) -> p s g", g=4)
        kb.op("dve", lambda e: e.tensor_tensor(out=gm3, in0=gs3, in1=gmax_bc, op=ALU.is_equal), r=[S4_b["gscore"], S1_b["gmax"]], w=[S4_b["gmask"]])
        kb.op("dve", lambda e: e.tensor_scalar(out=S4["pen"][:], in0=S4["gmask"][:], scalar1=1e30, scalar2=-1e30, op0=ALU.mult, op1=ALU.add), r=[S4_b["gmask"]], w=[S4_b["pen"]])
        gmask_bc = S4["gmask"][:].rearrange("p (q o) -> p q o", o=1).to_broadcast([128, 16, 4])
        pen_bc = S4["pen"][:].rearrange("p (q o) -> p q o", o=1).to_broadcast([128, 16, 4])
        msk = R["masked"][:].rearrange("p s (g k) -> p (s g) k", k=4)
        kb.op("dve", lambda e: e.tensor_tensor(out=msk, in0=sbg, in1=gmask_bc, op=ALU.mult), r=[R_b["sb"], S4_b["gmask"]], w=[R_b["masked"]])
        kb.op("dve", lambda e: e.tensor_tensor(out=msk, in0=msk, in1=pen_bc, op=ALU.add), r=[R_b["masked"], S4_b["pen"]], w=[R_b["masked"]])
        kb.op("dve", lambda e: e.tensor_reduce(out=S1["m1"][:], in_=R["masked"][:], axis=AX.X, op=ALU.max), r=[R_b["masked"]], w=[S1_b["m1"]])
        m1_bc = S1["m1"][:].rearrange("p (s o) -> p s o", o=1).to_broadcast([128, 4, 16])
        kb.op("dve", lambda e: e.tensor_tensor(out=R["sel1"][:], in0=R["masked"][:], in1=m1_bc, op=ALU.is_equal), r=[R_b["masked"], S1_b["m1"]], w=[R_b["sel1"]])
        kb.op("dve", lambda e: e.scalar_tensor_tensor(out=R["m2"][:], in0=R["sel1"][:], scalar=-1e30, in1=R["masked"][:], op0=ALU.mult, op1=ALU.add),
              r=[R_b["sel1"], R_b["masked"]], w=[R_b["m2"]])
        kb.op("dve", lambda e: e.tensor_reduce(out=S1["m2"][:], in_=R["m2"][:], axis=AX.X, op=ALU.max), r=[R_b["m2"]], w=[S1_b["m2"]])
        m2_bc = S1["m2"][:].rearrange("p (s o) -> p s o", o=1).to_broadcast([128, 4, 16])
        kb.op("dve", lambda e: e.tensor_tensor(out=R["sel2"][:], in0=R["m2"][:], in1=m2_bc, op=ALU.is_equal), r=[R_b["m2"], S1_b["m2"]], w=[R_b["sel2"]])
        kb.op("dve", lambda e: e.tensor_tensor(out=R["sel1"][:], in0=R["sel1"][:], in1=R["sel2"][:], op=ALU.add), r=[R_b["sel1"], R_b["sel2"]], w=[R_b["sel1"]])
        kb.op("dve", lambda e: e.tensor_tensor(out=R["ssel"][:], in0=R["sel1"][:], in1=R["s"][:], op=ALU.mult), r=[R_b["sel1"], R_b["s"]], w=[R_b["ssel"]])
        kb.op("dve", lambda e: e.tensor_reduce(out=S1["den"][:], in_=R["ssel"][:], axis=AX.X, op=ALU.add), r=[R_b["ssel"]], w=[S1_b["den"]])
        kb.op("dve", lambda e: e.reciprocal(S1["rden"][:], S1["den"][:]), r=[S1_b["den"]], w=[S1_b["rden"]])
        rden_bc = S1["rden"][:].rearrange("p (s o) -> p s o", o=1).to_broadcast([128, 4, 16])
        kb.op("dve", lambda e: e.tensor_tensor(out=R["gates"][:], in0=R["ssel"][:], in1=rden_bc, op=ALU.mult), r=[R_b["ssel"], S1_b["rden"]], w=[R_b["gates"]])
        for s in range(4):
            kb.op("pe", lambda e: e.transpose(pbank[5][0:16, s * 128:(s + 1) * 128], R["gates"][:, s, :], ident[:, :]), r=[R_b["gates"], ident_b], w=[pb[5]])
        kb.op("act", lambda e: e.copy(gatesT[:], pbank[5][0:16, :]), r=[pb[5]], w=[gatesT_b])
        for ex in range(16):
            mm(kb, pbank[4][:], sel[:, ex * 128:(ex + 1) * 128], gatesT[:], True, True, r=[sel_b, gatesT_b], w=[pb[4]])
            for j in range(4):
                def ld(st, sbuf_, ex=ex, j=j):
                    sv_ = st[:, :16 * 256].rearrange("p (c n) -> p c n", c=16)
                    src = wgu[ex].rearrange("(c p) n -> p c n", p=128)
                    kb.dma(sv_[:, :, 0:128], src[:, :, j * 128:(j + 1) * 128], w=[sbuf_])
                    kb.dma(sv_[:, :, 128:256], src[:, :, 512 + j * 128:512 + (j + 1) * 128], w=[sbuf_])
                wt, wtb = load_weight(ld, 16 * 256)
                pg, pu = (0, 1) if j % 2 == 0 else (2, 3)
                for k in range(16):
                    mm(kb, pbank[pg][:], wt[:, k * 256:k * 256 + 128], hT[:, k, :], k == 0, k == 15, r=[wtb, hT_b[k]], w=[pb[pg]])
                for k in range(16):
                    mm(kb, pbank[pu][:], wt[:, k * 256 + 128:k * 256 + 256], hT[:, k, :], k == 0, k == 15, r=[wtb, hT_b[k]], w=[pb[pu]])
                ti = nxt(tmp_i, 4)
                kb.op("act", lambda e: e.activation(out=tmp[ti][:], in_=pbank[pg][:], func=AF.Silu), r=[pb[pg]], w=[tmp_b[ti]])
                kb.op("dve", lambda e: e.tensor_tensor(out=tmp[ti][:], in0=tmp[ti][:], in1=pbank[pu][:], op=ALU.mult), r=[tmp_b[ti], pb[pu]], w=[tmp_b[ti]])
                kb.op("dve", lambda e: e.tensor_tensor(out=aT[:, j, :], in0=tmp[ti][:], in1=pbank[4][:], op=ALU.mult), r=[tmp_b[ti], pb[4]], w=[aT_b[j]])
            for mq in range(4):
                def ld(st, sbuf_, ex=ex, mq=mq):
                    kb.dma(st[:, :4 * 512].rearrange("p (c n) -> p c n", c=4), wd[ex].rearrange("(c p) n -> p c n", p=128)[:, :, mq * 512:(mq + 1) * 512], w=[sbuf_])
                wt, wtb = load_weight(ld, 4 * 512)
                for mi in range(4):
                    m = mq * 4 + mi
                    pi = 6 + (m % 2)
                    for k in range(4):
                        mm(kb, pbank[pi][:], wt[:, k * 512 + mi * 128:k * 512 + (mi + 1) * 128], aT[:, k, :], k == 0, k == 3, r=[wtb, aT_b[k]], w=[pb[pi]])
                    if ex == 0:
                        kb.op("act", lambda e: e.copy(yacc[:, m, :], pbank[pi][:]), r=[pb[pi]], w=[yacc_b[m]])
                    else:
                        kb.op("dve", lambda e: e.tensor_tensor(out=yacc[:, m, :], in0=pbank[pi][:], in1=yacc[:, m, :], op=ALU.add), r=[pb[pi], yacc_b[m]], w=[yacc_b[m]])
        for c in range(16):
            kb.op("act", lambda e: e.activation(out=yacc[:, c, :], in_=yacc[:, c, :], func=AF.Identity, scale=pcol(3, c, True)), r=[yacc_b[c], pv1_b], w=[yacc_b[c]])
            kb.op("dve", lambda e: e.scalar_tensor_tensor(out=xt[:, c, :], in0=xt[:, c, :], scalar=DN_ALPHA, in1=yacc[:, c, :], op0=ALU.mult, op1=ALU.add),
                  r=[xt_b[c], yacc_b[c]], w=[xt_b[c]])
        layernorm(6, 7, False, out_dma_t0=t0)
    return kb.finish()


def build_ada():
    kb = KB()
    cT_d = kb.din("cT", [128, 16 * 4])
    w_d = kb.din("w", [2, D, 1536])
    b_d = kb.din("b", [128, 24])
    out_d = kb.dout("modT", [128, 24 * 4])
    cT = kb.sb([128, 64]); cT_b = Buf()
    bb = kb.sb([128, 24]); bb_b = Buf()
    ot = kb.sb([128, 96]); ot_b = Buf()
    stg = [kb.sb([128, 16, 128]) for _ in range(3)]; stg_b = bufs(3)
    ps = [kb.ps([128, 512]) for _ in range(2)]; ps_b = bufs(2)
    kb.dma(cT[:], cT_d[:, :], w=[cT_b])
    kb.dma(bb[:], b_d[:, :], w=[bb_b])
    kb.op("act", lambda e: e.activation(out=cT[:], in_=cT[:], func=AF.Silu), r=[cT_b], w=[cT_b])
    for l in range(2):
        for m in range(12):
            u = l * 12 + m
            si = u % 3
            kb.dma(stg[si][:], w_d[l].rearrange("(c p) n -> p c n", p=128)[:, :, m * 128:(m + 1) * 128], w=[stg_b[si]])
            pi = u % 2
            for k in range(16):
                mm(kb, ps[pi][:, 0:4], stg[si][:, k, :], cT[:, k * 4:(k + 1) * 4], k == 0, k == 15, r=[stg_b[si], cT_b], w=[ps_b[pi]])
            kb.op("dve", lambda e: e.tensor_scalar(out=ot[:, u * 4:(u + 1) * 4], in0=ps[pi][:, 0:4], scalar1=bb[:, u:u + 1], scalar2=None, op0=ALU.add),
                  r=[ps_b[pi], bb_b], w=[ot_b])
    kb.dma(out_d[:, :], ot[:], r=[ot_b], final=True)
    return kb.finish()


def run_ada(c, ada_w, ada_b):
    nc = build_ada()
    cT = np.ascontiguousarray(c.T.reshape(16, 128, 4).transpose(1, 0, 2).reshape(128, 64))
    in_maps = []
    for j in range(NCORES):
        w = np.ascontiguousarray(ada_w[:, :, j * 1536:(j + 1) * 1536])
        b = ada_b[:, j * 1536:(j + 1) * 1536].reshape(2, 12, 128).transpose(2, 0, 1).reshape(128, 24)
        in_maps.append({"cT": cT, "w": w, "b": np.ascontiguousarray(b)})
    res = run_bass_kernel_spmd(nc, in_maps, core_ids=list(range(NCORES)))
    mod = np.zeros((2, 4, 6 * D), np.float32)
    for j in range(NCORES):
        o = res.results[j]["modT"].reshape(128, 2, 12, 4)
        mod[:, :, j * 1536:(j + 1) * 1536] = o.transpose(1, 3, 2, 0).reshape(2, 4, 1536)
    return mod


def build_pre(ncol, ntok=2048):
    assert ncol % 128 == 0
    NM = ncol // 128
    kb = KB()
    NT = ntok // TT
    xT = kb.din("xT", [D, ntok]).rearrange("(c p) t -> p c t", p=128)
    w_d = kb.din("w", [D, ncol]).rearrange("(c p) n -> p c n", p=128)
    pv_d = kb.din("pvec", [128, 32])
    zT = kb.dout("zT", [ncol, ntok]).rearrange("(c p) t -> p c t", p=128)
    xt = [kb.sb([128, 16, TT]) for _ in range(2)]; xt_b = bufs(2)
    hT = [kb.sb([128, 16, TT], BF16) for _ in range(2)]; hT_b = bufs(2)
    stg = [kb.sb([128, 16, 128]) for _ in range(3)]; stg_b = bufs(3)
    wb = [kb.sb([128, 16, 128], BF16) for _ in range(3)]; wb_b = bufs(3)
    ot = [kb.sb([128, TT]) for _ in range(4)]; ot_b = bufs(4)
    pv = kb.sb([128, 32]); pv_b = Buf()
    pv1 = kb.sb([128, 32]); pv1_b = Buf()
    ps = [kb.ps([128, TT]) for _ in range(4)]; ps_b = bufs(4)
    kb.dma(pv[:], pv_d[:, :], w=[pv_b])
    kb.op("dve", lambda e: e.tensor_scalar_add(pv1[:], pv[:], 1.0), r=[pv_b], w=[pv1_b])
    u = 0
    for tt in range(NT):
        t0 = tt * TT
        xi = tt % 2
        kb.dma(xt[xi][:], xT[:, :, t0:t0 + TT], w=[xt_b[xi]])
        for c in range(16):
            kb.op("act", lambda e: e.activation(out=hT[xi][:, c, :], in_=xt[xi][:, c, :], func=AF.Identity, scale=pv1[:, c:c + 1], bias=pv[:, 16 + c:17 + c]),
                  r=[xt_b[xi], pv_b, pv1_b], w=[hT_b[xi]])
        for m in range(NM):
            si = u % 3
            kb.dma(stg[si][:], w_d[:, :, m * 128:(m + 1) * 128], w=[stg_b[si]])
            kb.op("pool", lambda e: e.tensor_copy(out=wb[si][:], in_=stg[si][:]), r=[stg_b[si]], w=[wb_b[si]])
            pi = u % 4
            for k in range(16):
                mm(kb, ps[pi][:], wb[si][:, k, :], hT[xi][:, k, :], k == 0, k == 15, r=[wb_b[si], hT_b[xi]], w=[ps_b[pi]])
            if u % 2 == 0:
                kb.op("act", lambda e: e.copy(ot[pi][:], ps[pi][:]), r=[ps_b[pi]], w=[ot_b[pi]])
            else:
                kb.op("dve", lambda e: e.tensor_copy(out=ot[pi][:], in_=ps[pi][:]), r=[ps_b[pi]], w=[ot_b[pi]])
            kb.dma(zT[:, m, t0:t0 + TT], ot[pi][:], r=[ot_b[pi]], final=True)
            u += 1
    return kb.finish()


def fm16(v):
    return np.ascontiguousarray(v.reshape(16, 128).T)


def run_pre(x_tok_major, mod_l, w, sc_idx, sh_idx):
    ncol = w.shape[1]
    nc = build_pre(ncol)
    in_maps = []
    for j in range(NCORES):
        b, hf = j // 2, j % 2
        xT = np.ascontiguousarray(x_tok_major[b, hf * 2048:(hf + 1) * 2048].T)
        sc = mod_l[b, sc_idx * D:(sc_idx + 1) * D]
        sh = mod_l[b, sh_idx * D:(sh_idx + 1) * D]
        in_maps.append({"xT": xT, "w": w, "pvec": np.ascontiguousarray(np.concatenate([fm16(sc), fm16(sh)], 1))})
    res = run_bass_kernel_spmd(nc, in_maps, core_ids=list(range(NCORES)))
    z = np.zeros((4, ncol, 4096), np.float32)
    for j in range(NCORES):
        b, hf = j // 2, j % 2
        z[b, :, hf * 2048:(hf + 1) * 2048] = res.results[j]["zT"]
    return z


SEQ = 4096
MLA_SCALE = 192.0 ** -0.5


def build_mla(S=SEQ):
    kb = KB()
    NT = S // TT
    zq = kb.din("zq", [512, S]).rearrange("(c p) t -> p c t", p=128)
    zkv = kb.din("zkv", [256, S]).rearrange("(c p) t -> p c t", p=128)
    zkr = kb.din("zkr", [128, S]).rearrange("(c p) t -> p c t", p=64)
    cs_d = kb.din("cs", [128, S]).rearrange("(c p) t -> p c t", p=64)
    wq_d = kb.din("wq", [512, 1024]).rearrange("(c p) n -> p c n", p=128)
    wk_d = kb.din("wk", [256, 512]).rearrange("(c p) n -> p c n", p=128)
    wv_d = kb.din("wv", [256, 512]).rearrange("(c p) n -> p c n", p=128)
    g_d = kb.din("g", [128, 6])
    mask_d = kb.din("mask", [128, 4 * TT])
    attT = kb.dout("attT", [512, S]).rearrange("(c p) t -> p c t", p=128)

    qnope = [kb.sb([128, S], BF16) for _ in range(4)]; qnope_b = [bufs(NT) for _ in range(4)]
    qrope = [kb.sb([128, S], BF16) for _ in range(2)]; qrope_b = [bufs(NT) for _ in range(2)]
    knope = [kb.sb([128, S], BF16) for _ in range(4)]; knope_b = [bufs(NT) for _ in range(4)]
    krope = kb.sb([128, S], BF16); krope_b = bufs(NT)
    V = kb.sb([128, S // 128, 512], BF16); V_b = bufs(NT)
    wq = kb.sb([128, 4, 1024], BF16); wk = kb.sb([128, 2, 512], BF16); wv = kb.sb([128, 2, 512], BF16); w_b = Buf()
    g = kb.sb([128, 6]); g_b = Buf()
    mask = kb.sb([128, 4 * TT], BF16); mask_b = Buf()
    ones = kb.sb([128, 128]); ones_b = Buf()
    onesb = kb.sb([128, 128], BF16); onesb_b = Buf()
    stg = kb.sb([128, 2048]); stg_b = Buf()
    zin = [kb.sb([128, 6, TT]) for _ in range(1)]; zin_b = bufs(1)
    zr = [kb.sb([128, 4, TT]) for _ in range(1)]; zr_b = bufs(1)
    qn = kb.sb([128, 6, TT], BF16); qn_b = bufs(6)
    tmp = [kb.sb([128, TT]) for _ in range(4)]; tmp_b = bufs(4)
    rs = [kb.sb([128, TT]) for _ in range(2)]; rs_b = bufs(2)
    pT = [kb.sb([128, TT], BF16) for _ in range(3)]; pT_b = bufs(3)
    ot = [kb.sb([128, TT]) for _ in range(2)]; ot_b = bufs(2)
    ps = [kb.ps([128, TT]) for _ in range(8)]; pb = bufs(8)
    tmp_i = [0]

    def nxt(ctr, n):
        i = ctr[0] % n
        ctr[0] += 1
        return i

    kb.dma(g[:], g_d[:, :], w=[g_b])
    kb.op("dve", lambda e: e.memset(ones[:], 1.0), w=[ones_b])
    kb.op("dve", lambda e: e.memset(onesb[:], 1.0), w=[onesb_b])
    kb.dma(stg[:, :2048], mask_d[:, :], w=[stg_b])
    kb.op("dve", lambda e: e.tensor_copy(out=mask[:], in_=stg[:, :2048]), r=[stg_b], w=[mask_b])
    for hh in range(2):
        kb.dma(stg[:].rearrange("p (c n) -> p c n", c=2), wq_d[:, 2 * hh:2 * hh + 2, :], w=[stg_b])
        kb.op("dve", lambda e: e.tensor_copy(out=wq[:, 2 * hh:2 * hh + 2, :].rearrange("p c n -> p (c n)"), in_=stg[:]), r=[stg_b, w_b], w=[w_b])
    kb.dma(stg[:, :1024].rearrange("p (c n) -> p c n", c=2), wk_d[:, :, :], w=[stg_b])
    kb.op("dve", lambda e: e.tensor_copy(out=wk[:].rearrange("p c n -> p (c n)"), in_=stg[:, :1024]), r=[stg_b, w_b], w=[w_b])
    kb.dma(stg[:, :1024].rearrange("p (c n) -> p c n", c=2), wv_d[:, :, :], w=[stg_b])
    kb.op("dve", lambda e: e.tensor_copy(out=wv[:].rearrange("p c n -> p (c n)"), in_=stg[:, :1024]), r=[stg_b, w_b], w=[w_b])

    for tt in range(NT):
        t0 = tt * TT
        zi = 0
        kb.dma(zin[zi][:, 0:4, :], zq[:, :, t0:t0 + TT], w=[zin_b[zi]])
        kb.dma(zin[zi][:, 4:6, :], zkv[:, :, t0:t0 + TT], w=[zin_b[zi]])
        for hp in range(2):
            kb.dma(zr[zi][hp * 64:(hp + 1) * 64, 0:2, :], zkr[:, :, t0:t0 + TT], w=[zr_b[zi]])
            kb.dma(zr[zi][hp * 64:(hp + 1) * 64, 2:4, :], cs_d[:, :, t0:t0 + TT], w=[zr_b[zi]])
        for (c0, c1, pi, dim, eps, ri) in [(0, 4, 6, 512, 1e-6, 0), (4, 6, 7, 256, 1e-6, 1)]:
            for c in range(c0, c1):
                ti = nxt(tmp_i, 4)
                kb.op("act", lambda e: e.activation(out=tmp[ti][:], in_=zin[zi][:, c, :], func=AF.Square), r=[zin_b[zi]], w=[tmp_b[ti]])
                mm(kb, ps[pi][:], ones[:], tmp[ti][:], c == c0, c == c1 - 1, r=[ones_b, tmp_b[ti]], w=[pb[pi]])
            kb.op("dve", lambda e: e.tensor_scalar(out=rs[ri][:], in0=ps[pi][:], scalar1=1.0 / dim, scalar2=eps, op0=ALU.mult, op1=ALU.add), r=[pb[pi]], w=[rs_b[ri]])
            kb.op("act", lambda e: e.sqrt(rs[ri][:], rs[ri][:]), r=[rs_b[ri]], w=[rs_b[ri]])
            kb.op("dve", lambda e: e.reciprocal(rs[ri][:], rs[ri][:]), r=[rs_b[ri]], w=[rs_b[ri]])
            for c in range(c0, c1):
                kb.op("dve", lambda e: e.scalar_tensor_tensor(out=qn[:, c, :], in0=zin[zi][:, c, :], scalar=g[:, c:c + 1], in1=rs[ri][:], op0=ALU.mult, op1=ALU.mult),
                      r=[zin_b[zi], g_b, rs_b[ri]], w=[qn_b[c]])
        for h in range(4):
            for k in range(4):
                mm(kb, ps[0][:], wq[:, k, h * 128:(h + 1) * 128], qn[:, k, :], k == 0, k == 3, r=[w_b, qn_b[k]], w=[pb[0]])
            kb.op("act", lambda e: e.copy(qnope[h][:, t0:t0 + TT], ps[0][:]), r=[pb[0]], w=[qnope_b[h][tt]])
            for k in range(2):
                mm(kb, ps[3][:], wk[:, k, h * 128:(h + 1) * 128], qn[:, 4 + k, :], k == 0, k == 1, r=[w_b, qn_b[4 + k]], w=[pb[3]])
            kb.op("act", lambda e: e.copy(knope[h][:, t0:t0 + TT], ps[3][:]), r=[pb[3]], w=[knope_b[h][tt]])
        for hp in range(2):
            for k in range(4):
                mm(kb, ps[1][:], wq[:, k, 512 + hp * 128:512 + (hp + 1) * 128], qn[:, k, :], k == 0, k == 3, r=[w_b, qn_b[k]], w=[pb[1]])
            for k in range(4):
                mm(kb, ps[2][:], wq[:, k, 768 + hp * 128:768 + (hp + 1) * 128], qn[:, k, :], k == 0, k == 3, r=[w_b, qn_b[k]], w=[pb[2]])
            t1 = nxt(tmp_i, 4)
            kb.op("dve", lambda e: e.tensor_tensor(out=tmp[t1][:], in0=ps[1][:], in1=zr[zi][:, 2, :], op=ALU.mult), r=[pb[1], zr_b[zi]], w=[tmp_b[t1]])
            t2 = nxt(tmp_i, 4)
            kb.op("dve", lambda e: e.tensor_tensor(out=tmp[t2][:], in0=ps[2][:], in1=zr[zi][:, 3, :], op=ALU.mult), r=[pb[2], zr_b[zi]], w=[tmp_b[t2]])
            kb.op("dve", lambda e: e.tensor_tensor(out=qrope[hp][:, t0:t0 + TT], in0=tmp[t1][:], in1=tmp[t2][:], op=ALU.add),
                  r=[tmp_b[t1], tmp_b[t2]], w=[qrope_b[hp][tt]])
        for blk in range(4):
            pi = 4 + blk % 2
            for k in range(2):
                mm(kb, ps[pi][:], qn[:, 4 + k, blk * 128:(blk + 1) * 128], wv[:, k, :], k == 0, k == 1, r=[w_b, qn_b[4 + k]], w=[pb[pi]])
            kb.op("act", lambda e: e.copy(V[:, tt * 4 + blk, :], ps[pi][:]), r=[pb[pi]], w=[V_b[tt]])
        t1 = nxt(tmp_i, 4)
        kb.op("dve", lambda e: e.tensor_tensor(out=tmp[t1][:], in0=zr[zi][:, 0, :], in1=zr[zi][:, 2, :], op=ALU.mult), r=[zr_b[zi]], w=[tmp_b[t1]])
        t2 = nxt(tmp_i, 4)
        kb.op("dve", lambda e: e.tensor_tensor(out=tmp[t2][:], in0=zr[zi][:, 1, :], in1=zr[zi][:, 3, :], op=ALU.mult), r=[zr_b[zi]], w=[tmp_b[t2]])
        kb.op("dve", lambda e: e.tensor_tensor(out=krope[:, t0:t0 + TT], in0=tmp[t1][:], in1=tmp[t2][:], op=ALU.add), r=[tmp_b[t1], tmp_b[t2]], w=[krope_b[tt]])

    sbanks = [0, 1, 2]
    cnt = [0]
    for h in range(4):
        for qb in range(NT):
            q0 = qb * TT
            nkb = 4 * qb + 4
            po, pd = (3, 4) if (h * NT + qb) % 2 == 0 else (5, 6)

            def qk(kk):
                sb_ = sbanks[kk % 3]
                kt = kk // 4
                mm(kb, ps[sb_][:], knope[h][:, kk * 128:(kk + 1) * 128], qnope[h][:, q0:q0 + TT], True, False,
                   r=[knope_b[h][kt], qnope_b[h][qb]], w=[pb[sb_]])
                ph = (h % 2) * 64
                mm(kb, ps[sb_][:], krope[ph:ph + 64, kk * 128:(kk + 1) * 128], qrope[h // 2][ph:ph + 64, q0:q0 + TT], False, True,
                   r=[krope_b[kt], qrope_b[h // 2][qb]], w=[pb[sb_]])
            qk(0)
            for kk in range(nkb):
                if kk + 1 < nkb:
                    qk(kk + 1)
                sb_ = sbanks[kk % 3]
                pi = cnt[0] % 3
                cnt[0] += 1
                kb.op("act", lambda e: e.activation(out=pT[pi][:], in_=ps[sb_][:], func=AF.Exp, scale=MLA_SCALE), r=[pb[sb_]], w=[pT_b[pi]])
                j = kk - 4 * qb
                if j >= 0:
                    kb.op("dve", lambda e: e.tensor_tensor(out=pT[pi][:], in0=pT[pi][:], in1=mask[:, j * TT:(j + 1) * TT], op=ALU.mult), r=[pT_b[pi], mask_b], w=[pT_b[pi]])
                mm(kb, ps[po][:], V[:, kk, h * 128:(h + 1) * 128], pT[pi][:], kk == 0, kk == nkb - 1, r=[V_b[kk // 4], pT_b[pi]], w=[pb[po]])
                mm(kb, ps[pd][:], onesb[:], pT[pi][:], kk == 0, kk == nkb - 1, r=[onesb_b, pT_b[pi]], w=[pb[pd]])
            oi = (h * NT + qb) % 2
            ti = nxt(tmp_i, 4)
            kb.op("dve", lambda e: e.reciprocal(tmp[ti][:], ps[pd][:]), r=[pb[pd]], w=[tmp_b[ti]])
            kb.op("dve", lambda e: e.tensor_tensor(out=ot[oi][:], in0=ps[po][:], in1=tmp[ti][:], op=ALU.mult), r=[pb[po], tmp_b[ti]], w=[ot_b[oi]])
            kb.dma(attT[:, h, q0:q0 + TT], ot[oi][:], r=[ot_b[oi]], final=True)
    return kb.finish()


def rope_tables(S=SEQ):
    inv = 10000.0 ** (-np.arange(0, 64, 2, dtype=np.float32) / 64)
    ang = np.arange(S, dtype=np.float32)[None, :] * inv[:, None]
    cos, sin = np.cos(ang).astype(np.float32), np.sin(ang).astype(np.float32)
    return np.ascontiguousarray(np.concatenate([cos, cos, -sin, sin], 0))


def causal_masks():
    m = np.zeros((4, 128, TT), np.float32)
    k = np.arange(128)[:, None]
    q = np.arange(TT)[None, :]
    for j in range(4):
        m[j] = (q >= k + 128 * j)
    return np.ascontiguousarray(m.transpose(1, 0, 2).reshape(128, 4 * TT))


def run_mla(z0, w_uq, w_ukv, q_norm, kv_norm):
    nc = build_mla()
    cs = rope_tables()
    mask = causal_masks()
    g = np.ascontiguousarray(np.concatenate([q_norm.reshape(4, 128).T, kv_norm.reshape(2, 128).T], 1))
    in_maps = []
    for j in range(NCORES):
        b, hf = j // 2, j % 2
        wq_cols, wk_cols, wv_cols = [], [], []
        hs = list(range(4 * hf, 4 * hf + 4))
        for h in hs:
            wq_cols.append(w_uq[:, h * 192:h * 192 + 128])
            wk_cols.append(w_ukv[:, h * 256:h * 256 + 128])
            wv_cols.append(w_ukv[:, h * 256 + 128:h * 256 + 256])
        for h in hs:
            wq_cols.append(w_uq[:, h * 192 + 128:h * 192 + 192])
        for h in hs:
            wq_cols += [w_uq[:, h * 192 + 160:h * 192 + 192], w_uq[:, h * 192 + 128:h * 192 + 160]]
        in_maps.append({
            "zq": np.ascontiguousarray(z0[b, 0:512]), "zkv": np.ascontiguousarray(z0[b, 512:768]),
            "zkr": np.ascontiguousarray(np.concatenate([z0[b, 768:832], z0[b, 1856:1920]], 0)),
            "cs": cs, "wq": np.ascontiguousarray(np.concatenate(wq_cols, 1)), "wk": np.ascontiguousarray(np.concatenate(wk_cols, 1)),
            "wv": np.ascontiguousarray(np.concatenate(wv_cols, 1)), "g": g, "mask": mask})
    res = run_bass_kernel_spmd(nc, in_maps, core_ids=list(range(NCORES)))
    att = np.zeros((4, 1024, SEQ), np.float32)
    for j in range(NCORES):
        b, hf = j // 2, j % 2
        att[b, hf * 512:(hf + 1) * 512] = res.results[j]["attT"]
    return att


TWO_PI = 6.283185307179586
NG = 32


def build_s5(S=SEQ):
    kb = KB()
    NT = S // TT
    uT = kb.din("uT", [NG * 16, S])
    lamre_d = kb.din("lamre", [128, NG]); lamim_d = kb.din("lamim", [128, NG]); logdt_d = kb.din("logdt", [128, NG])
    bt_d = kb.din("bt", [16, NG * 128]); btsw_d = kb.din("btsw", [16, NG * 128])
    ca_d = kb.din("ca", [128, NG * 16]); cb_d = kb.din("cb", [128, NG * 16])
    d_d = kb.din("dsk", [16, NG])
    iota_d = kb.din("iota", [128, S])
    yT = kb.dout("yT", [NG * 16, S])

    def t32(name=None):
        return kb.sb([128, NG], name=name)
    lamre, lamim, dt_, r_, th, cth, sth, nre, nim, den, fre, fim, tA, tB = [t32() for _ in range(14)]
    s1, s2, s3, s4 = [t32() for _ in range(4)]
    prm_b = Buf()
    sgn = kb.sb([128, 1]); negpi = kb.sb([128, 1]); ki = kb.sb([128, NG], mybir.dt.int32)
    KI = kb.sb([128, S], mybir.dt.int32); KI_b = Buf()
    bt = kb.sb([16, NG * 128], BF16); btsw = kb.sb([16, NG * 128], BF16); bstg = kb.sb([16, NG * 128]); bt_b = Buf(); bstg_b = Buf()
    ca = kb.sb([128, NG, 16]); cb = kb.sb([128, NG, 16]); cstage = kb.sb([128, NG, 16]); L1 = kb.sb([128, NG, 16], BF16); L2 = kb.sb([128, NG, 16], BF16); c_b = Buf()
    dsk = kb.sb([16, NG]); dsk_b = Buf()
    iota = kb.sb([128, S]); iota_b = Buf()
    u32 = kb.sb([16, S]); u32_b = Buf()
    ubf = kb.sb([16, S], BF16); ubf_b = Buf()
    A1 = kb.sb([128, S]); A1_b = Buf()
    A2 = kb.sb([128, S]); A2_b = Buf()
    T2 = kb.sb([128, S]); T2_b = Buf()
    bz = kb.sb([128, S]); bz_b = bufs(NT); z_b = Buf()
    Zc = kb.sb([128, S], BF16); Zc_b = Buf()
    Zs = kb.sb([128, S], BF16); Zs_b = Buf()
    ysb = kb.sb([16, S]); ysb_b = Buf()
    tmp = [kb.sb([128, TT]) for _ in range(4)]; tmp_b = bufs(4)
    ps = [kb.ps([128, TT]) for _ in range(8)]; pb = bufs(8)

    P = [prm_b]
    kb.dma(lamre[:], lamre_d[:, :], w=P)
    kb.dma(lamim[:], lamim_d[:, :], w=P)
    kb.dma(dt_[:], logdt_d[:, :], w=P)
    kb.dma(iota[:], iota_d[:, :], w=[iota_b])
    kb.dma(dsk[:], d_d[:, :], w=[dsk_b])
    kb.dma(bstg[:], bt_d[:, :], w=[bstg_b])
    kb.op("dve", lambda e: e.tensor_copy(out=bt[:], in_=bstg[:]), r=[bstg_b], w=[bt_b])
    kb.dma(bstg[:], btsw_d[:, :], w=[bstg_b])
    kb.op("dve", lambda e: e.tensor_copy(out=btsw[:], in_=bstg[:]), r=[bstg_b, bt_b], w=[bt_b])
    kb.dma(ca[:].rearrange("p g c -> p (g c)"), ca_d[:, :], w=[c_b])
    kb.dma(cb[:].rearrange("p g c -> p (g c)"), cb_d[:, :], w=[c_b])
    V = lambda fn: kb.op("dve", fn, r=P, w=P)
    A = lambda fn: kb.op("act", fn, r=P, w=P)
    V(lambda e: e.memset(sgn[0:64, :], 1.0))
    V(lambda e: e.memset(sgn[64:128, :], -1.0))
    V(lambda e: e.memset(negpi[:], -3.141592653589793))
    A(lambda e: e.activation(out=dt_[:], in_=dt_[:], func=AF.Exp))
    V(lambda e: e.tensor_tensor(out=r_[:], in0=lamre[:], in1=dt_[:], op=ALU.mult))
    A(lambda e: e.activation(out=r_[:], in_=r_[:], func=AF.Exp))
    V(lambda e: e.tensor_tensor(out=th[:], in0=lamim[:], in1=dt_[:], op=ALU.mult))
    V(lambda e: e.tensor_single_scalar(out=th[:], in_=th[:], scalar=1.0 / TWO_PI, op=ALU.mult))
    V(lambda e: e.tensor_copy(out=ki[:], in_=th[:]))
    V(lambda e: e.tensor_tensor(out=th[:], in0=th[:], in1=ki[:], op=ALU.subtract))
    A(lambda e: e.activation(out=sth[:], in_=th[:], func=AF.Sin, scale=TWO_PI))
    V(lambda e: e.tensor_single_scalar(out=tA[:], in_=th[:], scalar=0.25, op=ALU.add))
    V(lambda e: e.tensor_copy(out=ki[:], in_=tA[:]))
    V(lambda e: e.tensor_tensor(out=tA[:], in0=tA[:], in1=ki[:], op=ALU.subtract))
    A(lambda e: e.activation(out=cth[:], in_=tA[:], func=AF.Sin, scale=TWO_PI))
    V(lambda e: e.tensor_tensor(out=nre[:], in0=r_[:], in1=cth[:], op=ALU.mult))
    V(lambda e: e.tensor_single_scalar(out=nre[:], in_=nre[:], scalar=-1.0, op=ALU.add))
    V(lambda e: e.tensor_tensor(out=nim[:], in0=r_[:], in1=sth[:], op=ALU.mult))
    V(lambda e: e.tensor_tensor(out=den[:], in0=lamre[:], in1=lamre[:], op=ALU.mult))
    V(lambda e: e.tensor_tensor(out=tA[:], in0=lamim[:], in1=lamim[:], op=ALU.mult))
    V(lambda e: e.tensor_tensor(out=den[:], in0=den[:], in1=tA[:], op=ALU.add))
    V(lambda e: e.reciprocal(den[:], den[:]))
    V(lambda e: e.tensor_tensor(out=fre[:], in0=nre[:], in1=lamre[:], op=ALU.mult))
    V(lambda e: e.tensor_tensor(out=tA[:], in0=nim[:], in1=lamim[:], op=ALU.mult))
    V(lambda e: e.tensor_tensor(out=fre[:], in0=fre[:], in1=tA[:], op=ALU.add))
    V(lambda e: e.tensor_tensor(out=fre[:], in0=fre[:], in1=den[:], op=ALU.mult))
    V(lambda e: e.tensor_tensor(out=fim[:], in0=nim[:], in1=lamre[:], op=ALU.mult))
    V(lambda e: e.tensor_tensor(out=tA[:], in0=nre[:], in1=lamim[:], op=ALU.mult))
    V(lambda e: e.tensor_tensor(out=fim[:], in0=fim[:], in1=tA[:], op=ALU.subtract))
    V(lambda e: e.tensor_tensor(out=fim[:], in0=fim[:], in1=den[:], op=ALU.mult))
    V(lambda e: e.tensor_scalar(out=s1[:], in0=fre[:], scalar1=sgn[:, 0:1], scalar2=None, op0=ALU.mult))
    V(lambda e: e.tensor_single_scalar(out=s2[:], in_=fim[:], scalar=-1.0, op=ALU.mult))
    V(lambda e: e.tensor_scalar(out=s3[:], in0=s2[:], scalar1=sgn[:, 0:1], scalar2=None, op0=ALU.mult))
    V(lambda e: e.tensor_single_scalar(out=s4[:], in_=fre[:], scalar=-1.0, op=ALU.mult))

    def bc(t):
        return t[:].rearrange("p (g o) -> p g o", o=1).to_broadcast([128, NG, 16])
    PC = [prm_b, c_b]
    kb.op("dve", lambda e: e.tensor_tensor(out=cstage[:], in0=ca[:], in1=bc(s1), op=ALU.mult), r=PC, w=PC)
    kb.op("dve", lambda e: e.tensor_tensor(out=ca[:], in0=ca[:], in1=bc(s3), op=ALU.mult), r=PC, w=PC)
    kb.op("dve", lambda e: e.tensor_tensor(out=tmp[0][:, :NG * 16].rearrange("p (g c) -> p g c", c=16), in0=cb[:], in1=bc(s2), op=ALU.mult), r=PC, w=PC + [tmp_b[0]])
    kb.op("dve", lambda e: e.tensor_tensor(out=L1[:], in0=cstage[:], in1=tmp[0][:, :NG * 16].rearrange("p (g c) -> p g c", c=16), op=ALU.add), r=PC + [tmp_b[0]], w=PC)
    kb.op("dve", lambda e: e.tensor_tensor(out=cb[:], in0=cb[:], in1=bc(s4), op=ALU.mult), r=PC, w=PC)
    kb.op("dve", lambda e: e.tensor_tensor(out=L2[:], in0=ca[:], in1=cb[:], op=ALU.add), r=PC, w=PC)

    tmp_i = [1]
    for g in range(NG):
        kb.dma(u32[:], uT[g * 16:(g + 1) * 16, :], w=[u32_b])
        kb.op("act", lambda e: e.copy(ubf[:], u32[:]), r=[u32_b], w=[ubf_b])
        kb.op("dve", lambda e: e.tensor_scalar(out=KI[:], in0=iota[:], scalar1=th[:, g:g + 1], scalar2=None, op0=ALU.mult), r=[iota_b, prm_b], w=[KI_b])
        kb.op("dve", lambda e: e.scalar_tensor_tensor(out=A1[:], in0=iota[:], scalar=th[:, g:g + 1], in1=KI[:], op0=ALU.mult, op1=ALU.subtract), r=[iota_b, prm_b, KI_b], w=[A1_b])
        kb.op("dve", lambda e: e.tensor_single_scalar(out=A2[:], in_=A1[:], scalar=0.25, op=ALU.add), r=[A1_b], w=[A2_b])
        kb.op("dve", lambda e: e.tensor_copy(out=KI[:], in_=A2[:]), r=[A2_b], w=[KI_b])
        kb.op("dve", lambda e: e.tensor_tensor(out=A2[:], in0=A2[:], in1=KI[:], op=ALU.subtract), r=[A2_b, KI_b], w=[A2_b])
        kb.op("act", lambda e: e.activation(out=A1[:], in_=A1[:], func=AF.Sin, scale=TWO_PI), r=[A1_b], w=[A1_b])
        kb.op("act", lambda e: e.activation(out=A2[:], in_=A2[:], func=AF.Sin, scale=TWO_PI), r=[A2_b], w=[A2_b])
        kb.op("pool", lambda e: e.tensor_scalar(out=T2[:], in0=A1[:], scalar1=sgn[:, 0:1], scalar2=None, op0=ALU.mult), r=[A1_b, prm_b], w=[T2_b])
        for tt in range(NT):
            t0 = tt * TT
            pa, pbk = (0, 1) if tt % 2 == 0 else (2, 3)
            mm(kb, ps[pa][:], bt[:, g * 128:(g + 1) * 128], ubf[:, t0:t0 + TT], True, True, r=[bt_b, ubf_b], w=[pb[pa]])
            mm(kb, ps[pbk][:], btsw[:, g * 128:(g + 1) * 128], ubf[:, t0:t0 + TT], True, True, r=[bt_b, ubf_b], w=[pb[pbk]])
            t1 = tmp_i[0] % 4; tmp_i[0] += 1
            kb.op("dve", lambda e: e.tensor_tensor(out=tmp[t1][:], in0=ps[pa][:], in1=A2[:, t0:t0 + TT], op=ALU.mult), r=[pb[pa], A2_b], w=[tmp_b[t1]])
            t2 = tmp_i[0] % 4; tmp_i[0] += 1
            kb.op("dve", lambda e: e.tensor_tensor(out=tmp[t2][:], in0=ps[pbk][:], in1=T2[:, t0:t0 + TT], op=ALU.mult), r=[pb[pbk], T2_b], w=[tmp_b[t2]])
            kb.op("pool", lambda e: e.tensor_tensor(out=bz[:, t0:t0 + TT], in0=tmp[t1][:], in1=tmp[t2][:], op=ALU.add), r=[tmp_b[t1], tmp_b[t2], z_b], w=[bz_b[tt]])
        kb.op("dve", lambda e: e.tensor_tensor_scan(out=bz[:], data0=r_[:, g:g + 1].to_broadcast([128, S]), data1=bz[:], initial=0.0, op0=ALU.mult, op1=ALU.add),
              r=bz_b + [prm_b], w=bz_b + [z_b])
        kb.op("dve", lambda e: e.tensor_tensor(out=Zc[:], in0=bz[:], in1=A2[:], op=ALU.mult), r=[z_b, A2_b], w=[Zc_b])
        kb.op("pool", lambda e: e.tensor_tensor(out=Zs[:], in0=bz[:], in1=A1[:], op=ALU.mult), r=[z_b, A1_b], w=[Zs_b])
        for tt in range(NT):
            t0 = tt * TT
            pi = 4 + tt % 4
            mm(kb, ps[pi][0:16, :], L1[:, g, :], Zc[:, t0:t0 + TT], True, False, r=[c_b, Zc_b], w=[pb[pi]])
            mm(kb, ps[pi][0:16, :], L2[:, g, :], Zs[:, t0:t0 + TT], False, True, r=[c_b, Zs_b], w=[pb[pi]])
            kb.op("dve", lambda e: e.scalar_tensor_tensor(out=ysb[:, t0:t0 + TT], in0=u32[:, t0:t0 + TT], scalar=dsk[:, g:g + 1], in1=ps[pi][0:16, :], op0=ALU.mult, op1=ALU.add),
                  r=[u32_b, dsk_b, pb[pi]], w=[ysb_b])
        kb.dma(yT[g * 16:(g + 1) * 16, :], ysb[:], r=[ysb_b], final=True)
    return kb.finish()


def run_s5(z0, lam_re, lam_im, b_re, b_im, c_re, c_im, d_skip, log_dt):
    nc = build_s5()
    iota = np.ascontiguousarray(np.broadcast_to(np.arange(SEQ, dtype=np.float32), (128, SEQ)))
    in_maps = []
    for j in range(NCORES):
        b, hf = j // 2, j % 2
        gs = slice(hf * NG, (hf + 1) * NG)
        lre = lam_re[gs].T; lim = lam_im[gs].T
        bre = b_re[gs].transpose(2, 0, 1); bim = b_im[gs].transpose(2, 0, 1)
        cre = c_re[gs].transpose(2, 0, 1); cim = c_im[gs].transpose(2, 0, 1)
        in_maps.append({
            "uT": np.ascontiguousarray(z0[b, 832 + hf * 512:832 + (hf + 1) * 512]),
            "lamre": np.ascontiguousarray(np.concatenate([lre, lre], 0)), "lamim": np.ascontiguousarray(np.concatenate([lim, lim], 0)),
            "logdt": np.ascontiguousarray(np.broadcast_to(log_dt[gs][None, :], (128, NG))),
            "bt": np.ascontiguousarray(np.concatenate([bre, bim], 2).reshape(16, NG * 128)),
            "btsw": np.ascontiguousarray(np.concatenate([bim, bre], 2).reshape(16, NG * 128)),
            "ca": np.ascontiguousarray(np.concatenate([cre, cim], 0).reshape(128, NG * 16)),
            "cb": np.ascontiguousarray(np.concatenate([cim, cre], 0).reshape(128, NG * 16)),
            "dsk": np.ascontiguousarray(d_skip[gs].T), "iota": iota})
    res = run_bass_kernel_spmd(nc, in_maps, core_ids=list(range(NCORES)))
    y = np.zeros((4, 1024, SEQ), np.float32)
    for j in range(NCORES):
        b, hf = j // 2, j % 2
        y[b, hf * 512:(hf + 1) * 512] = res.results[j]["yT"]
    return y


DILS = (1, 4, 16)


def build_dil(S=SEQ):
    kb = KB()
    NB = S // 128
    q_d = kb.din("q", [12, 64, S]); k_d = kb.din("k", [12, 64, S])
    v_d = kb.din("v", [12, 128, NB * 64])
    bm_d = kb.din("bm", [12, 128, 1024])
    attT = kb.dout("attT", [256, S])
    stg = [kb.sb([128, S]) for _ in range(2)]; stg_b = bufs(2)
    qs = [kb.sb([64, S], BF16) for _ in range(2)]; qs_b = bufs(2)
    ks = [kb.sb([64, S], BF16) for _ in range(2)]; ks_b = bufs(2)
    vs = [kb.sb([128, NB * 64], BF16) for _ in range(2)]; vs_b = bufs(2)
    eb = [kb.sb([128, 1024], BF16) for _ in range(2)]; eb_b = bufs(2)
    eb0 = [kb.sb([128, 512], BF16) for _ in range(2)]; eb0_b = bufs(2)
    num = kb.sb([64, S]); num_b = Buf()
    den = kb.sb([64, S]); den_b = Buf()
    onesb = kb.sb([128, 64], BF16); onesb_b = Buf()
    pT = [kb.sb([128, 512], BF16) for _ in range(4)]; pT_b = bufs(4)
    ps = [kb.ps([128, TT]) for _ in range(8)]; pb = bufs(8)
    kb.op("dve", lambda e: e.memset(onesb[:], 1.0), w=[onesb_b])
    u = 0
    pti = 0
    for h in range(4):
        for gi, d in enumerate(DILS):
            inst = gi * 4 + h
            bi = u % 2
            nb = NB // d
            kb.dma(stg[0][0:64, :], q_d[inst], w=[stg_b[0]])
            kb.op("pool", lambda e: e.tensor_copy(out=qs[bi][:], in_=stg[0][0:64, :]), r=[stg_b[0]], w=[qs_b[bi]])
            kb.dma(stg[1][0:64, :], k_d[inst], w=[stg_b[1]])
            kb.op("pool", lambda e: e.tensor_copy(out=ks[bi][:], in_=stg[1][0:64, :]), r=[stg_b[1]], w=[ks_b[bi]])
            kb.dma(stg[0][:, :NB * 64], v_d[inst], w=[stg_b[0]])
            kb.op("pool", lambda e: e.tensor_copy(out=vs[bi][:], in_=stg[0][:, :NB * 64]), r=[stg_b[0]], w=[vs_b[bi]])
            kb.dma(stg[1][:, :1024], bm_d[inst], w=[stg_b[1]])
            kb.op("act", lambda e: e.activation(out=eb[bi][:], in_=stg[1][:, :1024], func=AF.Exp), r=[stg_b[1]], w=[eb_b[bi]])
            kb.op("dve", lambda e: e.tensor_copy(out=eb0[bi][:], in_=eb[bi][:, 512:1024]), r=[eb_b[bi]], w=[eb0_b[bi]])
            kb.op("dve", lambda e: e.memset(eb0[bi][:, 0:128], 0.0), r=[eb0_b[bi]], w=[eb0_b[bi]])
            if d == 16:
                kb.op("dve", lambda e: e.memset(eb0[bi][:, 256:384], 0.0), r=[eb0_b[bi]], w=[eb0_b[bi]])
            for bt in range(NB // 4):
                B0 = bt * 4
                pss, psp, pso, psd = (0, 1, 2, 3) if bt % 2 == 0 else (4, 5, 6, 7)
                firsts = [(B0 + j) % nb == 0 for j in range(4)]
                for j in range(4):
                    B = B0 + j
                    mm(kb, ps[pss][:, j * 128:(j + 1) * 128], ks[bi][:, B * 128:(B + 1) * 128], qs[bi][:, B * 128:(B + 1) * 128], True, True,
                       r=[ks_b[bi], qs_b[bi]], w=[pb[pss]])
                    Bp = B if firsts[j] else B - 1
                    mm(kb, ps[psp][:, j * 128:(j + 1) * 128], ks[bi][:, Bp * 128:(Bp + 1) * 128], qs[bi][:, B * 128:(B + 1) * 128], True, True,
                       r=[ks_b[bi], qs_b[bi]], w=[pb[psp]])
                p1 = pti % 4; p2 = (pti + 1) % 4; pti += 2
                kb.op("act", lambda e: e.activation(out=pT[p1][:], in_=ps[pss][:], func=AF.Exp, scale=0.125), r=[pb[pss]], w=[pT_b[p1]])
                kb.op("act", lambda e: e.activation(out=pT[p2][:], in_=ps[psp][:], func=AF.Exp, scale=0.125), r=[pb[psp]], w=[pT_b[p2]])
                kb.op("dve", lambda e: e.tensor_tensor(out=pT[p1][:], in0=pT[p1][:], in1=eb[bi][:, 0:512], op=ALU.mult), r=[pT_b[p1], eb_b[bi]], w=[pT_b[p1]])
                ebp = eb0[bi][:] if any(firsts) else eb[bi][:, 512:1024]
                kb.op("dve", lambda e: e.tensor_tensor(out=pT[p2][:], in0=pT[p2][:], in1=ebp, op=ALU.mult), r=[pT_b[p2], eb_b[bi], eb0_b[bi]], w=[pT_b[p2]])
                for j in range(4):
                    B = B0 + j
                    Bp = B if firsts[j] else B - 1
                    cs_ = slice(j * 128, (j + 1) * 128)
                    mm(kb, ps[pso][0:64, cs_], vs[bi][:, B * 64:(B + 1) * 64], pT[p1][:, cs_], True, False, r=[vs_b[bi], pT_b[p1]], w=[pb[pso]])
                    mm(kb, ps[pso][0:64, cs_], vs[bi][:, Bp * 64:(Bp + 1) * 64], pT[p2][:, cs_], False, True, r=[vs_b[bi], pT_b[p2]], w=[pb[pso]])
                    mm(kb, ps[psd][0:64, cs_], onesb[:], pT[p1][:, cs_], True, False, r=[onesb_b, pT_b[p1]], w=[pb[psd]])
                    mm(kb, ps[psd][0:64, cs_], onesb[:], pT[p2][:, cs_], False, True, r=[onesb_b, pT_b[p2]], w=[pb[psd]])
                if d == 16:
                    r0 = B0 // nb
                    nv = num[:].rearrange("c (m r) -> c r m", r=16)[:, r0:r0 + 2, :]
                    dv_ = den[:].rearrange("c (m r) -> c r m", r=16)[:, r0:r0 + 2, :]
                    po = ps[pso][0:64, :].rearrange("c (r m) -> c r m", r=2)
                    pd = ps[psd][0:64, :].rearrange("c (r m) -> c r m", r=2)
                elif d == 4:
                    r0 = B0 // nb; m0 = (B0 % nb) * 128
                    nv = num[:].rearrange("c (m r) -> c r m", r=4)[:, r0, m0:m0 + 512]
                    dv_ = den[:].rearrange("c (m r) -> c r m", r=4)[:, r0, m0:m0 + 512]
                    po = ps[pso][0:64, :]; pd = ps[psd][0:64, :]
                else:
                    nv = num[:, B0 * 128:B0 * 128 + 512]; dv_ = den[:, B0 * 128:B0 * 128 + 512]
                    po = ps[pso][0:64, :]; pd = ps[psd][0:64, :]
                if gi == 0:
                    kb.op("act", lambda e: e.copy(nv, po), r=[pb[pso]], w=[num_b])
                    kb.op("act", lambda e: e.copy(dv_, pd), r=[pb[psd]], w=[den_b])
                else:
                    kb.op("dve", lambda e: e.tensor_tensor(out=nv, in0=po, in1=nv, op=ALU.add), r=[pb[pso], num_b], w=[num_b])
                    kb.op("dve", lambda e: e.tensor_tensor(out=dv_, in0=pd, in1=dv_, op=ALU.add), r=[pb[psd], den_b], w=[den_b])
            u += 1
        kb.op("dve", lambda e: e.reciprocal(den[:], den[:]), r=[den_b], w=[den_b])
        kb.op("dve", lambda e: e.tensor_tensor(out=num[:], in0=num[:], in1=den[:], op=ALU.mult), r=[num_b, den_b], w=[num_b])
        kb.dma(attT[h * 64:(h + 1) * 64, :], num[:], r=[num_b], final=True)
    return kb.finish()


def t5_bucket_np(dist):
    exact = 16
    logd = np.log(np.maximum(dist, 1).astype(np.float32) / exact) / np.float32(np.log(2048 / exact))
    large = np.minimum(exact + (logd * (32 - exact)).astype(np.int32), 31)
    return np.where(dist < exact, dist, large)


def run_dil(z1, rel_bias):
    nc = build_dil()
    S = SEQ
    NB = S // 128
    kk = np.arange(128)[:, None]; qq = np.arange(128)[None, :]
    in_maps = []
    for jc in range(NCORES):
        b, hf = jc // 2, jc % 2
        q_l, k_l, v_l, bm_l = [], [], [], []
        for gi, d in enumerate(DILS):
            for h in range(4 * hf, 4 * hf + 4):
                def rows(qkv):
                    r0 = gi * 1536 + qkv * 512 + h * 64
                    t = z1[b, r0:r0 + 64]
                    return t.reshape(64, S // d, d).transpose(0, 2, 1).reshape(64, S)
                q_l.append(rows(0)); k_l.append(rows(1))
                v = rows(2)
                v_l.append(v.reshape(64, NB, 128).transpose(2, 1, 0).reshape(128, NB * 64))
                bias_h = rel_bias[:, gi * 8 + h]
                same = np.where(qq >= kk, bias_h[t5_bucket_np(np.clip(qq - kk, 0, 128) * d)], -30000.0)
                prev = np.where(qq <= kk, bias_h[t5_bucket_np(np.clip(128 + qq - kk, 0, 128) * d)], -30000.0)
                bm_l.append(np.concatenate([np.tile(same, (1, 4)), np.tile(prev, (1, 4))], 1))
        in_maps.append({"q": np.ascontiguousarray(np.stack(q_l)), "k": np.ascontiguousarray(np.stack(k_l)),
                        "v": np.ascontiguousarray(np.stack(v_l)), "bm": np.ascontiguousarray(np.stack(bm_l).astype(np.float32))})
    res = run_bass_kernel_spmd(nc, in_maps, core_ids=list(range(NCORES)))
    att = np.zeros((4, 512, S), np.float32)
    for jc in range(NCORES):
        b, hf = jc // 2, jc % 2
        att[b, hf * 256:(hf + 1) * 256] = res.results[jc]["attT"]
    return att


CL = 64
RW_GN_EPS = 64e-5
WDEC = -0.6065306597126334


RW_STOP = None


def build_rwkv(S=SEQ, nh=12):
    kb = KB()
    NBT = S // 512
    FW = nh * 64
    zr_d = kb.din("zr", [FW, S]); zk_d = kb.din("zk", [FW, S])
    vt_d = kb.din("v_tok", [S, FW]); vp_d = kb.din("vprev_tok", [S, FW])
    zwd_d = kb.din("zwd", [64, S]); zad_d = kb.din("zad", [64, S]); zgd_d = kb.din("zgd", [224, S])
    cols_d = kb.din("cols", [64, 8 * nh])
    mul_d = kb.din("mu_lora", [128, 4])
    w2_d = kb.din("w2", [64, FW]); a2_d = kb.din("a2", [64, FW]); g2_d = kb.din("g2", [224, FW])
    rows_d = kb.din("rows", [64, 3 * FW])
    cst_d = kb.din("cst", [64, 2048])
    rmask_d = kb.din("rmask", [64, 512])
    out_d = kb.dout("tm_tok", [S, FW])

    cst = kb.sb([64, 2048]); cst_b = Buf()
    rmask = kb.sb([64, 512]); rmask_b = Buf()
    cols = kb.sb([64, 8 * nh]); cols_b = Buf()
    mul = kb.sb([128, 4]); mul_b = Buf()
    w2 = kb.sb([64, FW]); a2 = kb.sb([64, FW]); g2a = kb.sb([128, FW]); g2b = kb.sb([96, FW]); lw_b = Buf()
    rows = kb.sb([64, 3 * FW]); rows_b = Buf()
    onescol = kb.sb([64, 1]); onescol_b = Buf()
    tw = kb.sb([64, S]); xad = kb.sb([64, S]); sg0 = kb.sb([128, S]); sg1 = kb.sb([96, S]); lora_b = Buf()
    A_b = Buf()
    big = kb.sb([128, 4097]); big_b = A_b
    big2 = kb.sb([128, 4096]); big2_b = A_b

    kb.dma(cst[:], cst_d[:, :], w=[cst_b])
    kb.dma(rmask[:], rmask_d[:, :], w=[rmask_b])
    kb.dma(cols[:], cols_d[:, :], w=[cols_b])
    kb.dma(mul[:], mul_d[:, :], w=[mul_b])
    kb.dma(w2[:], w2_d[:, :], w=[lw_b]); kb.dma(a2[:], a2_d[:, :], w=[lw_b])
    kb.dma(g2a[:], g2_d[0:128, :], w=[lw_b]); kb.dma(g2b[:], g2_d[128:224, :], w=[lw_b])
    kb.dma(rows[:], rows_d[:, :], w=[rows_b])
    kb.op("dve", lambda e: e.memset(onescol[:], 1.0), w=[onescol_b])
    ones64 = cst[:, 0:64]; ident = cst[:, 64:128]; maskG2 = cst[:, 128:384]
    maskU8 = cst[:, 384:896]; maskL8 = cst[:, 896:1408]; I8 = cst[:, 1408:1920]

    def shifted(src_ap, P, mucol, dst, func):
        kb.op("dve", lambda e: e.memset(big[0:P, 0:1], 0.0), r=[big_b], w=[big_b])
        kb.dma(big[0:P, 1:S + 1], src_ap, w=[big_b])
        kb.op("dve", lambda e: e.tensor_tensor(out=big2[0:P, 0:S], in0=big[0:P, 0:S], in1=big[0:P, 1:S + 1], op=ALU.subtract), r=[big_b], w=[big2_b])
        kb.op("dve", lambda e: e.scalar_tensor_tensor(out=big2[0:P, 0:S], in0=big2[0:P, 0:S], scalar=mucol, in1=big[0:P, 1:S + 1], op0=ALU.mult, op1=ALU.add),
              r=[big_b, big2_b, mul_b], w=[big2_b])
        if func is None:
            kb.op("act", lambda e: e.copy(dst, big2[0:P, 0:S]), r=[big2_b], w=[lora_b])
        else:
            kb.op("act", lambda e: e.activation(out=dst, in_=big2[0:P, 0:S], func=func), r=[big2_b], w=[lora_b])
    shifted(zwd_d[:, :], 64, mul[0:64, 0:1], tw[:], AF.Tanh)
    shifted(zad_d[:, :], 64, mul[0:64, 1:2], xad[:], None)
    shifted(zgd_d[0:128, :], 128, mul[:, 2:3], sg0[:], AF.Sigmoid)
    shifted(zgd_d[128:224, :], 96, mul[0:96, 3:4], sg1[:], AF.Sigmoid)

    class _V:
        def __init__(self, t, i):
            self.t, self.i = t, i

        def __getitem__(self, idx):
            return self.t[0:64, self.i * 512:(self.i + 1) * 512][idx]
    R, Kx, lwt, av, kk, kkn, Kp, cum = [_V(big, i) for i in range(8)]
    Ep, Em, Epr, t1, t2 = [_V(big2, i) for i in range(5)]
    zrt = kb.sb([64, 513]); zkt = kb.sb([64, 513]); zin_b = Buf()
    AR = kb.sb([64, 8, 128]); BK = kb.sb([64, 8, 128]); rkr = kb.sb([64, 512]); ARBK_b = Buf()
    St = kb.sb([64, 64]); St_b = Buf()
    Stw = kb.sb([64, 64]); Stw_b = Buf()
    Vz = kb.sb([64, 8, 64]); Vp = kb.sb([64, 8, 64]); Vx = kb.sb([64, 8, 64]); Vin_b = Buf(); Vx_b = Buf()
    Pm = [kb.sb([64, 512]) for _ in range(2)]; Qm = [kb.sb([64, 512]) for _ in range(2)]; TTm = [kb.sb([64, 512]) for _ in range(2)]
    Tm = kb.sb([64, 512]); C_b = Buf()
    Gm = kb.sb([64, 256]); Gm_b = Buf()
    BKT = kb.sb([64, 128]); BKT_b = Buf()
    Xs = kb.sb([64, 64]); Xs_b = Buf()
    Us = kb.sb([64, 64]); Us_b = Buf()
    ep1 = kb.sb([64, 8, 64]); ep2 = kb.sb([64, 8, 64]); ep_b = Buf()
    st8 = [kb.sb([64, 8]) for _ in range(4)]
    ps = [kb.ps([128, 512]) for _ in range(8)]; pb = bufs(8)

    for h in range(nh):
        cc = lambda k: cols[:, k * nh + h:k * nh + h + 1]
        hc = slice(h * 64, (h + 1) * 64)
        kb.op("dve", lambda e: e.memset(St[:], 0.0), r=[St_b], w=[St_b])
        for bi in range(NBT):
            t0 = bi * 512
            A_ = lambda eng, fn, extra_r=(): kb.op(eng, fn, r=[A_b, zin_b] + list(extra_r), w=[A_b])
            for (zt, zd) in [(zrt, zr_d), (zkt, zk_d)]:
                if bi == 0:
                    kb.op("dve", lambda e: e.memset(zt[:, 0:1], 0.0), r=[zin_b, A_b], w=[zin_b])
                    kb.dma(zt[:, 1:513], zd[hc, 0:512], w=[zin_b])
                else:
                    kb.dma(zt[:], zd[hc, t0 - 1:t0 + 512], w=[zin_b])
            for (zt, dst, k) in [(zrt, R, 0), (zkt, Kx, 1)]:
                A_("dve", lambda e: e.tensor_tensor(out=t1[:], in0=zt[:, 0:512], in1=zt[:, 1:513], op=ALU.subtract))
                A_("dve", lambda e: e.scalar_tensor_tensor(out=dst[:], in0=t1[:], scalar=cc(k), in1=zt[:, 1:513], op0=ALU.mult, op1=ALU.add), [cols_b])
            mm(kb, ps[0][0:64, :], w2[:, hc], tw[:, t0:t0 + 512], True, True, r=[lw_b, lora_b], w=[pb[0]])
            A_("act", lambda e: e.activation(out=lwt[:], in_=ps[0][0:64, :], func=AF.Sigmoid, bias=cc(2)), [pb[0], cols_b])
            A_("act", lambda e: e.mul(lwt[:], lwt[:], WDEC))
            mm(kb, ps[0][0:64, :], a2[:, hc], xad[:, t0:t0 + 512], True, True, r=[lw_b, lora_b], w=[pb[0]])
            A_("act", lambda e: e.activation(out=av[:], in_=ps[0][0:64, :], func=AF.Sigmoid, bias=cc(3)), [pb[0], cols_b])
            A_("dve", lambda e: e.tensor_scalar(out=kk[:], in0=Kx[:], scalar1=cc(4), scalar2=None, op0=ALU.mult), [cols_b])
            A_("act", lambda e: e.activation(out=t1[:], in_=kk[:], func=AF.Square))
            mm(kb, ps[0][0:64, :], ones64, t1[:], True, True, r=[cst_b, A_b], w=[pb[0]])
            A_("act", lambda e: e.sqrt(t2[:], ps[0][0:64, :]), [pb[0]])
            A_("dve", lambda e: e.tensor_scalar_max(t2[:], t2[:], 1e-12))
            A_("dve", lambda e: e.reciprocal(t2[:], t2[:]))
            A_("dve", lambda e: e.tensor_tensor(out=kkn[:], in0=kk[:], in1=t2[:], op=ALU.mult))
            A_("dve", lambda e: e.tensor_scalar(out=t1[:], in0=av[:], scalar1=-1.0, scalar2=None, op0=ALU.add))
            A_("dve", lambda e: e.tensor_scalar(out=t1[:], in0=t1[:], scalar1=cc(5), scalar2=None, op0=ALU.mult), [cols_b])
            A_("dve", lambda e: e.scalar_tensor_tensor(out=Kp[:], in0=t1[:], scalar=1.0, in1=Kx[:], op0=ALU.add, op1=ALU.mult))
            A_("dve", lambda e: e.tensor_tensor_scan(out=cum[:], data0=rmask[:], data1=lwt[:], initial=0.0, op0=ALU.mult, op1=ALU.add), [rmask_b])
            A_("act", lambda e: e.activation(out=Ep[:], in_=cum[:], func=AF.Exp))
            A_("act", lambda e: e.activation(out=Em[:], in_=cum[:], func=AF.Exp, scale=-1.0))
            A_("dve", lambda e: e.tensor_tensor(out=t1[:], in0=cum[:], in1=lwt[:], op=ALU.subtract))
            A_("act", lambda e: e.activation(out=Epr[:], in_=t1[:], func=AF.Exp))
            v3 = lambda t: t[:].rearrange("p (c t) -> p c t", t=64)
            AB = [A_b, ARBK_b]
            kb.op("dve", lambda e: e.scalar_tensor_tensor(out=AR[:, :, 0:64], in0=v3(kkn), scalar=-1.0, in1=v3(Epr), op0=ALU.mult, op1=ALU.mult), r=AB, w=[ARBK_b])
            kb.op("dve", lambda e: e.tensor_tensor(out=AR[:, :, 64:128], in0=v3(R), in1=v3(Ep), op=ALU.mult), r=AB, w=[ARBK_b])
            A_("dve", lambda e: e.tensor_tensor(out=t1[:], in0=kkn[:], in1=av[:], op=ALU.mult))
            kb.op("dve", lambda e: e.tensor_tensor(out=BK[:, :, 0:64], in0=v3(t1), in1=v3(Em), op=ALU.mult), r=AB, w=[ARBK_b])
            kb.op("dve", lambda e: e.tensor_tensor(out=BK[:, :, 64:128], in0=v3(Kp), in1=v3(Em), op=ALU.mult), r=AB, w=[ARBK_b])
            kb.op("dve", lambda e: e.scalar_tensor_tensor(out=rkr[:], in0=R[:], scalar=cc(6), in1=Kp[:], op0=ALU.mult, op1=ALU.mult), r=AB + [cols_b], w=[ARBK_b])
            Ep3 = v3(Ep)
            if RW_STOP == 'A':
                return kb.finish()
            kb.dma(Vz[:], vt_d[t0:t0 + 512, hc].rearrange("(c t) i -> t c i", t=64), r=[Vx_b], w=[Vin_b])
            kb.dma(Vp[:], vp_d[t0:t0 + 512, hc].rearrange("(c t) i -> t c i", t=64), r=[Vx_b], w=[Vin_b])
            muv = rows[:, hc].rearrange("p (o i) -> p o i", o=1).to_broadcast([64, 8, 64])
            kb.op("dve", lambda e: e.tensor_tensor(out=Vp[:], in0=Vp[:], in1=Vz[:], op=ALU.subtract), r=[Vin_b], w=[Vin_b])
            kb.op("dve", lambda e: e.tensor_tensor(out=Vp[:], in0=Vp[:], in1=muv, op=ALU.mult), r=[Vin_b, rows_b], w=[Vin_b])
            kb.op("dve", lambda e: e.tensor_tensor(out=Vx[:], in0=Vp[:], in1=Vz[:], op=ALU.add), r=[Vin_b], w=[Vx_b])
            CB = [C_b]
            for ch in range(8):
                mm(kb, ps[1][0:64, ch * 64:(ch + 1) * 64], BK[:, ch, 0:64], AR[:, ch, 0:64], True, True, r=[ARBK_b], w=[pb[1]])
                mm(kb, ps[2][0:64, ch * 64:(ch + 1) * 64], AR[:, ch, 0:64], BK[:, ch, 0:64], True, True, r=[ARBK_b], w=[pb[2]])
            kb.op("dve", lambda e: e.tensor_tensor(out=Pm[0][:], in0=ps[1][0:64, :], in1=maskU8, op=ALU.mult), r=[pb[1], cst_b] + CB, w=CB)
            kb.op("dve", lambda e: e.tensor_tensor(out=Qm[0][:], in0=ps[2][0:64, :], in1=maskL8, op=ALU.mult), r=[pb[2], cst_b] + CB, w=CB)
            kb.op("dve", lambda e: e.tensor_tensor(out=Tm[:], in0=Pm[0][:], in1=I8, op=ALU.add), r=[cst_b] + CB, w=CB)
            kb.op("dve", lambda e: e.tensor_tensor(out=TTm[0][:], in0=Qm[0][:], in1=I8, op=ALU.add), r=[cst_b] + CB, w=CB)
            NL = 5
            for lv in range(NL):
                a_, b_ = lv % 2, (lv + 1) % 2
                last = lv == NL - 1
                for ch in range(8):
                    sl = slice(ch * 64, (ch + 1) * 64)
                    mm(kb, ps[1][0:64, sl], Qm[a_][:, sl], Pm[a_][:, sl], True, True, r=CB, w=[pb[1]])
                    if not last:
                        mm(kb, ps[2][0:64, sl], Pm[a_][:, sl], Qm[a_][:, sl], True, True, r=CB, w=[pb[2]])
                kb.op("act", lambda e: e.copy(Pm[b_][:], ps[1][0:64, :]), r=[pb[1]] + CB, w=CB)
                if not last:
                    kb.op("dve", lambda e: e.tensor_copy(out=Qm[b_][:], in_=ps[2][0:64, :]), r=[pb[2]] + CB, w=CB)
                for ch in range(8):
                    sl = slice(ch * 64, (ch + 1) * 64)
                    mm(kb, ps[3][0:64, sl], TTm[a_][:, sl], Pm[b_][:, sl], True, True, r=CB, w=[pb[3]])
                    if not last:
                        mm(kb, ps[4][0:64, sl], Pm[b_][:, sl], TTm[a_][:, sl], True, True, r=CB, w=[pb[4]])
                kb.op("dve", lambda e: e.tensor_tensor(out=Tm[:], in0=ps[3][0:64, :], in1=Tm[:], op=ALU.add), r=[pb[3]] + CB, w=CB)
                if not last:
                    kb.op("dve", lambda e: e.tensor_tensor(out=TTm[b_][:], in0=ps[4][0:64, :], in1=TTm[a_][:], op=ALU.add), r=[pb[4]] + CB, w=CB)
            if RW_STOP == 'C':
                return kb.finish()
            for ch in range(8):
                mm(kb, ps[5][0:64, 0:128], BK[:, ch, 0:64], AR[:, ch, :], True, True, r=[ARBK_b], w=[pb[5]])
                mm(kb, ps[5][0:64, 128:256], BK[:, ch, 64:128], AR[:, ch, :], True, True, r=[ARBK_b], w=[pb[5]])
                kb.op("dve", lambda e: e.tensor_tensor(out=Gm[:], in0=ps[5][0:64, 0:256], in1=maskG2, op=ALU.mult), r=[pb[5], cst_b], w=[Gm_b])
                kb.op("pe", lambda e: e.transpose(ps[5][0:64, 256:320], BK[:, ch, 0:64], ident), r=[ARBK_b, cst_b], w=[pb[5]])
                kb.op("pe", lambda e: e.transpose(ps[5][0:64, 320:384], BK[:, ch, 64:128], ident), r=[ARBK_b, cst_b], w=[pb[5]])
                kb.op("act", lambda e: e.copy(BKT[:], ps[5][0:64, 256:384]), r=[pb[5]], w=[BKT_b])
                mm(kb, ps[6][0:64, 0:64], AR[:, ch, 0:64], St[:], True, False, r=[ARBK_b, St_b], w=[pb[6]])
                mm(kb, ps[6][0:64, 0:64], Gm[:, 128:192], Vx[:, ch, :], False, True, r=[Gm_b, Vx_b], w=[pb[6]])
                kb.op("act", lambda e: e.copy(Xs[:], ps[6][0:64, 0:64]), r=[pb[6]], w=[Xs_b])
                mm(kb, ps[6][0:64, 64:128], Tm[:, ch * 64:(ch + 1) * 64], Xs[:], True, True, r=CB + [Xs_b], w=[pb[6]])
                kb.op("act", lambda e: e.copy(Us[:], ps[6][0:64, 64:128]), r=[pb[6]], w=[Us_b])
                ysl = slice(ch * 64, (ch + 1) * 64)
                mm(kb, ps[7][0:64, ysl], AR[:, ch, 64:128], St[:], True, False, r=[ARBK_b, St_b], w=[pb[7]])
                mm(kb, ps[7][0:64, ysl], Gm[:, 64:128], Us[:], False, False, r=[Gm_b, Us_b], w=[pb[7]])
                mm(kb, ps[7][0:64, ysl], Gm[:, 192:256], Vx[:, ch, :], False, True, r=[Gm_b, Vx_b], w=[pb[7]])
                kb.op("dve", lambda e: e.tensor_scalar(out=Stw[:], in0=St[:], scalar1=Ep3[:, ch, 63:64], scalar2=None, op0=ALU.mult), r=[St_b, A_b], w=[Stw_b])
                mm(kb, ps[6][0:64, 128:192], BKT[:, 0:64], Us[:], True, False, r=[BKT_b, Us_b], w=[pb[6]])
                mm(kb, ps[6][0:64, 128:192], BKT[:, 64:128], Vx[:, ch, :], False, True, r=[BKT_b, Vx_b], w=[pb[6]])
                kb.op("dve", lambda e: e.scalar_tensor_tensor(out=St[:], in0=ps[6][0:64, 128:192], scalar=Ep3[:, ch, 63:64], in1=Stw[:], op0=ALU.mult, op1=ALU.add),
                      r=[pb[6], A_b, Stw_b], w=[St_b])
                mm(kb, ps[2][0:64, ch:ch + 1], rkr[:, ch * 64:(ch + 1) * 64], onescol[:], True, True, r=[ARBK_b, onescol_b], w=[pb[2]])
                mm(kb, ps[1][0:64, ysl], sg0[:, t0 + ch * 64:t0 + (ch + 1) * 64], g2a[:, hc], True, False, r=[lora_b, lw_b], w=[pb[1]])
                mm(kb, ps[1][0:64, ysl], sg1[:, t0 + ch * 64:t0 + (ch + 1) * 64], g2b[:, hc], False, True, r=[lora_b, lw_b], w=[pb[1]])
            if RW_STOP == 'D':
                return kb.finish()
            E = [ep_b]
            Y3 = ps[7][0:64, :].rearrange("p (c i) -> p c i", i=64)
            bc8 = lambda t: t[:, :].rearrange("p (c o) -> p c o", o=1).to_broadcast([64, 8, 64])
            rowb = lambda k: rows[:, k * FW + h * 64:k * FW + (h + 1) * 64].rearrange("p (o i) -> p o i", o=1).to_broadcast([64, 8, 64])
            kb.op("dve", lambda e: e.tensor_reduce(out=st8[0][:], in_=Y3, axis=AX.X, op=ALU.add), r=[pb[7]] + E, w=E)
            kb.op("dve", lambda e: e.tensor_single_scalar(out=st8[0][:], in_=st8[0][:], scalar=1.0 / 64, op=ALU.mult), r=E, w=E)
            kb.op("dve", lambda e: e.tensor_tensor(out=ep1[:], in0=Y3, in1=bc8(st8[0]), op=ALU.subtract), r=[pb[7]] + E, w=E)
            kb.op("dve", lambda e: e.tensor_tensor(out=ep2[:], in0=ep1[:], in1=ep1[:], op=ALU.mult), r=E, w=E)
            kb.op("dve", lambda e: e.tensor_reduce(out=st8[1][:], in_=ep2[:], axis=AX.X, op=ALU.add), r=E, w=E)
            kb.op("dve", lambda e: e.tensor_scalar(out=st8[1][:], in0=st8[1][:], scalar1=1.0 / 64, scalar2=RW_GN_EPS, op0=ALU.mult, op1=ALU.add), r=E, w=E)
            kb.op("act", lambda e: e.sqrt(st8[1][:], st8[1][:]), r=E, w=E)
            kb.op("dve", lambda e: e.reciprocal(st8[1][:], st8[1][:]), r=E, w=E)
            kb.op("dve", lambda e: e.tensor_tensor(out=ep1[:], in0=ep1[:], in1=bc8(st8[1]), op=ALU.mult), r=E, w=E)
            kb.op("dve", lambda e: e.tensor_tensor(out=ep1[:], in0=ep1[:], in1=rowb(1), op=ALU.mult), r=E + [rows_b], w=E)
            kb.op("dve", lambda e: e.tensor_tensor(out=ep1[:], in0=ep1[:], in1=rowb(2), op=ALU.add), r=E + [rows_b], w=E)
            kb.op("act", lambda e: e.copy(st8[2][:], ps[2][0:64, 0:8]), r=[pb[2]] + E, w=E)
            kb.op("dve", lambda e: e.tensor_tensor(out=ep2[:], in0=Vx[:], in1=bc8(st8[2]), op=ALU.mult), r=E + [Vx_b], w=E)
            kb.op("dve", lambda e: e.tensor_tensor(out=ep1[:], in0=ep1[:], in1=ep2[:], op=ALU.add), r=E, w=E)
            kb.op("dve", lambda e: e.tensor_tensor(out=ep2[:], in0=ep1[:], in1=ps[1][0:64, :].rearrange("p (c i) -> p c i", i=64), op=ALU.mult), r=E + [pb[1]], w=E)
            kb.dma(out_d[t0:t0 + 512, hc].rearrange("(c t) i -> t c i", t=64), ep2[:], r=E, final=True)
    return kb.finish()


def run_rwkv(z1, rw, S=SEQ, nh=12, ncores=NCORES):
    nc = build_rwkv(S, nh)
    FW = nh * 64
    base = 4608
    mu = rw["mu"]
    s_ = np.arange(64)[:, None]; q_ = np.arange(128)[None, :]
    maskG = np.where(q_ < 64, s_ < q_, s_ <= (q_ - 64)).astype(np.float32)
    r64 = np.arange(64)[:, None]; c64 = np.arange(64)[None, :]
    U8 = np.tile((r64 < c64).astype(np.float32), (1, 8)); L8 = np.tile((r64 > c64).astype(np.float32), (1, 8)); I8 = np.tile(np.eye(64, dtype=np.float32), (1, 8))
    cst = np.zeros((64, 2048), np.float32)
    cst[:, 0:64] = 1.0; cst[:, 64:128] = np.eye(64); cst[:, 128:256] = maskG; cst[:, 256:384] = maskG
    cst[:, 384:896] = U8; cst[:, 896:1408] = L8; cst[:, 1408:1920] = I8
    rmask = np.ones((64, 512), np.float32); rmask[:, ::64] = 0
    in_maps = []
    for jc in range(ncores):
        b, hf = jc // 2, jc % 2
        fs = slice(hf * 768, hf * 768 + FW)
        def colv(v):
            return v[fs].reshape(nh, 64).T
        cols = np.zeros((64, 8 * nh), np.float32)
        for k, v in enumerate([mu[0:1536], mu[1536:3072], rw["w0"], rw["a0"], rw["k_k"], rw["k_a"], rw["r_k"].reshape(-1)]):
            cols[:, k * nh:(k + 1) * nh] = colv(v)
        mul = np.zeros((128, 4), np.float32)
        mul[0:64, 0] = mu[4608:4672]; mul[0:64, 1] = mu[4672:4736]; mul[:, 2] = mu[4736:4864]; mul[0:96, 3] = mu[4864:4960]
        rows = np.concatenate([np.broadcast_to(v[fs][None, :], (64, FW)) for v in [mu[3072:4608], rw["lnx_g"], rw["lnx_b"]]], 1)
        v_tok = np.ascontiguousarray(z1[b, base + 3072 + hf * 768: base + 3072 + hf * 768 + FW, :S].T)
        vprev = np.concatenate([np.zeros((1, FW), np.float32), v_tok[:-1]], 0)
        in_maps.append({
            "zr": np.ascontiguousarray(z1[b, base + hf * 768: base + hf * 768 + FW, :S]),
            "zk": np.ascontiguousarray(z1[b, base + 1536 + hf * 768: base + 1536 + hf * 768 + FW, :S]),
            "v_tok": v_tok, "vprev_tok": np.ascontiguousarray(vprev),
            "zwd": np.ascontiguousarray(z1[b, base + 4608:base + 4672, :S]), "zad": np.ascontiguousarray(z1[b, base + 4672:base + 4736, :S]),
            "zgd": np.ascontiguousarray(z1[b, base + 4736:base + 4960, :S]),
            "cols": cols, "mu_lora": mul, "w2": np.ascontiguousarray(rw["w2"][:, fs]), "a2": np.ascontiguousarray(rw["a2"][:, fs]),
            "g2": np.ascontiguousarray(rw["g2"][:, fs]), "rows": np.ascontiguousarray(rows), "cst": cst, "rmask": rmask})
    res = run_bass_kernel_spmd(nc, in_maps, core_ids=list(range(ncores)))
    tm = np.zeros((4, 1536, S), np.float32)
    for jc in range(ncores):
        b, hf = jc // 2, jc % 2
        tm[b, hf * 768:hf * 768 + FW] = res.results[jc]["tm_tok"].T
    return tm


def run_post(layer0, mixT, x_tok, mod_l, wout, wglu, ln, router_w, router_b, wgu, wd):
    nc = build_post(layer0)
    sel = np.zeros((16, 16, 128), np.float32)
    for e in range(16):
        sel[e, e, :] = 1.0
    sel = sel.reshape(16, 2048)
    ident = np.eye(128, dtype=np.float32)
    rb_bc = np.ascontiguousarray(np.broadcast_to(router_b[None, :], (128, 16)))
    in_maps = []
    for j in range(NCORES):
        b, hf = j // 2, j % 2
        ts = slice(hf * 2048, (hf + 1) * 2048)
        m = mod_l[b]
        vecs = [m[2 * D:3 * D], m[4 * D:5 * D], m[3 * D:4 * D], m[5 * D:6 * D], ln[0], ln[1], ln[2], ln[3]]
        pvec = np.ascontiguousarray(np.concatenate([fm16(v) for v in vecs], 1))
        im = {"mixT": np.ascontiguousarray(mixT[b][:, ts]), "xT": np.ascontiguousarray(x_tok[b, ts].T), "wout": wout, "pvec": pvec,
              "router_w": router_w, "router_b_bc": rb_bc, "wgu": wgu, "wd": wd, "ident": ident, "sel": sel}
        if layer0:
            im["wglu"] = wglu
        in_maps.append(im)
    res = run_bass_kernel_spmd(nc, in_maps, core_ids=list(range(NCORES)))
    out = np.zeros((4, SEQ, D), np.float32)
    for j in range(NCORES):
        b, hf = j // 2, j % 2
        out[b, hf * 2048:(hf + 1) * 2048] = res.results[j]["xoT"].T
    return out


def kernel(x, c, ada_w, ada_b, ln_mix_g, ln_mix_b, ln_ffn_g, ln_ffn_b, router_w, router_b,
           moe_w_gate_up, moe_w_down, rel_bias, ev_w_in, mla_q_norm, mla_w_uq, mla_kv_norm, mla_w_ukv,
           s5_lambda_re, s5_lambda_im, s5_b_re, s5_b_im, s5_c_re, s5_c_im, s5_d, s5_log_dt, s5_w_glu,
           ev_w_out, od_w_in, rw_mu, rw_w0, rw_w2, rw_a0, rw_a2, rw_g2, rw_k_k, rw_k_a, rw_r_k,
           rw_lnx_g, rw_lnx_b, od_w_out):
    f = lambda a: np.ascontiguousarray(np.asarray(a, dtype=np.float32))
    x = f(x)
    mod = run_ada(f(c), f(ada_w), f(ada_b))
    w = f(ev_w_in[0])
    wext = np.ascontiguousarray(np.concatenate([w, w[:, 800:832], w[:, 768:800]], 1))
    z0 = run_pre(x, mod[0], wext, 1, 0)
    att0 = run_mla(z0, f(mla_w_uq[0]), f(mla_w_ukv[0]), f(mla_q_norm[0]), f(mla_kv_norm[0]))
    ys5 = run_s5(z0, f(s5_lambda_re[0]), f(s5_lambda_im[0]), f(s5_b_re[0]), f(s5_b_im[0]), f(s5_c_re[0]), f(s5_c_im[0]), f(s5_d[0]), f(s5_log_dt[0]))
    del z0
    mix0 = np.concatenate([att0, ys5], 1)
    x1 = run_post(True, mix0, x, mod[0], f(ev_w_out[0]), f(s5_w_glu[0]), [f(ln_mix_g[0]), f(ln_mix_b[0]), f(ln_ffn_g[0]), f(ln_ffn_b[0])],
                  f(router_w), f(router_b), f(moe_w_gate_up[0]), f(moe_w_down[0]))
    del mix0, att0, ys5
    w = f(od_w_in[0])
    wext = np.ascontiguousarray(np.concatenate([w, np.zeros((D, 9600 - w.shape[1]), np.float32)], 1))
    z1 = run_pre(x1, mod[1], wext, 1, 0)
    att1 = run_dil(z1, f(rel_bias))
    rw = {"mu": f(rw_mu[0]), "w0": f(rw_w0[0]), "w2": f(rw_w2[0]), "a0": f(rw_a0[0]), "a2": f(rw_a2[0]), "g2": f(rw_g2[0]),
          "k_k": f(rw_k_k[0]), "k_a": f(rw_k_a[0]), "r_k": f(rw_r_k[0]), "lnx_g": f(rw_lnx_g[0]), "lnx_b": f(rw_lnx_b[0])}
    tm = run_rwkv(z1, rw)
    del z1
    mix1 = np.concatenate([att1, tm], 1)
    x2 = run_post(False, mix1, x1, mod[1], f(od_w_out[0]), None, [f(ln_mix_g[1]), f(ln_mix_b[1]), f(ln_ffn_g[1]), f(ln_ffn_b[1])],
                  f(router_w), f(router_b), f(moe_w_gate_up[1]), f(moe_w_down[1]))
    return x2.astype(np.float32)
```

```python
import contextlib
import numpy as np
import concourse.bass as bass
import concourse.mybir as mybir
from concourse.bass_utils import run_bass_kernel_spmd

F32 = mybir.dt.float32
BF16 = mybir.dt.bfloat16
AF = mybir.ActivationFunctionType
ALU = mybir.AluOpType
AX = mybir.AxisListType

D = 2048
NCORES = 8
DN_ALPHA = 4.0 ** 0.25
LN_EPS = 1e-5


SAME_ENGINE_WAIT = True


class Buf:
    __slots__ = ("w", "r", "dsem", "dval")

    def __init__(self):
        self.w = None
        self.r = {}
        self.dsem = None
        self.dval = 0


def bufs(n):
    return [Buf() for _ in range(n)]


class KB:
    def __init__(self):
        self.nc = bass.Bass("TRN2", target_bir_lowering=False)
        nc = self.nc
        self.eng = {"pe": nc.tensor, "act": nc.scalar, "dve": nc.vector, "pool": nc.gpsimd, "sp": nc.sync}
        self.stack = contextlib.ExitStack()
        self.sem, self.seq, self.seen = {}, {}, {}
        for e in self.eng:
            self.sem[e] = self.stack.enter_context(nc.semaphore("s_" + e))
            self.seq[e] = 0
            self.seen[e] = {}
        self.nsem = 0
        self.finals = []
        self.nm = 0

    def name(self, p):
        self.nm += 1
        return "%s%d" % (p, self.nm)

    def din(self, name, shape, dt=F32):
        return self.nc.dram_tensor(name, list(shape), dt, kind="ExternalInput").ap()

    def dout(self, name, shape, dt=F32):
        return self.nc.dram_tensor(name, list(shape), dt, kind="ExternalOutput").ap()

    def sb(self, shape, dt=F32, name=None):
        return self.stack.enter_context(self.nc.sbuf_tensor(name or self.name("sb"), list(shape), dt))

    def ps(self, shape, dt=F32, name=None):
        return self.stack.enter_context(self.nc.psum_tensor(name or self.name("ps"), list(shape), dt))

    def _wait(self, e, ev):
        sem, val, src = ev
        key = id(sem)
        if self.seen[e].get(key, 0) >= val:
            return
        if src == e and (e == "pe" or not SAME_ENGINE_WAIT):
            return
        self.eng[e].wait_ge(sem, val)
        self.seen[e][key] = val

    def _deps(self, e, r, w):
        for b in r:
            if b.w is not None:
                self._wait(e, b.w)
        for b in w:
            if b.w is not None:
                self._wait(e, b.w)
            for ev in b.r.values():
                self._wait(e, ev)

    def _record(self, ev, r, w):
        for b in r:
            b.r[id(ev[0])] = ev
        for b in w:
            b.w = ev
            b.r = {}

    def op(self, e, fn, r=(), w=()):
        self._deps(e, r, w)
        ins = fn(self.eng[e])
        self.seq[e] += 1
        ins.then_inc(self.sem[e], 1)
        self._record((self.sem[e], self.seq[e], e), r, w)
        return ins

    def dma(self, out, in_, r=(), w=(), q="sp", final=False):
        self._deps(q, r, w)
        ins = self.eng[q].dma_start(out=out, in_=in_)
        owner = w[0] if w else r[0]
        if owner.dsem is None:
            owner.dsem = self.stack.enter_context(self.nc.semaphore(self.name("sd")))
            self.nsem += 1
        owner.dval += 16
        ins.then_inc(owner.dsem, 16)
        ev = (owner.dsem, owner.dval, "dma")
        self._record(ev, r, w)
        if final:
            self.finals.append(ev)
        return ins

    def finish(self):
        for ev in self.finals:
            self._wait("sp", ev)
        self.stack.close()
        return self.nc


def mm(kb, out, lhsT, rhs, start, stop, r, w):
    return kb.op("pe", lambda e: e.matmul(out, lhsT=lhsT, rhs=rhs, start=start, stop=stop), r=r, w=w)


TT = 512


def build_post(layer0, ntok=2048):
    kb = KB()
    nc = kb.nc
    NT = ntok // TT
    mixT = kb.din("mixT", [D, ntok]).rearrange("(c p) t -> p c t", p=128)
    xT = kb.din("xT", [D, ntok]).rearrange("(c p) t -> p c t", p=128)
    wout = kb.din("wout", [D, D]).rearrange("(c p) n -> p c n", p=128)
    if layer0:
        wglu = kb.din("wglu", [1024, 1024]).rearrange("(c p) n -> p c n", p=128)
    pvec_d = kb.din("pvec", [128, 8 * 16])
    rw_d = kb.din("router_w", [D, 16]).rearrange("(c p) n -> p c n", p=128)
    rb_d = kb.din("router_b_bc", [128, 16])
    wgu = kb.din("wgu", [16, D, 1024])
    wd = kb.din("wd", [16, 512, D])
    ident_d = kb.din("ident", [128, 128])
    sel_d = kb.din("sel", [16, 16 * 128])
    xoT = kb.dout("xoT", [D, ntok]).rearrange("(c p) t -> p c t", p=128)

    xt = kb.sb([128, 16, TT]); xt_b = bufs(16)
    yacc = kb.sb([128, 16, TT]); yacc_b = bufs(16)
    mixb = kb.sb([128, 16, TT], BF16); mixb_b = bufs(16)
    hT = kb.sb([128, 16, TT], BF16); hT_b = bufs(16)
    NSTG = 2
    stg = [kb.sb([128, 2048]) for _ in range(NSTG)]; stg_b = bufs(NSTG)
    NWB = 6
    wb = [kb.sb([128, 4096], BF16) for _ in range(NWB)]; wb_b = bufs(NWB)
    aT = kb.sb([128, 4, TT], BF16); aT_b = bufs(4)
    tmp = [kb.sb([128, TT]) for _ in range(4)]; tmp_b = bufs(4)
    h32 = [kb.sb([128, TT]) for _ in range(2)]; h32_b = bufs(2)
    mean = kb.sb([128, TT]); mean_b = Buf()
    rstd = kb.sb([128, TT]); rstd_b = Buf()
    pvec = kb.sb([128, 8 * 16]); pvec_b = Buf()
    pv1 = kb.sb([128, 8 * 16]); pv1_b = Buf()
    rw = kb.sb([128, 16, 16]); rw_b = Buf()
    rb = kb.sb([128, 16]); rb_b = Buf()
    ident = kb.sb([128, 128]); ident_b = Buf()
    ones = kb.sb([128, 128]); ones_b = Buf()
    sel = kb.sb([16, 16 * 128]); sel_b = Buf()
    lgT = kb.sb([16, TT]); lgT_b = Buf()
    gatesT = kb.sb([16, TT]); gatesT_b = Buf()
    R = {n: kb.sb([128, 4, 16], name="r_" + n) for n in ["s", "sb", "masked", "m2", "sel1", "sel2", "ssel", "gates"]}
    R_b = {n: Buf() for n in R}
    S4 = {n: kb.sb([128, 16], name="q_" + n) for n in ["p0", "p1", "gscore", "gmask", "pen"]}
    S4_b = {n: Buf() for n in S4}
    S1 = {n: kb.sb([128, 4], name="o_" + n) for n in ["gmax", "m1", "m2", "den", "rden"]}
    S1_b = {n: Buf() for n in S1}

    pbank = [kb.ps([128, TT]) for _ in range(8)]; pb = bufs(8)

    stg_i = [0]; wb_i = [0]; tmp_i = [0]

    def nxt(ctr, n):
        i = ctr[0] % n
        ctr[0] += 1
        return i

    kb.dma(pvec[:], pvec_d[:, :], w=[pvec_b])
    kb.dma(rw[:], rw_d[:, :, :], w=[rw_b])
    kb.dma(rb[:], rb_d[:, :], w=[rb_b])
    kb.dma(ident[:], ident_d[:, :], w=[ident_b])
    kb.dma(sel[:], sel_d[:, :], w=[sel_b])
    kb.op("dve", lambda e: e.memset(ones[:], 1.0), w=[ones_b])
    kb.op("dve", lambda e: e.tensor_scalar_add(pv1[:], pvec[:], 1.0), r=[pvec_b], w=[pv1_b])

    def pcol(which, c, plus1=False):
        t = pv1 if plus1 else pvec
        return t[:, which * 16 + c: which * 16 + c + 1]

    def load_weight(src_ap_fn, ncols):
        wi = nxt(wb_i, NWB)
        src_ap_fn(wb[wi], wb_b[wi])
        return wb[wi], wb_b[wi]

    def layernorm(gi, bi, emit_h, out_dma_t0=None):
        ps_sum, ps_sq = pbank[6], pbank[7]
        for c in range(16):
            ti = nxt(tmp_i, 4)
            kb.op("act", lambda e: e.activation(out=tmp[ti][:], in_=xt[:, c, :], func=AF.Square), r=[xt_b[c]], w=[tmp_b[ti]])
            mm(kb, ps_sum[:], ones[:], xt[:, c, :], c == 0, c == 15, r=[ones_b, xt_b[c]], w=[pb[6]])
            mm(kb, ps_sq[:], ones[:], tmp[ti][:], c == 0, c == 15, r=[ones_b, tmp_b[ti]], w=[pb[7]])
        kb.op("act", lambda e: e.mul(mean[:], ps_sum[:], 1.0 / D), r=[pb[6]], w=[mean_b])
        ti = nxt(tmp_i, 4)
        kb.op("dve", lambda e: e.tensor_tensor(out=tmp[ti][:], in0=mean[:], in1=mean[:], op=ALU.mult), r=[mean_b], w=[tmp_b[ti]])
        kb.op("dve", lambda e: e.scalar_tensor_tensor(out=rstd[:], in0=ps_sq[:], scalar=1.0 / D, in1=tmp[ti][:], op0=ALU.mult, op1=ALU.subtract),
              r=[pb[7], tmp_b[ti]], w=[rstd_b])
        kb.op("dve", lambda e: e.tensor_scalar_add(rstd[:], rstd[:], LN_EPS), r=[rstd_b], w=[rstd_b])
        kb.op("act", lambda e: e.sqrt(rstd[:], rstd[:]), r=[rstd_b], w=[rstd_b])
        kb.op("dve", lambda e: e.reciprocal(rstd[:], rstd[:]), r=[rstd_b], w=[rstd_b])
        for c in range(16):
            kb.op("dve", lambda e: e.tensor_tensor(out=xt[:, c, :], in0=xt[:, c, :], in1=mean[:], op=ALU.subtract), r=[xt_b[c], mean_b], w=[xt_b[c]])
            kb.op("dve", lambda e: e.tensor_tensor(out=xt[:, c, :], in0=xt[:, c, :], in1=rstd[:], op=ALU.mult), r=[xt_b[c], rstd_b], w=[xt_b[c]])
            kb.op("act", lambda e: e.activation(out=xt[:, c, :], in_=xt[:, c, :], func=AF.Identity, scale=pcol(gi, c), bias=pcol(bi, c)),
                  r=[xt_b[c], pvec_b], w=[xt_b[c]])
            if emit_h:
                hi = c % 2
                kb.op("dve", lambda e: e.tensor_scalar(out=h32[hi][:], in0=xt[:, c, :], scalar1=pcol(1, c, True), scalar2=pcol(2, c), op0=ALU.mult, op1=ALU.add),
                      r=[xt_b[c], pvec_b, pv1_b], w=[h32_b[hi]])
                kb.op("act", lambda e: e.copy(hT[:, c, :], h32[hi][:]), r=[h32_b[hi]], w=[hT_b[c]])
                mm(kb, pbank[5][0:16, :], rw[:, c, :], h32[hi][:], c == 0, c == 15, r=[rw_b, h32_b[hi]], w=[pb[5]])
            if out_dma_t0 is not None:
                kb.dma(xoT[:, c, out_dma_t0:out_dma_t0 + TT], xt[:, c, :], r=[xt_b[c]], final=True)

    for tt in range(NT):
        t0 = tt * TT
        kb.dma(xt[:], xT[:, :, t0:t0 + TT], w=xt_b)
        for c in range(16):
            kb.op("act", lambda e: e.mul(xt[:, c, :], xt[:, c, :], DN_ALPHA), r=[xt_b[c]], w=[xt_b[c]])
        for q in range(4):
            si = nxt(stg_i, NSTG)
            sv = stg[si][:, :4 * TT].rearrange("p (c t) -> p c t", c=4)
            kb.dma(sv, mixT[:, 4 * q:4 * q + 4, t0:t0 + TT], w=[stg_b[si]])
            for cc in range(4):
                c = 4 * q + cc
                if layer0 and c >= 8:
                    ti = nxt(tmp_i, 4)
                    kb.op("dve", lambda e: e.tensor_tensor(out=tmp[ti][:], in0=sv[:, cc, :], in1=sv[:, cc, :], op=ALU.mult), r=[stg_b[si]], w=[tmp_b[ti]])
                    kb.op("dve", lambda e: e.tensor_scalar(out=tmp[ti][:], in0=tmp[ti][:], scalar1=0.044715, scalar2=1.0, op0=ALU.mult, op1=ALU.add), r=[tmp_b[ti]], w=[tmp_b[ti]])
                    kb.op("dve", lambda e: e.tensor_tensor(out=tmp[ti][:], in0=tmp[ti][:], in1=sv[:, cc, :], op=ALU.mult), r=[tmp_b[ti], stg_b[si]], w=[tmp_b[ti]])
                    kb.op("act", lambda e: e.activation(out=tmp[ti][:], in_=tmp[ti][:], func=AF.Sigmoid, scale=1.5957691216), r=[tmp_b[ti]], w=[tmp_b[ti]])
                    kb.op("dve", lambda e: e.tensor_tensor(out=hT[:, c, :], in0=tmp[ti][:], in1=sv[:, cc, :], op=ALU.mult), r=[tmp_b[ti], stg_b[si]], w=[hT_b[c]])
                else:
                    kb.op("act", lambda e: e.copy(mixb[:, c, :], sv[:, cc, :]), r=[stg_b[si]], w=[mixb_b[c]])
        if layer0:
            for m in range(8):
                def ld(st, sbuf_, m=m):
                    kb.dma(st[:, :8 * 128].rearrange("p (c n) -> p c n", c=8), wglu[:, :, m * 128:(m + 1) * 128], w=[sbuf_], q="pool")
                wt, wtb = load_weight(ld, 8 * 128)
                pi = m % 2
                for k in range(8):
                    mm(kb, pbank[pi][:], wt[:, k * 128:(k + 1) * 128], hT[:, 8 + k, :], k == 0, k == 7, r=[wtb, hT_b[8 + k]], w=[pb[pi]])
                ti = nxt(tmp_i, 4)
                kb.op("act", lambda e: e.activation(out=tmp[ti][:], in_=pbank[pi][:], func=AF.Sigmoid), r=[pb[pi]], w=[tmp_b[ti]])
                kb.op("dve", lambda e: e.tensor_tensor(out=mixb[:, 8 + m, :], in0=tmp[ti][:], in1=hT[:, 8 + m, :], op=ALU.mult), r=[tmp_b[ti], hT_b[8 + m]], w=[mixb_b[8 + m]])
        for m in range(16):
            def ld(st, sbuf_, m=m):
                kb.dma(st[:, :16 * 128].rearrange("p (c n) -> p c n", c=16), wout[:, :, m * 128:(m + 1) * 128], w=[sbuf_], q="pool")
            wt, wtb = load_weight(ld, 16 * 128)
            pi = m % 2
            for k in range(16):
                mm(kb, pbank[pi][:], wt[:, k * 128:(k + 1) * 128], mixb[:, k, :], k == 0, k == 15, r=[wtb, mixb_b[k]], w=[pb[pi]])
            kb.op("dve", lambda e: e.scalar_tensor_tensor(out=xt[:, m, :], in0=pbank[pi][:], scalar=pcol(0, m, True), in1=xt[:, m, :], op0=ALU.mult, op1=ALU.add),
                  r=[pb[pi], pv1_b, xt_b[m]], w=[xt_b[m]])
        layernorm(4, 5, True)
        kb.op("act", lambda e: e.copy(lgT[:], pbank[5][0:16, :]), r=[pb[5]], w=[lgT_b])
        for s in range(4):
            kb.op("pe", lambda e: e.transpose(pbank[4][:, s * 16:(s + 1) * 16], lgT[:, s * 128:(s + 1) * 128], ident[0:16, 0:16]), r=[lgT_b, ident_b], w=[pb[4]])
        lg = pbank[4][:, 0:64].rearrange("p (s e) -> p s e", s=4)
        kb.op("act", lambda e: e.activation(out=R["s"][:], in_=lg, func=AF.Sigmoid), r=[pb[4]], w=[R_b["s"]])
        rb_bc = rb[:].rearrange("p (o e) -> p o e", o=1).to_broadcast([128, 4, 16])
        kb.op("dve", lambda e: e.tensor_tensor(out=R["sb"][:], in0=R["s"][:], in1=rb_bc, op=ALU.add), r=[R_b["s"], rb_b], w=[R_b["sb"]])
        sbg = R["sb"][:].rearrange("p s (g k) -> p (s g) k", k=4)
        first = True
        for (i, j) in [(0, 1), (0, 2), (0, 3), (1, 2), (1, 3), (2, 3)]:
            if first:
                kb.op("dve", lambda e: e.tensor_tensor(out=S4["gscore"][:], in0=sbg[:, :, i], in1=sbg[:, :, j], op=ALU.add), r=[R_b["sb"]], w=[S4_b["gscore"]])
                first = False
            else:
                kb.op("dve", lambda e: e.tensor_tensor(out=S4["p0"][:], in0=sbg[:, :, i], in1=sbg[:, :, j], op=ALU.add), r=[R_b["sb"]], w=[S4_b["p0"]])
                kb.op("dve", lambda e: e.tensor_tensor(out=S4["gscore"][:], in0=S4["gscore"][:], in1=S4["p0"][:], op=ALU.max), r=[S4_b["p0"], S4_b["gscore"]], w=[S4_b["gscore"]])
        gs3 = S4["gscore"][:].rearrange("p (s g) -> p s g", g=4)
        kb.op("dve", lambda e: e.tensor_reduce(out=S1["gmax"][:], in_=gs3, axis=AX.X, op=ALU.max), r=[S4_b["gscore"]], w=[S1_b["gmax"]])
        gmax_bc = S1["gmax"][:].rearrange("p (s o) -> p s o", o=1).to_broadcast([128, 4, 4])
        gm3 = S4["gmask"][:].rearrange("p (s g) -> p s g", g=4)
        kb.op("dve", lambda e: e.tensor_tensor(out=gm3, in0=gs3, in1=gmax_bc, op=ALU.is_equal), r=[S4_b["gscore"], S1_b["gmax"]], w=[S4_b["gmask"]])
        kb.op("dve", lambda e: e.tensor_scalar(out=S4["pen"][:], in0=S4["gmask"][:], scalar1=1e30, scalar2=-1e30, op0=ALU.mult, op1=ALU.add), r=[S4_b["gmask"]], w=[S4_b["pen"]])
        gmask_bc = S4["gmask"][:].rearrange("p (q o) -> p q o", o=1).to_broadcast([128, 16, 4])
        pen_bc = S4["pen"][:].rearrange("p (q o) -> p q o", o=1).to_broadcast([128, 16, 4])
        msk = R["masked"][:].rearrange("p s (g k) -> p (s g) k", k=4)
        kb.op("dve", lambda e: e.tensor_tensor(out=msk, in0=sbg, in1=gmask_bc, op=ALU.mult), r=[R_b["sb"], S4_b["gmask"]], w=[R_b["masked"]])
        kb.op("dve", lambda e: e.tensor_tensor(out=msk, in0=msk, in1=pen_bc, op=ALU.add), r=[R_b["masked"], S4_b["pen"]], w=[R_b["masked"]])
        kb.op("dve", lambda e: e.tensor_reduce(out=S1["m1"][:], in_=R["masked"][:], axis=AX.X, op=ALU.max), r=[R_b["masked"]], w=[S1_b["m1"]])
        m1_bc = S1["m1"][:].rearrange("p (s o) -> p s o", o=1).to_broadcast([128, 4, 16])
        kb.op("dve", lambda e: e.tensor_tensor(out=R["sel1"][:], in0=R["masked"][:], in1=m1_bc, op=ALU.is_equal), r=[R_b["masked"], S1_b["m1"]], w=[R_b["sel1"]])
        kb.op("dve", lambda e: e.scalar_tensor_tensor(out=R["m2"][:], in0=R["sel1"][:], scalar=-1e30, in1=R["masked"][:], op0=ALU.mult, op1=ALU.add),
              r=[R_b["sel1"], R_b["masked"]], w=[R_b["m2"]])
        kb.op("dve", lambda e: e.tensor_reduce(out=S1["m2"][:], in_=R["m2"][:], axis=AX.X, op=ALU.max), r=[R_b["m2"]], w=[S1_b["m2"]])
        m2_bc = S1["m2"][:].rearrange("p (s o) -> p s o", o=1).to_broadcast([128, 4, 16])
        kb.op("dve", lambda e: e.tensor_tensor(out=R["sel2"][:], in0=R["m2"][:], in1=m2_bc, op=ALU.is_equal), r=[R_b["m2"], S1_b["m2"]], w=[R_b["sel2"]])
        kb.op("dve", lambda e: e.tensor_tensor(out=R["sel1"][:], in0=R["sel1"][:], in1=R["sel2"][:], op=ALU.add), r=[R_b["sel1"], R_b["sel2"]], w=[R_b["sel1"]])
        kb.op("dve", lambda e: e.tensor_tensor(out=R["ssel"][:], in0=R["sel1"][:], in1=R["s"][:], op=ALU.mult), r=[R_b["sel1"], R_b["s"]], w=[R_b["ssel"]])
        kb.op("dve", lambda e: e.tensor_reduce(out=S1["den"][:], in_=R["ssel"][:], axis=AX.X, op=ALU.add), r=[R_b["ssel"]], w=[S1_b["den"]])
        kb.op("dve", lambda e: e.reciprocal(S1["rden"][:], S1["den"][:]), r=[S1_b["den"]], w=[S1_b["rden"]])
        rden_bc = S1["rden"][:].rearrange("p (s o) -> p s o", o=1).to_broadcast([128, 4, 16])
        kb.op("dve", lambda e: e.tensor_tensor(out=R["gates"][:], in0=R["ssel"][:], in1=rden_bc, op=ALU.mult), r=[R_b["ssel"], S1_b["rden"]], w=[R_b["gates"]])
        for s in range(4):
            kb.op("pe", lambda e: e.transpose(pbank[5][0:16, s * 128:(s + 1) * 128], R["gates"][:, s, :], ident[:, :]), r=[R_b["gates"], ident_b], w=[pb[5]])
        kb.op("act", lambda e: e.copy(gatesT[:], pbank[5][0:16, :]), r=[pb[5]], w=[gatesT_b])
        for ex in range(16):
            mm(kb, pbank[4][:], sel[:, ex * 128:(ex + 1) * 128], gatesT[:], True, True, r=[sel_b, gatesT_b], w=[pb[4]])
            for j in range(4):
                def ld(st, sbuf_, ex=ex, j=j):
                    sv_ = st[:, :16 * 256].rearrange("p (c n) -> p c n", c=16)
                    src = wgu[ex].rearrange("(c p) n -> p c n", p=128)
                    kb.dma(sv_[:, :, 0:128], src[:, :, j * 128:(j + 1) * 128], w=[sbuf_], q="pool")
                    kb.dma(sv_[:, :, 128:256], src[:, :, 512 + j * 128:512 + (j + 1) * 128], w=[sbuf_], q="pool")
                wt, wtb = load_weight(ld, 16 * 256)
                pg, pu = (0, 1) if j % 2 == 0 else (2, 3)
                for k in range(16):
                    mm(kb, pbank[pg][:], wt[:, k * 256:k * 256 + 128], hT[:, k, :], k == 0, k == 15, r=[wtb, hT_b[k]], w=[pb[pg]])
                for k in range(16):
                    mm(kb, pbank[pu][:], wt[:, k * 256 + 128:k * 256 + 256], hT[:, k, :], k == 0, k == 15, r=[wtb, hT_b[k]], w=[pb[pu]])
                ti = nxt(tmp_i, 4)
                kb.op("act", lambda e: e.activation(out=tmp[ti][:], in_=pbank[pg][:], func=AF.Silu), r=[pb[pg]], w=[tmp_b[ti]])
                kb.op("dve", lambda e: e.tensor_tensor(out=tmp[ti][:], in0=tmp[ti][:], in1=pbank[pu][:], op=ALU.mult), r=[tmp_b[ti], pb[pu]], w=[tmp_b[ti]])
                kb.op("dve", lambda e: e.tensor_tensor(out=aT[:, j, :], in0=tmp[ti][:], in1=pbank[4][:], op=ALU.mult), r=[tmp_b[ti], pb[4]], w=[aT_b[j]])
            for mq in range(4):
                def ld(st, sbuf_, ex=ex, mq=mq):
                    kb.dma(st[:, :4 * 512].rearrange("p (c n) -> p c n", c=4), wd[ex].rearrange("(c p) n -> p c n", p=128)[:, :, mq * 512:(mq + 1) * 512], w=[sbuf_], q="pool")
                wt, wtb = load_weight(ld, 4 * 512)
                for mi in range(4):
                    m = mq * 4 + mi
                    pi = 6 + (m % 2)
                    for k in range(4):
                        mm(kb, pbank[pi][:], wt[:, k * 512 + mi * 128:k * 512 + (mi + 1) * 128], aT[:, k, :], k == 0, k == 3, r=[wtb, aT_b[k]], w=[pb[pi]])
                    if ex == 0:
                        kb.op("act", lambda e: e.copy(yacc[:, m, :], pbank[pi][:]), r=[pb[pi]], w=[yacc_b[m]])
                    else:
                        kb.op("dve", lambda e: e.tensor_tensor(out=yacc[:, m, :], in0=pbank[pi][:], in1=yacc[:, m, :], op=ALU.add), r=[pb[pi], yacc_b[m]], w=[yacc_b[m]])
        for c in range(16):
            kb.op("act", lambda e: e.activation(out=yacc[:, c, :], in_=yacc[:, c, :], func=AF.Identity, scale=pcol(3, c, True)), r=[yacc_b[c], pv1_b], w=[yacc_b[c]])
            kb.op("dve", lambda e: e.scalar_tensor_tensor(out=xt[:, c, :], in0=xt[:, c, :], scalar=DN_ALPHA, in1=yacc[:, c, :], op0=ALU.mult, op1=ALU.add),
                  r=[xt_b[c], yacc_b[c]], w=[xt_b[c]])
        layernorm(6, 7, False, out_dma_t0=t0)
    return kb.finish()


def build_ada():
    kb = KB()
    cT_d = kb.din("cT", [128, 16 * 4])
    w_d = kb.din("w", [2, D, 1536])
    b_d = kb.din("b", [128, 24])
    out_d = kb.dout("modT", [128, 24 * 4])
    cT = kb.sb([128, 64]); cT_b = Buf()
    bb = kb.sb([128, 24]); bb_b = Buf()
    ot = kb.sb([128, 96]); ot_b = Buf()
    stg = [kb.sb([128, 16, 128]) for _ in range(3)]; stg_b = bufs(3)
    ps = [kb.ps([128, 512]) for _ in range(2)]; ps_b = bufs(2)
    kb.dma(cT[:], cT_d[:, :], w=[cT_b])
    kb.dma(bb[:], b_d[:, :], w=[bb_b])
    kb.op("act", lambda e: e.activation(out=cT[:], in_=cT[:], func=AF.Silu), r=[cT_b], w=[cT_b])
    for l in range(2):
        for m in range(12):
            u = l * 12 + m
            si = u % 3
            kb.dma(stg[si][:], w_d[l].rearrange("(c p) n -> p c n", p=128)[:, :, m * 128:(m + 1) * 128], w=[stg_b[si]])
            pi = u % 2
            for k in range(16):
                mm(kb, ps[pi][:, 0:4], stg[si][:, k, :], cT[:, k * 4:(k + 1) * 4], k == 0, k == 15, r=[stg_b[si], cT_b], w=[ps_b[pi]])
            kb.op("dve", lambda e: e.tensor_scalar(out=ot[:, u * 4:(u + 1) * 4], in0=ps[pi][:, 0:4], scalar1=bb[:, u:u + 1], scalar2=None, op0=ALU.add),
                  r=[ps_b[pi], bb_b], w=[ot_b])
    kb.dma(out_d[:, :], ot[:], r=[ot_b], final=True)
    return kb.finish()


def run_ada(c, ada_w, ada_b):
    nc = build_ada()
    cT = np.ascontiguousarray(c.T.reshape(16, 128, 4).transpose(1, 0, 2).reshape(128, 64))
    in_maps = []
    for j in range(NCORES):
        w = np.ascontiguousarray(ada_w[:, :, j * 1536:(j + 1) * 1536])
        b = ada_b[:, j * 1536:(j + 1) * 1536].reshape(2, 12, 128).transpose(2, 0, 1).reshape(128, 24)
        in_maps.append({"cT": cT, "w": w, "b": np.ascontiguousarray(b)})
    res = run_bass_kernel_spmd(nc, in_maps, core_ids=list(range(NCORES)))
    mod = np.zeros((2, 4, 6 * D), np.float32)
    for j in range(NCORES):
        o = res.results[j]["modT"].reshape(128, 2, 12, 4)
        mod[:, :, j * 1536:(j + 1) * 1536] = o.transpose(1, 3, 2, 0).reshape(2, 4, 1536)
    return mod


def build_pre(ncol, ntok=2048):
    assert ncol % 128 == 0
    NM = ncol // 128
    kb = KB()
    NT = ntok // TT
    xT = kb.din("xT", [D, ntok]).rearrange("(c p) t -> p c t", p=128)
    w_d = kb.din("w", [D, ncol]).rearrange("(c p) n -> p c n", p=128)
    pv_d = kb.din("pvec", [128, 32])
    zT = kb.dout("zT", [ncol, ntok]).rearrange("(c p) t -> p c t", p=128)
    xt = [kb.sb([128, 16, TT]) for _ in range(2)]; xt_b = bufs(2)
    hT = [kb.sb([128, 16, TT], BF16) for _ in range(NT)]; hT_b = bufs(NT)
    stg = [kb.sb([128, 16, 128]) for _ in range(3)]; stg_b = bufs(3)
    wb = [kb.sb([128, 16, 128], BF16) for _ in range(3)]; wb_b = bufs(3)
    ot = [kb.sb([128, TT]) for _ in range(4)]; ot_b = bufs(4)
    pv = kb.sb([128, 32]); pv_b = Buf()
    pv1 = kb.sb([128, 32]); pv1_b = Buf()
    ps = [kb.ps([128, TT]) for _ in range(4)]; ps_b = bufs(4)
    kb.dma(pv[:], pv_d[:, :], w=[pv_b])
    kb.op("dve", lambda e: e.tensor_scalar_add(pv1[:], pv[:], 1.0), r=[pv_b], w=[pv1_b])
    for tt in range(NT):
        t0 = tt * TT
        xi = tt % 2
        kb.dma(xt[xi][:], xT[:, :, t0:t0 + TT], w=[xt_b[xi]])
        for c in range(16):
            kb.op("act", lambda e: e.activation(out=hT[tt][:, c, :], in_=xt[xi][:, c, :], func=AF.Identity, scale=pv1[:, c:c + 1], bias=pv[:, 16 + c:17 + c]),
                  r=[xt_b[xi], pv_b, pv1_b], w=[hT_b[tt]])
    u = 0
    for m in range(NM):
        si = m % 3
        kb.dma(wb[si][:], w_d[:, :, m * 128:(m + 1) * 128], w=[wb_b[si]], q="pool")
        for tt in range(NT):
            t0 = tt * TT
            pi = u % 4
            for k in range(16):
                mm(kb, ps[pi][:], wb[si][:, k, :], hT[tt][:, k, :], k == 0, k == 15, r=[wb_b[si], hT_b[tt]], w=[ps_b[pi]])
            if u % 2 == 0:
                kb.op("act", lambda e: e.copy(ot[pi][:], ps[pi][:]), r=[ps_b[pi]], w=[ot_b[pi]])
            else:
                kb.op("dve", lambda e: e.tensor_copy(out=ot[pi][:], in_=ps[pi][:]), r=[ps_b[pi]], w=[ot_b[pi]])
            kb.dma(zT[:, m, t0:t0 + TT], ot[pi][:], r=[ot_b[pi]], final=True, q="act" if u % 2 == 0 else "sp")
            u += 1
    return kb.finish()


def fm16(v):
    return np.ascontiguousarray(v.reshape(16, 128).T)


def run_pre(x_tok_major, mod_l, w, sc_idx, sh_idx):
    ncol = w.shape[1]
    nc = build_pre(ncol)
    in_maps = []
    for j in range(NCORES):
        b, hf = j // 2, j % 2
        xT = np.ascontiguousarray(x_tok_major[b, hf * 2048:(hf + 1) * 2048].T)
        sc = mod_l[b, sc_idx * D:(sc_idx + 1) * D]
        sh = mod_l[b, sh_idx * D:(sh_idx + 1) * D]
        in_maps.append({"xT": xT, "w": w, "pvec": np.ascontiguousarray(np.concatenate([fm16(sc), fm16(sh)], 1))})
    res = run_bass_kernel_spmd(nc, in_maps, core_ids=list(range(NCORES)))
    z = np.zeros((4, ncol, 4096), np.float32)
    for j in range(NCORES):
        b, hf = j // 2, j % 2
        z[b, :, hf * 2048:(hf + 1) * 2048] = res.results[j]["zT"]
    return z


SEQ = 4096
MLA_SCALE = 192.0 ** -0.5


def build_mla(S=SEQ):
    kb = KB()
    NT = S // TT
    zq = kb.din("zq", [512, S]).rearrange("(c p) t -> p c t", p=128)
    zkv = kb.din("zkv", [256, S]).rearrange("(c p) t -> p c t", p=128)
    zkr = kb.din("zkr", [128, S]).rearrange("(c p) t -> p c t", p=64)
    cs_d = kb.din("cs", [128, S]).rearrange("(c p) t -> p c t", p=64)
    wq_d = kb.din("wq", [512, 1024]).rearrange("(c p) n -> p c n", p=128)
    wk_d = kb.din("wk", [256, 512]).rearrange("(c p) n -> p c n", p=128)
    wv_d = kb.din("wv", [256, 512]).rearrange("(c p) n -> p c n", p=128)
    g_d = kb.din("g", [128, 6])
    mask_d = kb.din("mask", [128, 4 * TT])
    attT = kb.dout("attT", [512, S]).rearrange("(c p) t -> p c t", p=128)

    qnope = [kb.sb([128, S], BF16) for _ in range(4)]; qnope_b = [bufs(NT) for _ in range(4)]
    qrope = [kb.sb([128, S], BF16) for _ in range(2)]; qrope_b = [bufs(NT) for _ in range(2)]
    knope = [kb.sb([128, S], BF16) for _ in range(4)]; knope_b = [bufs(NT) for _ in range(4)]
    krope = kb.sb([128, S], BF16); krope_b = bufs(NT)
    V = kb.sb([128, S // 128, 512], BF16); V_b = bufs(NT)
    wq = kb.sb([128, 4, 1024], BF16); wk = kb.sb([128, 2, 512], BF16); wv = kb.sb([128, 2, 512], BF16); w_b = Buf()
    g = kb.sb([128, 6]); g_b = Buf()
    mask = kb.sb([128, 4 * TT], BF16); mask_b = Buf()
    ones = kb.sb([128, 128]); ones_b = Buf()
    onesb = kb.sb([128, 128], BF16); onesb_b = Buf()
    stg = kb.sb([128, 2048]); stg_b = Buf()
    zin = [kb.sb([128, 6, TT]) for _ in range(1)]; zin_b = bufs(1)
    zr = [kb.sb([128, 4, TT]) for _ in range(1)]; zr_b = bufs(1)
    qn = kb.sb([128, 6, TT], BF16); qn_b = bufs(6)
    tmp = [kb.sb([128, TT]) for _ in range(4)]; tmp_b = bufs(4)
    rs = [kb.sb([128, TT]) for _ in range(2)]; rs_b = bufs(2)
    pT = [kb.sb([128, TT], BF16) for _ in range(3)]; pT_b = bufs(3)
    ot = [kb.sb([128, TT]) for _ in range(2)]; ot_b = bufs(2)
    ps = [kb.ps([128, TT]) for _ in range(8)]; pb = bufs(8)
    tmp_i = [0]

    def nxt(ctr, n):
        i = ctr[0] % n
        ctr[0] += 1
        return i

    kb.dma(g[:], g_d[:, :], w=[g_b])
    kb.op("dve", lambda e: e.memset(ones[:], 1.0), w=[ones_b])
    kb.op("dve", lambda e: e.memset(onesb[:], 1.0), w=[onesb_b])
    kb.dma(stg[:, :2048], mask_d[:, :], w=[stg_b])
    kb.op("dve", lambda e: e.tensor_copy(out=mask[:], in_=stg[:, :2048]), r=[stg_b], w=[mask_b])
    for hh in range(2):
        kb.dma(stg[:].rearrange("p (c n) -> p c n", c=2), wq_d[:, 2 * hh:2 * hh + 2, :], w=[stg_b])
        kb.op("dve", lambda e: e.tensor_copy(out=wq[:, 2 * hh:2 * hh + 2, :].rearrange("p c n -> p (c n)"), in_=stg[:]), r=[stg_b, w_b], w=[w_b])
    kb.dma(stg[:, :1024].rearrange("p (c n) -> p c n", c=2), wk_d[:, :, :], w=[stg_b])
    kb.op("dve", lambda e: e.tensor_copy(out=wk[:].rearrange("p c n -> p (c n)"), in_=stg[:, :1024]), r=[stg_b, w_b], w=[w_b])
    kb.dma(stg[:, :1024].rearrange("p (c n) -> p c n", c=2), wv_d[:, :, :], w=[stg_b])
    kb.op("dve", lambda e: e.tensor_copy(out=wv[:].rearrange("p c n -> p (c n)"), in_=stg[:, :1024]), r=[stg_b, w_b], w=[w_b])

    for tt in range(NT):
        t0 = tt * TT
        zi = 0
        kb.dma(zin[zi][:, 0:4, :], zq[:, :, t0:t0 + TT], w=[zin_b[zi]])
        kb.dma(zin[zi][:, 4:6, :], zkv[:, :, t0:t0 + TT], w=[zin_b[zi]])
        for hp in range(2):
            kb.dma(zr[zi][hp * 64:(hp + 1) * 64, 0:2, :], zkr[:, :, t0:t0 + TT], w=[zr_b[zi]])
            kb.dma(zr[zi][hp * 64:(hp + 1) * 64, 2:4, :], cs_d[:, :, t0:t0 + TT], w=[zr_b[zi]])
        for (c0, c1, pi, dim, eps, ri) in [(0, 4, 6, 512, 1e-6, 0), (4, 6, 7, 256, 1e-6, 1)]:
            for c in range(c0, c1):
                ti = nxt(tmp_i, 4)
                kb.op("act", lambda e: e.activation(out=tmp[ti][:], in_=zin[zi][:, c, :], func=AF.Square), r=[zin_b[zi]], w=[tmp_b[ti]])
                mm(kb, ps[pi][:], ones[:], tmp[ti][:], c == c0, c == c1 - 1, r=[ones_b, tmp_b[ti]], w=[pb[pi]])
            kb.op("dve", lambda e: e.tensor_scalar(out=rs[ri][:], in0=ps[pi][:], scalar1=1.0 / dim, scalar2=eps, op0=ALU.mult, op1=ALU.add), r=[pb[pi]], w=[rs_b[ri]])
            kb.op("act", lambda e: e.sqrt(rs[ri][:], rs[ri][:]), r=[rs_b[ri]], w=[rs_b[ri]])
            kb.op("dve", lambda e: e.reciprocal(rs[ri][:], rs[ri][:]), r=[rs_b[ri]], w=[rs_b[ri]])
            for c in range(c0, c1):
                kb.op("dve", lambda e: e.scalar_tensor_tensor(out=qn[:, c, :], in0=zin[zi][:, c, :], scalar=g[:, c:c + 1], in1=rs[ri][:], op0=ALU.mult, op1=ALU.mult),
                      r=[zin_b[zi], g_b, rs_b[ri]], w=[qn_b[c]])
        for h in range(4):
            for k in range(4):
                mm(kb, ps[0][:], wq[:, k, h * 128:(h + 1) * 128], qn[:, k, :], k == 0, k == 3, r=[w_b, qn_b[k]], w=[pb[0]])
            kb.op("act", lambda e: e.copy(qnope[h][:, t0:t0 + TT], ps[0][:]), r=[pb[0]], w=[qnope_b[h][tt]])
            for k in range(2):
                mm(kb, ps[3][:], wk[:, k, h * 128:(h + 1) * 128], qn[:, 4 + k, :], k == 0, k == 1, r=[w_b, qn_b[4 + k]], w=[pb[3]])
            kb.op("act", lambda e: e.copy(knope[h][:, t0:t0 + TT], ps[3][:]), r=[pb[3]], w=[knope_b[h][tt]])
        for hp in range(2):
            for k in range(4):
                mm(kb, ps[1][:], wq[:, k, 512 + hp * 128:512 + (hp + 1) * 128], qn[:, k, :], k == 0, k == 3, r=[w_b, qn_b[k]], w=[pb[1]])
            for k in range(4):
                mm(kb, ps[2][:], wq[:, k, 768 + hp * 128:768 + (hp + 1) * 128], qn[:, k, :], k == 0, k == 3, r=[w_b, qn_b[k]], w=[pb[2]])
            t1 = nxt(tmp_i, 4)
            kb.op("dve", lambda e: e.tensor_tensor(out=tmp[t1][:], in0=ps[1][:], in1=zr[zi][:, 2, :], op=ALU.mult), r=[pb[1], zr_b[zi]], w=[tmp_b[t1]])
            t2 = nxt(tmp_i, 4)
            kb.op("dve", lambda e: e.tensor_tensor(out=tmp[t2][:], in0=ps[2][:], in1=zr[zi][:, 3, :], op=ALU.mult), r=[pb[2], zr_b[zi]], w=[tmp_b[t2]])
            kb.op("dve", lambda e: e.tensor_tensor(out=qrope[hp][:, t0:t0 + TT], in0=tmp[t1][:], in1=tmp[t2][:], op=ALU.add),
                  r=[tmp_b[t1], tmp_b[t2]], w=[qrope_b[hp][tt]])
        for blk in range(4):
            pi = 4 + blk % 2
            for k in range(2):
                mm(kb, ps[pi][:], qn[:, 4 + k, blk * 128:(blk + 1) * 128], wv[:, k, :], k == 0, k == 1, r=[w_b, qn_b[4 + k]], w=[pb[pi]])
            kb.op("act", lambda e: e.copy(V[:, tt * 4 + blk, :], ps[pi][:]), r=[pb[pi]], w=[V_b[tt]])
        t1 = nxt(tmp_i, 4)
        kb.op("dve", lambda e: e.tensor_tensor(out=tmp[t1][:], in0=zr[zi][:, 0, :], in1=zr[zi][:, 2, :], op=ALU.mult), r=[zr_b[zi]], w=[tmp_b[t1]])
        t2 = nxt(tmp_i, 4)
        kb.op("dve", lambda e: e.tensor_tensor(out=tmp[t2][:], in0=zr[zi][:, 1, :], in1=zr[zi][:, 3, :], op=ALU.mult), r=[zr_b[zi]], w=[tmp_b[t2]])
        kb.op("dve", lambda e: e.tensor_tensor(out=krope[:, t0:t0 + TT], in0=tmp[t1][:], in1=tmp[t2][:], op=ALU.add), r=[tmp_b[t1], tmp_b[t2]], w=[krope_b[tt]])

    sbanks = [0, 1, 2]
    cnt = [0]
    for h in range(4):
        for qb in range(NT):
            q0 = qb * TT
            nkb = 4 * qb + 4
            po, pd = (3, 4) if (h * NT + qb) % 2 == 0 else (5, 6)

            def qk(kk):
                sb_ = sbanks[kk % 3]
                kt = kk // 4
                mm(kb, ps[sb_][:], knope[h][:, kk * 128:(kk + 1) * 128], qnope[h][:, q0:q0 + TT], True, False,
                   r=[knope_b[h][kt], qnope_b[h][qb]], w=[pb[sb_]])
                ph = (h % 2) * 64
                mm(kb, ps[sb_][:], krope[ph:ph + 64, kk * 128:(kk + 1) * 128], qrope[h // 2][ph:ph + 64, q0:q0 + TT], False, True,
                   r=[krope_b[kt], qrope_b[h // 2][qb]], w=[pb[sb_]])
            qk(0)
            for kk in range(nkb):
                if kk + 1 < nkb:
                    qk(kk + 1)
                sb_ = sbanks[kk % 3]
                pi = cnt[0] % 3
                cnt[0] += 1
                kb.op("act", lambda e: e.activation(out=pT[pi][:], in_=ps[sb_][:], func=AF.Exp, scale=MLA_SCALE), r=[pb[sb_]], w=[pT_b[pi]])
                j = kk - 4 * qb
                if j >= 0:
                    kb.op("dve", lambda e: e.tensor_tensor(out=pT[pi][:], in0=pT[pi][:], in1=mask[:, j * TT:(j + 1) * TT], op=ALU.mult), r=[pT_b[pi], mask_b], w=[pT_b[pi]])
                mm(kb, ps[po][:], V[:, kk, h * 128:(h + 1) * 128], pT[pi][:], kk == 0, kk == nkb - 1, r=[V_b[kk // 4], pT_b[pi]], w=[pb[po]])
                mm(kb, ps[pd][:], onesb[:], pT[pi][:], kk == 0, kk == nkb - 1, r=[onesb_b, pT_b[pi]], w=[pb[pd]])
            oi = (h * NT + qb) % 2
            ti = nxt(tmp_i, 4)
            kb.op("dve", lambda e: e.reciprocal(tmp[ti][:], ps[pd][:]), r=[pb[pd]], w=[tmp_b[ti]])
            kb.op("dve", lambda e: e.tensor_tensor(out=ot[oi][:], in0=ps[po][:], in1=tmp[ti][:], op=ALU.mult), r=[pb[po], tmp_b[ti]], w=[ot_b[oi]])
            kb.dma(attT[:, h, q0:q0 + TT], ot[oi][:], r=[ot_b[oi]], final=True)
    return kb.finish()


def rope_tables(S=SEQ):
    inv = 10000.0 ** (-np.arange(0, 64, 2, dtype=np.float32) / 64)
    ang = np.arange(S, dtype=np.float32)[None, :] * inv[:, None]
    cos, sin = np.cos(ang).astype(np.float32), np.sin(ang).astype(np.float32)
    return np.ascontiguousarray(np.concatenate([cos, cos, -sin, sin], 0))


def causal_masks():
    m = np.zeros((4, 128, TT), np.float32)
    k = np.arange(128)[:, None]
    q = np.arange(TT)[None, :]
    for j in range(4):
        m[j] = (q >= k + 128 * j)
    return np.ascontiguousarray(m.transpose(1, 0, 2).reshape(128, 4 * TT))


def run_mla(z0, w_uq, w_ukv, q_norm, kv_norm):
    nc = build_mla()
    cs = rope_tables()
    mask = causal_masks()
    g = np.ascontiguousarray(np.concatenate([q_norm.reshape(4, 128).T, kv_norm.reshape(2, 128).T], 1))
    in_maps = []
    for j in range(NCORES):
        b, hf = j // 2, j % 2
        wq_cols, wk_cols, wv_cols = [], [], []
        hs = list(range(4 * hf, 4 * hf + 4))
        for h in hs:
            wq_cols.append(w_uq[:, h * 192:h * 192 + 128])
            wk_cols.append(w_ukv[:, h * 256:h * 256 + 128])
            wv_cols.append(w_ukv[:, h * 256 + 128:h * 256 + 256])
        for h in hs:
            wq_cols.append(w_uq[:, h * 192 + 128:h * 192 + 192])
        for h in hs:
            wq_cols += [w_uq[:, h * 192 + 160:h * 192 + 192], w_uq[:, h * 192 + 128:h * 192 + 160]]
        in_maps.append({
            "zq": np.ascontiguousarray(z0[b, 0:512]), "zkv": np.ascontiguousarray(z0[b, 512:768]),
            "zkr": np.ascontiguousarray(np.concatenate([z0[b, 768:832], z0[b, 1856:1920]], 0)),
            "cs": cs, "wq": np.ascontiguousarray(np.concatenate(wq_cols, 1)), "wk": np.ascontiguousarray(np.concatenate(wk_cols, 1)),
            "wv": np.ascontiguousarray(np.concatenate(wv_cols, 1)), "g": g, "mask": mask})
    res = run_bass_kernel_spmd(nc, in_maps, core_ids=list(range(NCORES)))
    att = np.zeros((4, 1024, SEQ), np.float32)
    for j in range(NCORES):
        b, hf = j // 2, j % 2
        att[b, hf * 512:(hf + 1) * 512] = res.results[j]["attT"]
    return att


TWO_PI = 6.283185307179586
NG = 32


def build_s5(S=SEQ):
    kb = KB()
    NT = S // TT
    uT = kb.din("uT", [NG * 16, S])
    lamre_d = kb.din("lamre", [128, NG]); lamim_d = kb.din("lamim", [128, NG]); logdt_d = kb.din("logdt", [128, NG])
    bt_d = kb.din("bt", [16, NG * 128]); btsw_d = kb.din("btsw", [16, NG * 128])
    ca_d = kb.din("ca", [128, NG * 16]); cb_d = kb.din("cb", [128, NG * 16])
    d_d = kb.din("dsk", [16, NG])
    iota_d = kb.din("iota", [128, S])
    yT = kb.dout("yT", [NG * 16, S])

    def t32(name=None):
        return kb.sb([128, NG], name=name)
    lamre, lamim, dt_, r_, th, cth, sth, nre, nim, den, fre, fim, tA, tB = [t32() for _ in range(14)]
    s1, s2, s3, s4 = [t32() for _ in range(4)]
    prm_b = Buf()
    sgn = kb.sb([128, 1]); negpi = kb.sb([128, 1]); ki = kb.sb([128, NG], mybir.dt.int32)
    KI = kb.sb([128, S], mybir.dt.int32); KI_b = Buf()
    bt = kb.sb([16, NG * 128], BF16); btsw = kb.sb([16, NG * 128], BF16); bstg = kb.sb([16, NG * 128]); bt_b = Buf(); bstg_b = Buf()
    ca = kb.sb([128, NG, 16]); cb = kb.sb([128, NG, 16]); cstage = kb.sb([128, NG, 16]); L1 = kb.sb([128, NG, 16], BF16); L2 = kb.sb([128, NG, 16], BF16); c_b = Buf()
    dsk = kb.sb([16, NG]); dsk_b = Buf()
    iota = kb.sb([128, S]); iota_b = Buf()
    u32 = kb.sb([16, S]); u32_b = Buf()
    ubf = kb.sb([16, S], BF16); ubf_b = Buf()
    A1 = kb.sb([128, S]); A1_b = Buf()
    A2 = kb.sb([128, S]); A2_b = Buf()
    T2 = kb.sb([128, S]); T2_b = Buf()
    bz = kb.sb([128, S]); bz_b = bufs(NT); z_b = Buf()
    Zc = kb.sb([128, S], BF16); Zc_b = Buf()
    Zs = kb.sb([128, S], BF16); Zs_b = Buf()
    ysb = kb.sb([16, S]); ysb_b = Buf()
    tmp = [kb.sb([128, TT]) for _ in range(4)]; tmp_b = bufs(4)
    ps = [kb.ps([128, TT]) for _ in range(8)]; pb = bufs(8)

    P = [prm_b]
    kb.dma(lamre[:], lamre_d[:, :], w=P)
    kb.dma(lamim[:], lamim_d[:, :], w=P)
    kb.dma(dt_[:], logdt_d[:, :], w=P)
    kb.dma(iota[:], iota_d[:, :], w=[iota_b])
    kb.dma(dsk[:], d_d[:, :], w=[dsk_b])
    kb.dma(bstg[:], bt_d[:, :], w=[bstg_b])
    kb.op("dve", lambda e: e.tensor_copy(out=bt[:], in_=bstg[:]), r=[bstg_b], w=[bt_b])
    kb.dma(bstg[:], btsw_d[:, :], w=[bstg_b])
    kb.op("dve", lambda e: e.tensor_copy(out=btsw[:], in_=bstg[:]), r=[bstg_b, bt_b], w=[bt_b])
    kb.dma(ca[:].rearrange("p g c -> p (g c)"), ca_d[:, :], w=[c_b])
    kb.dma(cb[:].rearrange("p g c -> p (g c)"), cb_d[:, :], w=[c_b])
    V = lambda fn: kb.op("dve", fn, r=P, w=P)
    A = lambda fn: kb.op("act", fn, r=P, w=P)
    V(lambda e: e.memset(sgn[0:64, :], 1.0))
    V(lambda e: e.memset(sgn[64:128, :], -1.0))
    V(lambda e: e.memset(negpi[:], -3.141592653589793))
    A(lambda e: e.activation(out=dt_[:], in_=dt_[:], func=AF.Exp))
    V(lambda e: e.tensor_tensor(out=r_[:], in0=lamre[:], in1=dt_[:], op=ALU.mult))
    A(lambda e: e.activation(out=r_[:], in_=r_[:], func=AF.Exp))
    V(lambda e: e.tensor_tensor(out=th[:], in0=lamim[:], in1=dt_[:], op=ALU.mult))
    V(lambda e: e.tensor_single_scalar(out=th[:], in_=th[:], scalar=1.0 / TWO_PI, op=ALU.mult))
    V(lambda e: e.tensor_copy(out=ki[:], in_=th[:]))
    V(lambda e: e.tensor_tensor(out=th[:], in0=th[:], in1=ki[:], op=ALU.subtract))
    A(lambda e: e.activation(out=sth[:], in_=th[:], func=AF.Sin, scale=TWO_PI))
    V(lambda e: e.tensor_single_scalar(out=tA[:], in_=th[:], scalar=0.25, op=ALU.add))
    V(lambda e: e.tensor_copy(out=ki[:], in_=tA[:]))
    V(lambda e: e.tensor_tensor(out=tA[:], in0=tA[:], in1=ki[:], op=ALU.subtract))
    A(lambda e: e.activation(out=cth[:], in_=tA[:], func=AF.Sin, scale=TWO_PI))
    V(lambda e: e.tensor_tensor(out=nre[:], in0=r_[:], in1=cth[:], op=ALU.mult))
    V(lambda e: e.tensor_single_scalar(out=nre[:], in_=nre[:], scalar=-1.0, op=ALU.add))
    V(lambda e: e.tensor_tensor(out=nim[:], in0=r_[:], in1=sth[:], op=ALU.mult))
    V(lambda e: e.tensor_tensor(out=den[:], in0=lamre[:], in1=lamre[:], op=ALU.mult))
    V(lambda e: e.tensor_tensor(out=tA[:], in0=lamim[:], in1=lamim[:], op=ALU.mult))
    V(lambda e: e.tensor_tensor(out=den[:], in0=den[:], in1=tA[:], op=ALU.add))
    V(lambda e: e.reciprocal(den[:], den[:]))
    V(lambda e: e.tensor_tensor(out=fre[:], in0=nre[:], in1=lamre[:], op=ALU.mult))
    V(lambda e: e.tensor_tensor(out=tA[:], in0=nim[:], in1=lamim[:], op=ALU.mult))
    V(lambda e: e.tensor_tensor(out=fre[:], in0=fre[:], in1=tA[:], op=ALU.add))
    V(lambda e: e.tensor_tensor(out=fre[:], in0=fre[:], in1=den[:], op=ALU.mult))
    V(lambda e: e.tensor_tensor(out=fim[:], in0=nim[:], in1=lamre[:], op=ALU.mult))
    V(lambda e: e.tensor_tensor(out=tA[:], in0=nre[:], in1=lamim[:], op=ALU.mult))
    V(lambda e: e.tensor_tensor(out=fim[:], in0=fim[:], in1=tA[:], op=ALU.subtract))
    V(lambda e: e.tensor_tensor(out=fim[:], in0=fim[:], in1=den[:], op=ALU.mult))
    V(lambda e: e.tensor_scalar(out=s1[:], in0=fre[:], scalar1=sgn[:, 0:1], scalar2=None, op0=ALU.mult))
    V(lambda e: e.tensor_single_scalar(out=s2[:], in_=fim[:], scalar=-1.0, op=ALU.mult))
    V(lambda e: e.tensor_scalar(out=s3[:], in0=s2[:], scalar1=sgn[:, 0:1], scalar2=None, op0=ALU.mult))
    V(lambda e: e.tensor_single_scalar(out=s4[:], in_=fre[:], scalar=-1.0, op=ALU.mult))

    def bc(t):
        return t[:].rearrange("p (g o) -> p g o", o=1).to_broadcast([128, NG, 16])
    PC = [prm_b, c_b]
    kb.op("dve", lambda e: e.tensor_tensor(out=cstage[:], in0=ca[:], in1=bc(s1), op=ALU.mult), r=PC, w=PC)
    kb.op("dve", lambda e: e.tensor_tensor(out=ca[:], in0=ca[:], in1=bc(s3), op=ALU.mult), r=PC, w=PC)
    kb.op("dve", lambda e: e.tensor_tensor(out=tmp[0][:, :NG * 16].rearrange("p (g c) -> p g c", c=16), in0=cb[:], in1=bc(s2), op=ALU.mult), r=PC, w=PC + [tmp_b[0]])
    kb.op("dve", lambda e: e.tensor_tensor(out=L1[:], in0=cstage[:], in1=tmp[0][:, :NG * 16].rearrange("p (g c) -> p g c", c=16), op=ALU.add), r=PC + [tmp_b[0]], w=PC)
    kb.op("dve", lambda e: e.tensor_tensor(out=cb[:], in0=cb[:], in1=bc(s4), op=ALU.mult), r=PC, w=PC)
    kb.op("dve", lambda e: e.tensor_tensor(out=L2[:], in0=ca[:], in1=cb[:], op=ALU.add), r=PC, w=PC)

    tmp_i = [1]
    for g in range(NG):
        kb.dma(u32[:], uT[g * 16:(g + 1) * 16, :], w=[u32_b])
        kb.op("act", lambda e: e.copy(ubf[:], u32[:]), r=[u32_b], w=[ubf_b])
        kb.op("dve", lambda e: e.tensor_scalar(out=KI[:], in0=iota[:], scalar1=th[:, g:g + 1], scalar2=None, op0=ALU.mult), r=[iota_b, prm_b], w=[KI_b])
        kb.op("dve", lambda e: e.scalar_tensor_tensor(out=A1[:], in0=iota[:], scalar=th[:, g:g + 1], in1=KI[:], op0=ALU.mult, op1=ALU.subtract), r=[iota_b, prm_b, KI_b], w=[A1_b])
        kb.op("dve", lambda e: e.tensor_single_scalar(out=A2[:], in_=A1[:], scalar=0.25, op=ALU.add), r=[A1_b], w=[A2_b])
        kb.op("dve", lambda e: e.tensor_copy(out=KI[:], in_=A2[:]), r=[A2_b], w=[KI_b])
        kb.op("dve", lambda e: e.tensor_tensor(out=A2[:], in0=A2[:], in1=KI[:], op=ALU.subtract), r=[A2_b, KI_b], w=[A2_b])
        kb.op("act", lambda e: e.activation(out=A1[:], in_=A1[:], func=AF.Sin, scale=TWO_PI), r=[A1_b], w=[A1_b])
        kb.op("act", lambda e: e.activation(out=A2[:], in_=A2[:], func=AF.Sin, scale=TWO_PI), r=[A2_b], w=[A2_b])
        kb.op("act", lambda e: e.activation(out=T2[:], in_=A1[:], func=AF.Identity, scale=sgn[:, 0:1]), r=[A1_b, prm_b], w=[T2_b])
        for tt in range(NT):
            t0 = tt * TT
            pa, pbk = (0, 1) if tt % 2 == 0 else (2, 3)
            mm(kb, ps[pa][:], bt[:, g * 128:(g + 1) * 128], ubf[:, t0:t0 + TT], True, True, r=[bt_b, ubf_b], w=[pb[pa]])
            mm(kb, ps[pbk][:], btsw[:, g * 128:(g + 1) * 128], ubf[:, t0:t0 + TT], True, True, r=[bt_b, ubf_b], w=[pb[pbk]])
            t1 = tmp_i[0] % 4; tmp_i[0] += 1
            kb.op("dve", lambda e: e.tensor_tensor(out=tmp[t1][:], in0=ps[pa][:], in1=A2[:, t0:t0 + TT], op=ALU.mult), r=[pb[pa], A2_b], w=[tmp_b[t1]])
            t2 = tmp_i[0] % 4; tmp_i[0] += 1
            kb.op("dve", lambda e: e.tensor_tensor(out=tmp[t2][:], in0=ps[pbk][:], in1=T2[:, t0:t0 + TT], op=ALU.mult), r=[pb[pbk], T2_b], w=[tmp_b[t2]])
            kb.op("dve", lambda e: e.tensor_tensor(out=bz[:, t0:t0 + TT], in0=tmp[t1][:], in1=tmp[t2][:], op=ALU.add), r=[tmp_b[t1], tmp_b[t2], z_b], w=[bz_b[tt]])
        kb.op("dve", lambda e: e.tensor_tensor_scan(out=bz[:], data0=r_[:, g:g + 1].to_broadcast([128, S]), data1=bz[:], initial=0.0, op0=ALU.mult, op1=ALU.add),
              r=bz_b + [prm_b], w=bz_b + [z_b])
        kb.op("dve", lambda e: e.tensor_tensor(out=Zc[:], in0=bz[:], in1=A2[:], op=ALU.mult), r=[z_b, A2_b], w=[Zc_b])
        kb.op("dve", lambda e: e.tensor_tensor(out=Zs[:], in0=bz[:], in1=A1[:], op=ALU.mult), r=[z_b, A1_b], w=[Zs_b])
        for tt in range(NT):
            t0 = tt * TT
            pi = 4 + tt % 4
            mm(kb, ps[pi][0:16, :], L1[:, g, :], Zc[:, t0:t0 + TT], True, False, r=[c_b, Zc_b], w=[pb[pi]])
            mm(kb, ps[pi][0:16, :], L2[:, g, :], Zs[:, t0:t0 + TT], False, True, r=[c_b, Zs_b], w=[pb[pi]])
            kb.op("dve", lambda e: e.scalar_tensor_tensor(out=ysb[:, t0:t0 + TT], in0=u32[:, t0:t0 + TT], scalar=dsk[:, g:g + 1], in1=ps[pi][0:16, :], op0=ALU.mult, op1=ALU.add),
                  r=[u32_b, dsk_b, pb[pi]], w=[ysb_b])
        kb.dma(yT[g * 16:(g + 1) * 16, :], ysb[:], r=[ysb_b], final=True)
    return kb.finish()


def run_s5(z0, lam_re, lam_im, b_re, b_im, c_re, c_im, d_skip, log_dt):
    nc = build_s5()
    iota = np.ascontiguousarray(np.broadcast_to(np.arange(SEQ, dtype=np.float32), (128, SEQ)))
    in_maps = []
    for j in range(NCORES):
        b, hf = j // 2, j % 2
        gs = slice(hf * NG, (hf + 1) * NG)
        lre = lam_re[gs].T; lim = lam_im[gs].T
        bre = b_re[gs].transpose(2, 0, 1); bim = b_im[gs].transpose(2, 0, 1)
        cre = c_re[gs].transpose(2, 0, 1); cim = c_im[gs].transpose(2, 0, 1)
        in_maps.append({
            "uT": np.ascontiguousarray(z0[b, 832 + hf * 512:832 + (hf + 1) * 512]),
            "lamre": np.ascontiguousarray(np.concatenate([lre, lre], 0)), "lamim": np.ascontiguousarray(np.concatenate([lim, lim], 0)),
            "logdt": np.ascontiguousarray(np.broadcast_to(log_dt[gs][None, :], (128, NG))),
            "bt": np.ascontiguousarray(np.concatenate([bre, bim], 2).reshape(16, NG * 128)),
            "btsw": np.ascontiguousarray(np.concatenate([bim, bre], 2).reshape(16, NG * 128)),
            "ca": np.ascontiguousarray(np.concatenate([cre, cim], 0).reshape(128, NG * 16)),
            "cb": np.ascontiguousarray(np.concatenate([cim, cre], 0).reshape(128, NG * 16)),
            "dsk": np.ascontiguousarray(d_skip[gs].T), "iota": iota})
    res = run_bass_kernel_spmd(nc, in_maps, core_ids=list(range(NCORES)))
    y = np.zeros((4, 1024, SEQ), np.float32)
    for j in range(NCORES):
        b, hf = j // 2, j % 2
        y[b, hf * 512:(hf + 1) * 512] = res.results[j]["yT"]
    return y


DILS = (1, 4, 16)


def build_dil(S=SEQ):
    kb = KB()
    NB = S // 128
    q_d = kb.din("q", [12, 64, S]); k_d = kb.din("k", [12, 64, S])
    v_d = kb.din("v", [12, 128, NB * 64])
    bm_d = kb.din("bm", [12, 128, 1024])
    attT = kb.dout("attT", [256, S])
    stg = [kb.sb([128, S]) for _ in range(2)]; stg_b = bufs(2)
    qs = [kb.sb([64, S], BF16) for _ in range(2)]; qs_b = bufs(2)
    ks = [kb.sb([64, S], BF16) for _ in range(2)]; ks_b = bufs(2)
    vs = [kb.sb([128, NB * 64], BF16) for _ in range(2)]; vs_b = bufs(2)
    eb = [kb.sb([128, 1024], BF16) for _ in range(2)]; eb_b = bufs(2)
    eb0 = [kb.sb([128, 512], BF16) for _ in range(2)]; eb0_b = bufs(2)
    num = kb.sb([64, S]); num_b = Buf()
    den = kb.sb([64, S]); den_b = Buf()
    onesb = kb.sb([128, 64], BF16); onesb_b = Buf()
    pT = [kb.sb([128, 512], BF16) for _ in range(4)]; pT_b = bufs(4)
    ps = [kb.ps([128, TT]) for _ in range(8)]; pb = bufs(8)
    kb.op("dve", lambda e: e.memset(onesb[:], 1.0), w=[onesb_b])
    u = 0
    pti = 0
    for h in range(4):
        for gi, d in enumerate(DILS):
            inst = gi * 4 + h
            bi = u % 2
            nb = NB // d
            kb.dma(stg[0][0:64, :], q_d[inst], w=[stg_b[0]])
            kb.op("pool", lambda e: e.tensor_copy(out=qs[bi][:], in_=stg[0][0:64, :]), r=[stg_b[0]], w=[qs_b[bi]])
            kb.dma(stg[1][0:64, :], k_d[inst], w=[stg_b[1]])
            kb.op("pool", lambda e: e.tensor_copy(out=ks[bi][:], in_=stg[1][0:64, :]), r=[stg_b[1]], w=[ks_b[bi]])
            kb.dma(stg[0][:, :NB * 64], v_d[inst], w=[stg_b[0]])
            kb.op("pool", lambda e: e.tensor_copy(out=vs[bi][:], in_=stg[0][:, :NB * 64]), r=[stg_b[0]], w=[vs_b[bi]])
            kb.dma(stg[1][:, :1024], bm_d[inst], w=[stg_b[1]])
            kb.op("act", lambda e: e.activation(out=eb[bi][:], in_=stg[1][:, :1024], func=AF.Exp), r=[stg_b[1]], w=[eb_b[bi]])
            kb.op("dve", lambda e: e.tensor_copy(out=eb0[bi][:], in_=eb[bi][:, 512:1024]), r=[eb_b[bi]], w=[eb0_b[bi]])
            kb.op("dve", lambda e: e.memset(eb0[bi][:, 0:128], 0.0), r=[eb0_b[bi]], w=[eb0_b[bi]])
            if d == 16:
                kb.op("dve", lambda e: e.memset(eb0[bi][:, 256:384], 0.0), r=[eb0_b[bi]], w=[eb0_b[bi]])
            for bt in range(NB // 4):
                B0 = bt * 4
                pss, psp, pso, psd = (0, 1, 2, 3) if bt % 2 == 0 else (4, 5, 6, 7)
                firsts = [(B0 + j) % nb == 0 for j in range(4)]
                for j in range(4):
                    B = B0 + j
                    mm(kb, ps[pss][:, j * 128:(j + 1) * 128], ks[bi][:, B * 128:(B + 1) * 128], qs[bi][:, B * 128:(B + 1) * 128], True, True,
                       r=[ks_b[bi], qs_b[bi]], w=[pb[pss]])
                    Bp = B if firsts[j] else B - 1
                    mm(kb, ps[psp][:, j * 128:(j + 1) * 128], ks[bi][:, Bp * 128:(Bp + 1) * 128], qs[bi][:, B * 128:(B + 1) * 128], True, True,
                       r=[ks_b[bi], qs_b[bi]], w=[pb[psp]])
                p1 = pti % 4; p2 = (pti + 1) % 4; pti += 2
                kb.op("act", lambda e: e.activation(out=pT[p1][:], in_=ps[pss][:], func=AF.Exp, scale=0.125), r=[pb[pss]], w=[pT_b[p1]])
                kb.op("act", lambda e: e.activation(out=pT[p2][:], in_=ps[psp][:], func=AF.Exp, scale=0.125), r=[pb[psp]], w=[pT_b[p2]])
                kb.op("dve", lambda e: e.tensor_tensor(out=pT[p1][:], in0=pT[p1][:], in1=eb[bi][:, 0:512], op=ALU.mult), r=[pT_b[p1], eb_b[bi]], w=[pT_b[p1]])
                ebp = eb0[bi][:] if any(firsts) else eb[bi][:, 512:1024]
                kb.op("dve", lambda e: e.tensor_tensor(out=pT[p2][:], in0=pT[p2][:], in1=ebp, op=ALU.mult), r=[pT_b[p2], eb_b[bi], eb0_b[bi]], w=[pT_b[p2]])
                for j in range(4):
                    B = B0 + j
                    Bp = B if firsts[j] else B - 1
                    cs_ = slice(j * 128, (j + 1) * 128)
                    mm(kb, ps[pso][0:64, cs_], vs[bi][:, B * 64:(B + 1) * 64], pT[p1][:, cs_], True, False, r=[vs_b[bi], pT_b[p1]], w=[pb[pso]])
                    mm(kb, ps[pso][0:64, cs_], vs[bi][:, Bp * 64:(Bp + 1) * 64], pT[p2][:, cs_], False, True, r=[vs_b[bi], pT_b[p2]], w=[pb[pso]])
                    mm(kb, ps[psd][0:64, cs_], onesb[:], pT[p1][:, cs_], True, False, r=[onesb_b, pT_b[p1]], w=[pb[psd]])
                    mm(kb, ps[psd][0:64, cs_], onesb[:], pT[p2][:, cs_], False, True, r=[onesb_b, pT_b[p2]], w=[pb[psd]])
                if d == 16:
                    r0 = B0 // nb
                    nv = num[:].rearrange("c (m r) -> c r m", r=16)[:, r0:r0 + 2, :]
                    dv_ = den[:].rearrange("c (m r) -> c r m", r=16)[:, r0:r0 + 2, :]
                    po = ps[pso][0:64, :].rearrange("c (r m) -> c r m", r=2)
                    pd = ps[psd][0:64, :].rearrange("c (r m) -> c r m", r=2)
                elif d == 4:
                    r0 = B0 // nb; m0 = (B0 % nb) * 128
                    nv = num[:].rearrange("c (m r) -> c r m", r=4)[:, r0, m0:m0 + 512]
                    dv_ = den[:].rearrange("c (m r) -> c r m", r=4)[:, r0, m0:m0 + 512]
                    po = ps[pso][0:64, :]; pd = ps[psd][0:64, :]
                else:
                    nv = num[:, B0 * 128:B0 * 128 + 512]; dv_ = den[:, B0 * 128:B0 * 128 + 512]
                    po = ps[pso][0:64, :]; pd = ps[psd][0:64, :]
                if gi == 0:
                    kb.op("act", lambda e: e.copy(nv, po), r=[pb[pso]], w=[num_b])
                    kb.op("act", lambda e: e.copy(dv_, pd), r=[pb[psd]], w=[den_b])
                else:
                    kb.op("dve", lambda e: e.tensor_tensor(out=nv, in0=po, in1=nv, op=ALU.add), r=[pb[pso], num_b], w=[num_b])
                    kb.op("dve", lambda e: e.tensor_tensor(out=dv_, in0=pd, in1=dv_, op=ALU.add), r=[pb[psd], den_b], w=[den_b])
            u += 1
        kb.op("dve", lambda e: e.reciprocal(den[:], den[:]), r=[den_b], w=[den_b])
        kb.op("dve", lambda e: e.tensor_tensor(out=num[:], in0=num[:], in1=den[:], op=ALU.mult), r=[num_b, den_b], w=[num_b])
        kb.dma(attT[h * 64:(h + 1) * 64, :], num[:], r=[num_b], final=True)
    return kb.finish()


def t5_bucket_np(dist):
    exact = 16
    logd = np.log(np.maximum(dist, 1).astype(np.float32) / exact) / np.float32(np.log(2048 / exact))
    large = np.minimum(exact + (logd * (32 - exact)).astype(np.int32), 31)
    return np.where(dist < exact, dist, large)


def run_dil(z1, rel_bias):
    nc = build_dil()
    S = SEQ
    NB = S // 128
    kk = np.arange(128)[:, None]; qq = np.arange(128)[None, :]
    in_maps = []
    for jc in range(NCORES):
        b, hf = jc // 2, jc % 2
        q_l, k_l, v_l, bm_l = [], [], [], []
        for gi, d in enumerate(DILS):
            for h in range(4 * hf, 4 * hf + 4):
                def rows(qkv):
                    r0 = gi * 1536 + qkv * 512 + h * 64
                    t = z1[b, r0:r0 + 64]
                    return t.reshape(64, S // d, d).transpose(0, 2, 1).reshape(64, S)
                q_l.append(rows(0)); k_l.append(rows(1))
                v = rows(2)
                v_l.append(v.reshape(64, NB, 128).transpose(2, 1, 0).reshape(128, NB * 64))
                bias_h = rel_bias[:, gi * 8 + h]
                same = np.where(qq >= kk, bias_h[t5_bucket_np(np.clip(qq - kk, 0, 128) * d)], -30000.0)
                prev = np.where(qq <= kk, bias_h[t5_bucket_np(np.clip(128 + qq - kk, 0, 128) * d)], -30000.0)
                bm_l.append(np.concatenate([np.tile(same, (1, 4)), np.tile(prev, (1, 4))], 1))
        in_maps.append({"q": np.ascontiguousarray(np.stack(q_l)), "k": np.ascontiguousarray(np.stack(k_l)),
                        "v": np.ascontiguousarray(np.stack(v_l)), "bm": np.ascontiguousarray(np.stack(bm_l).astype(np.float32))})
    res = run_bass_kernel_spmd(nc, in_maps, core_ids=list(range(NCORES)))
    att = np.zeros((4, 512, S), np.float32)
    for jc in range(NCORES):
        b, hf = jc // 2, jc % 2
        att[b, hf * 256:(hf + 1) * 256] = res.results[jc]["attT"]
    return att


CL = 64
RW_GN_EPS = 64e-5
WDEC = -0.6065306597126334


RW_STOP = None


def build_rwkv(S=SEQ, nh=12):
    kb = KB()
    NBT = S // 512
    FW = nh * 64
    zr_d = kb.din("zr", [FW, S]); zk_d = kb.din("zk", [FW, S])
    vt_d = kb.din("v_tok", [S, FW]); vp_d = kb.din("vprev_tok", [S, FW])
    zwd_d = kb.din("zwd", [64, S]); zad_d = kb.din("zad", [64, S]); zgd_d = kb.din("zgd", [224, S])
    cols_d = kb.din("cols", [64, 8 * nh])
    mul_d = kb.din("mu_lora", [128, 4])
    w2_d = kb.din("w2", [64, FW]); a2_d = kb.din("a2", [64, FW]); g2_d = kb.din("g2", [224, FW])
    rows_d = kb.din("rows", [64, 3 * FW])
    cst_d = kb.din("cst", [64, 2048])
    rmask_d = kb.din("rmask", [64, 512])
    out_d = kb.dout("tm_tok", [S, FW])

    cst = kb.sb([64, 2048]); cst_b = Buf()
    rmask = kb.sb([64, 512]); rmask_b = Buf()
    cols = kb.sb([64, 8 * nh]); cols_b = Buf()
    mul = kb.sb([128, 4]); mul_b = Buf()
    w2 = kb.sb([64, FW]); a2 = kb.sb([64, FW]); lw_b = Buf()
    g2s = kb.sb([128, FW]); g2a = kb.sb([128, FW], BF16); g2b = kb.sb([96, FW], BF16)
    rows = kb.sb([64, 3 * FW]); rows_b = Buf()
    onescol = kb.sb([64, 1]); onescol_b = Buf()
    tw = kb.sb([64, S]); xad = kb.sb([64, S]); sg0 = kb.sb([128, S], BF16); sg1 = kb.sb([96, S], BF16); lora_b = Buf()
    P_b = Buf()
    big = kb.sb([128, 4097]); big2 = kb.sb([128, 4096])

    kb.dma(cst[:], cst_d[:, :], w=[cst_b])
    kb.dma(rmask[:], rmask_d[:, :], w=[rmask_b])
    kb.dma(cols[:], cols_d[:, :], w=[cols_b])
    kb.dma(mul[:], mul_d[:, :], w=[mul_b])
    kb.dma(w2[:], w2_d[:, :], w=[lw_b]); kb.dma(a2[:], a2_d[:, :], w=[lw_b])
    kb.dma(g2s[:], g2_d[0:128, :], w=[P_b])
    kb.op("dve", lambda e: e.tensor_copy(out=g2a[:], in_=g2s[:]), r=[P_b], w=[lw_b])
    kb.dma(g2s[0:96, :], g2_d[128:224, :], r=[lw_b], w=[P_b])
    kb.op("dve", lambda e: e.tensor_copy(out=g2b[:], in_=g2s[0:96, :]), r=[P_b], w=[lw_b])
    kb.dma(rows[:], rows_d[:, :], w=[rows_b])
    kb.op("dve", lambda e: e.memset(onescol[:], 1.0), w=[onescol_b])
    ones64 = cst[:, 0:64]; ident = cst[:, 64:128]; maskG2 = cst[:, 128:384]
    maskU8 = cst[:, 384:896]; maskL8 = cst[:, 896:1408]; I8 = cst[:, 1408:1920]

    def shifted(src_ap, P, mucol, dst, func):
        kb.op("dve", lambda e: e.memset(big[0:P, 0:1], 0.0), r=[P_b], w=[P_b])
        kb.dma(big[0:P, 1:S + 1], src_ap, w=[P_b])
        kb.op("dve", lambda e: e.tensor_tensor(out=big2[0:P, 0:S], in0=big[0:P, 0:S], in1=big[0:P, 1:S + 1], op=ALU.subtract), r=[P_b], w=[P_b])
        kb.op("dve", lambda e: e.scalar_tensor_tensor(out=big2[0:P, 0:S], in0=big2[0:P, 0:S], scalar=mucol, in1=big[0:P, 1:S + 1], op0=ALU.mult, op1=ALU.add),
              r=[P_b, mul_b], w=[P_b])
        if func is None:
            kb.op("act", lambda e: e.copy(dst, big2[0:P, 0:S]), r=[P_b], w=[lora_b])
        else:
            kb.op("act", lambda e: e.activation(out=dst, in_=big2[0:P, 0:S], func=func), r=[P_b], w=[lora_b])
    shifted(zwd_d[:, :], 64, mul[0:64, 0:1], tw[:], AF.Tanh)
    shifted(zad_d[:, :], 64, mul[0:64, 1:2], xad[:], None)
    shifted(zgd_d[0:128, :], 128, mul[:, 2:3], sg0[:], AF.Sigmoid)
    shifted(zgd_d[128:224, :], 96, mul[0:96, 3:4], sg1[:], AF.Sigmoid)

    class _V:
        def __init__(self, t, i):
            self.t, self.i = t, i

        def __getitem__(self, idx):
            return self.t[0:64, self.i * 512:(self.i + 1) * 512][idx]

    ps = [kb.ps([128, 512]) for _ in range(8)]; pb = bufs(8)

    def make_set(si):
        T = {}
        if si == 0:
            scr = [_V(big, i) for i in range(8)] + [_V(big2, i) for i in range(5)]
        else:
            extra = kb.sb([64, 10 * 512])
            scr = [_V(big2, i) for i in range(5, 8)] + [_V(extra, i) for i in range(10)]
        T["scr"] = scr
        T["A_b"] = P_b if si == 0 else Buf()
        T["P_extra"] = [P_b]
        T["zrt"] = kb.sb([64, 513]); T["zkt"] = kb.sb([64, 513]); T["zin_b"] = Buf()
        T["AR"] = kb.sb([64, 8, 128]); T["BK"] = kb.sb([64, 8, 128]); T["rkr"] = kb.sb([64, 512]); T["ARBK_b"] = Buf()
        T["St"] = kb.sb([64, 64]); T["St_b"] = Buf(); T["Stw"] = kb.sb([64, 64]); T["Stw_b"] = Buf()
        T["Vx"] = kb.sb([64, 8, 64]); T["Vx_b"] = Buf()
        T["Pm"] = [kb.sb([64, 512]) for _ in range(2)]; T["Qm"] = [kb.sb([64, 512]) for _ in range(2)]; T["TTm"] = [kb.sb([64, 512]) for _ in range(2)]
        T["Tm"] = kb.sb([64, 512]); T["C_b"] = Buf()
        T["Gm"] = kb.sb([64, 256]); T["Gm_b"] = Buf()
        T["BKT"] = kb.sb([64, 128]); T["BKT_b"] = Buf()
        T["Xs"] = kb.sb([64, 64]); T["Xs_b"] = Buf(); T["Us"] = kb.sb([64, 64]); T["Us_b"] = Buf()
        T["ep1"] = kb.sb([64, 8, 64]); T["ep2"] = kb.sb([64, 8, 64]); T["ep_b"] = Buf()
        T["st8"] = [kb.sb([64, 8]) for _ in range(3)]
        T["bank"] = [4 * si + k for k in range(4)]
        return T

    def head_gen(h, T):
        R, Kx, lwt, av, kk, kkn, Kp, cum, Ep, Em, Epr, t1, t2 = T["scr"]
        A_b = T["A_b"]; zrt, zkt, zin_b = T["zrt"], T["zkt"], T["zin_b"]
        AR, BK, rkr, ARBK_b = T["AR"], T["BK"], T["rkr"], T["ARBK_b"]
        St, St_b, Stw, Stw_b = T["St"], T["St_b"], T["Stw"], T["Stw_b"]
        Vx, Vx_b = T["Vx"], T["Vx_b"]
        Pm, Qm, TTm, Tm, C_b = T["Pm"], T["Qm"], T["TTm"], T["Tm"], T["C_b"]
        Gm, Gm_b, BKT, BKT_b, Xs, Xs_b, Us, Us_b = T["Gm"], T["Gm_b"], T["BKT"], T["BKT_b"], T["Xs"], T["Xs_b"], T["Us"], T["Us_b"]
        ep1, ep2, ep_b, st8 = T["ep1"], T["ep2"], T["ep_b"], T["st8"]
        b0, b1, b2, b3 = T["bank"]
        AX_ = [A_b] + ([P_b] if A_b is not P_b else [])
        cc = lambda k: cols[:, k * nh + h:k * nh + h + 1]
        hc = slice(h * 64, (h + 1) * 64)
        kb.op("dve", lambda e: e.memset(St[:], 0.0), r=[St_b], w=[St_b])
        for bi in range(NBT):
            t0 = bi * 512
            A_ = lambda eng, fn, extra_r=(): kb.op(eng, fn, r=AX_ + [zin_b] + list(extra_r), w=[A_b])
            for (zt, zd) in [(zrt, zr_d), (zkt, zk_d)]:
                if bi == 0:
                    kb.op("dve", lambda e: e.memset(zt[:, 0:1], 0.0), r=[zin_b, A_b], w=[zin_b])
                    kb.dma(zt[:, 1:513], zd[hc, 0:512], w=[zin_b])
                else:
                    kb.dma(zt[:], zd[hc, t0 - 1:t0 + 512], w=[zin_b])
            E = [ep_b]
            kb.dma(ep1[:], vt_d[t0:t0 + 512, hc].rearrange("(c t) i -> t c i", t=64), w=E, q="pool")
            kb.dma(ep2[:], vp_d[t0:t0 + 512, hc].rearrange("(c t) i -> t c i", t=64), w=E, q="pool")
            for (zt, dst, k) in [(zrt, R, 0), (zkt, Kx, 1)]:
                A_("dve", lambda e: e.tensor_tensor(out=t1[:], in0=zt[:, 0:512], in1=zt[:, 1:513], op=ALU.subtract))
                A_("dve", lambda e: e.scalar_tensor_tensor(out=dst[:], in0=t1[:], scalar=cc(k), in1=zt[:, 1:513], op0=ALU.mult, op1=ALU.add), [cols_b])
            mm(kb, ps[b0][0:64, :], w2[:, hc], tw[:, t0:t0 + 512], True, True, r=[lw_b, lora_b], w=[pb[b0]])
            A_("act", lambda e: e.activation(out=lwt[:], in_=ps[b0][0:64, :], func=AF.Sigmoid, bias=cc(2)), [pb[b0], cols_b])
            A_("act", lambda e: e.mul(lwt[:], lwt[:], WDEC))
            mm(kb, ps[b0][0:64, :], a2[:, hc], xad[:, t0:t0 + 512], True, True, r=[lw_b, lora_b], w=[pb[b0]])
            A_("act", lambda e: e.activation(out=av[:], in_=ps[b0][0:64, :], func=AF.Sigmoid, bias=cc(3)), [pb[b0], cols_b])
            A_("dve", lambda e: e.tensor_scalar(out=kk[:], in0=Kx[:], scalar1=cc(4), scalar2=None, op0=ALU.mult), [cols_b])
            A_("act", lambda e: e.activation(out=t1[:], in_=kk[:], func=AF.Square))
            mm(kb, ps[b0][0:64, :], ones64, t1[:], True, True, r=[cst_b, A_b], w=[pb[b0]])
            A_("act", lambda e: e.sqrt(t2[:], ps[b0][0:64, :]), [pb[b0]])
            yield
            A_("dve", lambda e: e.tensor_scalar_max(t2[:], t2[:], 1e-12))
            A_("dve", lambda e: e.reciprocal(t2[:], t2[:]))
            A_("dve", lambda e: e.tensor_tensor(out=kkn[:], in0=kk[:], in1=t2[:], op=ALU.mult))
            A_("dve", lambda e: e.tensor_scalar(out=t1[:], in0=av[:], scalar1=-1.0, scalar2=None, op0=ALU.add))
            A_("dve", lambda e: e.tensor_scalar(out=t1[:], in0=t1[:], scalar1=cc(5), scalar2=None, op0=ALU.mult), [cols_b])
            A_("dve", lambda e: e.scalar_tensor_tensor(out=Kp[:], in0=t1[:], scalar=1.0, in1=Kx[:], op0=ALU.add, op1=ALU.mult))
            A_("dve", lambda e: e.tensor_tensor_scan(out=cum[:], data0=rmask[:], data1=lwt[:], initial=0.0, op0=ALU.mult, op1=ALU.add), [rmask_b])
            A_("act", lambda e: e.activation(out=Ep[:], in_=cum[:], func=AF.Exp))
            A_("act", lambda e: e.activation(out=Em[:], in_=cum[:], func=AF.Exp, scale=-1.0))
            A_("dve", lambda e: e.tensor_tensor(out=t1[:], in0=cum[:], in1=lwt[:], op=ALU.subtract))
            A_("act", lambda e: e.activation(out=Epr[:], in_=t1[:], func=AF.Exp))
            yield
            v3 = lambda t: t[:].rearrange("p (c t) -> p c t", t=64)
            AB = AX_ + [ARBK_b]
            kb.op("dve", lambda e: e.scalar_tensor_tensor(out=AR[:, :, 0:64], in0=v3(kkn), scalar=-1.0, in1=v3(Epr), op0=ALU.mult, op1=ALU.mult), r=AB, w=[ARBK_b])
            kb.op("dve", lambda e: e.tensor_tensor(out=AR[:, :, 64:128], in0=v3(R), in1=v3(Ep), op=ALU.mult), r=AB, w=[ARBK_b])
            A_("dve", lambda e: e.tensor_tensor(out=t1[:], in0=kkn[:], in1=av[:], op=ALU.mult))
            kb.op("dve", lambda e: e.tensor_tensor(out=BK[:, :, 0:64], in0=v3(t1), in1=v3(Em), op=ALU.mult), r=AB, w=[ARBK_b])
            kb.op("dve", lambda e: e.tensor_tensor(out=BK[:, :, 64:128], in0=v3(Kp), in1=v3(Em), op=ALU.mult), r=AB, w=[ARBK_b])
            kb.op("dve", lambda e: e.scalar_tensor_tensor(out=rkr[:], in0=R[:], scalar=cc(6), in1=Kp[:], op0=ALU.mult, op1=ALU.mult), r=AB + [cols_b], w=[ARBK_b])
            Ep3 = v3(Ep)
            muv = rows[:, hc].rearrange("p (o i) -> p o i", o=1).to_broadcast([64, 8, 64])
            kb.op("dve", lambda e: e.tensor_tensor(out=ep2[:], in0=ep2[:], in1=ep1[:], op=ALU.subtract), r=E, w=E)
            kb.op("dve", lambda e: e.tensor_tensor(out=ep2[:], in0=ep2[:], in1=muv, op=ALU.mult), r=E + [rows_b], w=E)
            kb.op("dve", lambda e: e.tensor_tensor(out=Vx[:], in0=ep2[:], in1=ep1[:], op=ALU.add), r=E, w=[Vx_b])
            yield
            CB = [C_b]
            for ch in range(8):
                mm(kb, ps[b0][0:64, ch * 64:(ch + 1) * 64], BK[:, ch, 0:64], AR[:, ch, 0:64], True, True, r=[ARBK_b], w=[pb[b0]])
                mm(kb, ps[b1][0:64, ch * 64:(ch + 1) * 64], AR[:, ch, 0:64], BK[:, ch, 0:64], True, True, r=[ARBK_b], w=[pb[b1]])
            kb.op("dve", lambda e: e.tensor_tensor(out=Pm[0][:], in0=ps[b0][0:64, :], in1=maskU8, op=ALU.mult), r=[pb[b0], cst_b] + CB, w=CB)
            kb.op("dve", lambda e: e.tensor_tensor(out=Qm[0][:], in0=ps[b1][0:64, :], in1=maskL8, op=ALU.mult), r=[pb[b1], cst_b] + CB, w=CB)
            kb.op("dve", lambda e: e.tensor_tensor(out=Tm[:], in0=Pm[0][:], in1=I8, op=ALU.add), r=[cst_b] + CB, w=CB)
            kb.op("dve", lambda e: e.tensor_tensor(out=TTm[0][:], in0=Qm[0][:], in1=I8, op=ALU.add), r=[cst_b] + CB, w=CB)
            yield
            NL = 5
            for lv in range(NL):
                a_, b_ = lv % 2, (lv + 1) % 2
                last = lv == NL - 1
                for ch in range(8):
                    sl = slice(ch * 64, (ch + 1) * 64)
                    mm(kb, ps[b0][0:64, sl], Qm[a_][:, sl], Pm[a_][:, sl], True, True, r=CB, w=[pb[b0]])
                    if not last:
                        mm(kb, ps[b1][0:64, sl], Pm[a_][:, sl], Qm[a_][:, sl], True, True, r=CB, w=[pb[b1]])
                kb.op("act", lambda e: e.copy(Pm[b_][:], ps[b0][0:64, :]), r=[pb[b0]] + CB, w=CB)
                if not last:
                    kb.op("dve", lambda e: e.tensor_copy(out=Qm[b_][:], in_=ps[b1][0:64, :]), r=[pb[b1]] + CB, w=CB)
                yield
                for ch in range(8):
                    sl = slice(ch * 64, (ch + 1) * 64)
                    mm(kb, ps[b2][0:64, sl], TTm[a_][:, sl], Pm[b_][:, sl], True, True, r=CB, w=[pb[b2]])
                    if not last:
                        mm(kb, ps[b3][0:64, sl], Pm[b_][:, sl], TTm[a_][:, sl], True, True, r=CB, w=[pb[b3]])
                kb.op("dve", lambda e: e.tensor_tensor(out=Tm[:], in0=ps[b2][0:64, :], in1=Tm[:], op=ALU.add), r=[pb[b2]] + CB, w=CB)
                if not last:
                    kb.op("dve", lambda e: e.tensor_tensor(out=TTm[b_][:], in0=ps[b3][0:64, :], in1=TTm[a_][:], op=ALU.add), r=[pb[b3]] + CB, w=CB)
                yield
            for ch in range(8):
                mm(kb, ps[b0][0:64, 0:128], BK[:, ch, 0:64], AR[:, ch, :], True, True, r=[ARBK_b], w=[pb[b0]])
                mm(kb, ps[b0][0:64, 128:256], BK[:, ch, 64:128], AR[:, ch, :], True, True, r=[ARBK_b], w=[pb[b0]])
                kb.op("dve", lambda e: e.tensor_tensor(out=Gm[:], in0=ps[b0][0:64, 0:256], in1=maskG2, op=ALU.mult), r=[pb[b0], cst_b], w=[Gm_b])
                kb.op("pe", lambda e: e.transpose(ps[b0][0:64, 256:320], BK[:, ch, 0:64], ident), r=[ARBK_b, cst_b], w=[pb[b0]])
                kb.op("pe", lambda e: e.transpose(ps[b0][0:64, 320:384], BK[:, ch, 64:128], ident), r=[ARBK_b, cst_b], w=[pb[b0]])
                kb.op("act", lambda e: e.copy(BKT[:], ps[b0][0:64, 256:384]), r=[pb[b0]], w=[BKT_b])
                mm(kb, ps[b1][0:64, 0:64], AR[:, ch, 0:64], St[:], True, False, r=[ARBK_b, St_b], w=[pb[b1]])
                mm(kb, ps[b1][0:64, 0:64], Gm[:, 128:192], Vx[:, ch, :], False, True, r=[Gm_b, Vx_b], w=[pb[b1]])
                kb.op("act", lambda e: e.copy(Xs[:], ps[b1][0:64, 0:64]), r=[pb[b1]], w=[Xs_b])
                yield
                mm(kb, ps[b1][0:64, 64:128], Tm[:, ch * 64:(ch + 1) * 64], Xs[:], True, True, r=CB + [Xs_b], w=[pb[b1]])
                kb.op("act", lambda e: e.copy(Us[:], ps[b1][0:64, 64:128]), r=[pb[b1]], w=[Us_b])
                yield
                ysl = slice(ch * 64, (ch + 1) * 64)
                mm(kb, ps[b2][0:64, ysl], AR[:, ch, 64:128], St[:], True, False, r=[ARBK_b, St_b], w=[pb[b2]])
                mm(kb, ps[b2][0:64, ysl], Gm[:, 64:128], Us[:], False, False, r=[Gm_b, Us_b], w=[pb[b2]])
                mm(kb, ps[b2][0:64, ysl], Gm[:, 192:256], Vx[:, ch, :], False, True, r=[Gm_b, Vx_b], w=[pb[b2]])
                kb.op("dve", lambda e: e.tensor_scalar(out=Stw[:], in0=St[:], scalar1=Ep3[:, ch, 63:64], scalar2=None, op0=ALU.mult), r=[St_b, A_b], w=[Stw_b])
                mm(kb, ps[b1][0:64, 128:192], BKT[:, 0:64], Us[:], True, False, r=[BKT_b, Us_b], w=[pb[b1]])
                mm(kb, ps[b1][0:64, 128:192], BKT[:, 64:128], Vx[:, ch, :], False, True, r=[BKT_b, Vx_b], w=[pb[b1]])
                kb.op("dve", lambda e: e.scalar_tensor_tensor(out=St[:], in0=ps[b1][0:64, 128:192], scalar=Ep3[:, ch, 63:64], in1=Stw[:], op0=ALU.mult, op1=ALU.add),
                      r=[pb[b1], A_b, Stw_b], w=[St_b])
                mm(kb, ps[b1][0:64, 192 + ch:193 + ch], rkr[:, ch * 64:(ch + 1) * 64], onescol[:], True, True, r=[ARBK_b, onescol_b], w=[pb[b1]])
                mm(kb, ps[b3][0:64, ysl], sg0[:, t0 + ch * 64:t0 + (ch + 1) * 64], g2a[:, hc], True, False, r=[lora_b, lw_b], w=[pb[b3]])
                mm(kb, ps[b3][0:64, ysl], sg1[:, t0 + ch * 64:t0 + (ch + 1) * 64], g2b[:, hc], False, True, r=[lora_b, lw_b], w=[pb[b3]])
                yield
            Y3 = ps[b2][0:64, :].rearrange("p (c i) -> p c i", i=64)
            bc8 = lambda t: t[:, :].rearrange("p (c o) -> p c o", o=1).to_broadcast([64, 8, 64])
            rowb = lambda k: rows[:, k * FW + h * 64:k * FW + (h + 1) * 64].rearrange("p (o i) -> p o i", o=1).to_broadcast([64, 8, 64])
            kb.op("dve", lambda e: e.tensor_reduce(out=st8[0][:], in_=Y3, axis=AX.X, op=ALU.add), r=[pb[b2]] + E, w=E)
            kb.op("dve", lambda e: e.tensor_single_scalar(out=st8[0][:], in_=st8[0][:], scalar=1.0 / 64, op=ALU.mult), r=E, w=E)
            kb.op("dve", lambda e: e.tensor_tensor(out=ep1[:], in0=Y3, in1=bc8(st8[0]), op=ALU.subtract), r=[pb[b2]] + E, w=E)
            kb.op("dve", lambda e: e.tensor_tensor(out=ep2[:], in0=ep1[:], in1=ep1[:], op=ALU.mult), r=E, w=E)
            kb.op("dve", lambda e: e.tensor_reduce(out=st8[1][:], in_=ep2[:], axis=AX.X, op=ALU.add), r=E, w=E)
            kb.op("dve", lambda e: e.tensor_scalar(out=st8[1][:], in0=st8[1][:], scalar1=1.0 / 64, scalar2=RW_GN_EPS, op0=ALU.mult, op1=ALU.add), r=E, w=E)
            kb.op("act", lambda e: e.sqrt(st8[1][:], st8[1][:]), r=E, w=E)
            kb.op("dve", lambda e: e.reciprocal(st8[1][:], st8[1][:]), r=E, w=E)
            yield
            kb.op("dve", lambda e: e.tensor_tensor(out=ep1[:], in0=ep1[:], in1=bc8(st8[1]), op=ALU.mult), r=E, w=E)
            kb.op("dve", lambda e: e.tensor_tensor(out=ep1[:], in0=ep1[:], in1=rowb(1), op=ALU.mult), r=E + [rows_b], w=E)
            kb.op("dve", lambda e: e.tensor_tensor(out=ep1[:], in0=ep1[:], in1=rowb(2), op=ALU.add), r=E + [rows_b], w=E)
            kb.op("act", lambda e: e.copy(st8[2][:], ps[b1][0:64, 192:200]), r=[pb[b1]] + E, w=E)
            kb.op("dve", lambda e: e.tensor_tensor(out=ep2[:], in0=Vx[:], in1=bc8(st8[2]), op=ALU.mult), r=E + [Vx_b], w=E)
            kb.op("dve", lambda e: e.tensor_tensor(out=ep1[:], in0=ep1[:], in1=ep2[:], op=ALU.add), r=E, w=E)
            kb.op("dve", lambda e: e.tensor_tensor(out=ep2[:], in0=ep1[:], in1=ps[b3][0:64, :].rearrange("p (c i) -> p c i", i=64), op=ALU.mult), r=E + [pb[b3]], w=E)
            kb.dma(out_d[t0:t0 + 512, hc].rearrange("(c t) i -> t c i", t=64), ep2[:], r=E, final=True)
            yield

    sets = [make_set(0), make_set(1)]
    for h0 in range(0, nh, 2):
        gens = [head_gen(h0 + i, sets[i]) for i in range(min(2, nh - h0))]
        alive = list(gens)
        while alive:
            for g in list(alive):
                try:
                    next(g)
                except StopIteration:
                    alive.remove(g)
    return kb.finish()


def run_rwkv(z1, rw, S=SEQ, nh=12, ncores=NCORES):
    nc = build_rwkv(S, nh)
    FW = nh * 64
    base = 4608
    mu = rw["mu"]
    s_ = np.arange(64)[:, None]; q_ = np.arange(128)[None, :]
    maskG = np.where(q_ < 64, s_ < q_, s_ <= (q_ - 64)).astype(np.float32)
    r64 = np.arange(64)[:, None]; c64 = np.arange(64)[None, :]
    U8 = np.tile((r64 < c64).astype(np.float32), (1, 8)); L8 = np.tile((r64 > c64).astype(np.float32), (1, 8)); I8 = np.tile(np.eye(64, dtype=np.float32), (1, 8))
    cst = np.zeros((64, 2048), np.float32)
    cst[:, 0:64] = 1.0; cst[:, 64:128] = np.eye(64); cst[:, 128:256] = maskG; cst[:, 256:384] = maskG
    cst[:, 384:896] = U8; cst[:, 896:1408] = L8; cst[:, 1408:1920] = I8
    rmask = np.ones((64, 512), np.float32); rmask[:, ::64] = 0
    in_maps = []
    for jc in range(ncores):
        b, hf = jc // 2, jc % 2
        fs = slice(hf * 768, hf * 768 + FW)
        def colv(v):
            return v[fs].reshape(nh, 64).T
        cols = np.zeros((64, 8 * nh), np.float32)
        for k, v in enumerate([mu[0:1536], mu[1536:3072], rw["w0"], rw["a0"], rw["k_k"], rw["k_a"], rw["r_k"].reshape(-1)]):
            cols[:, k * nh:(k + 1) * nh] = colv(v)
        mul = np.zeros((128, 4), np.float32)
        mul[0:64, 0] = mu[4608:4672]; mul[0:64, 1] = mu[4672:4736]; mul[:, 2] = mu[4736:4864]; mul[0:96, 3] = mu[4864:4960]
        rows = np.concatenate([np.broadcast_to(v[fs][None, :], (64, FW)) for v in [mu[3072:4608], rw["lnx_g"], rw["lnx_b"]]], 1)
        v_tok = np.ascontiguousarray(z1[b, base + 3072 + hf * 768: base + 3072 + hf * 768 + FW, :S].T)
        vprev = np.concatenate([np.zeros((1, FW), np.float32), v_tok[:-1]], 0)
        in_maps.append({
            "zr": np.ascontiguousarray(z1[b, base + hf * 768: base + hf * 768 + FW, :S]),
            "zk": np.ascontiguousarray(z1[b, base + 1536 + hf * 768: base + 1536 + hf * 768 + FW, :S]),
            "v_tok": v_tok, "vprev_tok": np.ascontiguousarray(vprev),
            "zwd": np.ascontiguousarray(z1[b, base + 4608:base + 4672, :S]), "zad": np.ascontiguousarray(z1[b, base + 4672:base + 4736, :S]),
            "zgd": np.ascontiguousarray(z1[b, base + 4736:base + 4960, :S]),
            "cols": cols, "mu_lora": mul, "w2": np.ascontiguousarray(rw["w2"][:, fs]), "a2": np.ascontiguousarray(rw["a2"][:, fs]),
            "g2": np.ascontiguousarray(rw["g2"][:, fs]), "rows": np.ascontiguousarray(rows), "cst": cst, "rmask": rmask})
    res = run_bass_kernel_spmd(nc, in_maps, core_ids=list(range(ncores)))
    tm = np.zeros((4, 1536, S), np.float32)
    for jc in range(ncores):
        b, hf = jc // 2, jc % 2
        tm[b, hf * 768:hf * 768 + FW] = res.results[jc]["tm_tok"].T
    return tm


def run_post(layer0, mixT, x_tok, mod_l, wout, wglu, ln, router_w, router_b, wgu, wd):
    nc = build_post(layer0)
    sel = np.zeros((16, 16, 128), np.float32)
    for e in range(16):
        sel[e, e, :] = 1.0
    sel = sel.reshape(16, 2048)
    ident = np.eye(128, dtype=np.float32)
    rb_bc = np.ascontiguousarray(np.broadcast_to(router_b[None, :], (128, 16)))
    in_maps = []
    for j in range(NCORES):
        b, hf = j // 2, j % 2
        ts = slice(hf * 2048, (hf + 1) * 2048)
        m = mod_l[b]
        vecs = [m[2 * D:3 * D], m[4 * D:5 * D], m[3 * D:4 * D], m[5 * D:6 * D], ln[0], ln[1], ln[2], ln[3]]
        pvec = np.ascontiguousarray(np.concatenate([fm16(v) for v in vecs], 1))
        im = {"mixT": np.ascontiguousarray(mixT[b][:, ts]), "xT": np.ascontiguousarray(x_tok[b, ts].T), "wout": wout, "pvec": pvec,
              "router_w": router_w, "router_b_bc": rb_bc, "wgu": wgu, "wd": wd, "ident": ident, "sel": sel}
        if layer0:
            im["wglu"] = wglu
        in_maps.append(im)
    res = run_bass_kernel_spmd(nc, in_maps, core_ids=list(range(NCORES)))
    out = np.zeros((4, SEQ, D), np.float32)
    for j in range(NCORES):
        b, hf = j // 2, j % 2
        out[b, hf * 2048:(hf + 1) * 2048] = res.results[j]["xoT"].T
    return out


def kernel(x, c, ada_w, ada_b, ln_mix_g, ln_mix_b, ln_ffn_g, ln_ffn_b, router_w, router_b,
           moe_w_gate_up, moe_w_down, rel_bias, ev_w_in, mla_q_norm, mla_w_uq, mla_kv_norm, mla_w_ukv,
           s5_lambda_re, s5_lambda_im, s5_b_re, s5_b_im, s5_c_re, s5_c_im, s5_d, s5_log_dt, s5_w_glu,
           ev_w_out, od_w_in, rw_mu, rw_w0, rw_w2, rw_a0, rw_a2, rw_g2, rw_k_k, rw_k_a, rw_r_k,
           rw_lnx_g, rw_lnx_b, od_w_out):
    f = lambda a: np.ascontiguousarray(np.asarray(a, dtype=np.float32))
    x = f(x)
    mod = run_ada(f(c), f(ada_w), f(ada_b))
    w = f(ev_w_in[0])
    wext = np.ascontiguousarray(np.concatenate([w, w[:, 800:832], w[:, 768:800]], 1))
    z0 = run_pre(x, mod[0], wext, 1, 0)
    att0 = run_mla(z0, f(mla_w_uq[0]), f(mla_w_ukv[0]), f(mla_q_norm[0]), f(mla_kv_norm[0]))
    ys5 = run_s5(z0, f(s5_lambda_re[0]), f(s5_lambda_im[0]), f(s5_b_re[0]), f(s5_b_im[0]), f(s5_c_re[0]), f(s5_c_im[0]), f(s5_d[0]), f(s5_log_dt[0]))
    del z0
    mix0 = np.concatenate([att0, ys5], 1)
    x1 = run_post(True, mix0, x, mod[0], f(ev_w_out[0]), f(s5_w_glu[0]), [f(ln_mix_g[0]), f(ln_mix_b[0]), f(ln_ffn_g[0]), f(ln_ffn_b[0])],
                  f(router_w), f(router_b), f(moe_w_gate_up[0]), f(moe_w_down[0]))
    del mix0, att0, ys5
    w = f(od_w_in[0])
    wext = np.ascontiguousarray(np.concatenate([w, np.zeros((D, 9600 - w.shape[1]), np.float32)], 1))
    z1 = run_pre(x1, mod[1], wext, 1, 0)
    att1 = run_dil(z1, f(rel_bias))
    rw = {"mu": f(rw_mu[0]), "w0": f(rw_w0[0]), "w2": f(rw_w2[0]), "a0": f(rw_a0[0]), "a2": f(rw_a2[0]), "g2": f(rw_g2[0]),
          "k_k": f(rw_k_k[0]), "k_a": f(rw_k_a[0]), "r_k": f(rw_r_k[0]), "lnx_g": f(rw_lnx_g[0]), "lnx_b": f(rw_lnx_b[0])}
    tm = run_rwkv(z1, rw)
    del z1
    mix1 = np.concatenate([att1, tm], 1)
    x2 = run_post(False, mix1, x1, mod[1], f(od_w_out[0]), None, [f(ln_mix_g[1]), f(ln_mix_b[1]), f(ln_ffn_g[1]), f(ln_ffn_b[1])],
                  f(router_w), f(router_b), f(moe_w_gate_up[1]), f(moe_w_down[1]))
    return x2.astype(np.float32)
```

```python
import contextlib
import numpy as np
import concourse.bass as bass
import concourse.mybir as mybir
from concourse.bass_utils import run_bass_kernel_spmd

F32 = mybir.dt.float32
BF16 = mybir.dt.bfloat16
AF = mybir.ActivationFunctionType
ALU = mybir.AluOpType
AX = mybir.AxisListType

D = 2048
NCORES = 8
DN_ALPHA = 4.0 ** 0.25
LN_EPS = 1e-5


SAME_ENGINE_WAIT = True


class Buf:
    __slots__ = ("w", "r", "dsem", "dval")

    def __init__(self):
        self.w = None
        self.r = {}
        self.dsem = None
        self.dval = 0


def bufs(n):
    return [Buf() for _ in range(n)]


class KB:
    def __init__(self):
        self.nc = bass.Bass("TRN2", target_bir_lowering=False)
        nc = self.nc
        self.eng = {"pe": nc.tensor, "act": nc.scalar, "dve": nc.vector, "pool": nc.gpsimd, "sp": nc.sync}
        self.stack = contextlib.ExitStack()
        self.sem, self.seq, self.seen = {}, {}, {}
        for e in self.eng:
            self.sem[e] = self.stack.enter_context(nc.semaphore("s_" + e))
            self.seq[e] = 0
            self.seen[e] = {}
        self.nsem = 0
        self.finals = []
        self.nm = 0

    def name(self, p):
        self.nm += 1
        return "%s%d" % (p, self.nm)

    def din(self, name, shape, dt=F32):
        return self.nc.dram_tensor(name, list(shape), dt, kind="ExternalInput").ap()

    def dout(self, name, shape, dt=F32):
        return self.nc.dram_tensor(name, list(shape), dt, kind="ExternalOutput").ap()

    def sb(self, shape, dt=F32, name=None):
        return self.stack.enter_context(self.nc.sbuf_tensor(name or self.name("sb"), list(shape), dt))

    def ps(self, shape, dt=F32, name=None):
        return self.stack.enter_context(self.nc.psum_tensor(name or self.name("ps"), list(shape), dt))

    def _wait(self, e, ev):
        sem, val, src = ev
        key = id(sem)
        if self.seen[e].get(key, 0) >= val:
            return
        if src == e and (e == "pe" or not SAME_ENGINE_WAIT):
            return
        self.eng[e].wait_ge(sem, val)
        self.seen[e][key] = val

    def _deps(self, e, r, w):
        for b in r:
            if b.w is not None:
                self._wait(e, b.w)
        for b in w:
            if b.w is not None:
                self._wait(e, b.w)
            for ev in b.r.values():
                self._wait(e, ev)

    def _record(self, ev, r, w):
        for b in r:
            b.r[id(ev[0])] = ev
        for b in w:
            b.w = ev
            b.r = {}

    def op(self, e, fn, r=(), w=()):
        self._deps(e, r, w)
        ins = fn(self.eng[e])
        self.seq[e] += 1
        ins.then_inc(self.sem[e], 1)
        self._record((self.sem[e], self.seq[e], e), r, w)
        return ins

    def dma(self, out, in_, r=(), w=(), q="sp", final=False):
        self._deps(q, r, w)
        ins = self.eng[q].dma_start(out=out, in_=in_)
        owner = w[0] if w else r[0]
        if owner.dsem is None:
            owner.dsem = self.stack.enter_context(self.nc.semaphore(self.name("sd")))
            self.nsem += 1
        owner.dval += 16
        ins.then_inc(owner.dsem, 16)
        ev = (owner.dsem, owner.dval, "dma")
        self._record(ev, r, w)
        if final:
            self.finals.append(ev)
        return ins

    def finish(self):
        for ev in self.finals:
            self._wait("sp", ev)
        self.stack.close()
        return self.nc


def mm(kb, out, lhsT, rhs, start, stop, r, w):
    return kb.op("pe", lambda e: e.matmul(out, lhsT=lhsT, rhs=rhs, start=start, stop=stop), r=r, w=w)


TT = 512


def build_post(layer0, ntok=2048):
    kb = KB()
    nc = kb.nc
    NT = ntok // TT
    mixT = kb.din("mixT", [D, ntok]).rearrange("(c p) t -> p c t", p=128)
    xT = kb.din("xT", [D, ntok]).rearrange("(c p) t -> p c t", p=128)
    wout = kb.din("wout", [D, D]).rearrange("(c p) n -> p c n", p=128)
    if layer0:
        wglu = kb.din("wglu", [1024, 1024]).rearrange("(c p) n -> p c n", p=128)
    pvec_d = kb.din("pvec", [128, 8 * 16])
    rw_d = kb.din("router_w", [D, 16]).rearrange("(c p) n -> p c n", p=128)
    rb_d = kb.din("router_b_bc", [128, 16])
    wgu = kb.din("wgu", [16, D, 1024])
    wd = kb.din("wd", [16, 512, D])
    ident_d = kb.din("ident", [128, 128])
    sel_d = kb.din("sel", [16, 16 * 128])
    xoT = kb.dout("xoT", [D, ntok]).rearrange("(c p) t -> p c t", p=128)

    xt = kb.sb([128, 16, TT]); xt_b = bufs(16)
    yacc = kb.sb([128, 16, TT]); yacc_b = bufs(16)
    mixb = kb.sb([128, 16, TT], BF16); mixb_b = bufs(16)
    hT = kb.sb([128, 16, TT], BF16); hT_b = bufs(16)
    NSTG = 2
    stg = [kb.sb([128, 2048]) for _ in range(NSTG)]; stg_b = bufs(NSTG)
    NWB = 6
    wb = [kb.sb([128, 4096], BF16) for _ in range(NWB)]; wb_b = bufs(NWB)
    aT = kb.sb([128, 4, TT], BF16); aT_b = bufs(4)
    tmp = [kb.sb([128, TT]) for _ in range(4)]; tmp_b = bufs(4)
    h32 = [kb.sb([128, TT]) for _ in range(2)]; h32_b = bufs(2)
    mean = kb.sb([128, TT]); mean_b = Buf()
    rstd = kb.sb([128, TT]); rstd_b = Buf()
    pvec = kb.sb([128, 8 * 16]); pvec_b = Buf()
    pv1 = kb.sb([128, 8 * 16]); pv1_b = Buf()
    rw = kb.sb([128, 16, 16]); rw_b = Buf()
    rb = kb.sb([128, 16]); rb_b = Buf()
    ident = kb.sb([128, 128]); ident_b = Buf()
    ones = kb.sb([128, 128]); ones_b = Buf()
    sel = kb.sb([16, 16 * 128]); sel_b = Buf()
    lgT = kb.sb([16, TT]); lgT_b = Buf()
    gatesT = kb.sb([16, TT]); gatesT_b = Buf()
    R = {n: kb.sb([128, 4, 16], name="r_" + n) for n in ["s", "sb", "masked", "m2", "sel1", "sel2", "ssel", "gates"]}
    R_b = {n: Buf() for n in R}
    S4 = {n: kb.sb([128, 16], name="q_" + n) for n in ["p0", "p1", "gscore", "gmask", "pen"]}
    S4_b = {n: Buf() for n in S4}
    S1 = {n: kb.sb([128, 4], name="o_" + n) for n in ["gmax", "m1", "m2", "den", "rden"]}
    S1_b = {n: Buf() for n in S1}

    pbank = [kb.ps([128, TT]) for _ in range(8)]; pb = bufs(8)

    stg_i = [0]; wb_i = [0]; tmp_i = [0]

    def nxt(ctr, n):
        i = ctr[0] % n
        ctr[0] += 1
        return i

    kb.dma(pvec[:], pvec_d[:, :], w=[pvec_b])
    kb.dma(rw[:], rw_d[:, :, :], w=[rw_b])
    kb.dma(rb[:], rb_d[:, :], w=[rb_b])
    kb.dma(ident[:], ident_d[:, :], w=[ident_b])
    kb.dma(sel[:], sel_d[:, :], w=[sel_b])
    kb.op("dve", lambda e: e.memset(ones[:], 1.0), w=[ones_b])
    kb.op("dve", lambda e: e.tensor_scalar_add(pv1[:], pvec[:], 1.0), r=[pvec_b], w=[pv1_b])

    def pcol(which, c, plus1=False):
        t = pv1 if plus1 else pvec
        return t[:, which * 16 + c: which * 16 + c + 1]

    def load_weight(src_ap_fn, ncols):
        wi = nxt(wb_i, NWB)
        src_ap_fn(wb[wi], wb_b[wi])
        return wb[wi], wb_b[wi]

    def layernorm(gi, bi, emit_h, out_dma_t0=None):
        ps_sum, ps_sq = pbank[6], pbank[7]
        for c in range(16):
            ti = nxt(tmp_i, 4)
            kb.op("act", lambda e: e.activation(out=tmp[ti][:], in_=xt[:, c, :], func=AF.Square), r=[xt_b[c]], w=[tmp_b[ti]])
            mm(kb, ps_sum[:], ones[:], xt[:, c, :], c == 0, c == 15, r=[ones_b, xt_b[c]], w=[pb[6]])
            mm(kb, ps_sq[:], ones[:], tmp[ti][:], c == 0, c == 15, r=[ones_b, tmp_b[ti]], w=[pb[7]])
        kb.op("act", lambda e: e.mul(mean[:], ps_sum[:], 1.0 / D), r=[pb[6]], w=[mean_b])
        ti = nxt(tmp_i, 4)
        kb.op("dve", lambda e: e.tensor_tensor(out=tmp[ti][:], in0=mean[:], in1=mean[:], op=ALU.mult), r=[mean_b], w=[tmp_b[ti]])
        kb.op("dve", lambda e: e.scalar_tensor_tensor(out=rstd[:], in0=ps_sq[:], scalar=1.0 / D, in1=tmp[ti][:], op0=ALU.mult, op1=ALU.subtract),
              r=[pb[7], tmp_b[ti]], w=[rstd_b])
        kb.op("dve", lambda e: e.tensor_scalar_add(rstd[:], rstd[:], LN_EPS), r=[rstd_b], w=[rstd_b])
        kb.op("act", lambda e: e.sqrt(rstd[:], rstd[:]), r=[rstd_b], w=[rstd_b])
        kb.op("dve", lambda e: e.reciprocal(rstd[:], rstd[:]), r=[rstd_b], w=[rstd_b])
        for c in range(16):
            kb.op("dve", lambda e: e.tensor_tensor(out=xt[:, c, :], in0=xt[:, c, :], in1=mean[:], op=ALU.subtract), r=[xt_b[c], mean_b], w=[xt_b[c]])
            kb.op("dve", lambda e: e.tensor_tensor(out=xt[:, c, :], in0=xt[:, c, :], in1=rstd[:], op=ALU.mult), r=[xt_b[c], rstd_b], w=[xt_b[c]])
            kb.op("act", lambda e: e.activation(out=xt[:, c, :], in_=xt[:, c, :], func=AF.Identity, scale=pcol(gi, c), bias=pcol(bi, c)),
                  r=[xt_b[c], pvec_b], w=[xt_b[c]])
            if emit_h:
                hi = c % 2
                kb.op("dve", lambda e: e.tensor_scalar(out=h32[hi][:], in0=xt[:, c, :], scalar1=pcol(1, c, True), scalar2=pcol(2, c), op0=ALU.mult, op1=ALU.add),
                      r=[xt_b[c], pvec_b, pv1_b], w=[h32_b[hi]])
                kb.op("act", lambda e: e.copy(hT[:, c, :], h32[hi][:]), r=[h32_b[hi]], w=[hT_b[c]])
                mm(kb, pbank[5][0:16, :], rw[:, c, :], h32[hi][:], c == 0, c == 15, r=[rw_b, h32_b[hi]], w=[pb[5]])
            if out_dma_t0 is not None:
                kb.dma(xoT[:, c, out_dma_t0:out_dma_t0 + TT], xt[:, c, :], r=[xt_b[c]], final=True)

    for tt in range(NT):
        t0 = tt * TT
        kb.dma(xt[:], xT[:, :, t0:t0 + TT], w=xt_b)
        for c in range(16):
            kb.op("act", lambda e: e.mul(xt[:, c, :], xt[:, c, :], DN_ALPHA), r=[xt_b[c]], w=[xt_b[c]])
        for q in range(4):
            si = nxt(stg_i, NSTG)
            sv = stg[si][:, :4 * TT].rearrange("p (c t) -> p c t", c=4)
            kb.dma(sv, mixT[:, 4 * q:4 * q + 4, t0:t0 + TT], w=[stg_b[si]])
            for cc in range(4):
                c = 4 * q + cc
                if layer0 and c >= 8:
                    ti = nxt(tmp_i, 4)
                    kb.op("dve", lambda e: e.tensor_tensor(out=tmp[ti][:], in0=sv[:, cc, :], in1=sv[:, cc, :], op=ALU.mult), r=[stg_b[si]], w=[tmp_b[ti]])
                    kb.op("dve", lambda e: e.tensor_scalar(out=tmp[ti][:], in0=tmp[ti][:], scalar1=0.044715, scalar2=1.0, op0=ALU.mult, op1=ALU.add), r=[tmp_b[ti]], w=[tmp_b[ti]])
                    kb.op("dve", lambda e: e.tensor_tensor(out=tmp[ti][:], in0=tmp[ti][:], in1=sv[:, cc, :], op=ALU.mult), r=[tmp_b[ti], stg_b[si]], w=[tmp_b[ti]])
                    kb.op("act", lambda e: e.activation(out=tmp[ti][:], in_=tmp[ti][:], func=AF.Sigmoid, scale=1.5957691216), r=[tmp_b[ti]], w=[tmp_b[ti]])
                    kb.op("dve", lambda e: e.tensor_tensor(out=hT[:, c, :], in0=tmp[ti][:], in1=sv[:, cc, :], op=ALU.mult), r=[tmp_b[ti], stg_b[si]], w=[hT_b[c]])
                else:
                    kb.op("act", lambda e: e.copy(mixb[:, c, :], sv[:, cc, :]), r=[stg_b[si]], w=[mixb_b[c]])
        if layer0:
            for m in range(8):
                def ld(st, sbuf_, m=m):
                    kb.dma(st[:, :8 * 128].rearrange("p (c n) -> p c n", c=8), wglu[:, :, m * 128:(m + 1) * 128], w=[sbuf_], q="pool")
                wt, wtb = load_weight(ld, 8 * 128)
                pi = m % 2
                for k in range(8):
                    mm(kb, pbank[pi][:], wt[:, k * 128:(k + 1) * 128], hT[:, 8 + k, :], k == 0, k == 7, r=[wtb, hT_b[8 + k]], w=[pb[pi]])
                ti = nxt(tmp_i, 4)
                kb.op("act", lambda e: e.activation(out=tmp[ti][:], in_=pbank[pi][:], func=AF.Sigmoid), r=[pb[pi]], w=[tmp_b[ti]])
                kb.op("dve", lambda e: e.tensor_tensor(out=mixb[:, 8 + m, :], in0=tmp[ti][:], in1=hT[:, 8 + m, :], op=ALU.mult), r=[tmp_b[ti], hT_b[8 + m]], w=[mixb_b[8 + m]])
        for m in range(16):
            def ld(st, sbuf_, m=m):
                kb.dma(st[:, :16 * 128].rearrange("p (c n) -> p c n", c=16), wout[:, :, m * 128:(m + 1) * 128], w=[sbuf_], q="pool")
            wt, wtb = load_weight(ld, 16 * 128)
            pi = m % 2
            for k in range(16):
                mm(kb, pbank[pi][:], wt[:, k * 128:(k + 1) * 128], mixb[:, k, :], k == 0, k == 15, r=[wtb, mixb_b[k]], w=[pb[pi]])
            kb.op("dve", lambda e: e.scalar_tensor_tensor(out=xt[:, m, :], in0=pbank[pi][:], scalar=pcol(0, m, True), in1=xt[:, m, :], op0=ALU.mult, op1=ALU.add),
                  r=[pb[pi], pv1_b, xt_b[m]], w=[xt_b[m]])
        layernorm(4, 5, True)
        kb.op("act", lambda e: e.copy(lgT[:], pbank[5][0:16, :]), r=[pb[5]], w=[lgT_b])
        for s in range(4):
            kb.op("pe", lambda e: e.transpose(pbank[4][:, s * 16:(s + 1) * 16], lgT[:, s * 128:(s + 1) * 128], ident[0:16, 0:16]), r=[lgT_b, ident_b], w=[pb[4]])
        lg = pbank[4][:, 0:64].rearrange("p (s e) -> p s e", s=4)
        kb.op("act", lambda e: e.activation(out=R["s"][:], in_=lg, func=AF.Sigmoid), r=[pb[4]], w=[R_b["s"]])
        rb_bc = rb[:].rearrange("p (o e) -> p o e", o=1).to_broadcast([128, 4, 16])
        kb.op("dve", lambda e: e.tensor_tensor(out=R["sb"][:], in0=R["s"][:], in1=rb_bc, op=ALU.add), r=[R_b["s"], rb_b], w=[R_b["sb"]])
        sbg = R["sb"][:].rearrange("p s (g k) -> p (s g) k", k=4)
        first = True
        for (i, j) in [(0, 1), (0, 2), (0, 3), (1, 2), (1, 3), (2, 3)]:
            if first:
                kb.op("dve", lambda e: e.tensor_tensor(out=S4["gscore"][:], in0=sbg[:, :, i], in1=sbg[:, :, j], op=ALU.add), r=[R_b["sb"]], w=[S4_b["gscore"]])
                first = False
            else:
                kb.op("dve", lambda e: e.tensor_tensor(out=S4["p0"][:], in0=sbg[:, :, i], in1=sbg[:, :, j], op=ALU.add), r=[R_b["sb"]], w=[S4_b["p0"]])
                kb.op("dve", lambda e: e.tensor_tensor(out=S4["gscore"][:], in0=S4["gscore"][:], in1=S4["p0"][:], op=ALU.max), r=[S4_b["p0"], S4_b["gscore"]], w=[S4_b["gscore"]])
        gs3 = S4["gscore"][:].rearrange("p (s g) -> p s g", g=4)
        kb.op("dve", lambda e: e.tensor_reduce(out=S1["gmax"][:], in_=gs3, axis=AX.X, op=ALU.max), r=[S4_b["gscore"]], w=[S1_b["gmax"]])
        gmax_bc = S1["gmax"][:].rearrange("p (s o) -> p s o", o=1).to_broadcast([128, 4, 4])
        gm3 = S4["gmask"][:].rearrange("p (s g) -> p s g", g=4)
        kb.op("dve", lambda e: e.tensor_tensor(out=gm3, in0=gs3, in1=gmax_bc, op=ALU.is_equal), r=[S4_b["gscore"], S1_b["gmax"]], w=[S4_b["gmask"]])
        kb.op("dve", lambda e: e.tensor_scalar(out=S4["pen"][:], in0=S4["gmask"][:], scalar1=1e30, scalar2=-1e30, op0=ALU.mult, op1=ALU.add), r=[S4_b["gmask"]], w=[S4_b["pen"]])
        gmask_bc = S4["gmask"][:].rearrange("p (q o) -> p q o", o=1).to_broadcast([128, 16, 4])
        pen_bc = S4["pen"][:].rearrange("p (q o) -> p q o", o=1).to_broadcast([128, 16, 4])
        msk = R["masked"][:].rearrange("p s (g k) -> p (s g) k", k=4)
        kb.op("dve", lambda e: e.tensor_tensor(out=msk, in0=sbg, in1=gmask_bc, op=ALU.mult), r=[R_b["sb"], S4_b["gmask"]], w=[R_b["masked"]])
        kb.op("dve", lambda e: e.tensor_tensor(out=msk, in0=msk, in1=pen_bc, op=ALU.add), r=[R_b["masked"], S4_b["pen"]], w=[R_b["masked"]])
        kb.op("dve", lambda e: e.tensor_reduce(out=S1["m1"][:], in_=R["masked"][:], axis=AX.X, op=ALU.max), r=[R_b["masked"]], w=[S1_b["m1"]])
        m1_bc = S1["m1"][:].rearrange("p (s o) -> p s o", o=1).to_broadcast([128, 4, 16])
        kb.op("dve", lambda e: e.tensor_tensor(out=R["sel1"][:], in0=R["masked"][:], in1=m1_bc, op=ALU.is_equal), r=[R_b["masked"], S1_b["m1"]], w=[R_b["sel1"]])
        kb.op("dve", lambda e: e.scalar_tensor_tensor(out=R["m2"][:], in0=R["sel1"][:], scalar=-1e30, in1=R["masked"][:], op0=ALU.mult, op1=ALU.add),
              r=[R_b["sel1"], R_b["masked"]], w=[R_b["m2"]])
        kb.op("dve", lambda e: e.tensor_reduce(out=S1["m2"][:], in_=R["m2"][:], axis=AX.X, op=ALU.max), r=[R_b["m2"]], w=[S1_b["m2"]])
        m2_bc = S1["m2"][:].rearrange("p (s o) -> p s o", o=1).to_broadcast([128, 4, 16])
        kb.op("dve", lambda e: e.tensor_tensor(out=R["sel2"][:], in0=R["m2"][:], in1=m2_bc, op=ALU.is_equal), r=[R_b["m2"], S1_b["m2"]], w=[R_b["sel2"]])
        kb.op("dve", lambda e: e.tensor_tensor(out=R["sel1"][:], in0=R["sel1"][:], in1=R["sel2"][:], op=ALU.add), r=[R_b["sel1"], R_b["sel2"]], w=[R_b["sel1"]])
        kb.op("dve", lambda e: e.tensor_tensor(out=R["ssel"][:], in0=R["sel1"][:], in1=R["s"][:], op=ALU.mult), r=[R_b["sel1"], R_b["s"]], w=[R_b["ssel"]])
        kb.op("dve", lambda e: e.tensor_reduce(out=S1["den"][:], in_=R["ssel"][:], axis=AX.X, op=ALU.add), r=[R_b["ssel"]], w=[S1_b["den"]])
        kb.op("dve", lambda e: e.reciprocal(S1["rden"][:], S1["den"][:]), r=[S1_b["den"]], w=[S1_b["rden"]])
        rden_bc = S1["rden"][:].rearrange("p (s o) -> p s o", o=1).to_broadcast([128, 4, 16])
        kb.op("dve", lambda e: e.tensor_tensor(out=R["gates"][:], in0=R["ssel"][:], in1=rden_bc, op=ALU.mult), r=[R_b["ssel"], S1_b["rden"]], w=[R_b["gates"]])
        for s in range(4):
            kb.op("pe", lambda e: e.transpose(pbank[5][0:16, s * 128:(s + 1) * 128], R["gates"][:, s, :], ident[:, :]), r=[R_b["gates"], ident_b], w=[pb[5]])
        kb.op("act", lambda e: e.copy(gatesT[:], pbank[5][0:16, :]), r=[pb[5]], w=[gatesT_b])
        for ex in range(16):
            mm(kb, pbank[4][:], sel[:, ex * 128:(ex + 1) * 128], gatesT[:], True, True, r=[sel_b, gatesT_b], w=[pb[4]])
            for j in range(4):
                def ld(st, sbuf_, ex=ex, j=j):
                    sv_ = st[:, :16 * 256].rearrange("p (c n) -> p c n", c=16)
                    src = wgu[ex].rearrange("(c p) n -> p c n", p=128)
                    kb.dma(sv_[:, :, 0:128], src[:, :, j * 128:(j + 1) * 128], w=[sbuf_], q="pool")
                    kb.dma(sv_[:, :, 128:256], src[:, :, 512 + j * 128:512 + (j + 1) * 128], w=[sbuf_], q="pool")
                wt, wtb = load_weight(ld, 16 * 256)
                pg, pu = (0, 1) if j % 2 == 0 else (2, 3)
                for k in range(16):
                    mm(kb, pbank[pg][:], wt[:, k * 256:k * 256 + 128], hT[:, k, :], k == 0, k == 15, r=[wtb, hT_b[k]], w=[pb[pg]])
                for k in range(16):
                    mm(kb, pbank[pu][:], wt[:, k * 256 + 128:k * 256 + 256], hT[:, k, :], k == 0, k == 15, r=[wtb, hT_b[k]], w=[pb[pu]])
                ti = nxt(tmp_i, 4)
                kb.op("act", lambda e: e.activation(out=tmp[ti][:], in_=pbank[pg][:], func=AF.Silu), r=[pb[pg]], w=[tmp_b[ti]])
                kb.op("dve", lambda e: e.tensor_tensor(out=tmp[ti][:], in0=tmp[ti][:], in1=pbank[pu][:], op=ALU.mult), r=[tmp_b[ti], pb[pu]], w=[tmp_b[ti]])
                kb.op("dve", lambda e: e.tensor_tensor(out=aT[:, j, :], in0=tmp[ti][:], in1=pbank[4][:], op=ALU.mult), r=[tmp_b[ti], pb[4]], w=[aT_b[j]])
            for mq in range(4):
                def ld(st, sbuf_, ex=ex, mq=mq):
                    kb.dma(st[:, :4 * 512].rearrange("p (c n) -> p c n", c=4), wd[ex].rearrange("(c p) n -> p c n", p=128)[:, :, mq * 512:(mq + 1) * 512], w=[sbuf_], q="pool")
                wt, wtb = load_weight(ld, 4 * 512)
                for mi in range(4):
                    m = mq * 4 + mi
                    pi = 6 + (m % 2)
                    for k in range(4):
                        mm(kb, pbank[pi][:], wt[:, k * 512 + mi * 128:k * 512 + (mi + 1) * 128], aT[:, k, :], k == 0, k == 3, r=[wtb, aT_b[k]], w=[pb[pi]])
                    if ex == 0:
                        kb.op("act", lambda e: e.copy(yacc[:, m, :], pbank[pi][:]), r=[pb[pi]], w=[yacc_b[m]])
                    else:
                        kb.op("dve", lambda e: e.tensor_tensor(out=yacc[:, m, :], in0=pbank[pi][:], in1=yacc[:, m, :], op=ALU.add), r=[pb[pi], yacc_b[m]], w=[yacc_b[m]])
        for c in range(16):
            kb.op("act", lambda e: e.activation(out=yacc[:, c, :], in_=yacc[:, c, :], func=AF.Identity, scale=pcol(3, c, True)), r=[yacc_b[c], pv1_b], w=[yacc_b[c]])
            kb.op("dve", lambda e: e.scalar_tensor_tensor(out=xt[:, c, :], in0=xt[:, c, :], scalar=DN_ALPHA, in1=yacc[:, c, :], op0=ALU.mult, op1=ALU.add),
                  r=[xt_b[c], yacc_b[c]], w=[xt_b[c]])
        layernorm(6, 7, False, out_dma_t0=t0)
    return kb.finish()


def build_ada():
    kb = KB()
    cT_d = kb.din("cT", [128, 16 * 4])
    w_d = kb.din("w", [2, D, 1536])
    b_d = kb.din("b", [128, 24])
    out_d = kb.dout("modT", [128, 24 * 4])
    cT = kb.sb([128, 64]); cT_b = Buf()
    bb = kb.sb([128, 24]); bb_b = Buf()
    ot = kb.sb([128, 96]); ot_b = Buf()
    stg = [kb.sb([128, 16, 128]) for _ in range(3)]; stg_b = bufs(3)
    ps = [kb.ps([128, 512]) for _ in range(2)]; ps_b = bufs(2)
    kb.dma(cT[:], cT_d[:, :], w=[cT_b])
    kb.dma(bb[:], b_d[:, :], w=[bb_b])
    kb.op("act", lambda e: e.activation(out=cT[:], in_=cT[:], func=AF.Silu), r=[cT_b], w=[cT_b])
    for l in range(2):
        for m in range(12):
            u = l * 12 + m
            si = u % 3
            kb.dma(stg[si][:], w_d[l].rearrange("(c p) n -> p c n", p=128)[:, :, m * 128:(m + 1) * 128], w=[stg_b[si]])
            pi = u % 2
            for k in range(16):
                mm(kb, ps[pi][:, 0:4], stg[si][:, k, :], cT[:, k * 4:(k + 1) * 4], k == 0, k == 15, r=[stg_b[si], cT_b], w=[ps_b[pi]])
            kb.op("dve", lambda e: e.tensor_scalar(out=ot[:, u * 4:(u + 1) * 4], in0=ps[pi][:, 0:4], scalar1=bb[:, u:u + 1], scalar2=None, op0=ALU.add),
                  r=[ps_b[pi], bb_b], w=[ot_b])
    kb.dma(out_d[:, :], ot[:], r=[ot_b], final=True)
    return kb.finish()


def run_ada(c, ada_w, ada_b):
    nc = build_ada()
    cT = np.ascontiguousarray(c.T.reshape(16, 128, 4).transpose(1, 0, 2).reshape(128, 64))
    in_maps = []
    for j in range(NCORES):
        w = np.ascontiguousarray(ada_w[:, :, j * 1536:(j + 1) * 1536])
        b = ada_b[:, j * 1536:(j + 1) * 1536].reshape(2, 12, 128).transpose(2, 0, 1).reshape(128, 24)
        in_maps.append({"cT": cT, "w": w, "b": np.ascontiguousarray(b)})
    res = run_bass_kernel_spmd(nc, in_maps, core_ids=list(range(NCORES)))
    mod = np.zeros((2, 4, 6 * D), np.float32)
    for j in range(NCORES):
        o = res.results[j]["modT"].reshape(128, 2, 12, 4)
        mod[:, :, j * 1536:(j + 1) * 1536] = o.transpose(1, 3, 2, 0).reshape(2, 4, 1536)
    return mod


def build_pre(ncol, ntok=2048):
    assert ncol % 128 == 0
    NM = ncol // 128
    kb = KB()
    NT = ntok // TT
    xT = kb.din("xT", [D, ntok]).rearrange("(c p) t -> p c t", p=128)
    w_d = kb.din("w", [D, ncol]).rearrange("(c p) n -> p c n", p=128)
    pv_d = kb.din("pvec", [128, 32])
    zT = kb.dout("zT", [ncol, ntok]).rearrange("(c p) t -> p c t", p=128)
    xt = [kb.sb([128, 16, TT]) for _ in range(2)]; xt_b = bufs(2)
    hT = [kb.sb([128, 16, TT], BF16) for _ in range(NT)]; hT_b = bufs(NT)
    stg = [kb.sb([128, 16, 128]) for _ in range(3)]; stg_b = bufs(3)
    wb = [kb.sb([128, 16, 128], BF16) for _ in range(3)]; wb_b = bufs(3)
    ot = [kb.sb([128, TT]) for _ in range(4)]; ot_b = bufs(4)
    pv = kb.sb([128, 32]); pv_b = Buf()
    pv1 = kb.sb([128, 32]); pv1_b = Buf()
    ps = [kb.ps([128, TT]) for _ in range(4)]; ps_b = bufs(4)
    kb.dma(pv[:], pv_d[:, :], w=[pv_b])
    kb.op("dve", lambda e: e.tensor_scalar_add(pv1[:], pv[:], 1.0), r=[pv_b], w=[pv1_b])
    for tt in range(NT):
        t0 = tt * TT
        xi = tt % 2
        kb.dma(xt[xi][:], xT[:, :, t0:t0 + TT], w=[xt_b[xi]])
        for c in range(16):
            kb.op("act", lambda e: e.activation(out=hT[tt][:, c, :], in_=xt[xi][:, c, :], func=AF.Identity, scale=pv1[:, c:c + 1], bias=pv[:, 16 + c:17 + c]),
                  r=[xt_b[xi], pv_b, pv1_b], w=[hT_b[tt]])
    u = 0
    for m in range(NM):
        si = m % 3
        kb.dma(wb[si][:], w_d[:, :, m * 128:(m + 1) * 128], w=[wb_b[si]], q="pool")
        for tt in range(NT):
            t0 = tt * TT
            pi = u % 4
            for k in range(16):
                mm(kb, ps[pi][:], wb[si][:, k, :], hT[tt][:, k, :], k == 0, k == 15, r=[wb_b[si], hT_b[tt]], w=[ps_b[pi]])
            if u % 2 == 0:
                kb.op("act", lambda e: e.copy(ot[pi][:], ps[pi][:]), r=[ps_b[pi]], w=[ot_b[pi]])
            else:
                kb.op("dve", lambda e: e.tensor_copy(out=ot[pi][:], in_=ps[pi][:]), r=[ps_b[pi]], w=[ot_b[pi]])
            kb.dma(zT[:, m, t0:t0 + TT], ot[pi][:], r=[ot_b[pi]], final=True, q="act" if u % 2 == 0 else "sp")
            u += 1
    return kb.finish()


def fm16(v):
    return np.ascontiguousarray(v.reshape(16, 128).T)


def run_pre(x_tok_major, mod_l, w, sc_idx, sh_idx):
    ncol = w.shape[1]
    nc = build_pre(ncol)
    in_maps = []
    for j in range(NCORES):
        b, hf = j // 2, j % 2
        xT = np.ascontiguousarray(x_tok_major[b, hf * 2048:(hf + 1) * 2048].T)
        sc = mod_l[b, sc_idx * D:(sc_idx + 1) * D]
        sh = mod_l[b, sh_idx * D:(sh_idx + 1) * D]
        in_maps.append({"xT": xT, "w": w, "pvec": np.ascontiguousarray(np.concatenate([fm16(sc), fm16(sh)], 1))})
    res = run_bass_kernel_spmd(nc, in_maps, core_ids=list(range(NCORES)))
    z = np.zeros((4, ncol, 4096), np.float32)
    for j in range(NCORES):
        b, hf = j // 2, j % 2
        z[b, :, hf * 2048:(hf + 1) * 2048] = res.results[j]["zT"]
    return z


SEQ = 4096
MLA_SCALE = 192.0 ** -0.5


def build_mla(S=SEQ):
    kb = KB()
    NT = S // TT
    zq = kb.din("zq", [512, S]).rearrange("(c p) t -> p c t", p=128)
    zkv = kb.din("zkv", [256, S]).rearrange("(c p) t -> p c t", p=128)
    zkr = kb.din("zkr", [128, S]).rearrange("(c p) t -> p c t", p=64)
    cs_d = kb.din("cs", [128, S]).rearrange("(c p) t -> p c t", p=64)
    wq_d = kb.din("wq", [512, 1024]).rearrange("(c p) n -> p c n", p=128)
    wk_d = kb.din("wk", [256, 512]).rearrange("(c p) n -> p c n", p=128)
    wv_d = kb.din("wv", [256, 512]).rearrange("(c p) n -> p c n", p=128)
    g_d = kb.din("g", [128, 6])
    mask_d = kb.din("mask", [128, 4 * TT])
    attT = kb.dout("attT", [512, S]).rearrange("(c p) t -> p c t", p=128)

    qnope = [kb.sb([128, S], BF16) for _ in range(4)]; qnope_b = [bufs(NT) for _ in range(4)]
    qrope = [kb.sb([128, S], BF16) for _ in range(2)]; qrope_b = [bufs(NT) for _ in range(2)]
    knope = [kb.sb([128, S], BF16) for _ in range(4)]; knope_b = [bufs(NT) for _ in range(4)]
    krope = kb.sb([128, S], BF16); krope_b = bufs(NT)
    V = kb.sb([128, S // 128, 512], BF16); V_b = bufs(NT)
    wq = kb.sb([128, 4, 1024], BF16); wk = kb.sb([128, 2, 512], BF16); wv = kb.sb([128, 2, 512], BF16); w_b = Buf()
    g = kb.sb([128, 6]); g_b = Buf()
    mask = kb.sb([128, 4 * TT], BF16); mask_b = Buf()
    ones = kb.sb([128, 128]); ones_b = Buf()
    onesb = kb.sb([128, 128], BF16); onesb_b = Buf()
    stg = kb.sb([128, 2048]); stg_b = Buf()
    zin = [kb.sb([128, 6, TT]) for _ in range(1)]; zin_b = bufs(1)
    zr = [kb.sb([128, 4, TT]) for _ in range(1)]; zr_b = bufs(1)
    qn = kb.sb([128, 6, TT], BF16); qn_b = bufs(6)
    tmp = [kb.sb([128, TT]) for _ in range(4)]; tmp_b = bufs(4)
    rs = [kb.sb([128, TT]) for _ in range(2)]; rs_b = bufs(2)
    pT = [kb.sb([128, TT], BF16) for _ in range(3)]; pT_b = bufs(3)
    ot = [kb.sb([128, TT]) for _ in range(2)]; ot_b = bufs(2)
    ps = [kb.ps([128, TT]) for _ in range(8)]; pb = bufs(8)
    tmp_i = [0]

    def nxt(ctr, n):
        i = ctr[0] % n
        ctr[0] += 1
        return i

    kb.dma(g[:], g_d[:, :], w=[g_b])
    kb.op("dve", lambda e: e.memset(ones[:], 1.0), w=[ones_b])
    kb.op("dve", lambda e: e.memset(onesb[:], 1.0), w=[onesb_b])
    kb.dma(stg[:, :2048], mask_d[:, :], w=[stg_b])
    kb.op("dve", lambda e: e.tensor_copy(out=mask[:], in_=stg[:, :2048]), r=[stg_b], w=[mask_b])
    for hh in range(2):
        kb.dma(stg[:].rearrange("p (c n) -> p c n", c=2), wq_d[:, 2 * hh:2 * hh + 2, :], w=[stg_b])
        kb.op("dve", lambda e: e.tensor_copy(out=wq[:, 2 * hh:2 * hh + 2, :].rearrange("p c n -> p (c n)"), in_=stg[:]), r=[stg_b, w_b], w=[w_b])
    kb.dma(stg[:, :1024].rearrange("p (c n) -> p c n", c=2), wk_d[:, :, :], w=[stg_b])
    kb.op("dve", lambda e: e.tensor_copy(out=wk[:].rearrange("p c n -> p (c n)"), in_=stg[:, :1024]), r=[stg_b, w_b], w=[w_b])
    kb.dma(stg[:, :1024].rearrange("p (c n) -> p c n", c=2), wv_d[:, :, :], w=[stg_b])
    kb.op("dve", lambda e: e.tensor_copy(out=wv[:].rearrange("p c n -> p (c n)"), in_=stg[:, :1024]), r=[stg_b, w_b], w=[w_b])

    for tt in range(NT):
        t0 = tt * TT
        zi = 0
        kb.dma(zin[zi][:, 0:4, :], zq[:, :, t0:t0 + TT], w=[zin_b[zi]])
        kb.dma(zin[zi][:, 4:6, :], zkv[:, :, t0:t0 + TT], w=[zin_b[zi]])
        for hp in range(2):
            kb.dma(zr[zi][hp * 64:(hp + 1) * 64, 0:2, :], zkr[:, :, t0:t0 + TT], w=[zr_b[zi]])
            kb.dma(zr[zi][hp * 64:(hp + 1) * 64, 2:4, :], cs_d[:, :, t0:t0 + TT], w=[zr_b[zi]])
        for (c0, c1, pi, dim, eps, ri) in [(0, 4, 6, 512, 1e-6, 0), (4, 6, 7, 256, 1e-6, 1)]:
            for c in range(c0, c1):
                ti = nxt(tmp_i, 4)
                kb.op("act", lambda e: e.activation(out=tmp[ti][:], in_=zin[zi][:, c, :], func=AF.Square), r=[zin_b[zi]], w=[tmp_b[ti]])
                mm(kb, ps[pi][:], ones[:], tmp[ti][:], c == c0, c == c1 - 1, r=[ones_b, tmp_b[ti]], w=[pb[pi]])
            kb.op("dve", lambda e: e.tensor_scalar(out=rs[ri][:], in0=ps[pi][:], scalar1=1.0 / dim, scalar2=eps, op0=ALU.mult, op1=ALU.add), r=[pb[pi]], w=[rs_b[ri]])
            kb.op("act", lambda e: e.sqrt(rs[ri][:], rs[ri][:]), r=[rs_b[ri]], w=[rs_b[ri]])
            kb.op("dve", lambda e: e.reciprocal(rs[ri][:], rs[ri][:]), r=[rs_b[ri]], w=[rs_b[ri]])
            for c in range(c0, c1):
                kb.op("dve", lambda e: e.scalar_tensor_tensor(out=qn[:, c, :], in0=zin[zi][:, c, :], scalar=g[:, c:c + 1], in1=rs[ri][:], op0=ALU.mult, op1=ALU.mult),
                      r=[zin_b[zi], g_b, rs_b[ri]], w=[qn_b[c]])
        for h in range(4):
            for k in range(4):
                mm(kb, ps[0][:], wq[:, k, h * 128:(h + 1) * 128], qn[:, k, :], k == 0, k == 3, r=[w_b, qn_b[k]], w=[pb[0]])
            kb.op("act", lambda e: e.copy(qnope[h][:, t0:t0 + TT], ps[0][:]), r=[pb[0]], w=[qnope_b[h][tt]])
            for k in range(2):
                mm(kb, ps[3][:], wk[:, k, h * 128:(h + 1) * 128], qn[:, 4 + k, :], k == 0, k == 1, r=[w_b, qn_b[4 + k]], w=[pb[3]])
            kb.op("act", lambda e: e.copy(knope[h][:, t0:t0 + TT], ps[3][:]), r=[pb[3]], w=[knope_b[h][tt]])
        for hp in range(2):
            for k in range(4):
                mm(kb, ps[1][:], wq[:, k, 512 + hp * 128:512 + (hp + 1) * 128], qn[:, k, :], k == 0, k == 3, r=[w_b, qn_b[k]], w=[pb[1]])
            for k in range(4):
                mm(kb, ps[2][:], wq[:, k, 768 + hp * 128:768 + (hp + 1) * 128], qn[:, k, :], k == 0, k == 3, r=[w_b, qn_b[k]], w=[pb[2]])
            t1 = nxt(tmp_i, 4)
            kb.op("dve", lambda e: e.tensor_tensor(out=tmp[t1][:], in0=ps[1][:], in1=zr[zi][:, 2, :], op=ALU.mult), r=[pb[1], zr_b[zi]], w=[tmp_b[t1]])
            t2 = nxt(tmp_i, 4)
            kb.op("dve", lambda e: e.tensor_tensor(out=tmp[t2][:], in0=ps[2][:], in1=zr[zi][:, 3, :], op=ALU.mult), r=[pb[2], zr_b[zi]], w=[tmp_b[t2]])
            kb.op("dve", lambda e: e.tensor_tensor(out=qrope[hp][:, t0:t0 + TT], in0=tmp[t1][:], in1=tmp[t2][:], op=ALU.add),
                  r=[tmp_b[t1], tmp_b[t2]], w=[qrope_b[hp][tt]])
        for blk in range(4):
            pi = 4 + blk % 2
            for k in range(2):
                mm(kb, ps[pi][:], qn[:, 4 + k, blk * 128:(blk + 1) * 128], wv[:, k, :], k == 0, k == 1, r=[w_b, qn_b[4 + k]], w=[pb[pi]])
            kb.op("act", lambda e: e.copy(V[:, tt * 4 + blk, :], ps[pi][:]), r=[pb[pi]], w=[V_b[tt]])
        t1 = nxt(tmp_i, 4)
        kb.op("dve", lambda e: e.tensor_tensor(out=tmp[t1][:], in0=zr[zi][:, 0, :], in1=zr[zi][:, 2, :], op=ALU.mult), r=[zr_b[zi]], w=[tmp_b[t1]])
        t2 = nxt(tmp_i, 4)
        kb.op("dve", lambda e: e.tensor_tensor(out=tmp[t2][:], in0=zr[zi][:, 1, :], in1=zr[zi][:, 3, :], op=ALU.mult), r=[zr_b[zi]], w=[tmp_b[t2]])
        kb.op("dve", lambda e: e.tensor_tensor(out=krope[:, t0:t0 + TT], in0=tmp[t1][:], in1=tmp[t2][:], op=ALU.add), r=[tmp_b[t1], tmp_b[t2]], w=[krope_b[tt]])

    sbanks = [0, 1, 2]
    cnt = [0]
    for h in range(4):
        for qb in range(NT):
            q0 = qb * TT
            nkb = 4 * qb + 4
            po, pd = (3, 4) if (h * NT + qb) % 2 == 0 else (5, 6)

            def qk(kk):
                sb_ = sbanks[kk % 3]
                kt = kk // 4
                mm(kb, ps[sb_][:], knope[h][:, kk * 128:(kk + 1) * 128], qnope[h][:, q0:q0 + TT], True, False,
                   r=[knope_b[h][kt], qnope_b[h][qb]], w=[pb[sb_]])
                ph = (h % 2) * 64
                mm(kb, ps[sb_][:], krope[ph:ph + 64, kk * 128:(kk + 1) * 128], qrope[h // 2][ph:ph + 64, q0:q0 + TT], False, True,
                   r=[krope_b[kt], qrope_b[h // 2][qb]], w=[pb[sb_]])
            qk(0)
            for kk in range(nkb):
                if kk + 1 < nkb:
                    qk(kk + 1)
                sb_ = sbanks[kk % 3]
                pi = cnt[0] % 3
                cnt[0] += 1
                kb.op("act", lambda e: e.activation(out=pT[pi][:], in_=ps[sb_][:], func=AF.Exp, scale=MLA_SCALE), r=[pb[sb_]], w=[pT_b[pi]])
                j = kk - 4 * qb
                if j >= 0:
                    kb.op("dve", lambda e: e.tensor_tensor(out=pT[pi][:], in0=pT[pi][:], in1=mask[:, j * TT:(j + 1) * TT], op=ALU.mult), r=[pT_b[pi], mask_b], w=[pT_b[pi]])
                mm(kb, ps[po][:], V[:, kk, h * 128:(h + 1) * 128], pT[pi][:], kk == 0, kk == nkb - 1, r=[V_b[kk // 4], pT_b[pi]], w=[pb[po]])
                mm(kb, ps[pd][:], onesb[:], pT[pi][:], kk == 0, kk == nkb - 1, r=[onesb_b, pT_b[pi]], w=[pb[pd]])
            oi = (h * NT + qb) % 2
            ti = nxt(tmp_i, 4)
            kb.op("dve", lambda e: e.reciprocal(tmp[ti][:], ps[pd][:]), r=[pb[pd]], w=[tmp_b[ti]])
            kb.op("dve", lambda e: e.tensor_tensor(out=ot[oi][:], in0=ps[po][:], in1=tmp[ti][:], op=ALU.mult), r=[pb[po], tmp_b[ti]], w=[ot_b[oi]])
            kb.dma(attT[:, h, q0:q0 + TT], ot[oi][:], r=[ot_b[oi]], final=True)
    return kb.finish()


def rope_tables(S=SEQ):
    inv = 10000.0 ** (-np.arange(0, 64, 2, dtype=np.float32) / 64)
    ang = np.arange(S, dtype=np.float32)[None, :] * inv[:, None]
    cos, sin = np.cos(ang).astype(np.float32), np.sin(ang).astype(np.float32)
    return np.ascontiguousarray(np.concatenate([cos, cos, -sin, sin], 0))


def causal_masks():
    m = np.zeros((4, 128, TT), np.float32)
    k = np.arange(128)[:, None]
    q = np.arange(TT)[None, :]
    for j in range(4):
        m[j] = (q >= k + 128 * j)
    return np.ascontiguousarray(m.transpose(1, 0, 2).reshape(128, 4 * TT))


def run_mla(z0, w_uq, w_ukv, q_norm, kv_norm):
    nc = build_mla()
    cs = rope_tables()
    mask = causal_masks()
    g = np.ascontiguousarray(np.concatenate([q_norm.reshape(4, 128).T, kv_norm.reshape(2, 128).T], 1))
    in_maps = []
    for j in range(NCORES):
        b, hf = j // 2, j % 2
        wq_cols, wk_cols, wv_cols = [], [], []
        hs = list(range(4 * hf, 4 * hf + 4))
        for h in hs:
            wq_cols.append(w_uq[:, h * 192:h * 192 + 128])
            wk_cols.append(w_ukv[:, h * 256:h * 256 + 128])
            wv_cols.append(w_ukv[:, h * 256 + 128:h * 256 + 256])
        for h in hs:
            wq_cols.append(w_uq[:, h * 192 + 128:h * 192 + 192])
        for h in hs:
            wq_cols += [w_uq[:, h * 192 + 160:h * 192 + 192], w_uq[:, h * 192 + 128:h * 192 + 160]]
        in_maps.append({
            "zq": np.ascontiguousarray(z0[b, 0:512]), "zkv": np.ascontiguousarray(z0[b, 512:768]),
            "zkr": np.ascontiguousarray(np.concatenate([z0[b, 768:832], z0[b, 1856:1920]], 0)),
            "cs": cs, "wq": np.ascontiguousarray(np.concatenate(wq_cols, 1)), "wk": np.ascontiguousarray(np.concatenate(wk_cols, 1)),
            "wv": np.ascontiguousarray(np.concatenate(wv_cols, 1)), "g": g, "mask": mask})
    res = run_bass_kernel_spmd(nc, in_maps, core_ids=list(range(NCORES)))
    att = np.zeros((4, 1024, SEQ), np.float32)
    for j in range(NCORES):
        b, hf = j // 2, j % 2
        att[b, hf * 512:(hf + 1) * 512] = res.results[j]["attT"]
    return att


TWO_PI = 6.283185307179586
NG = 32


def build_s5(S=SEQ):
    kb = KB()
    NT = S // TT
    uT = kb.din("uT", [NG * 16, S])
    lamre_d = kb.din("lamre", [128, NG]); lamim_d = kb.din("lamim", [128, NG]); logdt_d = kb.din("logdt", [128, NG])
    bt_d = kb.din("bt", [16, NG * 128]); btsw_d = kb.din("btsw", [16, NG * 128])
    ca_d = kb.din("ca", [128, NG * 16]); cb_d = kb.din("cb", [128, NG * 16])
    d_d = kb.din("dsk", [16, NG])
    iota_d = kb.din("iota", [128, S])
    yT = kb.dout("yT", [NG * 16, S])

    def t32(name=None):
        return kb.sb([128, NG], name=name)
    lamre, lamim, dt_, r_, th, cth, sth, nre, nim, den, fre, fim, tA, tB = [t32() for _ in range(14)]
    s1, s2, s3, s4 = [t32() for _ in range(4)]
    prm_b = Buf()
    sgn = kb.sb([128, 1]); negpi = kb.sb([128, 1]); ki = kb.sb([128, NG], mybir.dt.int32)
    KI = kb.sb([128, S], mybir.dt.int32); KI_b = Buf()
    bt = kb.sb([16, NG * 128], BF16); btsw = kb.sb([16, NG * 128], BF16); bstg = kb.sb([16, NG * 128]); bt_b = Buf(); bstg_b = Buf()
    ca = kb.sb([128, NG, 16]); cb = kb.sb([128, NG, 16]); cstage = kb.sb([128, NG, 16]); L1 = kb.sb([128, NG, 16], BF16); L2 = kb.sb([128, NG, 16], BF16); c_b = Buf()
    dsk = kb.sb([16, NG]); dsk_b = Buf()
    iota = kb.sb([128, S]); iota_b = Buf()
    u32 = kb.sb([16, S]); u32_b = Buf()
    ubf = kb.sb([16, S], BF16); ubf_b = Buf()
    A1 = kb.sb([128, S]); A1_b = Buf()
    A2 = kb.sb([128, S]); A2_b = Buf()
    T2 = kb.sb([128, S]); T2_b = Buf()
    bz = kb.sb([128, S]); bz_b = bufs(NT); z_b = Buf()
    Zc = kb.sb([128, S], BF16); Zc_b = Buf()
    Zs = kb.sb([128, S], BF16); Zs_b = Buf()
    ysb = kb.sb([16, S]); ysb_b = Buf()
    tmp = [kb.sb([128, TT]) for _ in range(4)]; tmp_b = bufs(4)
    ps = [kb.ps([128, TT]) for _ in range(8)]; pb = bufs(8)

    P = [prm_b]
    kb.dma(lamre[:], lamre_d[:, :], w=P)
    kb.dma(lamim[:], lamim_d[:, :], w=P)
    kb.dma(dt_[:], logdt_d[:, :], w=P)
    kb.dma(iota[:], iota_d[:, :], w=[iota_b])
    kb.dma(dsk[:], d_d[:, :], w=[dsk_b])
    kb.dma(bstg[:], bt_d[:, :], w=[bstg_b])
    kb.op("dve", lambda e: e.tensor_copy(out=bt[:], in_=bstg[:]), r=[bstg_b], w=[bt_b])
    kb.dma(bstg[:], btsw_d[:, :], w=[bstg_b])
    kb.op("dve", lambda e: e.tensor_copy(out=btsw[:], in_=bstg[:]), r=[bstg_b, bt_b], w=[bt_b])
    kb.dma(ca[:].rearrange("p g c -> p (g c)"), ca_d[:, :], w=[c_b])
    kb.dma(cb[:].rearrange("p g c -> p (g c)"), cb_d[:, :], w=[c_b])
    V = lambda fn: kb.op("dve", fn, r=P, w=P)
    A = lambda fn: kb.op("act", fn, r=P, w=P)
    V(lambda e: e.memset(sgn[0:64, :], 1.0))
    V(lambda e: e.memset(sgn[64:128, :], -1.0))
    V(lambda e: e.memset(negpi[:], -3.141592653589793))
    A(lambda e: e.activation(out=dt_[:], in_=dt_[:], func=AF.Exp))
    V(lambda e: e.tensor_tensor(out=r_[:], in0=lamre[:], in1=dt_[:], op=ALU.mult))
    A(lambda e: e.activation(out=r_[:], in_=r_[:], func=AF.Exp))
    V(lambda e: e.tensor_tensor(out=th[:], in0=lamim[:], in1=dt_[:], op=ALU.mult))
    V(lambda e: e.tensor_single_scalar(out=th[:], in_=th[:], scalar=1.0 / TWO_PI, op=ALU.mult))
    V(lambda e: e.tensor_copy(out=ki[:], in_=th[:]))
    V(lambda e: e.tensor_tensor(out=th[:], in0=th[:], in1=ki[:], op=ALU.subtract))
    A(lambda e: e.activation(out=sth[:], in_=th[:], func=AF.Sin, scale=TWO_PI))
    V(lambda e: e.tensor_single_scalar(out=tA[:], in_=th[:], scalar=0.25, op=ALU.add))
    V(lambda e: e.tensor_copy(out=ki[:], in_=tA[:]))
    V(lambda e: e.tensor_tensor(out=tA[:], in0=tA[:], in1=ki[:], op=ALU.subtract))
    A(lambda e: e.activation(out=cth[:], in_=tA[:], func=AF.Sin, scale=TWO_PI))
    V(lambda e: e.tensor_tensor(out=nre[:], in0=r_[:], in1=cth[:], op=ALU.mult))
    V(lambda e: e.tensor_single_scalar(out=nre[:], in_=nre[:], scalar=-1.0, op=ALU.add))
    V(lambda e: e.tensor_tensor(out=nim[:], in0=r_[:], in1=sth[:], op=ALU.mult))
    V(lambda e: e.tensor_tensor(out=den[:], in0=lamre[:], in1=lamre[:], op=ALU.mult))
    V(lambda e: e.tensor_tensor(out=tA[:], in0=lamim[:], in1=lamim[:], op=ALU.mult))
    V(lambda e: e.tensor_tensor(out=den[:], in0=den[:], in1=tA[:], op=ALU.add))
    V(lambda e: e.reciprocal(den[:], den[:]))
    V(lambda e: e.tensor_tensor(out=fre[:], in0=nre[:], in1=lamre[:], op=ALU.mult))
    V(lambda e: e.tensor_tensor(out=tA[:], in0=nim[:], in1=lamim[:], op=ALU.mult))
    V(lambda e: e.tensor_tensor(out=fre[:], in0=fre[:], in1=tA[:], op=ALU.add))
    V(lambda e: e.tensor_tensor(out=fre[:], in0=fre[:], in1=den[:], op=ALU.mult))
    V(lambda e: e.tensor_tensor(out=fim[:], in0=nim[:], in1=lamre[:], op=ALU.mult))
    V(lambda e: e.tensor_tensor(out=tA[:], in0=nre[:], in1=lamim[:], op=ALU.mult))
    V(lambda e: e.tensor_tensor(out=fim[:], in0=fim[:], in1=tA[:], op=ALU.subtract))
    V(lambda e: e.tensor_tensor(out=fim[:], in0=fim[:], in1=den[:], op=ALU.mult))
    V(lambda e: e.tensor_scalar(out=s1[:], in0=fre[:], scalar1=sgn[:, 0:1], scalar2=None, op0=ALU.mult))
    V(lambda e: e.tensor_single_scalar(out=s2[:], in_=fim[:], scalar=-1.0, op=ALU.mult))
    V(lambda e: e.tensor_scalar(out=s3[:], in0=s2[:], scalar1=sgn[:, 0:1], scalar2=None, op0=ALU.mult))
    V(lambda e: e.tensor_single_scalar(out=s4[:], in_=fre[:], scalar=-1.0, op=ALU.mult))

    def bc(t):
        return t[:].rearrange("p (g o) -> p g o", o=1).to_broadcast([128, NG, 16])
    PC = [prm_b, c_b]
    kb.op("dve", lambda e: e.tensor_tensor(out=cstage[:], in0=ca[:], in1=bc(s1), op=ALU.mult), r=PC, w=PC)
    kb.op("dve", lambda e: e.tensor_tensor(out=ca[:], in0=ca[:], in1=bc(s3), op=ALU.mult), r=PC, w=PC)
    kb.op("dve", lambda e: e.tensor_tensor(out=tmp[0][:, :NG * 16].rearrange("p (g c) -> p g c", c=16), in0=cb[:], in1=bc(s2), op=ALU.mult), r=PC, w=PC + [tmp_b[0]])
    kb.op("dve", lambda e: e.tensor_tensor(out=L1[:], in0=cstage[:], in1=tmp[0][:, :NG * 16].rearrange("p (g c) -> p g c", c=16), op=ALU.add), r=PC + [tmp_b[0]], w=PC)
    kb.op("dve", lambda e: e.tensor_tensor(out=cb[:], in0=cb[:], in1=bc(s4), op=ALU.mult), r=PC, w=PC)
    kb.op("dve", lambda e: e.tensor_tensor(out=L2[:], in0=ca[:], in1=cb[:], op=ALU.add), r=PC, w=PC)

    tmp_i = [1]
    for g in range(NG):
        kb.dma(u32[:], uT[g * 16:(g + 1) * 16, :], w=[u32_b])
        kb.op("act", lambda e: e.copy(ubf[:], u32[:]), r=[u32_b], w=[ubf_b])
        kb.op("dve", lambda e: e.tensor_scalar(out=KI[:], in0=iota[:], scalar1=th[:, g:g + 1], scalar2=None, op0=ALU.mult), r=[iota_b, prm_b], w=[KI_b])
        kb.op("dve", lambda e: e.scalar_tensor_tensor(out=A1[:], in0=iota[:], scalar=th[:, g:g + 1], in1=KI[:], op0=ALU.mult, op1=ALU.subtract), r=[iota_b, prm_b, KI_b], w=[A1_b])
        kb.op("dve", lambda e: e.tensor_single_scalar(out=A2[:], in_=A1[:], scalar=0.25, op=ALU.add), r=[A1_b], w=[A2_b])
        kb.op("dve", lambda e: e.tensor_copy(out=KI[:], in_=A2[:]), r=[A2_b], w=[KI_b])
        kb.op("dve", lambda e: e.tensor_tensor(out=A2[:], in0=A2[:], in1=KI[:], op=ALU.subtract), r=[A2_b, KI_b], w=[A2_b])
        kb.op("act", lambda e: e.activation(out=A1[:], in_=A1[:], func=AF.Sin, scale=TWO_PI), r=[A1_b], w=[A1_b])
        kb.op("act", lambda e: e.activation(out=A2[:], in_=A2[:], func=AF.Sin, scale=TWO_PI), r=[A2_b], w=[A2_b])
        kb.op("act", lambda e: e.activation(out=T2[:], in_=A1[:], func=AF.Identity, scale=sgn[:, 0:1]), r=[A1_b, prm_b], w=[T2_b])
        for tt in range(NT):
            t0 = tt * TT
            pa, pbk = (0, 1) if tt % 2 == 0 else (2, 3)
            mm(kb, ps[pa][:], bt[:, g * 128:(g + 1) * 128], ubf[:, t0:t0 + TT], True, True, r=[bt_b, ubf_b], w=[pb[pa]])
            mm(kb, ps[pbk][:], btsw[:, g * 128:(g + 1) * 128], ubf[:, t0:t0 + TT], True, True, r=[bt_b, ubf_b], w=[pb[pbk]])
            t1 = tmp_i[0] % 4; tmp_i[0] += 1
            kb.op("dve", lambda e: e.tensor_tensor(out=tmp[t1][:], in0=ps[pa][:], in1=A2[:, t0:t0 + TT], op=ALU.mult), r=[pb[pa], A2_b], w=[tmp_b[t1]])
            t2 = tmp_i[0] % 4; tmp_i[0] += 1
            kb.op("dve", lambda e: e.tensor_tensor(out=tmp[t2][:], in0=ps[pbk][:], in1=T2[:, t0:t0 + TT], op=ALU.mult), r=[pb[pbk], T2_b], w=[tmp_b[t2]])
            kb.op("dve", lambda e: e.tensor_tensor(out=bz[:, t0:t0 + TT], in0=tmp[t1][:], in1=tmp[t2][:], op=ALU.add), r=[tmp_b[t1], tmp_b[t2], z_b], w=[bz_b[tt]])
        kb.op("dve", lambda e: e.tensor_tensor_scan(out=bz[:], data0=r_[:, g:g + 1].to_broadcast([128, S]), data1=bz[:], initial=0.0, op0=ALU.mult, op1=ALU.add),
              r=bz_b + [prm_b], w=bz_b + [z_b])
        kb.op("dve", lambda e: e.tensor_tensor(out=Zc[:], in0=bz[:], in1=A2[:], op=ALU.mult), r=[z_b, A2_b], w=[Zc_b])
        kb.op("dve", lambda e: e.tensor_tensor(out=Zs[:], in0=bz[:], in1=A1[:], op=ALU.mult), r=[z_b, A1_b], w=[Zs_b])
        for tt in range(NT):
            t0 = tt * TT
            pi = 4 + tt % 4
            mm(kb, ps[pi][0:16, :], L1[:, g, :], Zc[:, t0:t0 + TT], True, False, r=[c_b, Zc_b], w=[pb[pi]])
            mm(kb, ps[pi][0:16, :], L2[:, g, :], Zs[:, t0:t0 + TT], False, True, r=[c_b, Zs_b], w=[pb[pi]])
            kb.op("dve", lambda e: e.scalar_tensor_tensor(out=ysb[:, t0:t0 + TT], in0=u32[:, t0:t0 + TT], scalar=dsk[:, g:g + 1], in1=ps[pi][0:16, :], op0=ALU.mult, op1=ALU.add),
                  r=[u32_b, dsk_b, pb[pi]], w=[ysb_b])
        kb.dma(yT[g * 16:(g + 1) * 16, :], ysb[:], r=[ysb_b], final=True)
    return kb.finish()


def run_s5(z0, lam_re, lam_im, b_re, b_im, c_re, c_im, d_skip, log_dt):
    nc = build_s5()
    iota = np.ascontiguousarray(np.broadcast_to(np.arange(SEQ, dtype=np.float32), (128, SEQ)))
    in_maps = []
    for j in range(NCORES):
        b, hf = j // 2, j % 2
        gs = slice(hf * NG, (hf + 1) * NG)
        lre = lam_re[gs].T; lim = lam_im[gs].T
        bre = b_re[gs].transpose(2, 0, 1); bim = b_im[gs].transpose(2, 0, 1)
        cre = c_re[gs].transpose(2, 0, 1); cim = c_im[gs].transpose(2, 0, 1)
        in_maps.append({
            "uT": np.ascontiguousarray(z0[b, 832 + hf * 512:832 + (hf + 1) * 512]),
            "lamre": np.ascontiguousarray(np.concatenate([lre, lre], 0)), "lamim": np.ascontiguousarray(np.concatenate([lim, lim], 0)),
            "logdt": np.ascontiguousarray(np.broadcast_to(log_dt[gs][None, :], (128, NG))),
            "bt": np.ascontiguousarray(np.concatenate([bre, bim], 2).reshape(16, NG * 128)),
            "btsw": np.ascontiguousarray(np.concatenate([bim, bre], 2).reshape(16, NG * 128)),
            "ca": np.ascontiguousarray(np.concatenate([cre, cim], 0).reshape(128, NG * 16)),
            "cb": np.ascontiguousarray(np.concatenate([cim, cre], 0).reshape(128, NG * 16)),
            "dsk": np.ascontiguousarray(d_skip[gs].T), "iota": iota})
    res = run_bass_kernel_spmd(nc, in_maps, core_ids=list(range(NCORES)))
    y = np.zeros((4, 1024, SEQ), np.float32)
    for j in range(NCORES):
        b, hf = j // 2, j % 2
        y[b, hf * 512:(hf + 1) * 512] = res.results[j]["yT"]
    return y


DILS = (1, 4, 16)


def build_dil(S=SEQ):
    kb = KB()
    NB = S // 128
    q_d = kb.din("q", [12, 64, S]); k_d = kb.din("k", [12, 64, S])
    v_d = kb.din("v", [12, 128, NB * 64])
    bm_d = kb.din("bm", [12, 128, 1024])
    attT = kb.dout("attT", [256, S])
    stg = [kb.sb([128, S]) for _ in range(2)]; stg_b = bufs(2)
    qs = [kb.sb([64, S], BF16) for _ in range(2)]; qs_b = bufs(2)
    ks = [kb.sb([64, S], BF16) for _ in range(2)]; ks_b = bufs(2)
    vs = [kb.sb([128, NB * 64], BF16) for _ in range(2)]; vs_b = bufs(2)
    eb = [kb.sb([128, 1024], BF16) for _ in range(2)]; eb_b = bufs(2)
    eb0 = [kb.sb([128, 512], BF16) for _ in range(2)]; eb0_b = bufs(2)
    num = kb.sb([64, S]); num_b = Buf()
    den = kb.sb([64, S]); den_b = Buf()
    onesb = kb.sb([128, 64], BF16); onesb_b = Buf()
    pT = [kb.sb([128, 512], BF16) for _ in range(4)]; pT_b = bufs(4)
    ps = [kb.ps([128, TT]) for _ in range(8)]; pb = bufs(8)
    kb.op("dve", lambda e: e.memset(onesb[:], 1.0), w=[onesb_b])
    u = 0
    pti = 0
    for h in range(4):
        for gi, d in enumerate(DILS):
            inst = gi * 4 + h
            bi = u % 2
            nb = NB // d
            kb.dma(stg[0][0:64, :], q_d[inst], w=[stg_b[0]])
            kb.op("pool", lambda e: e.tensor_copy(out=qs[bi][:], in_=stg[0][0:64, :]), r=[stg_b[0]], w=[qs_b[bi]])
            kb.dma(stg[1][0:64, :], k_d[inst], w=[stg_b[1]])
            kb.op("pool", lambda e: e.tensor_copy(out=ks[bi][:], in_=stg[1][0:64, :]), r=[stg_b[1]], w=[ks_b[bi]])
            kb.dma(stg[0][:, :NB * 64], v_d[inst], w=[stg_b[0]])
            kb.op("pool", lambda e: e.tensor_copy(out=vs[bi][:], in_=stg[0][:, :NB * 64]), r=[stg_b[0]], w=[vs_b[bi]])
            kb.dma(stg[1][:, :1024], bm_d[inst], w=[stg_b[1]])
            kb.op("act", lambda e: e.activation(out=eb[bi][:], in_=stg[1][:, :1024], func=AF.Exp), r=[stg_b[1]], w=[eb_b[bi]])
            kb.op("dve", lambda e: e.tensor_copy(out=eb0[bi][:], in_=eb[bi][:, 512:1024]), r=[eb_b[bi]], w=[eb0_b[bi]])
            kb.op("dve", lambda e: e.memset(eb0[bi][:, 0:128], 0.0), r=[eb0_b[bi]], w=[eb0_b[bi]])
            if d == 16:
                kb.op("dve", lambda e: e.memset(eb0[bi][:, 256:384], 0.0), r=[eb0_b[bi]], w=[eb0_b[bi]])
            for bt in range(NB // 4):
                B0 = bt * 4
                pss, psp, pso, psd = (0, 1, 2, 3) if bt % 2 == 0 else (4, 5, 6, 7)
                firsts = [(B0 + j) % nb == 0 for j in range(4)]
                for j in range(4):
                    B = B0 + j
                    mm(kb, ps[pss][:, j * 128:(j + 1) * 128], ks[bi][:, B * 128:(B + 1) * 128], qs[bi][:, B * 128:(B + 1) * 128], True, True,
                       r=[ks_b[bi], qs_b[bi]], w=[pb[pss]])
                    Bp = B if firsts[j] else B - 1
                    mm(kb, ps[psp][:, j * 128:(j + 1) * 128], ks[bi][:, Bp * 128:(Bp + 1) * 128], qs[bi][:, B * 128:(B + 1) * 128], True, True,
                       r=[ks_b[bi], qs_b[bi]], w=[pb[psp]])
                p1 = pti % 4; p2 = (pti + 1) % 4; pti += 2
                kb.op("act", lambda e: e.activation(out=pT[p1][:], in_=ps[pss][:], func=AF.Exp, scale=0.125), r=[pb[pss]], w=[pT_b[p1]])
                kb.op("act", lambda e: e.activation(out=pT[p2][:], in_=ps[psp][:], func=AF.Exp, scale=0.125), r=[pb[psp]], w=[pT_b[p2]])
                kb.op("dve", lambda e: e.tensor_tensor(out=pT[p1][:], in0=pT[p1][:], in1=eb[bi][:, 0:512], op=ALU.mult), r=[pT_b[p1], eb_b[bi]], w=[pT_b[p1]])
                ebp = eb0[bi][:] if any(firsts) else eb[bi][:, 512:1024]
                kb.op("dve", lambda e: e.tensor_tensor(out=pT[p2][:], in0=pT[p2][:], in1=ebp, op=ALU.mult), r=[pT_b[p2], eb_b[bi], eb0_b[bi]], w=[pT_b[p2]])
                for j in range(4):
                    B = B0 + j
                    Bp = B if firsts[j] else B - 1
                    cs_ = slice(j * 128, (j + 1) * 128)
                    mm(kb, ps[pso][0:64, cs_], vs[bi][:, B * 64:(B + 1) * 64], pT[p1][:, cs_], True, False, r=[vs_b[bi], pT_b[p1]], w=[pb[pso]])
                    mm(kb, ps[pso][0:64, cs_], vs[bi][:, Bp * 64:(Bp + 1) * 64], pT[p2][:, cs_], False, True, r=[vs_b[bi], pT_b[p2]], w=[pb[pso]])
                    mm(kb, ps[psd][0:64, cs_], onesb[:], pT[p1][:, cs_], True, False, r=[onesb_b, pT_b[p1]], w=[pb[psd]])
                    mm(kb, ps[psd][0:64, cs_], onesb[:], pT[p2][:, cs_], False, True, r=[onesb_b, pT_b[p2]], w=[pb[psd]])
                if d == 16:
                    r0 = B0 // nb
                    nv = num[:].rearrange("c (m r) -> c r m", r=16)[:, r0:r0 + 2, :]
                    dv_ = den[:].rearrange("c (m r) -> c r m", r=16)[:, r0:r0 + 2, :]
                    po = ps[pso][0:64, :].rearrange("c (r m) -> c r m", r=2)
                    pd = ps[psd][0:64, :].rearrange("c (r m) -> c r m", r=2)
                elif d == 4:
                    r0 = B0 // nb; m0 = (B0 % nb) * 128
                    nv = num[:].rearrange("c (m r) -> c r m", r=4)[:, r0, m0:m0 + 512]
                    dv_ = den[:].rearrange("c (m r) -> c r m", r=4)[:, r0, m0:m0 + 512]
                    po = ps[pso][0:64, :]; pd = ps[psd][0:64, :]
                else:
                    nv = num[:, B0 * 128:B0 * 128 + 512]; dv_ = den[:, B0 * 128:B0 * 128 + 512]
                    po = ps[pso][0:64, :]; pd = ps[psd][0:64, :]
                if gi == 0:
                    kb.op("act", lambda e: e.copy(nv, po), r=[pb[pso]], w=[num_b])
                    kb.op("act", lambda e: e.copy(dv_, pd), r=[pb[psd]], w=[den_b])
                else:
                    kb.op("dve", lambda e: e.tensor_tensor(out=nv, in0=po, in1=nv, op=ALU.add), r=[pb[pso], num_b], w=[num_b])
                    kb.op("dve", lambda e: e.tensor_tensor(out=dv_, in0=pd, in1=dv_, op=ALU.add), r=[pb[psd], den_b], w=[den_b])
            u += 1
        kb.op("dve", lambda e: e.reciprocal(den[:], den[:]), r=[den_b], w=[den_b])
        kb.op("dve", lambda e: e.tensor_tensor(out=num[:], in0=num[:], in1=den[:], op=ALU.mult), r=[num_b, den_b], w=[num_b])
        kb.dma(attT[h * 64:(h + 1) * 64, :], num[:], r=[num_b], final=True)
    return kb.finish()


def t5_bucket_np(dist):
    exact = 16
    logd = np.log(np.maximum(dist, 1).astype(np.float32) / exact) / np.float32(np.log(2048 / exact))
    large = np.minimum(exact + (logd * (32 - exact)).astype(np.int32), 31)
    return np.where(dist < exact, dist, large)


def run_dil(z1, rel_bias):
    nc = build_dil()
    S = SEQ
    NB = S // 128
    kk = np.arange(128)[:, None]; qq = np.arange(128)[None, :]
    in_maps = []
    for jc in range(NCORES):
        b, hf = jc // 2, jc % 2
        q_l, k_l, v_l, bm_l = [], [], [], []
        for gi, d in enumerate(DILS):
            for h in range(4 * hf, 4 * hf + 4):
                def rows(qkv):
                    r0 = gi * 1536 + qkv * 512 + h * 64
                    t = z1[b, r0:r0 + 64]
                    return t.reshape(64, S // d, d).transpose(0, 2, 1).reshape(64, S)
                q_l.append(rows(0)); k_l.append(rows(1))
                v = rows(2)
                v_l.append(v.reshape(64, NB, 128).transpose(2, 1, 0).reshape(128, NB * 64))
                bias_h = rel_bias[:, gi * 8 + h]
                same = np.where(qq >= kk, bias_h[t5_bucket_np(np.clip(qq - kk, 0, 128) * d)], -30000.0)
                prev = np.where(qq <= kk, bias_h[t5_bucket_np(np.clip(128 + qq - kk, 0, 128) * d)], -30000.0)
                bm_l.append(np.concatenate([np.tile(same, (1, 4)), np.tile(prev, (1, 4))], 1))
        in_maps.append({"q": np.ascontiguousarray(np.stack(q_l)), "k": np.ascontiguousarray(np.stack(k_l)),
                        "v": np.ascontiguousarray(np.stack(v_l)), "bm": np.ascontiguousarray(np.stack(bm_l).astype(np.float32))})
    res = run_bass_kernel_spmd(nc, in_maps, core_ids=list(range(NCORES)))
    att = np.zeros((4, 512, S), np.float32)
    for jc in range(NCORES):
        b, hf = jc // 2, jc % 2
        att[b, hf * 256:(hf + 1) * 256] = res.results[jc]["attT"]
    return att


CL = 64
RW_GN_EPS = 64e-5
WDEC = -0.6065306597126334


RW_STOP = None
RW_OFFSET = 0


def build_rwkv(S=SEQ, nh=12):
    kb = KB()
    NBT = S // 512
    FW = nh * 64
    zr_d = kb.din("zr", [FW, S]); zk_d = kb.din("zk", [FW, S])
    vt_d = kb.din("v_tok", [S, FW]); vp_d = kb.din("vprev_tok", [S, FW])
    zwd_d = kb.din("zwd", [64, S]); zad_d = kb.din("zad", [64, S]); zgd_d = kb.din("zgd", [224, S])
    cols_d = kb.din("cols", [64, 8 * nh])
    mul_d = kb.din("mu_lora", [128, 4])
    w2_d = kb.din("w2", [64, FW]); a2_d = kb.din("a2", [64, FW]); g2_d = kb.din("g2", [224, FW])
    rows_d = kb.din("rows", [64, 3 * FW])
    cst_d = kb.din("cst", [64, 2048])
    rmask_d = kb.din("rmask", [64, 512])
    out_d = kb.dout("tm_tok", [S, FW])

    cst = kb.sb([64, 2048]); cst_b = Buf()
    rmask = kb.sb([64, 512]); rmask_b = Buf()
    cols = kb.sb([64, 8 * nh]); cols_b = Buf()
    mul = kb.sb([128, 4]); mul_b = Buf()
    w2 = kb.sb([64, FW]); a2 = kb.sb([64, FW]); lw_b = Buf()
    g2s = kb.sb([128, FW]); g2a = kb.sb([128, FW], BF16); g2b = kb.sb([96, FW], BF16)
    rows = kb.sb([64, 3 * FW]); rows_b = Buf()
    onescol = kb.sb([64, 1]); onescol_b = Buf()
    tw = kb.sb([64, S]); xad = kb.sb([64, S]); sg0 = kb.sb([128, S], BF16); sg1 = kb.sb([96, S], BF16); lora_b = Buf()
    P_b = Buf()
    big = kb.sb([128, 4097]); big2 = kb.sb([128, 4096])

    kb.dma(cst[:], cst_d[:, :], w=[cst_b])
    kb.dma(rmask[:], rmask_d[:, :], w=[rmask_b])
    kb.dma(cols[:], cols_d[:, :], w=[cols_b])
    kb.dma(mul[:], mul_d[:, :], w=[mul_b])
    kb.dma(w2[:], w2_d[:, :], w=[lw_b]); kb.dma(a2[:], a2_d[:, :], w=[lw_b])
    kb.dma(g2s[:], g2_d[0:128, :], w=[P_b])
    kb.op("dve", lambda e: e.tensor_copy(out=g2a[:], in_=g2s[:]), r=[P_b], w=[lw_b])
    kb.dma(g2s[0:96, :], g2_d[128:224, :], r=[lw_b], w=[P_b])
    kb.op("dve", lambda e: e.tensor_copy(out=g2b[:], in_=g2s[0:96, :]), r=[P_b], w=[lw_b])
    kb.dma(rows[:], rows_d[:, :], w=[rows_b])
    kb.op("dve", lambda e: e.memset(onescol[:], 1.0), w=[onescol_b])
    ones64 = cst[:, 0:64]; ident = cst[:, 64:128]; maskG2 = cst[:, 128:384]
    maskU8 = cst[:, 384:896]; maskL8 = cst[:, 896:1408]; I8 = cst[:, 1408:1920]

    def shifted(src_ap, P, mucol, dst, func):
        kb.op("dve", lambda e: e.memset(big[0:P, 0:1], 0.0), r=[P_b], w=[P_b])
        kb.dma(big[0:P, 1:S + 1], src_ap, w=[P_b])
        kb.op("dve", lambda e: e.tensor_tensor(out=big2[0:P, 0:S], in0=big[0:P, 0:S], in1=big[0:P, 1:S + 1], op=ALU.subtract), r=[P_b], w=[P_b])
        kb.op("dve", lambda e: e.scalar_tensor_tensor(out=big2[0:P, 0:S], in0=big2[0:P, 0:S], scalar=mucol, in1=big[0:P, 1:S + 1], op0=ALU.mult, op1=ALU.add),
              r=[P_b, mul_b], w=[P_b])
        if func is None:
            kb.op("act", lambda e: e.copy(dst, big2[0:P, 0:S]), r=[P_b], w=[lora_b])
        else:
            kb.op("act", lambda e: e.activation(out=dst, in_=big2[0:P, 0:S], func=func), r=[P_b], w=[lora_b])
    shifted(zwd_d[:, :], 64, mul[0:64, 0:1], tw[:], AF.Tanh)
    shifted(zad_d[:, :], 64, mul[0:64, 1:2], xad[:], None)
    shifted(zgd_d[0:128, :], 128, mul[:, 2:3], sg0[:], AF.Sigmoid)
    shifted(zgd_d[128:224, :], 96, mul[0:96, 3:4], sg1[:], AF.Sigmoid)

    class _V:
        def __init__(self, t, i):
            self.t, self.i = t, i

        def __getitem__(self, idx):
            return self.t[0:64, self.i * 512:(self.i + 1) * 512][idx]

    ps = [kb.ps([128, 512]) for _ in range(8)]; pb = bufs(8)

    def make_set(si):
        T = {}
        if si == 0:
            scr = [_V(big, i) for i in range(8)] + [_V(big2, i) for i in range(5)]
        else:
            extra = kb.sb([64, 10 * 512])
            scr = [_V(big2, i) for i in range(5, 8)] + [_V(extra, i) for i in range(10)]
        T["scr"] = scr
        T["A_b"] = P_b if si == 0 else Buf()
        T["P_extra"] = [P_b]
        T["zrt"] = kb.sb([64, 513]); T["zkt"] = kb.sb([64, 513]); T["zin_b"] = Buf()
        T["AR"] = kb.sb([64, 8, 128]); T["BK"] = kb.sb([64, 8, 128]); T["rkr"] = kb.sb([64, 512]); T["ARBK_b"] = Buf()
        T["St"] = kb.sb([64, 64]); T["St_b"] = Buf(); T["Stw"] = kb.sb([64, 64]); T["Stw_b"] = Buf()
        T["Vx"] = kb.sb([64, 8, 64]); T["Vx_b"] = Buf()
        T["Pm"] = [kb.sb([64, 512], BF16) for _ in range(2)]; T["Qm"] = [kb.sb([64, 512], BF16) for _ in range(2)]; T["TTm"] = [kb.sb([64, 512], BF16) for _ in range(2)]
        T["Tm"] = kb.sb([64, 512]); T["C_b"] = Buf()
        T["Gm"] = kb.sb([64, 256]); T["Gm_b"] = Buf()
        T["BKT"] = kb.sb([64, 128]); T["BKT_b"] = Buf()
        T["Xs"] = kb.sb([64, 64]); T["Xs_b"] = Buf(); T["Us"] = kb.sb([64, 64]); T["Us_b"] = Buf()
        T["ep1"] = kb.sb([64, 8, 64]); T["ep2"] = kb.sb([64, 8, 64]); T["ep_b"] = Buf()
        T["st8"] = [kb.sb([64, 8]) for _ in range(3)]
        T["bank"] = [4 * si + k for k in range(4)]
        return T

    def head_gen(h, T):
        R, Kx, lwt, av, kk, kkn, Kp, cum, Ep, Em, Epr, t1, t2 = T["scr"]
        A_b = T["A_b"]; zrt, zkt, zin_b = T["zrt"], T["zkt"], T["zin_b"]
        AR, BK, rkr, ARBK_b = T["AR"], T["BK"], T["rkr"], T["ARBK_b"]
        St, St_b, Stw, Stw_b = T["St"], T["St_b"], T["Stw"], T["Stw_b"]
        Vx, Vx_b = T["Vx"], T["Vx_b"]
        Pm, Qm, TTm, Tm, C_b = T["Pm"], T["Qm"], T["TTm"], T["Tm"], T["C_b"]
        Gm, Gm_b, BKT, BKT_b, Xs, Xs_b, Us, Us_b = T["Gm"], T["Gm_b"], T["BKT"], T["BKT_b"], T["Xs"], T["Xs_b"], T["Us"], T["Us_b"]
        ep1, ep2, ep_b, st8 = T["ep1"], T["ep2"], T["ep_b"], T["st8"]
        b0, b1, b2, b3 = T["bank"]
        AX_ = [A_b] + ([P_b] if A_b is not P_b else [])
        cc = lambda k: cols[:, k * nh + h:k * nh + h + 1]
        hc = slice(h * 64, (h + 1) * 64)
        kb.op("dve", lambda e: e.memset(St[:], 0.0), r=[St_b], w=[St_b])
        for bi in range(NBT):
            t0 = bi * 512
            A_ = lambda eng, fn, extra_r=(): kb.op(eng, fn, r=AX_ + [zin_b] + list(extra_r), w=[A_b])
            for (zt, zd) in [(zrt, zr_d), (zkt, zk_d)]:
                if bi == 0:
                    kb.op("dve", lambda e: e.memset(zt[:, 0:1], 0.0), r=[zin_b, A_b], w=[zin_b])
                    kb.dma(zt[:, 1:513], zd[hc, 0:512], w=[zin_b])
                else:
                    kb.dma(zt[:], zd[hc, t0 - 1:t0 + 512], w=[zin_b])
            E = [ep_b]
            kb.dma(ep1[:], vt_d[t0:t0 + 512, hc].rearrange("(c t) i -> t c i", t=64), w=E, q="pool")
            kb.dma(ep2[:], vp_d[t0:t0 + 512, hc].rearrange("(c t) i -> t c i", t=64), w=E, q="pool")
            for (zt, dst, k) in [(zrt, R, 0), (zkt, Kx, 1)]:
                A_("dve", lambda e: e.tensor_tensor(out=t1[:], in0=zt[:, 0:512], in1=zt[:, 1:513], op=ALU.subtract))
                A_("dve", lambda e: e.scalar_tensor_tensor(out=dst[:], in0=t1[:], scalar=cc(k), in1=zt[:, 1:513], op0=ALU.mult, op1=ALU.add), [cols_b])
            mm(kb, ps[b0][0:64, :], w2[:, hc], tw[:, t0:t0 + 512], True, True, r=[lw_b, lora_b], w=[pb[b0]])
            A_("act", lambda e: e.activation(out=lwt[:], in_=ps[b0][0:64, :], func=AF.Sigmoid, bias=cc(2)), [pb[b0], cols_b])
            A_("act", lambda e: e.mul(lwt[:], lwt[:], WDEC))
            mm(kb, ps[b0][0:64, :], a2[:, hc], xad[:, t0:t0 + 512], True, True, r=[lw_b, lora_b], w=[pb[b0]])
            A_("act", lambda e: e.activation(out=av[:], in_=ps[b0][0:64, :], func=AF.Sigmoid, bias=cc(3)), [pb[b0], cols_b])
            A_("dve", lambda e: e.tensor_scalar(out=kk[:], in0=Kx[:], scalar1=cc(4), scalar2=None, op0=ALU.mult), [cols_b])
            A_("act", lambda e: e.activation(out=t1[:], in_=kk[:], func=AF.Square))
            mm(kb, ps[b0][0:64, :], ones64, t1[:], True, True, r=[cst_b, A_b], w=[pb[b0]])
            A_("act", lambda e: e.sqrt(t2[:], ps[b0][0:64, :]), [pb[b0]])
            yield
            A_("dve", lambda e: e.tensor_scalar_max(t2[:], t2[:], 1e-12))
            A_("dve", lambda e: e.reciprocal(t2[:], t2[:]))
            A_("dve", lambda e: e.tensor_tensor(out=kkn[:], in0=kk[:], in1=t2[:], op=ALU.mult))
            A_("dve", lambda e: e.tensor_scalar(out=t1[:], in0=av[:], scalar1=-1.0, scalar2=None, op0=ALU.add))
            A_("dve", lambda e: e.tensor_scalar(out=t1[:], in0=t1[:], scalar1=cc(5), scalar2=None, op0=ALU.mult), [cols_b])
            A_("dve", lambda e: e.scalar_tensor_tensor(out=Kp[:], in0=t1[:], scalar=1.0, in1=Kx[:], op0=ALU.add, op1=ALU.mult))
            A_("dve", lambda e: e.tensor_tensor_scan(out=cum[:], data0=rmask[:], data1=lwt[:], initial=0.0, op0=ALU.mult, op1=ALU.add), [rmask_b])
            A_("act", lambda e: e.activation(out=Ep[:], in_=cum[:], func=AF.Exp))
            A_("act", lambda e: e.activation(out=Em[:], in_=cum[:], func=AF.Exp, scale=-1.0))
            A_("dve", lambda e: e.tensor_tensor(out=t1[:], in0=cum[:], in1=lwt[:], op=ALU.subtract))
            A_("act", lambda e: e.activation(out=Epr[:], in_=t1[:], func=AF.Exp))
            yield
            v3 = lambda t: t[:].rearrange("p (c t) -> p c t", t=64)
            AB = AX_ + [ARBK_b]
            kb.op("dve", lambda e: e.scalar_tensor_tensor(out=AR[:, :, 0:64], in0=v3(kkn), scalar=-1.0, in1=v3(Epr), op0=ALU.mult, op1=ALU.mult), r=AB, w=[ARBK_b])
            kb.op("dve", lambda e: e.tensor_tensor(out=AR[:, :, 64:128], in0=v3(R), in1=v3(Ep), op=ALU.mult), r=AB, w=[ARBK_b])
            A_("dve", lambda e: e.tensor_tensor(out=t1[:], in0=kkn[:], in1=av[:], op=ALU.mult))
            kb.op("dve", lambda e: e.tensor_tensor(out=BK[:, :, 0:64], in0=v3(t1), in1=v3(Em), op=ALU.mult), r=AB, w=[ARBK_b])
            kb.op("dve", lambda e: e.tensor_tensor(out=BK[:, :, 64:128], in0=v3(Kp), in1=v3(Em), op=ALU.mult), r=AB, w=[ARBK_b])
            kb.op("dve", lambda e: e.scalar_tensor_tensor(out=rkr[:], in0=R[:], scalar=cc(6), in1=Kp[:], op0=ALU.mult, op1=ALU.mult), r=AB + [cols_b], w=[ARBK_b])
            Ep3 = v3(Ep)
            muv = rows[:, hc].rearrange("p (o i) -> p o i", o=1).to_broadcast([64, 8, 64])
            kb.op("dve", lambda e: e.tensor_tensor(out=ep2[:], in0=ep2[:], in1=ep1[:], op=ALU.subtract), r=E, w=E)
            kb.op("dve", lambda e: e.tensor_tensor(out=ep2[:], in0=ep2[:], in1=muv, op=ALU.mult), r=E + [rows_b], w=E)
            kb.op("dve", lambda e: e.tensor_tensor(out=Vx[:], in0=ep2[:], in1=ep1[:], op=ALU.add), r=E, w=[Vx_b])
            yield
            CB = [C_b]
            for ch in range(8):
                mm(kb, ps[b0][0:64, ch * 64:(ch + 1) * 64], BK[:, ch, 0:64], AR[:, ch, 0:64], True, True, r=[ARBK_b], w=[pb[b0]])
                mm(kb, ps[b1][0:64, ch * 64:(ch + 1) * 64], AR[:, ch, 0:64], BK[:, ch, 0:64], True, True, r=[ARBK_b], w=[pb[b1]])
            kb.op("dve", lambda e: e.tensor_tensor(out=Tm[:], in0=ps[b0][0:64, :], in1=maskU8, op=ALU.mult), r=[pb[b0], cst_b] + CB, w=CB)
            kb.op("act", lambda e: e.copy(Pm[0][:], Tm[:]), r=CB, w=CB)
            kb.op("dve", lambda e: e.tensor_tensor(out=Qm[0][:], in0=ps[b1][0:64, :], in1=maskL8, op=ALU.mult), r=[pb[b1], cst_b] + CB, w=CB)
            kb.op("dve", lambda e: e.tensor_tensor(out=Tm[:], in0=Tm[:], in1=I8, op=ALU.add), r=[cst_b] + CB, w=CB)
            kb.op("dve", lambda e: e.tensor_tensor(out=TTm[0][:], in0=Qm[0][:], in1=I8, op=ALU.add), r=[cst_b] + CB, w=CB)
            yield
            NL = 5
            for lv in range(NL):
                a_, b_ = lv % 2, (lv + 1) % 2
                last = lv == NL - 1
                for ch in range(8):
                    sl = slice(ch * 64, (ch + 1) * 64)
                    mm(kb, ps[b0][0:64, sl], Qm[a_][:, sl], Pm[a_][:, sl], True, True, r=CB, w=[pb[b0]])
                    if not last:
                        mm(kb, ps[b1][0:64, sl], Pm[a_][:, sl], Qm[a_][:, sl], True, True, r=CB, w=[pb[b1]])
                kb.op("act", lambda e: e.copy(Pm[b_][:], ps[b0][0:64, :]), r=[pb[b0]] + CB, w=CB)
                if not last:
                    kb.op("dve", lambda e: e.tensor_copy(out=Qm[b_][:], in_=ps[b1][0:64, :]), r=[pb[b1]] + CB, w=CB)
                yield
                for ch in range(8):
                    sl = slice(ch * 64, (ch + 1) * 64)
                    mm(kb, ps[b2][0:64, sl], TTm[a_][:, sl], Pm[b_][:, sl], True, True, r=CB, w=[pb[b2]])
                    if not last:
                        mm(kb, ps[b3][0:64, sl], Pm[b_][:, sl], TTm[a_][:, sl], True, True, r=CB, w=[pb[b3]])
                kb.op("dve", lambda e: e.tensor_tensor(out=Tm[:], in0=ps[b2][0:64, :], in1=Tm[:], op=ALU.add), r=[pb[b2]] + CB, w=CB)
                if not last:
                    kb.op("dve", lambda e: e.tensor_tensor(out=TTm[b_][:], in0=ps[b3][0:64, :], in1=TTm[a_][:], op=ALU.add), r=[pb[b3]] + CB, w=CB)
                yield
            for ch in range(8):
                mm(kb, ps[b0][0:64, 0:128], BK[:, ch, 0:64], AR[:, ch, :], True, True, r=[ARBK_b], w=[pb[b0]])
                mm(kb, ps[b0][0:64, 128:256], BK[:, ch, 64:128], AR[:, ch, :], True, True, r=[ARBK_b], w=[pb[b0]])
                kb.op("dve", lambda e: e.tensor_tensor(out=Gm[:], in0=ps[b0][0:64, 0:256], in1=maskG2, op=ALU.mult), r=[pb[b0], cst_b], w=[Gm_b])
                kb.op("pe", lambda e: e.transpose(ps[b0][0:64, 256:320], BK[:, ch, 0:64], ident), r=[ARBK_b, cst_b], w=[pb[b0]])
                kb.op("pe", lambda e: e.transpose(ps[b0][0:64, 320:384], BK[:, ch, 64:128], ident), r=[ARBK_b, cst_b], w=[pb[b0]])
                kb.op("act", lambda e: e.copy(BKT[:], ps[b0][0:64, 256:384]), r=[pb[b0]], w=[BKT_b])
                mm(kb, ps[b1][0:64, 0:64], AR[:, ch, 0:64], St[:], True, False, r=[ARBK_b, St_b], w=[pb[b1]])
                mm(kb, ps[b1][0:64, 0:64], Gm[:, 128:192], Vx[:, ch, :], False, True, r=[Gm_b, Vx_b], w=[pb[b1]])
                kb.op("act", lambda e: e.copy(Xs[:], ps[b1][0:64, 0:64]), r=[pb[b1]], w=[Xs_b])
                yield
                mm(kb, ps[b1][0:64, 64:128], Tm[:, ch * 64:(ch + 1) * 64], Xs[:], True, True, r=CB + [Xs_b], w=[pb[b1]])
                kb.op("act", lambda e: e.copy(Us[:], ps[b1][0:64, 64:128]), r=[pb[b1]], w=[Us_b])
                yield
                ysl = slice(ch * 64, (ch + 1) * 64)
                mm(kb, ps[b2][0:64, ysl], AR[:, ch, 64:128], St[:], True, False, r=[ARBK_b, St_b], w=[pb[b2]])
                mm(kb, ps[b2][0:64, ysl], Gm[:, 64:128], Us[:], False, False, r=[Gm_b, Us_b], w=[pb[b2]])
                mm(kb, ps[b2][0:64, ysl], Gm[:, 192:256], Vx[:, ch, :], False, True, r=[Gm_b, Vx_b], w=[pb[b2]])
                kb.op("dve", lambda e: e.tensor_scalar(out=Stw[:], in0=St[:], scalar1=Ep3[:, ch, 63:64], scalar2=None, op0=ALU.mult), r=[St_b, A_b], w=[Stw_b])
                mm(kb, ps[b1][0:64, 128:192], BKT[:, 0:64], Us[:], True, False, r=[BKT_b, Us_b], w=[pb[b1]])
                mm(kb, ps[b1][0:64, 128:192], BKT[:, 64:128], Vx[:, ch, :], False, True, r=[BKT_b, Vx_b], w=[pb[b1]])
                kb.op("dve", lambda e: e.scalar_tensor_tensor(out=St[:], in0=ps[b1][0:64, 128:192], scalar=Ep3[:, ch, 63:64], in1=Stw[:], op0=ALU.mult, op1=ALU.add),
                      r=[pb[b1], A_b, Stw_b], w=[St_b])
                mm(kb, ps[b1][0:64, 192 + ch:193 + ch], rkr[:, ch * 64:(ch + 1) * 64], onescol[:], True, True, r=[ARBK_b, onescol_b], w=[pb[b1]])
                mm(kb, ps[b3][0:64, ysl], sg0[:, t0 + ch * 64:t0 + (ch + 1) * 64], g2a[:, hc], True, False, r=[lora_b, lw_b], w=[pb[b3]])
                mm(kb, ps[b3][0:64, ysl], sg1[:, t0 + ch * 64:t0 + (ch + 1) * 64], g2b[:, hc], False, True, r=[lora_b, lw_b], w=[pb[b3]])
                yield
            Y3 = ps[b2][0:64, :].rearrange("p (c i) -> p c i", i=64)
            bc8 = lambda t: t[:, :].rearrange("p (c o) -> p c o", o=1).to_broadcast([64, 8, 64])
            rowb = lambda k: rows[:, k * FW + h * 64:k * FW + (h + 1) * 64].rearrange("p (o i) -> p o i", o=1).to_broadcast([64, 8, 64])
            kb.op("dve", lambda e: e.tensor_reduce(out=st8[0][:], in_=Y3, axis=AX.X, op=ALU.add), r=[pb[b2]] + E, w=E)
            kb.op("dve", lambda e: e.tensor_single_scalar(out=st8[0][:], in_=st8[0][:], scalar=1.0 / 64, op=ALU.mult), r=E, w=E)
            kb.op("dve", lambda e: e.tensor_tensor(out=ep1[:], in0=Y3, in1=bc8(st8[0]), op=ALU.subtract), r=[pb[b2]] + E, w=E)
            kb.op("dve", lambda e: e.tensor_tensor(out=ep2[:], in0=ep1[:], in1=ep1[:], op=ALU.mult), r=E, w=E)
            kb.op("dve", lambda e: e.tensor_reduce(out=st8[1][:], in_=ep2[:], axis=AX.X, op=ALU.add), r=E, w=E)
            kb.op("dve", lambda e: e.tensor_scalar(out=st8[1][:], in0=st8[1][:], scalar1=1.0 / 64, scalar2=RW_GN_EPS, op0=ALU.mult, op1=ALU.add), r=E, w=E)
            kb.op("act", lambda e: e.sqrt(st8[1][:], st8[1][:]), r=E, w=E)
            kb.op("dve", lambda e: e.reciprocal(st8[1][:], st8[1][:]), r=E, w=E)
            yield
            kb.op("dve", lambda e: e.tensor_tensor(out=ep1[:], in0=ep1[:], in1=bc8(st8[1]), op=ALU.mult), r=E, w=E)
            kb.op("dve", lambda e: e.tensor_tensor(out=ep1[:], in0=ep1[:], in1=rowb(1), op=ALU.mult), r=E + [rows_b], w=E)
            kb.op("dve", lambda e: e.tensor_tensor(out=ep1[:], in0=ep1[:], in1=rowb(2), op=ALU.add), r=E + [rows_b], w=E)
            kb.op("act", lambda e: e.copy(st8[2][:], ps[b1][0:64, 192:200]), r=[pb[b1]] + E, w=E)
            kb.op("dve", lambda e: e.tensor_tensor(out=ep2[:], in0=Vx[:], in1=bc8(st8[2]), op=ALU.mult), r=E + [Vx_b], w=E)
            kb.op("dve", lambda e: e.tensor_tensor(out=ep1[:], in0=ep1[:], in1=ep2[:], op=ALU.add), r=E, w=E)
            kb.op("dve", lambda e: e.tensor_tensor(out=ep2[:], in0=ep1[:], in1=ps[b3][0:64, :].rearrange("p (c i) -> p c i", i=64), op=ALU.mult), r=E + [pb[b3]], w=E)
            kb.dma(out_d[t0:t0 + 512, hc].rearrange("(c t) i -> t c i", t=64), ep2[:], r=E, final=True)
            yield

    sets = [make_set(0), make_set(1)]
    for h0 in range(0, nh, 2):
        gens = [head_gen(h0 + i, sets[i]) for i in range(min(2, nh - h0))]
        alive = list(gens)
        for _ in range(RW_OFFSET):
            try:
                next(gens[0])
            except StopIteration:
                alive.remove(gens[0])
                break
        while alive:
            for g in list(alive):
                try:
                    next(g)
                except StopIteration:
                    alive.remove(g)
    return kb.finish()


def run_rwkv(z1, rw, S=SEQ, nh=12, ncores=NCORES):
    nc = build_rwkv(S, nh)
    FW = nh * 64
    base = 4608
    mu = rw["mu"]
    s_ = np.arange(64)[:, None]; q_ = np.arange(128)[None, :]
    maskG = np.where(q_ < 64, s_ < q_, s_ <= (q_ - 64)).astype(np.float32)
    r64 = np.arange(64)[:, None]; c64 = np.arange(64)[None, :]
    U8 = np.tile((r64 < c64).astype(np.float32), (1, 8)); L8 = np.tile((r64 > c64).astype(np.float32), (1, 8)); I8 = np.tile(np.eye(64, dtype=np.float32), (1, 8))
    cst = np.zeros((64, 2048), np.float32)
    cst[:, 0:64] = 1.0; cst[:, 64:128] = np.eye(64); cst[:, 128:256] = maskG; cst[:, 256:384] = maskG
    cst[:, 384:896] = U8; cst[:, 896:1408] = L8; cst[:, 1408:1920] = I8
    rmask = np.ones((64, 512), np.float32); rmask[:, ::64] = 0
    in_maps = []
    for jc in range(ncores):
        b, hf = jc // 2, jc % 2
        fs = slice(hf * 768, hf * 768 + FW)
        def colv(v):
            return v[fs].reshape(nh, 64).T
        cols = np.zeros((64, 8 * nh), np.float32)
        for k, v in enumerate([mu[0:1536], mu[1536:3072], rw["w0"], rw["a0"], rw["k_k"], rw["k_a"], rw["r_k"].reshape(-1)]):
            cols[:, k * nh:(k + 1) * nh] = colv(v)
        mul = np.zeros((128, 4), np.float32)
        mul[0:64, 0] = mu[4608:4672]; mul[0:64, 1] = mu[4672:4736]; mul[:, 2] = mu[4736:4864]; mul[0:96, 3] = mu[4864:4960]
        rows = np.concatenate([np.broadcast_to(v[fs][None, :], (64, FW)) for v in [mu[3072:4608], rw["lnx_g"], rw["lnx_b"]]], 1)
        v_tok = np.ascontiguousarray(z1[b, base + 3072 + hf * 768: base + 3072 + hf * 768 + FW, :S].T)
        vprev = np.concatenate([np.zeros((1, FW), np.float32), v_tok[:-1]], 0)
        in_maps.append({
            "zr": np.ascontiguousarray(z1[b, base + hf * 768: base + hf * 768 + FW, :S]),
            "zk": np.ascontiguousarray(z1[b, base + 1536 + hf * 768: base + 1536 + hf * 768 + FW, :S]),
            "v_tok": v_tok, "vprev_tok": np.ascontiguousarray(vprev),
            "zwd": np.ascontiguousarray(z1[b, base + 4608:base + 4672, :S]), "zad": np.ascontiguousarray(z1[b, base + 4672:base + 4736, :S]),
            "zgd": np.ascontiguousarray(z1[b, base + 4736:base + 4960, :S]),
            "cols": cols, "mu_lora": mul, "w2": np.ascontiguousarray(rw["w2"][:, fs]), "a2": np.ascontiguousarray(rw["a2"][:, fs]),
            "g2": np.ascontiguousarray(rw["g2"][:, fs]), "rows": np.ascontiguousarray(rows), "cst": cst, "rmask": rmask})
    res = run_bass_kernel_spmd(nc, in_maps, core_ids=list(range(ncores)))
    tm = np.zeros((4, 1536, S), np.float32)
    for jc in range(ncores):
        b, hf = jc // 2, jc % 2
        tm[b, hf * 768:hf * 768 + FW] = res.results[jc]["tm_tok"].T
    return tm


def run_post(layer0, mixT, x_tok, mod_l, wout, wglu, ln, router_w, router_b, wgu, wd):
    nc = build_post(layer0)
    sel = np.zeros((16, 16, 128), np.float32)
    for e in range(16):
        sel[e, e, :] = 1.0
    sel = sel.reshape(16, 2048)
    ident = np.eye(128, dtype=np.float32)
    rb_bc = np.ascontiguousarray(np.broadcast_to(router_b[None, :], (128, 16)))
    in_maps = []
    for j in range(NCORES):
        b, hf = j // 2, j % 2
        ts = slice(hf * 2048, (hf + 1) * 2048)
        m = mod_l[b]
        vecs = [m[2 * D:3 * D], m[4 * D:5 * D], m[3 * D:4 * D], m[5 * D:6 * D], ln[0], ln[1], ln[2], ln[3]]
        pvec = np.ascontiguousarray(np.concatenate([fm16(v) for v in vecs], 1))
        im = {"mixT": np.ascontiguousarray(mixT[b][:, ts]), "xT": np.ascontiguousarray(x_tok[b, ts].T), "wout": wout, "pvec": pvec,
              "router_w": router_w, "router_b_bc": rb_bc, "wgu": wgu, "wd": wd, "ident": ident, "sel": sel}
        if layer0:
            im["wglu"] = wglu
        in_maps.append(im)
    res = run_bass_kernel_spmd(nc, in_maps, core_ids=list(range(NCORES)))
    out = np.zeros((4, SEQ, D), np.float32)
    for j in range(NCORES):
        b, hf = j // 2, j % 2
        out[b, hf * 2048:(hf + 1) * 2048] = res.results[j]["xoT"].T
    return out


def kernel(x, c, ada_w, ada_b, ln_mix_g, ln_mix_b, ln_ffn_g, ln_ffn_b, router_w, router_b,
           moe_w_gate_up, moe_w_down, rel_bias, ev_w_in, mla_q_norm, mla_w_uq, mla_kv_norm, mla_w_ukv,
           s5_lambda_re, s5_lambda_im, s5_b_re, s5_b_im, s5_c_re, s5_c_im, s5_d, s5_log_dt, s5_w_glu,
           ev_w_out, od_w_in, rw_mu, rw_w0, rw_w2, rw_a0, rw_a2, rw_g2, rw_k_k, rw_k_a, rw_r_k,
           rw_lnx_g, rw_lnx_b, od_w_out):
    f = lambda a: np.ascontiguousarray(np.asarray(a, dtype=np.float32))
    x = f(x)
    mod = run_ada(f(c), f(ada_w), f(ada_b))
    w = f(ev_w_in[0])
    wext = np.ascontiguousarray(np.concatenate([w, w[:, 800:832], w[:, 768:800]], 1))
    z0 = run_pre(x, mod[0], wext, 1, 0)
    att0 = run_mla(z0, f(mla_w_uq[0]), f(mla_w_ukv[0]), f(mla_q_norm[0]), f(mla_kv_norm[0]))
    ys5 = run_s5(z0, f(s5_lambda_re[0]), f(s5_lambda_im[0]), f(s5_b_re[0]), f(s5_b_im[0]), f(s5_c_re[0]), f(s5_c_im[0]), f(s5_d[0]), f(s5_log_dt[0]))
    del z0
    mix0 = np.concatenate([att0, ys5], 1)
    x1 = run_post(True, mix0, x, mod[0], f(ev_w_out[0]), f(s5_w_glu[0]), [f(ln_mix_g[0]), f(ln_mix_b[0]), f(ln_ffn_g[0]), f(ln_ffn_b[0])],
                  f(router_w), f(router_b), f(moe_w_gate_up[0]), f(moe_w_down[0]))
    del mix0, att0, ys5
    w = f(od_w_in[0])
    wext = np.ascontiguousarray(np.concatenate([w, np.zeros((D, 9600 - w.shape[1]), np.float32)], 1))
    z1 = run_pre(x1, mod[1], wext, 1, 0)
    att1 = run_dil(z1, f(rel_bias))
    rw = {"mu": f(rw_mu[0]), "w0": f(rw_w0[0]), "w2": f(rw_w2[0]), "a0": f(rw_a0[0]), "a2": f(rw_a2[0]), "g2": f(rw_g2[0]),
          "k_k": f(rw_k_k[0]), "k_a": f(rw_k_a[0]), "r_k": f(rw_r_k[0]), "lnx_g": f(rw_lnx_g[0]), "lnx_b": f(rw_lnx_b[0])}
    tm = run_rwkv(z1, rw)
    del z1
    mix1 = np.concatenate([att1, tm], 1)
    x2 = run_post(False, mix1, x1, mod[1], f(od_w_out[0]), None, [f(ln_mix_g[1]), f(ln_mix_b[1]), f(ln_ffn_g[1]), f(ln_ffn_b[1])],
                  f(router_w), f(router_b), f(moe_w_gate_up[1]), f(moe_w_down[1]))
    return x2.astype(np.float32)
```

```python
import contextlib
import numpy as np
import concourse.bass as bass
import concourse.mybir as mybir
from concourse.bass_utils import run_bass_kernel_spmd

F32 = mybir.dt.float32
BF16 = mybir.dt.bfloat16
AF = mybir.ActivationFunctionType
ALU = mybir.AluOpType
AX = mybir.AxisListType

D = 2048
NCORES = 8
DN_ALPHA = 4.0 ** 0.25
LN_EPS = 1e-5


SAME_ENGINE_WAIT = True


class Buf:
    __slots__ = ("w", "r", "dsem", "dval")

    def __init__(self):
        self.w = None
        self.r = {}
        self.dsem = None
        self.dval = 0


def bufs(n):
    return [Buf() for _ in range(n)]


class KB:
    def __init__(self):
        self.nc = bass.Bass("TRN2", target_bir_lowering=False)
        nc = self.nc
        self.eng = {"pe": nc.tensor, "act": nc.scalar, "dve": nc.vector, "pool": nc.gpsimd, "sp": nc.sync}
        self.stack = contextlib.ExitStack()
        self.sem, self.seq, self.seen = {}, {}, {}
        for e in self.eng:
            self.sem[e] = self.stack.enter_context(nc.semaphore("s_" + e))
            self.seq[e] = 0
            self.seen[e] = {}
        self.nsem = 0
        self.finals = []
        self.nm = 0

    def name(self, p):
        self.nm += 1
        return "%s%d" % (p, self.nm)

    def din(self, name, shape, dt=F32):
        return self.nc.dram_tensor(name, list(shape), dt, kind="ExternalInput").ap()

    def dout(self, name, shape, dt=F32):
        return self.nc.dram_tensor(name, list(shape), dt, kind="ExternalOutput").ap()

    def sb(self, shape, dt=F32, name=None):
        return self.stack.enter_context(self.nc.sbuf_tensor(name or self.name("sb"), list(shape), dt))

    def ps(self, shape, dt=F32, name=None):
        return self.stack.enter_context(self.nc.psum_tensor(name or self.name("ps"), list(shape), dt))

    def _wait(self, e, ev):
        sem, val, src = ev
        key = id(sem)
        if self.seen[e].get(key, 0) >= val:
            return
        if src == e and (e == "pe" or not SAME_ENGINE_WAIT):
            return
        self.eng[e].wait_ge(sem, val)
        self.seen[e][key] = val

    def _deps(self, e, r, w):
        for b in r:
            if b.w is not None:
                self._wait(e, b.w)
        for b in w:
            if b.w is not None:
                self._wait(e, b.w)
            for ev in b.r.values():
                self._wait(e, ev)

    def _record(self, ev, r, w):
        for b in r:
            b.r[id(ev[0])] = ev
        for b in w:
            b.w = ev
            b.r = {}

    def op(self, e, fn, r=(), w=()):
        self._deps(e, r, w)
        ins = fn(self.eng[e])
        self.seq[e] += 1
        ins.then_inc(self.sem[e], 1)
        self._record((self.sem[e], self.seq[e], e), r, w)
        return ins

    def dma(self, out, in_, r=(), w=(), q="sp", final=False):
        self._deps(q, r, w)
        ins = self.eng[q].dma_start(out=out, in_=in_)
        owner = w[0] if w else r[0]
        if owner.dsem is None:
            owner.dsem = self.stack.enter_context(self.nc.semaphore(self.name("sd")))
            self.nsem += 1
        owner.dval += 16
        ins.then_inc(owner.dsem, 16)
        ev = (owner.dsem, owner.dval, "dma")
        self._record(ev, r, w)
        if final:
            self.finals.append(ev)
        return ins

    def finish(self):
        for ev in self.finals:
            self._wait("sp", ev)
        self.stack.close()
        return self.nc


def mm(kb, out, lhsT, rhs, start, stop, r, w):
    return kb.op("pe", lambda e: e.matmul(out, lhsT=lhsT, rhs=rhs, start=start, stop=stop), r=r, w=w)


TT = 512


def build_post(layer0, ntok=2048):
    kb = KB()
    nc = kb.nc
    NT = ntok // TT
    mixT = kb.din("mixT", [D, ntok]).rearrange("(c p) t -> p c t", p=128)
    xT = kb.din("xT", [D, ntok]).rearrange("(c p) t -> p c t", p=128)
    wout = kb.din("wout", [D, D]).rearrange("(c p) n -> p c n", p=128)
    if layer0:
        wglu = kb.din("wglu", [1024, 1024]).rearrange("(c p) n -> p c n", p=128)
    pvec_d = kb.din("pvec", [128, 8 * 16])
    rw_d = kb.din("router_w", [D, 16]).rearrange("(c p) n -> p c n", p=128)
    rb_d = kb.din("router_b_bc", [128, 16])
    wgu = kb.din("wgu", [16, D, 1024])
    wd = kb.din("wd", [16, 512, D])
    ident_d = kb.din("ident", [128, 128])
    sel_d = kb.din("sel", [16, 16 * 128])
    xoT = kb.dout("xoT", [D, ntok]).rearrange("(c p) t -> p c t", p=128)

    xt = kb.sb([128, 16, TT]); xt_b = bufs(16)
    yacc = kb.sb([128, 16, TT]); yacc_b = bufs(16)
    mixb = kb.sb([128, 16, TT], BF16); mixb_b = bufs(16)
    hT = kb.sb([128, 16, TT], BF16); hT_b = bufs(16)
    NSTG = 2
    stg = [kb.sb([128, 2048]) for _ in range(NSTG)]; stg_b = bufs(NSTG)
    NWB = 6
    wb = [kb.sb([128, 4096], BF16) for _ in range(NWB)]; wb_b = bufs(NWB)
    aT = kb.sb([128, 4, TT], BF16); aT_b = bufs(4)
    tmp = [kb.sb([128, TT]) for _ in range(4)]; tmp_b = bufs(4)
    h32 = [kb.sb([128, TT]) for _ in range(2)]; h32_b = bufs(2)
    mean = kb.sb([128, TT]); mean_b = Buf()
    rstd = kb.sb([128, TT]); rstd_b = Buf()
    pvec = kb.sb([128, 8 * 16]); pvec_b = Buf()
    pv1 = kb.sb([128, 8 * 16]); pv1_b = Buf()
    rw = kb.sb([128, 16, 16]); rw_b = Buf()
    rb = kb.sb([128, 16]); rb_b = Buf()
    ident = kb.sb([128, 128]); ident_b = Buf()
    ones = kb.sb([128, 128]); ones_b = Buf()
    sel = kb.sb([16, 16 * 128]); sel_b = Buf()
    lgT = kb.sb([16, TT]); lgT_b = Buf()
    gatesT = kb.sb([16, TT]); gatesT_b = Buf()
    R = {n: kb.sb([128, 4, 16], name="r_" + n) for n in ["s", "sb", "masked", "m2", "sel1", "sel2", "ssel", "gates"]}
    R_b = {n: Buf() for n in R}
    S4 = {n: kb.sb([128, 16], name="q_" + n) for n in ["p0", "p1", "gscore", "gmask", "pen"]}
    S4_b = {n: Buf() for n in S4}
    S1 = {n: kb.sb([128, 4], name="o_" + n) for n in ["gmax", "m1", "m2", "den", "rden"]}
    S1_b = {n: Buf() for n in S1}

    pbank = [kb.ps([128, TT]) for _ in range(8)]; pb = bufs(8)

    stg_i = [0]; wb_i = [0]; tmp_i = [0]

    def nxt(ctr, n):
        i = ctr[0] % n
        ctr[0] += 1
        return i

    kb.dma(pvec[:], pvec_d[:, :], w=[pvec_b])
    kb.dma(rw[:], rw_d[:, :, :], w=[rw_b])
    kb.dma(rb[:], rb_d[:, :], w=[rb_b])
    kb.dma(ident[:], ident_d[:, :], w=[ident_b])
    kb.dma(sel[:], sel_d[:, :], w=[sel_b])
    kb.op("dve", lambda e: e.memset(ones[:], 1.0), w=[ones_b])
    kb.op("dve", lambda e: e.tensor_scalar_add(pv1[:], pvec[:], 1.0), r=[pvec_b], w=[pv1_b])

    def pcol(which, c, plus1=False):
        t = pv1 if plus1 else pvec
        return t[:, which * 16 + c: which * 16 + c + 1]

    def load_weight(src_ap_fn, ncols):
        wi = nxt(wb_i, NWB)
        src_ap_fn(wb[wi], wb_b[wi])
        return wb[wi], wb_b[wi]

    def layernorm(gi, bi, emit_h, out_dma_t0=None):
        ps_sum, ps_sq = pbank[6], pbank[7]
        for c in range(16):
            ti = nxt(tmp_i, 4)
            kb.op("act", lambda e: e.activation(out=tmp[ti][:], in_=xt[:, c, :], func=AF.Square), r=[xt_b[c]], w=[tmp_b[ti]])
            mm(kb, ps_sum[:], ones[:], xt[:, c, :], c == 0, c == 15, r=[ones_b, xt_b[c]], w=[pb[6]])
            mm(kb, ps_sq[:], ones[:], tmp[ti][:], c == 0, c == 15, r=[ones_b, tmp_b[ti]], w=[pb[7]])
        kb.op("act", lambda e: e.mul(mean[:], ps_sum[:], 1.0 / D), r=[pb[6]], w=[mean_b])
        ti = nxt(tmp_i, 4)
        kb.op("dve", lambda e: e.tensor_tensor(out=tmp[ti][:], in0=mean[:], in1=mean[:], op=ALU.mult), r=[mean_b], w=[tmp_b[ti]])
        kb.op("dve", lambda e: e.scalar_tensor_tensor(out=rstd[:], in0=ps_sq[:], scalar=1.0 / D, in1=tmp[ti][:], op0=ALU.mult, op1=ALU.subtract),
              r=[pb[7], tmp_b[ti]], w=[rstd_b])
        kb.op("dve", lambda e: e.tensor_scalar_add(rstd[:], rstd[:], LN_EPS), r=[rstd_b], w=[rstd_b])
        kb.op("act", lambda e: e.sqrt(rstd[:], rstd[:]), r=[rstd_b], w=[rstd_b])
        kb.op("dve", lambda e: e.reciprocal(rstd[:], rstd[:]), r=[rstd_b], w=[rstd_b])
        for c in range(16):
            kb.op("dve", lambda e: e.tensor_tensor(out=xt[:, c, :], in0=xt[:, c, :], in1=mean[:], op=ALU.subtract), r=[xt_b[c], mean_b], w=[xt_b[c]])
            kb.op("dve", lambda e: e.tensor_tensor(out=xt[:, c, :], in0=xt[:, c, :], in1=rstd[:], op=ALU.mult), r=[xt_b[c], rstd_b], w=[xt_b[c]])
            kb.op("act", lambda e: e.activation(out=xt[:, c, :], in_=xt[:, c, :], func=AF.Identity, scale=pcol(gi, c), bias=pcol(bi, c)),
                  r=[xt_b[c], pvec_b], w=[xt_b[c]])
            if emit_h:
                hi = c % 2
                kb.op("dve", lambda e: e.tensor_scalar(out=h32[hi][:], in0=xt[:, c, :], scalar1=pcol(1, c, True), scalar2=pcol(2, c), op0=ALU.mult, op1=ALU.add),
                      r=[xt_b[c], pvec_b, pv1_b], w=[h32_b[hi]])
                kb.op("act", lambda e: e.copy(hT[:, c, :], h32[hi][:]), r=[h32_b[hi]], w=[hT_b[c]])
                mm(kb, pbank[5][0:16, :], rw[:, c, :], h32[hi][:], c == 0, c == 15, r=[rw_b, h32_b[hi]], w=[pb[5]])
            if out_dma_t0 is not None:
                kb.dma(xoT[:, c, out_dma_t0:out_dma_t0 + TT], xt[:, c, :], r=[xt_b[c]], final=True)

    for tt in range(NT):
        t0 = tt * TT
        kb.dma(xt[:], xT[:, :, t0:t0 + TT], w=xt_b)
        for c in range(16):
            kb.op("act", lambda e: e.mul(xt[:, c, :], xt[:, c, :], DN_ALPHA), r=[xt_b[c]], w=[xt_b[c]])
        for q in range(4):
            si = nxt(stg_i, NSTG)
            sv = stg[si][:, :4 * TT].rearrange("p (c t) -> p c t", c=4)
            kb.dma(sv, mixT[:, 4 * q:4 * q + 4, t0:t0 + TT], w=[stg_b[si]])
            for cc in range(4):
                c = 4 * q + cc
                if layer0 and c >= 8:
                    ti = nxt(tmp_i, 4)
                    kb.op("dve", lambda e: e.tensor_tensor(out=tmp[ti][:], in0=sv[:, cc, :], in1=sv[:, cc, :], op=ALU.mult), r=[stg_b[si]], w=[tmp_b[ti]])
                    kb.op("dve", lambda e: e.tensor_scalar(out=tmp[ti][:], in0=tmp[ti][:], scalar1=0.044715, scalar2=1.0, op0=ALU.mult, op1=ALU.add), r=[tmp_b[ti]], w=[tmp_b[ti]])
                    kb.op("dve", lambda e: e.tensor_tensor(out=tmp[ti][:], in0=tmp[ti][:], in1=sv[:, cc, :], op=ALU.mult), r=[tmp_b[ti], stg_b[si]], w=[tmp_b[ti]])
                    kb.op("act", lambda e: e.activation(out=tmp[ti][:], in_=tmp[ti][:], func=AF.Sigmoid, scale=1.5957691216), r=[tmp_b[ti]], w=[tmp_b[ti]])
                    kb.op("dve", lambda e: e.tensor_tensor(out=hT[:, c, :], in0=tmp[ti][:], in1=sv[:, cc, :], op=ALU.mult), r=[tmp_b[ti], stg_b[si]], w=[hT_b[c]])
                else:
                    kb.op("act", lambda e: e.copy(mixb[:, c, :], sv[:, cc, :]), r=[stg_b[si]], w=[mixb_b[c]])
        if layer0:
            for m in range(8):
                def ld(st, sbuf_, m=m):
                    kb.dma(st[:, :8 * 128].rearrange("p (c n) -> p c n", c=8), wglu[:, :, m * 128:(m + 1) * 128], w=[sbuf_], q="pool")
                wt, wtb = load_weight(ld, 8 * 128)
                pi = m % 2
                for k in range(8):
                    mm(kb, pbank[pi][:], wt[:, k * 128:(k + 1) * 128], hT[:, 8 + k, :], k == 0, k == 7, r=[wtb, hT_b[8 + k]], w=[pb[pi]])
                ti = nxt(tmp_i, 4)
                kb.op("act", lambda e: e.activation(out=tmp[ti][:], in_=pbank[pi][:], func=AF.Sigmoid), r=[pb[pi]], w=[tmp_b[ti]])
                kb.op("dve", lambda e: e.tensor_tensor(out=mixb[:, 8 + m, :], in0=tmp[ti][:], in1=hT[:, 8 + m, :], op=ALU.mult), r=[tmp_b[ti], hT_b[8 + m]], w=[mixb_b[8 + m]])
        for m in range(16):
            def ld(st, sbuf_, m=m):
                kb.dma(st[:, :16 * 128].rearrange("p (c n) -> p c n", c=16), wout[:, :, m * 128:(m + 1) * 128], w=[sbuf_], q="pool")
            wt, wtb = load_weight(ld, 16 * 128)
            pi = m % 2
            for k in range(16):
                mm(kb, pbank[pi][:], wt[:, k * 128:(k + 1) * 128], mixb[:, k, :], k == 0, k == 15, r=[wtb, mixb_b[k]], w=[pb[pi]])
            kb.op("dve", lambda e: e.scalar_tensor_tensor(out=xt[:, m, :], in0=pbank[pi][:], scalar=pcol(0, m, True), in1=xt[:, m, :], op0=ALU.mult, op1=ALU.add),
                  r=[pb[pi], pv1_b, xt_b[m]], w=[xt_b[m]])
        layernorm(4, 5, True)
        kb.op("act", lambda e: e.copy(lgT[:], pbank[5][0:16, :]), r=[pb[5]], w=[lgT_b])
        for s in range(4):
            kb.op("pe", lambda e: e.transpose(pbank[4][:, s * 16:(s + 1) * 16], lgT[:, s * 128:(s + 1) * 128], ident[0:16, 0:16]), r=[lgT_b, ident_b], w=[pb[4]])
        lg = pbank[4][:, 0:64].rearrange("p (s e) -> p s e", s=4)
        kb.op("act", lambda e: e.activation(out=R["s"][:], in_=lg, func=AF.Sigmoid), r=[pb[4]], w=[R_b["s"]])
        rb_bc = rb[:].rearrange("p (o e) -> p o e", o=1).to_broadcast([128, 4, 16])
        kb.op("dve", lambda e: e.tensor_tensor(out=R["sb"][:], in0=R["s"][:], in1=rb_bc, op=ALU.add), r=[R_b["s"], rb_b], w=[R_b["sb"]])
        sbg = R["sb"][:].rearrange("p s (g k) -> p (s g) k", k=4)
        first = True
        for (i, j) in [(0, 1), (0, 2), (0, 3), (1, 2), (1, 3), (2, 3)]:
            if first:
                kb.op("dve", lambda e: e.tensor_tensor(out=S4["gscore"][:], in0=sbg[:, :, i], in1=sbg[:, :, j], op=ALU.add), r=[R_b["sb"]], w=[S4_b["gscore"]])
                first = False
            else:
                kb.op("dve", lambda e: e.tensor_tensor(out=S4["p0"][:], in0=sbg[:, :, i], in1=sbg[:, :, j], op=ALU.add), r=[R_b["sb"]], w=[S4_b["p0"]])
                kb.op("dve", lambda e: e.tensor_tensor(out=S4["gscore"][:], in0=S4["gscore"][:], in1=S4["p0"][:], op=ALU.max), r=[S4_b["p0"], S4_b["gscore"]], w=[S4_b["gscore"]])
        gs3 = S4["gscore"][:].rearrange("p (s g) -> p s g", g=4)
        kb.op("dve", lambda e: e.tensor_reduce(out=S1["gmax"][:], in_=gs3, axis=AX.X, op=ALU.max), r=[S4_b["gscore"]], w=[S1_b["gmax"]])
        gmax_bc = S1["gmax"][:].rearrange("p (s o) -> p s o", o=1).to_broadcast([128, 4, 4])
        gm3 = S4["gmask"][:].rearrange("p (s g) -> p s g", g=4)
        kb.op("dve", lambda e: e.tensor_tensor(out=gm3, in0=gs3, in1=gmax_bc, op=ALU.is_equal), r=[S4_b["gscore"], S1_b["gmax"]], w=[S4_b["gmask"]])
        kb.op("dve", lambda e: e.tensor_scalar(out=S4["pen"][:], in0=S4["gmask"][:], scalar1=1e30, scalar2=-1e30, op0=ALU.mult, op1=ALU.add), r=[S4_b["gmask"]], w=[S4_b["pen"]])
        gmask_bc = S4["gmask"][:].rearrange("p (q o) -> p q o", o=1).to_broadcast([128, 16, 4])
        pen_bc = S4["pen"][:].rearrange("p (q o) -> p q o", o=1).to_broadcast([128, 16, 4])
        msk = R["masked"][:].rearrange("p s (g k) -> p (s g) k", k=4)
        kb.op("dve", lambda e: e.tensor_tensor(out=msk, in0=sbg, in1=gmask_bc, op=ALU.mult), r=[R_b["sb"], S4_b["gmask"]], w=[R_b["masked"]])
        kb.op("dve", lambda e: e.tensor_tensor(out=msk, in0=msk, in1=pen_bc, op=ALU.add), r=[R_b["masked"], S4_b["pen"]], w=[R_b["masked"]])
        kb.op("dve", lambda e: e.tensor_reduce(out=S1["m1"][:], in_=R["masked"][:], axis=AX.X, op=ALU.max), r=[R_b["masked"]], w=[S1_b["m1"]])
        m1_bc = S1["m1"][:].rearrange("p (s o) -> p s o", o=1).to_broadcast([128, 4, 16])
        kb.op("dve", lambda e: e.tensor_tensor(out=R["sel1"][:], in0=R["masked"][:], in1=m1_bc, op=ALU.is_equal), r=[R_b["masked"], S1_b["m1"]], w=[R_b["sel1"]])
        kb.op("dve", lambda e: e.scalar_tensor_tensor(out=R["m2"][:], in0=R["sel1"][:], scalar=-1e30, in1=R["masked"][:], op0=ALU.mult, op1=ALU.add),
              r=[R_b["sel1"], R_b["masked"]], w=[R_b["m2"]])
        kb.op("dve", lambda e: e.tensor_reduce(out=S1["m2"][:], in_=R["m2"][:], axis=AX.X, op=ALU.max), r=[R_b["m2"]], w=[S1_b["m2"]])
        m2_bc = S1["m2"][:].rearrange("p (s o) -> p s o", o=1).to_broadcast([128, 4, 16])
        kb.op("dve", lambda e: e.tensor_tensor(out=R["sel2"][:], in0=R["m2"][:], in1=m2_bc, op=ALU.is_equal), r=[R_b["m2"], S1_b["m2"]], w=[R_b["sel2"]])
        kb.op("dve", lambda e: e.tensor_tensor(out=R["sel1"][:], in0=R["sel1"][:], in1=R["sel2"][:], op=ALU.add), r=[R_b["sel1"], R_b["sel2"]], w=[R_b["sel1"]])
        kb.op("dve", lambda e: e.tensor_tensor(out=R["ssel"][:], in0=R["sel1"][:], in1=R["s"][:], op=ALU.mult), r=[R_b["sel1"], R_b["s"]], w=[R_b["ssel"]])
        kb.op("dve", lambda e: e.tensor_reduce(out=S1["den"][:], in_=R["ssel"][:], axis=AX.X, op=ALU.add), r=[R_b["ssel"]], w=[S1_b["den"]])
        kb.op("dve", lambda e: e.reciprocal(S1["rden"][:], S1["den"][:]), r=[S1_b["den"]], w=[S1_b["rden"]])
        rden_bc = S1["rden"][:].rearrange("p (s o) -> p s o", o=1).to_broadcast([128, 4, 16])
        kb.op("dve", lambda e: e.tensor_tensor(out=R["gates"][:], in0=R["ssel"][:], in1=rden_bc, op=ALU.mult), r=[R_b["ssel"], S1_b["rden"]], w=[R_b["gates"]])
        for s in range(4):
            kb.op("pe", lambda e: e.transpose(pbank[5][0:16, s * 128:(s + 1) * 128], R["gates"][:, s, :], ident[:, :]), r=[R_b["gates"], ident_b], w=[pb[5]])
        kb.op("act", lambda e: e.copy(gatesT[:], pbank[5][0:16, :]), r=[pb[5]], w=[gatesT_b])
        for ex in range(16):
            mm(kb, pbank[4][:], sel[:, ex * 128:(ex + 1) * 128], gatesT[:], True, True, r=[sel_b, gatesT_b], w=[pb[4]])
            for j in range(4):
                def ld(st, sbuf_, ex=ex, j=j):
                    sv_ = st[:, :16 * 256].rearrange("p (c n) -> p c n", c=16)
                    src = wgu[ex].rearrange("(c p) n -> p c n", p=128)
                    kb.dma(sv_[:, :, 0:128], src[:, :, j * 128:(j + 1) * 128], w=[sbuf_], q="pool")
                    kb.dma(sv_[:, :, 128:256], src[:, :, 512 + j * 128:512 + (j + 1) * 128], w=[sbuf_], q="pool")
                wt, wtb = load_weight(ld, 16 * 256)
                pg, pu = (0, 1) if j % 2 == 0 else (2, 3)
                for k in range(16):
                    mm(kb, pbank[pg][:], wt[:, k * 256:k * 256 + 128], hT[:, k, :], k == 0, k == 15, r=[wtb, hT_b[k]], w=[pb[pg]])
                for k in range(16):
                    mm(kb, pbank[pu][:], wt[:, k * 256 + 128:k * 256 + 256], hT[:, k, :], k == 0, k == 15, r=[wtb, hT_b[k]], w=[pb[pu]])
                ti = nxt(tmp_i, 4)
                kb.op("act", lambda e: e.activation(out=tmp[ti][:], in_=pbank[pg][:], func=AF.Silu), r=[pb[pg]], w=[tmp_b[ti]])
                kb.op("dve", lambda e: e.tensor_tensor(out=tmp[ti][:], in0=tmp[ti][:], in1=pbank[pu][:], op=ALU.mult), r=[tmp_b[ti], pb[pu]], w=[tmp_b[ti]])
                kb.op("dve", lambda e: e.tensor_tensor(out=aT[:, j, :], in0=tmp[ti][:], in1=pbank[4][:], op=ALU.mult), r=[tmp_b[ti], pb[4]], w=[aT_b[j]])
            for mq in range(4):
                def ld(st, sbuf_, ex=ex, mq=mq):
                    kb.dma(st[:, :4 * 512].rearrange("p (c n) -> p c n", c=4), wd[ex].rearrange("(c p) n -> p c n", p=128)[:, :, mq * 512:(mq + 1) * 512], w=[sbuf_], q="pool")
                wt, wtb = load_weight(ld, 4 * 512)
                for mi in range(4):
                    m = mq * 4 + mi
                    pi = 6 + (m % 2)
                    for k in range(4):
                        mm(kb, pbank[pi][:], wt[:, k * 512 + mi * 128:k * 512 + (mi + 1) * 128], aT[:, k, :], k == 0, k == 3, r=[wtb, aT_b[k]], w=[pb[pi]])
                    if ex == 0:
                        kb.op("act", lambda e: e.copy(yacc[:, m, :], pbank[pi][:]), r=[pb[pi]], w=[yacc_b[m]])
                    else:
                        kb.op("dve", lambda e: e.tensor_tensor(out=yacc[:, m, :], in0=pbank[pi][:], in1=yacc[:, m, :], op=ALU.add), r=[pb[pi], yacc_b[m]], w=[yacc_b[m]])
        for c in range(16):
            kb.op("act", lambda e: e.activation(out=yacc[:, c, :], in_=yacc[:, c, :], func=AF.Identity, scale=pcol(3, c, True)), r=[yacc_b[c], pv1_b], w=[yacc_b[c]])
            kb.op("dve", lambda e: e.scalar_tensor_tensor(out=xt[:, c, :], in0=xt[:, c, :], scalar=DN_ALPHA, in1=yacc[:, c, :], op0=ALU.mult, op1=ALU.add),
                  r=[xt_b[c], yacc_b[c]], w=[xt_b[c]])
        layernorm(6, 7, False, out_dma_t0=t0)
    return kb.finish()


def build_ada():
    kb = KB()
    cT_d = kb.din("cT", [128, 16 * 4])
    w_d = kb.din("w", [2, D, 1536])
    b_d = kb.din("b", [128, 24])
    out_d = kb.dout("modT", [128, 24 * 4])
    cT = kb.sb([128, 64]); cT_b = Buf()
    bb = kb.sb([128, 24]); bb_b = Buf()
    ot = kb.sb([128, 96]); ot_b = Buf()
    stg = [kb.sb([128, 16, 128]) for _ in range(3)]; stg_b = bufs(3)
    ps = [kb.ps([128, 512]) for _ in range(2)]; ps_b = bufs(2)
    kb.dma(cT[:], cT_d[:, :], w=[cT_b])
    kb.dma(bb[:], b_d[:, :], w=[bb_b])
    kb.op("act", lambda e: e.activation(out=cT[:], in_=cT[:], func=AF.Silu), r=[cT_b], w=[cT_b])
    for l in range(2):
        for m in range(12):
            u = l * 12 + m
            si = u % 3
            kb.dma(stg[si][:], w_d[l].rearrange("(c p) n -> p c n", p=128)[:, :, m * 128:(m + 1) * 128], w=[stg_b[si]])
            pi = u % 2
            for k in range(16):
                mm(kb, ps[pi][:, 0:4], stg[si][:, k, :], cT[:, k * 4:(k + 1) * 4], k == 0, k == 15, r=[stg_b[si], cT_b], w=[ps_b[pi]])
            kb.op("dve", lambda e: e.tensor_scalar(out=ot[:, u * 4:(u + 1) * 4], in0=ps[pi][:, 0:4], scalar1=bb[:, u:u + 1], scalar2=None, op0=ALU.add),
                  r=[ps_b[pi], bb_b], w=[ot_b])
    kb.dma(out_d[:, :], ot[:], r=[ot_b], final=True)
    return kb.finish()


def run_ada(c, ada_w, ada_b):
    nc = build_ada()
    cT = np.ascontiguousarray(c.T.reshape(16, 128, 4).transpose(1, 0, 2).reshape(128, 64))
    in_maps = []
    for j in range(NCORES):
        w = np.ascontiguousarray(ada_w[:, :, j * 1536:(j + 1) * 1536])
        b = ada_b[:, j * 1536:(j + 1) * 1536].reshape(2, 12, 128).transpose(2, 0, 1).reshape(128, 24)
        in_maps.append({"cT": cT, "w": w, "b": np.ascontiguousarray(b)})
    res = run_bass_kernel_spmd(nc, in_maps, core_ids=list(range(NCORES)))
    mod = np.zeros((2, 4, 6 * D), np.float32)
    for j in range(NCORES):
        o = res.results[j]["modT"].reshape(128, 2, 12, 4)
        mod[:, :, j * 1536:(j + 1) * 1536] = o.transpose(1, 3, 2, 0).reshape(2, 4, 1536)
    return mod


def build_pre(ncol, ntok=2048):
    assert ncol % 128 == 0
    NM = ncol // 128
    kb = KB()
    NT = ntok // TT
    xT = kb.din("xT", [D, ntok]).rearrange("(c p) t -> p c t", p=128)
    w_d = kb.din("w", [D, ncol]).rearrange("(c p) n -> p c n", p=128)
    pv_d = kb.din("pvec", [128, 32])
    zT = kb.dout("zT", [ncol, ntok]).rearrange("(c p) t -> p c t", p=128)
    xt = [kb.sb([128, 16, TT]) for _ in range(2)]; xt_b = bufs(2)
    hT = [kb.sb([128, 16, TT], BF16) for _ in range(NT)]; hT_b = bufs(NT)
    stg = [kb.sb([128, 16, 128]) for _ in range(3)]; stg_b = bufs(3)
    wb = [kb.sb([128, 16, 128], BF16) for _ in range(3)]; wb_b = bufs(3)
    ot = [kb.sb([128, TT]) for _ in range(4)]; ot_b = bufs(4)
    pv = kb.sb([128, 32]); pv_b = Buf()
    pv1 = kb.sb([128, 32]); pv1_b = Buf()
    ps = [kb.ps([128, TT]) for _ in range(4)]; ps_b = bufs(4)
    kb.dma(pv[:], pv_d[:, :], w=[pv_b])
    kb.op("dve", lambda e: e.tensor_scalar_add(pv1[:], pv[:], 1.0), r=[pv_b], w=[pv1_b])
    for tt in range(NT):
        t0 = tt * TT
        xi = tt % 2
        kb.dma(xt[xi][:], xT[:, :, t0:t0 + TT], w=[xt_b[xi]])
        for c in range(16):
            kb.op("act", lambda e: e.activation(out=hT[tt][:, c, :], in_=xt[xi][:, c, :], func=AF.Identity, scale=pv1[:, c:c + 1], bias=pv[:, 16 + c:17 + c]),
                  r=[xt_b[xi], pv_b, pv1_b], w=[hT_b[tt]])
    u = 0
    for m in range(NM):
        si = m % 3
        kb.dma(wb[si][:], w_d[:, :, m * 128:(m + 1) * 128], w=[wb_b[si]], q="pool")
        for tt in range(NT):
            t0 = tt * TT
            pi = u % 4
            for k in range(16):
                mm(kb, ps[pi][:], wb[si][:, k, :], hT[tt][:, k, :], k == 0, k == 15, r=[wb_b[si], hT_b[tt]], w=[ps_b[pi]])
            if u % 2 == 0:
                kb.op("act", lambda e: e.copy(ot[pi][:], ps[pi][:]), r=[ps_b[pi]], w=[ot_b[pi]])
            else:
                kb.op("dve", lambda e: e.tensor_copy(out=ot[pi][:], in_=ps[pi][:]), r=[ps_b[pi]], w=[ot_b[pi]])
            kb.dma(zT[:, m, t0:t0 + TT], ot[pi][:], r=[ot_b[pi]], final=True, q="act" if u % 2 == 0 else "sp")
            u += 1
    return kb.finish()


def fm16(v):
    return np.ascontiguousarray(v.reshape(16, 128).T)


def run_pre(x_tok_major, mod_l, w, sc_idx, sh_idx):
    ncol = w.shape[1]
    nc = build_pre(ncol)
    in_maps = []
    for j in range(NCORES):
        b, hf = j // 2, j % 2
        xT = np.ascontiguousarray(x_tok_major[b, hf * 2048:(hf + 1) * 2048].T)
        sc = mod_l[b, sc_idx * D:(sc_idx + 1) * D]
        sh = mod_l[b, sh_idx * D:(sh_idx + 1) * D]
        in_maps.append({"xT": xT, "w": w, "pvec": np.ascontiguousarray(np.concatenate([fm16(sc), fm16(sh)], 1))})
    res = run_bass_kernel_spmd(nc, in_maps, core_ids=list(range(NCORES)))
    z = np.zeros((4, ncol, 4096), np.float32)
    for j in range(NCORES):
        b, hf = j // 2, j % 2
        z[b, :, hf * 2048:(hf + 1) * 2048] = res.results[j]["zT"]
    return z


SEQ = 4096
MLA_SCALE = 192.0 ** -0.5


def build_mla(S=SEQ):
    kb = KB()
    NT = S // TT
    zq = kb.din("zq", [512, S]).rearrange("(c p) t -> p c t", p=128)
    zkv = kb.din("zkv", [256, S]).rearrange("(c p) t -> p c t", p=128)
    zkr = kb.din("zkr", [128, S]).rearrange("(c p) t -> p c t", p=64)
    cs_d = kb.din("cs", [128, S]).rearrange("(c p) t -> p c t", p=64)
    wq_d = kb.din("wq", [512, 1024]).rearrange("(c p) n -> p c n", p=128)
    wk_d = kb.din("wk", [256, 512]).rearrange("(c p) n -> p c n", p=128)
    wv_d = kb.din("wv", [256, 512]).rearrange("(c p) n -> p c n", p=128)
    g_d = kb.din("g", [128, 6])
    mask_d = kb.din("mask", [128, 4 * TT])
    attT = kb.dout("attT", [512, S]).rearrange("(c p) t -> p c t", p=128)

    qnope = [kb.sb([128, S], BF16) for _ in range(4)]; qnope_b = [bufs(NT) for _ in range(4)]
    qrope = [kb.sb([128, S], BF16) for _ in range(2)]; qrope_b = [bufs(NT) for _ in range(2)]
    knope = [kb.sb([128, S], BF16) for _ in range(4)]; knope_b = [bufs(NT) for _ in range(4)]
    krope = kb.sb([128, S], BF16); krope_b = bufs(NT)
    V = kb.sb([128, S // 128, 512], BF16); V_b = bufs(NT)
    wq = kb.sb([128, 4, 1024], BF16); wk = kb.sb([128, 2, 512], BF16); wv = kb.sb([128, 2, 512], BF16); w_b = Buf()
    g = kb.sb([128, 6]); g_b = Buf()
    mask = kb.sb([128, 4 * TT], BF16); mask_b = Buf()
    ones = kb.sb([128, 128]); ones_b = Buf()
    onesb = kb.sb([128, 128], BF16); onesb_b = Buf()
    stg = kb.sb([128, 2048]); stg_b = Buf()
    zin = [kb.sb([128, 6, TT]) for _ in range(1)]; zin_b = bufs(1)
    zr = [kb.sb([128, 4, TT]) for _ in range(1)]; zr_b = bufs(1)
    qn = kb.sb([128, 6, TT], BF16); qn_b = bufs(6)
    tmp = [kb.sb([128, TT]) for _ in range(4)]; tmp_b = bufs(4)
    rs = [kb.sb([128, TT]) for _ in range(2)]; rs_b = bufs(2)
    pT = [kb.sb([128, TT], BF16) for _ in range(3)]; pT_b = bufs(3)
    ot = [kb.sb([128, TT]) for _ in range(2)]; ot_b = bufs(2)
    ps = [kb.ps([128, TT]) for _ in range(8)]; pb = bufs(8)
    tmp_i = [0]

    def nxt(ctr, n):
        i = ctr[0] % n
        ctr[0] += 1
        return i

    kb.dma(g[:], g_d[:, :], w=[g_b])
    kb.op("dve", lambda e: e.memset(ones[:], 1.0), w=[ones_b])
    kb.op("dve", lambda e: e.memset(onesb[:], 1.0), w=[onesb_b])
    kb.dma(stg[:, :2048], mask_d[:, :], w=[stg_b])
    kb.op("dve", lambda e: e.tensor_copy(out=mask[:], in_=stg[:, :2048]), r=[stg_b], w=[mask_b])
    for hh in range(2):
        kb.dma(stg[:].rearrange("p (c n) -> p c n", c=2), wq_d[:, 2 * hh:2 * hh + 2, :], w=[stg_b])
        kb.op("dve", lambda e: e.tensor_copy(out=wq[:, 2 * hh:2 * hh + 2, :].rearrange("p c n -> p (c n)"), in_=stg[:]), r=[stg_b, w_b], w=[w_b])
    kb.dma(stg[:, :1024].rearrange("p (c n) -> p c n", c=2), wk_d[:, :, :], w=[stg_b])
    kb.op("dve", lambda e: e.tensor_copy(out=wk[:].rearrange("p c n -> p (c n)"), in_=stg[:, :1024]), r=[stg_b, w_b], w=[w_b])
    kb.dma(stg[:, :1024].rearrange("p (c n) -> p c n", c=2), wv_d[:, :, :], w=[stg_b])
    kb.op("dve", lambda e: e.tensor_copy(out=wv[:].rearrange("p c n -> p (c n)"), in_=stg[:, :1024]), r=[stg_b, w_b], w=[w_b])

    for tt in range(NT):
        t0 = tt * TT
        zi = 0
        kb.dma(zin[zi][:, 0:4, :], zq[:, :, t0:t0 + TT], w=[zin_b[zi]])
        kb.dma(zin[zi][:, 4:6, :], zkv[:, :, t0:t0 + TT], w=[zin_b[zi]])
        for hp in range(2):
            kb.dma(zr[zi][hp * 64:(hp + 1) * 64, 0:2, :], zkr[:, :, t0:t0 + TT], w=[zr_b[zi]])
            kb.dma(zr[zi][hp * 64:(hp + 1) * 64, 2:4, :], cs_d[:, :, t0:t0 + TT], w=[zr_b[zi]])
        for (c0, c1, pi, dim, eps, ri) in [(0, 4, 6, 512, 1e-6, 0), (4, 6, 7, 256, 1e-6, 1)]:
            for c in range(c0, c1):
                ti = nxt(tmp_i, 4)
                kb.op("act", lambda e: e.activation(out=tmp[ti][:], in_=zin[zi][:, c, :], func=AF.Square), r=[zin_b[zi]], w=[tmp_b[ti]])
                mm(kb, ps[pi][:], ones[:], tmp[ti][:], c == c0, c == c1 - 1, r=[ones_b, tmp_b[ti]], w=[pb[pi]])
            kb.op("dve", lambda e: e.tensor_scalar(out=rs[ri][:], in0=ps[pi][:], scalar1=1.0 / dim, scalar2=eps, op0=ALU.mult, op1=ALU.add), r=[pb[pi]], w=[rs_b[ri]])
            kb.op("act", lambda e: e.sqrt(rs[ri][:], rs[ri][:]), r=[rs_b[ri]], w=[rs_b[ri]])
            kb.op("dve", lambda e: e.reciprocal(rs[ri][:], rs[ri][:]), r=[rs_b[ri]], w=[rs_b[ri]])
            for c in range(c0, c1):
                kb.op("dve", lambda e: e.scalar_tensor_tensor(out=qn[:, c, :], in0=zin[zi][:, c, :], scalar=g[:, c:c + 1], in1=rs[ri][:], op0=ALU.mult, op1=ALU.mult),
                      r=[zin_b[zi], g_b, rs_b[ri]], w=[qn_b[c]])
        for h in range(4):
            for k in range(4):
                mm(kb, ps[0][:], wq[:, k, h * 128:(h + 1) * 128], qn[:, k, :], k == 0, k == 3, r=[w_b, qn_b[k]], w=[pb[0]])
            kb.op("act", lambda e: e.copy(qnope[h][:, t0:t0 + TT], ps[0][:]), r=[pb[0]], w=[qnope_b[h][tt]])
            for k in range(2):
                mm(kb, ps[3][:], wk[:, k, h * 128:(h + 1) * 128], qn[:, 4 + k, :], k == 0, k == 1, r=[w_b, qn_b[4 + k]], w=[pb[3]])
            kb.op("act", lambda e: e.copy(knope[h][:, t0:t0 + TT], ps[3][:]), r=[pb[3]], w=[knope_b[h][tt]])
        for hp in range(2):
            for k in range(4):
                mm(kb, ps[1][:], wq[:, k, 512 + hp * 128:512 + (hp + 1) * 128], qn[:, k, :], k == 0, k == 3, r=[w_b, qn_b[k]], w=[pb[1]])
            for k in range(4):
                mm(kb, ps[2][:], wq[:, k, 768 + hp * 128:768 + (hp + 1) * 128], qn[:, k, :], k == 0, k == 3, r=[w_b, qn_b[k]], w=[pb[2]])
            t1 = nxt(tmp_i, 4)
            kb.op("dve", lambda e: e.tensor_tensor(out=tmp[t1][:], in0=ps[1][:], in1=zr[zi][:, 2, :], op=ALU.mult), r=[pb[1], zr_b[zi]], w=[tmp_b[t1]])
            t2 = nxt(tmp_i, 4)
            kb.op("dve", lambda e: e.tensor_tensor(out=tmp[t2][:], in0=ps[2][:], in1=zr[zi][:, 3, :], op=ALU.mult), r=[pb[2], zr_b[zi]], w=[tmp_b[t2]])
            kb.op("dve", lambda e: e.tensor_tensor(out=qrope[hp][:, t0:t0 + TT], in0=tmp[t1][:], in1=tmp[t2][:], op=ALU.add),
                  r=[tmp_b[t1], tmp_b[t2]], w=[qrope_b[hp][tt]])
        for blk in range(4):
            pi = 4 + blk % 2
            for k in range(2):
                mm(kb, ps[pi][:], qn[:, 4 + k, blk * 128:(blk + 1) * 128], wv[:, k, :], k == 0, k == 1, r=[w_b, qn_b[4 + k]], w=[pb[pi]])
            kb.op("act", lambda e: e.copy(V[:, tt * 4 + blk, :], ps[pi][:]), r=[pb[pi]], w=[V_b[tt]])
        t1 = nxt(tmp_i, 4)
        kb.op("dve", lambda e: e.tensor_tensor(out=tmp[t1][:], in0=zr[zi][:, 0, :], in1=zr[zi][:, 2, :], op=ALU.mult), r=[zr_b[zi]], w=[tmp_b[t1]])
        t2 = nxt(tmp_i, 4)
        kb.op("dve", lambda e: e.tensor_tensor(out=tmp[t2][:], in0=zr[zi][:, 1, :], in1=zr[zi][:, 3, :], op=ALU.mult), r=[zr_b[zi]], w=[tmp_b[t2]])
        kb.op("dve", lambda e: e.tensor_tensor(out=krope[:, t0:t0 + TT], in0=tmp[t1][:], in1=tmp[t2][:], op=ALU.add), r=[tmp_b[t1], tmp_b[t2]], w=[krope_b[tt]])

    sbanks = [0, 1, 2]
    cnt = [0]
    for h in range(4):
        for qb in range(NT):
            q0 = qb * TT
            nkb = 4 * qb + 4
            po, pd = (3, 4) if (h * NT + qb) % 2 == 0 else (5, 6)

            def qk(kk):
                sb_ = sbanks[kk % 3]
                kt = kk // 4
                mm(kb, ps[sb_][:], knope[h][:, kk * 128:(kk + 1) * 128], qnope[h][:, q0:q0 + TT], True, False,
                   r=[knope_b[h][kt], qnope_b[h][qb]], w=[pb[sb_]])
                ph = (h % 2) * 64
                mm(kb, ps[sb_][:], krope[ph:ph + 64, kk * 128:(kk + 1) * 128], qrope[h // 2][ph:ph + 64, q0:q0 + TT], False, True,
                   r=[krope_b[kt], qrope_b[h // 2][qb]], w=[pb[sb_]])
            qk(0)
            for kk in range(nkb):
                if kk + 1 < nkb:
                    qk(kk + 1)
                sb_ = sbanks[kk % 3]
                pi = cnt[0] % 3
                cnt[0] += 1
                kb.op("act", lambda e: e.activation(out=pT[pi][:], in_=ps[sb_][:], func=AF.Exp, scale=MLA_SCALE), r=[pb[sb_]], w=[pT_b[pi]])
                j = kk - 4 * qb
                if j >= 0:
                    kb.op("dve", lambda e: e.tensor_tensor(out=pT[pi][:], in0=pT[pi][:], in1=mask[:, j * TT:(j + 1) * TT], op=ALU.mult), r=[pT_b[pi], mask_b], w=[pT_b[pi]])
                mm(kb, ps[po][:], V[:, kk, h * 128:(h + 1) * 128], pT[pi][:], kk == 0, kk == nkb - 1, r=[V_b[kk // 4], pT_b[pi]], w=[pb[po]])
                mm(kb, ps[pd][:], onesb[:], pT[pi][:], kk == 0, kk == nkb - 1, r=[onesb_b, pT_b[pi]], w=[pb[pd]])
            oi = (h * NT + qb) % 2
            ti = nxt(tmp_i, 4)
            kb.op("dve", lambda e: e.reciprocal(tmp[ti][:], ps[pd][:]), r=[pb[pd]], w=[tmp_b[ti]])
            kb.op("dve", lambda e: e.tensor_tensor(out=ot[oi][:], in0=ps[po][:], in1=tmp[ti][:], op=ALU.mult), r=[pb[po], tmp_b[ti]], w=[ot_b[oi]])
            kb.dma(attT[:, h, q0:q0 + TT], ot[oi][:], r=[ot_b[oi]], final=True)
    return kb.finish()


def rope_tables(S=SEQ):
    inv = 10000.0 ** (-np.arange(0, 64, 2, dtype=np.float32) / 64)
    ang = np.arange(S, dtype=np.float32)[None, :] * inv[:, None]
    cos, sin = np.cos(ang).astype(np.float32), np.sin(ang).astype(np.float32)
    return np.ascontiguousarray(np.concatenate([cos, cos, -sin, sin], 0))


def causal_masks():
    m = np.zeros((4, 128, TT), np.float32)
    k = np.arange(128)[:, None]
    q = np.arange(TT)[None, :]
    for j in range(4):
        m[j] = (q >= k + 128 * j)
    return np.ascontiguousarray(m.transpose(1, 0, 2).reshape(128, 4 * TT))


def run_mla(z0, w_uq, w_ukv, q_norm, kv_norm):
    nc = build_mla()
    cs = rope_tables()
    mask = causal_masks()
    g = np.ascontiguousarray(np.concatenate([q_norm.reshape(4, 128).T, kv_norm.reshape(2, 128).T], 1))
    in_maps = []
    for j in range(NCORES):
        b, hf = j // 2, j % 2
        wq_cols, wk_cols, wv_cols = [], [], []
        hs = list(range(4 * hf, 4 * hf + 4))
        for h in hs:
            wq_cols.append(w_uq[:, h * 192:h * 192 + 128])
            wk_cols.append(w_ukv[:, h * 256:h * 256 + 128])
            wv_cols.append(w_ukv[:, h * 256 + 128:h * 256 + 256])
        for h in hs:
            wq_cols.append(w_uq[:, h * 192 + 128:h * 192 + 192])
        for h in hs:
            wq_cols += [w_uq[:, h * 192 + 160:h * 192 + 192], w_uq[:, h * 192 + 128:h * 192 + 160]]
        in_maps.append({
            "zq": np.ascontiguousarray(z0[b, 0:512]), "zkv": np.ascontiguousarray(z0[b, 512:768]),
            "zkr": np.ascontiguousarray(np.concatenate([z0[b, 768:832], z0[b, 1856:1920]], 0)),
            "cs": cs, "wq": np.ascontiguousarray(np.concatenate(wq_cols, 1)), "wk": np.ascontiguousarray(np.concatenate(wk_cols, 1)),
            "wv": np.ascontiguousarray(np.concatenate(wv_cols, 1)), "g": g, "mask": mask})
    res = run_bass_kernel_spmd(nc, in_maps, core_ids=list(range(NCORES)))
    att = np.zeros((4, 1024, SEQ), np.float32)
    for j in range(NCORES):
        b, hf = j // 2, j % 2
        att[b, hf * 512:(hf + 1) * 512] = res.results[j]["attT"]
    return att


TWO_PI = 6.283185307179586
NG = 32


def build_s5(S=SEQ):
    kb = KB()
    NT = S // TT
    uT = kb.din("uT", [NG * 16, S])
    lamre_d = kb.din("lamre", [128, NG]); lamim_d = kb.din("lamim", [128, NG]); logdt_d = kb.din("logdt", [128, NG])
    bt_d = kb.din("bt", [16, NG * 128]); btsw_d = kb.din("btsw", [16, NG * 128])
    ca_d = kb.din("ca", [128, NG * 16]); cb_d = kb.din("cb", [128, NG * 16])
    d_d = kb.din("dsk", [16, NG])
    iota_d = kb.din("iota", [128, S])
    yT = kb.dout("yT", [NG * 16, S])

    def t32(name=None):
        return kb.sb([128, NG], name=name)
    lamre, lamim, dt_, r_, th, cth, sth, nre, nim, den, fre, fim, tA, tB = [t32() for _ in range(14)]
    s1, s2, s3, s4 = [t32() for _ in range(4)]
    prm_b = Buf()
    sgn = kb.sb([128, 1]); negpi = kb.sb([128, 1]); ki = kb.sb([128, NG], mybir.dt.int32)
    KI = kb.sb([128, S], mybir.dt.int32); KI_b = Buf()
    bt = kb.sb([16, NG * 128], BF16); btsw = kb.sb([16, NG * 128], BF16); bstg = kb.sb([16, NG * 128]); bt_b = Buf(); bstg_b = Buf()
    ca = kb.sb([128, NG, 16]); cb = kb.sb([128, NG, 16]); cstage = kb.sb([128, NG, 16]); L1 = kb.sb([128, NG, 16], BF16); L2 = kb.sb([128, NG, 16], BF16); c_b = Buf()
    dsk = kb.sb([16, NG]); dsk_b = Buf()
    iota = kb.sb([128, S]); iota_b = Buf()
    u32 = kb.sb([16, S]); u32_b = Buf()
    ubf = kb.sb([16, S], BF16); ubf_b = Buf()
    A1 = kb.sb([128, S]); A1_b = Buf()
    A2 = kb.sb([128, S]); A2_b = Buf()
    T2 = kb.sb([128, S]); T2_b = Buf()
    bz = kb.sb([128, S]); bz_b = bufs(NT); z_b = Buf()
    Zc = kb.sb([128, S], BF16); Zc_b = Buf()
    Zs = kb.sb([128, S], BF16); Zs_b = Buf()
    ysb = kb.sb([16, S]); ysb_b = Buf()
    tmp = [kb.sb([128, TT]) for _ in range(4)]; tmp_b = bufs(4)
    ps = [kb.ps([128, TT]) for _ in range(8)]; pb = bufs(8)

    P = [prm_b]
    kb.dma(lamre[:], lamre_d[:, :], w=P)
    kb.dma(lamim[:], lamim_d[:, :], w=P)
    kb.dma(dt_[:], logdt_d[:, :], w=P)
    kb.dma(iota[:], iota_d[:, :], w=[iota_b])
    kb.dma(dsk[:], d_d[:, :], w=[dsk_b])
    kb.dma(bstg[:], bt_d[:, :], w=[bstg_b])
    kb.op("dve", lambda e: e.tensor_copy(out=bt[:], in_=bstg[:]), r=[bstg_b], w=[bt_b])
    kb.dma(bstg[:], btsw_d[:, :], w=[bstg_b])
    kb.op("dve", lambda e: e.tensor_copy(out=btsw[:], in_=bstg[:]), r=[bstg_b, bt_b], w=[bt_b])
    kb.dma(ca[:].rearrange("p g c -> p (g c)"), ca_d[:, :], w=[c_b])
    kb.dma(cb[:].rearrange("p g c -> p (g c)"), cb_d[:, :], w=[c_b])
    V = lambda fn: kb.op("dve", fn, r=P, w=P)
    A = lambda fn: kb.op("act", fn, r=P, w=P)
    V(lambda e: e.memset(sgn[0:64, :], 1.0))
    V(lambda e: e.memset(sgn[64:128, :], -1.0))
    V(lambda e: e.memset(negpi[:], -3.141592653589793))
    A(lambda e: e.activation(out=dt_[:], in_=dt_[:], func=AF.Exp))
    V(lambda e: e.tensor_tensor(out=r_[:], in0=lamre[:], in1=dt_[:], op=ALU.mult))
    A(lambda e: e.activation(out=r_[:], in_=r_[:], func=AF.Exp))
    V(lambda e: e.tensor_tensor(out=th[:], in0=lamim[:], in1=dt_[:], op=ALU.mult))
    V(lambda e: e.tensor_single_scalar(out=th[:], in_=th[:], scalar=1.0 / TWO_PI, op=ALU.mult))
    V(lambda e: e.tensor_copy(out=ki[:], in_=th[:]))
    V(lambda e: e.tensor_tensor(out=th[:], in0=th[:], in1=ki[:], op=ALU.subtract))
    A(lambda e: e.activation(out=sth[:], in_=th[:], func=AF.Sin, scale=TWO_PI))
    V(lambda e: e.tensor_single_scalar(out=tA[:], in_=th[:], scalar=0.25, op=ALU.add))
    V(lambda e: e.tensor_copy(out=ki[:], in_=tA[:]))
    V(lambda e: e.tensor_tensor(out=tA[:], in0=tA[:], in1=ki[:], op=ALU.subtract))
    A(lambda e: e.activation(out=cth[:], in_=tA[:], func=AF.Sin, scale=TWO_PI))
    V(lambda e: e.tensor_tensor(out=nre[:], in0=r_[:], in1=cth[:], op=ALU.mult))
    V(lambda e: e.tensor_single_scalar(out=nre[:], in_=nre[:], scalar=-1.0, op=ALU.add))
    V(lambda e: e.tensor_tensor(out=nim[:], in0=r_[:], in1=sth[:], op=ALU.mult))
    V(lambda e: e.tensor_tensor(out=den[:], in0=lamre[:], in1=lamre[:], op=ALU.mult))
    V(lambda e: e.tensor_tensor(out=tA[:], in0=lamim[:], in1=lamim[:], op=ALU.mult))
    V(lambda e: e.tensor_tensor(out=den[:], in0=den[:], in1=tA[:], op=ALU.add))
    V(lambda e: e.reciprocal(den[:], den[:]))
    V(lambda e: e.tensor_tensor(out=fre[:], in0=nre[:], in1=lamre[:], op=ALU.mult))
    V(lambda e: e.tensor_tensor(out=tA[:], in0=nim[:], in1=lamim[:], op=ALU.mult))
    V(lambda e: e.tensor_tensor(out=fre[:], in0=fre[:], in1=tA[:], op=ALU.add))
    V(lambda e: e.tensor_tensor(out=fre[:], in0=fre[:], in1=den[:], op=ALU.mult))
    V(lambda e: e.tensor_tensor(out=fim[:], in0=nim[:], in1=lamre[:], op=ALU.mult))
    V(lambda e: e.tensor_tensor(out=tA[:], in0=nre[:], in1=lamim[:], op=ALU.mult))
    V(lambda e: e.tensor_tensor(out=fim[:], in0=fim[:], in1=tA[:], op=ALU.subtract))
    V(lambda e: e.tensor_tensor(out=fim[:], in0=fim[:], in1=den[:], op=ALU.mult))
    V(lambda e: e.tensor_scalar(out=s1[:], in0=fre[:], scalar1=sgn[:, 0:1], scalar2=None, op0=ALU.mult))
    V(lambda e: e.tensor_single_scalar(out=s2[:], in_=fim[:], scalar=-1.0, op=ALU.mult))
    V(lambda e: e.tensor_scalar(out=s3[:], in0=s2[:], scalar1=sgn[:, 0:1], scalar2=None, op0=ALU.mult))
    V(lambda e: e.tensor_single_scalar(out=s4[:], in_=fre[:], scalar=-1.0, op=ALU.mult))

    def bc(t):
        return t[:].rearrange("p (g o) -> p g o", o=1).to_broadcast([128, NG, 16])
    PC = [prm_b, c_b]
    kb.op("dve", lambda e: e.tensor_tensor(out=cstage[:], in0=ca[:], in1=bc(s1), op=ALU.mult), r=PC, w=PC)
    kb.op("dve", lambda e: e.tensor_tensor(out=ca[:], in0=ca[:], in1=bc(s3), op=ALU.mult), r=PC, w=PC)
    kb.op("dve", lambda e: e.tensor_tensor(out=tmp[0][:, :NG * 16].rearrange("p (g c) -> p g c", c=16), in0=cb[:], in1=bc(s2), op=ALU.mult), r=PC, w=PC + [tmp_b[0]])
    kb.op("dve", lambda e: e.tensor_tensor(out=L1[:], in0=cstage[:], in1=tmp[0][:, :NG * 16].rearrange("p (g c) -> p g c", c=16), op=ALU.add), r=PC + [tmp_b[0]], w=PC)
    kb.op("dve", lambda e: e.tensor_tensor(out=cb[:], in0=cb[:], in1=bc(s4), op=ALU.mult), r=PC, w=PC)
    kb.op("dve", lambda e: e.tensor_tensor(out=L2[:], in0=ca[:], in1=cb[:], op=ALU.add), r=PC, w=PC)

    tmp_i = [1]
    for g in range(NG):
        kb.dma(u32[:], uT[g * 16:(g + 1) * 16, :], w=[u32_b])
        kb.op("act", lambda e: e.copy(ubf[:], u32[:]), r=[u32_b], w=[ubf_b])
        kb.op("dve", lambda e: e.tensor_scalar(out=KI[:], in0=iota[:], scalar1=th[:, g:g + 1], scalar2=None, op0=ALU.mult), r=[iota_b, prm_b], w=[KI_b])
        kb.op("dve", lambda e: e.scalar_tensor_tensor(out=A1[:], in0=iota[:], scalar=th[:, g:g + 1], in1=KI[:], op0=ALU.mult, op1=ALU.subtract), r=[iota_b, prm_b, KI_b], w=[A1_b])
        kb.op("dve", lambda e: e.tensor_single_scalar(out=A2[:], in_=A1[:], scalar=0.25, op=ALU.add), r=[A1_b], w=[A2_b])
        kb.op("dve", lambda e: e.tensor_copy(out=KI[:], in_=A2[:]), r=[A2_b], w=[KI_b])
        kb.op("dve", lambda e: e.tensor_tensor(out=A2[:], in0=A2[:], in1=KI[:], op=ALU.subtract), r=[A2_b, KI_b], w=[A2_b])
        kb.op("act", lambda e: e.activation(out=A1[:], in_=A1[:], func=AF.Sin, scale=TWO_PI), r=[A1_b], w=[A1_b])
        kb.op("act", lambda e: e.activation(out=A2[:], in_=A2[:], func=AF.Sin, scale=TWO_PI), r=[A2_b], w=[A2_b])
        kb.op("act", lambda e: e.activation(out=T2[:], in_=A1[:], func=AF.Identity, scale=sgn[:, 0:1]), r=[A1_b, prm_b], w=[T2_b])
        for tt in range(NT):
            t0 = tt * TT
            pa, pbk = (0, 1) if tt % 2 == 0 else (2, 3)
            mm(kb, ps[pa][:], bt[:, g * 128:(g + 1) * 128], ubf[:, t0:t0 + TT], True, True, r=[bt_b, ubf_b], w=[pb[pa]])
            mm(kb, ps[pbk][:], btsw[:, g * 128:(g + 1) * 128], ubf[:, t0:t0 + TT], True, True, r=[bt_b, ubf_b], w=[pb[pbk]])
            t1 = tmp_i[0] % 4; tmp_i[0] += 1
            kb.op("dve", lambda e: e.tensor_tensor(out=tmp[t1][:], in0=ps[pa][:], in1=A2[:, t0:t0 + TT], op=ALU.mult), r=[pb[pa], A2_b], w=[tmp_b[t1]])
            t2 = tmp_i[0] % 4; tmp_i[0] += 1
            kb.op("dve", lambda e: e.tensor_tensor(out=tmp[t2][:], in0=ps[pbk][:], in1=T2[:, t0:t0 + TT], op=ALU.mult), r=[pb[pbk], T2_b], w=[tmp_b[t2]])
            kb.op("dve", lambda e: e.tensor_tensor(out=bz[:, t0:t0 + TT], in0=tmp[t1][:], in1=tmp[t2][:], op=ALU.add), r=[tmp_b[t1], tmp_b[t2], z_b], w=[bz_b[tt]])
        kb.op("dve", lambda e: e.tensor_tensor_scan(out=bz[:], data0=r_[:, g:g + 1].to_broadcast([128, S]), data1=bz[:], initial=0.0, op0=ALU.mult, op1=ALU.add),
              r=bz_b + [prm_b], w=bz_b + [z_b])
        kb.op("dve", lambda e: e.tensor_tensor(out=Zc[:], in0=bz[:], in1=A2[:], op=ALU.mult), r=[z_b, A2_b], w=[Zc_b])
        kb.op("dve", lambda e: e.tensor_tensor(out=Zs[:], in0=bz[:], in1=A1[:], op=ALU.mult), r=[z_b, A1_b], w=[Zs_b])
        for tt in range(NT):
            t0 = tt * TT
            pi = 4 + tt % 4
            mm(kb, ps[pi][0:16, :], L1[:, g, :], Zc[:, t0:t0 + TT], True, False, r=[c_b, Zc_b], w=[pb[pi]])
            mm(kb, ps[pi][0:16, :], L2[:, g, :], Zs[:, t0:t0 + TT], False, True, r=[c_b, Zs_b], w=[pb[pi]])
            kb.op("dve", lambda e: e.scalar_tensor_tensor(out=ysb[:, t0:t0 + TT], in0=u32[:, t0:t0 + TT], scalar=dsk[:, g:g + 1], in1=ps[pi][0:16, :], op0=ALU.mult, op1=ALU.add),
                  r=[u32_b, dsk_b, pb[pi]], w=[ysb_b])
        kb.dma(yT[g * 16:(g + 1) * 16, :], ysb[:], r=[ysb_b], final=True)
    return kb.finish()


def run_s5(z0, lam_re, lam_im, b_re, b_im, c_re, c_im, d_skip, log_dt):
    nc = build_s5()
    iota = np.ascontiguousarray(np.broadcast_to(np.arange(SEQ, dtype=np.float32), (128, SEQ)))
    in_maps = []
    for j in range(NCORES):
        b, hf = j // 2, j % 2
        gs = slice(hf * NG, (hf + 1) * NG)
        lre = lam_re[gs].T; lim = lam_im[gs].T
        bre = b_re[gs].transpose(2, 0, 1); bim = b_im[gs].transpose(2, 0, 1)
        cre = c_re[gs].transpose(2, 0, 1); cim = c_im[gs].transpose(2, 0, 1)
        in_maps.append({
            "uT": np.ascontiguousarray(z0[b, 832 + hf * 512:832 + (hf + 1) * 512]),
            "lamre": np.ascontiguousarray(np.concatenate([lre, lre], 0)), "lamim": np.ascontiguousarray(np.concatenate([lim, lim], 0)),
            "logdt": np.ascontiguousarray(np.broadcast_to(log_dt[gs][None, :], (128, NG))),
            "bt": np.ascontiguousarray(np.concatenate([bre, bim], 2).reshape(16, NG * 128)),
            "btsw": np.ascontiguousarray(np.concatenate([bim, bre], 2).reshape(16, NG * 128)),
            "ca": np.ascontiguousarray(np.concatenate([cre, cim], 0).reshape(128, NG * 16)),
            "cb": np.ascontiguousarray(np.concatenate([cim, cre], 0).reshape(128, NG * 16)),
            "dsk": np.ascontiguousarray(d_skip[gs].T), "iota": iota})
    res = run_bass_kernel_spmd(nc, in_maps, core_ids=list(range(NCORES)))
    y = np.zeros((4, 1024, SEQ), np.float32)
    for j in range(NCORES):
        b, hf = j // 2, j % 2
        y[b, hf * 512:(hf + 1) * 512] = res.results[j]["yT"]
    return y


DILS = (1, 4, 16)


def build_dil(S=SEQ):
    kb = KB()
    NB = S // 128
    q_d = kb.din("q", [12, 64, S]); k_d = kb.din("k", [12, 64, S])
    v_d = kb.din("v", [12, 128, NB * 64])
    bm_d = kb.din("bm", [12, 128, 1024])
    attT = kb.dout("attT", [256, S])
    stg = [kb.sb([128, S]) for _ in range(2)]; stg_b = bufs(2)
    qs = [kb.sb([64, S], BF16) for _ in range(2)]; qs_b = bufs(2)
    ks = [kb.sb([64, S], BF16) for _ in range(2)]; ks_b = bufs(2)
    vs = [kb.sb([128, NB * 64], BF16) for _ in range(2)]; vs_b = bufs(2)
    eb = [kb.sb([128, 1024], BF16) for _ in range(2)]; eb_b = bufs(2)
    eb0 = [kb.sb([128, 512], BF16) for _ in range(2)]; eb0_b = bufs(2)
    num = kb.sb([64, S]); num_b = Buf()
    den = kb.sb([64, S]); den_b = Buf()
    onesb = kb.sb([128, 64], BF16); onesb_b = Buf()
    pT = [kb.sb([128, 512], BF16) for _ in range(4)]; pT_b = bufs(4)
    ps = [kb.ps([128, TT]) for _ in range(8)]; pb = bufs(8)
    kb.op("dve", lambda e: e.memset(onesb[:], 1.0), w=[onesb_b])
    u = 0
    pti = 0
    for h in range(4):
        for gi, d in enumerate(DILS):
            inst = gi * 4 + h
            bi = u % 2
            nb = NB // d
            kb.dma(stg[0][0:64, :], q_d[inst], w=[stg_b[0]])
            kb.op("pool", lambda e: e.tensor_copy(out=qs[bi][:], in_=stg[0][0:64, :]), r=[stg_b[0]], w=[qs_b[bi]])
            kb.dma(stg[1][0:64, :], k_d[inst], w=[stg_b[1]])
            kb.op("pool", lambda e: e.tensor_copy(out=ks[bi][:], in_=stg[1][0:64, :]), r=[stg_b[1]], w=[ks_b[bi]])
            kb.dma(stg[0][:, :NB * 64], v_d[inst], w=[stg_b[0]])
            kb.op("pool", lambda e: e.tensor_copy(out=vs[bi][:], in_=stg[0][:, :NB * 64]), r=[stg_b[0]], w=[vs_b[bi]])
            kb.dma(stg[1][:, :1024], bm_d[inst], w=[stg_b[1]])
            kb.op("act", lambda e: e.activation(out=eb[bi][:], in_=stg[1][:, :1024], func=AF.Exp), r=[stg_b[1]], w=[eb_b[bi]])
            kb.op("dve", lambda e: e.tensor_copy(out=eb0[bi][:], in_=eb[bi][:, 512:1024]), r=[eb_b[bi]], w=[eb0_b[bi]])
            kb.op("dve", lambda e: e.memset(eb0[bi][:, 0:128], 0.0), r=[eb0_b[bi]], w=[eb0_b[bi]])
            if d == 16:
                kb.op("dve", lambda e: e.memset(eb0[bi][:, 256:384], 0.0), r=[eb0_b[bi]], w=[eb0_b[bi]])
            for bt in range(NB // 4):
                B0 = bt * 4
                pss, psp, pso, psd = (0, 1, 2, 3) if bt % 2 == 0 else (4, 5, 6, 7)
                firsts = [(B0 + j) % nb == 0 for j in range(4)]
                for j in range(4):
                    B = B0 + j
                    mm(kb, ps[pss][:, j * 128:(j + 1) * 128], ks[bi][:, B * 128:(B + 1) * 128], qs[bi][:, B * 128:(B + 1) * 128], True, True,
                       r=[ks_b[bi], qs_b[bi]], w=[pb[pss]])
                    Bp = B if firsts[j] else B - 1
                    mm(kb, ps[psp][:, j * 128:(j + 1) * 128], ks[bi][:, Bp * 128:(Bp + 1) * 128], qs[bi][:, B * 128:(B + 1) * 128], True, True,
                       r=[ks_b[bi], qs_b[bi]], w=[pb[psp]])
                p1 = pti % 4; p2 = (pti + 1) % 4; pti += 2
                kb.op("act", lambda e: e.activation(out=pT[p1][:], in_=ps[pss][:], func=AF.Exp, scale=0.125), r=[pb[pss]], w=[pT_b[p1]])
                kb.op("act", lambda e: e.activation(out=pT[p2][:], in_=ps[psp][:], func=AF.Exp, scale=0.125), r=[pb[psp]], w=[pT_b[p2]])
                kb.op("dve", lambda e: e.tensor_tensor(out=pT[p1][:], in0=pT[p1][:], in1=eb[bi][:, 0:512], op=ALU.mult), r=[pT_b[p1], eb_b[bi]], w=[pT_b[p1]])
                ebp = eb0[bi][:] if any(firsts) else eb[bi][:, 512:1024]
                kb.op("dve", lambda e: e.tensor_tensor(out=pT[p2][:], in0=pT[p2][:], in1=ebp, op=ALU.mult), r=[pT_b[p2], eb_b[bi], eb0_b[bi]], w=[pT_b[p2]])
                for j in range(4):
                    B = B0 + j
                    Bp = B if firsts[j] else B - 1
                    cs_ = slice(j * 128, (j + 1) * 128)
                    mm(kb, ps[pso][0:64, cs_], vs[bi][:, B * 64:(B + 1) * 64], pT[p1][:, cs_], True, False, r=[vs_b[bi], pT_b[p1]], w=[pb[pso]])
                    mm(kb, ps[pso][0:64, cs_], vs[bi][:, Bp * 64:(Bp + 1) * 64], pT[p2][:, cs_], False, True, r=[vs_b[bi], pT_b[p2]], w=[pb[pso]])
                    mm(kb, ps[psd][0:64, cs_], onesb[:], pT[p1][:, cs_], True, False, r=[onesb_b, pT_b[p1]], w=[pb[psd]])
                    mm(kb, ps[psd][0:64, cs_], onesb[:], pT[p2][:, cs_], False, True, r=[onesb_b, pT_b[p2]], w=[pb[psd]])
                if d == 16:
                    r0 = B0 // nb
                    nv = num[:].rearrange("c (m r) -> c r m", r=16)[:, r0:r0 + 2, :]
                    dv_ = den[:].rearrange("c (m r) -> c r m", r=16)[:, r0:r0 + 2, :]
                    po = ps[pso][0:64, :].rearrange("c (r m) -> c r m", r=2)
                    pd = ps[psd][0:64, :].rearrange("c (r m) -> c r m", r=2)
                elif d == 4:
                    r0 = B0 // nb; m0 = (B0 % nb) * 128
                    nv = num[:].rearrange("c (m r) -> c r m", r=4)[:, r0, m0:m0 + 512]
                    dv_ = den[:].rearrange("c (m r) -> c r m", r=4)[:, r0, m0:m0 + 512]
                    po = ps[pso][0:64, :]; pd = ps[psd][0:64, :]
                else:
                    nv = num[:, B0 * 128:B0 * 128 + 512]; dv_ = den[:, B0 * 128:B0 * 128 + 512]
                    po = ps[pso][0:64, :]; pd = ps[psd][0:64, :]
                if gi == 0:
                    kb.op("act", lambda e: e.copy(nv, po), r=[pb[pso]], w=[num_b])
                    kb.op("act", lambda e: e.copy(dv_, pd), r=[pb[psd]], w=[den_b])
                else:
                    kb.op("dve", lambda e: e.tensor_tensor(out=nv, in0=po, in1=nv, op=ALU.add), r=[pb[pso], num_b], w=[num_b])
                    kb.op("dve", lambda e: e.tensor_tensor(out=dv_, in0=pd, in1=dv_, op=ALU.add), r=[pb[psd], den_b], w=[den_b])
            u += 1
        kb.op("dve", lambda e: e.reciprocal(den[:], den[:]), r=[den_b], w=[den_b])
        kb.op("dve", lambda e: e.tensor_tensor(out=num[:], in0=num[:], in1=den[:], op=ALU.mult), r=[num_b, den_b], w=[num_b])
        kb.dma(attT[h * 64:(h + 1) * 64, :], num[:], r=[num_b], final=True)
    return kb.finish()


def t5_bucket_np(dist):
    exact = 16
    logd = np.log(np.maximum(dist, 1).astype(np.float32) / exact) / np.float32(np.log(2048 / exact))
    large = np.minimum(exact + (logd * (32 - exact)).astype(np.int32), 31)
    return np.where(dist < exact, dist, large)


def run_dil(z1, rel_bias):
    nc = build_dil()
    S = SEQ
    NB = S // 128
    kk = np.arange(128)[:, None]; qq = np.arange(128)[None, :]
    in_maps = []
    for jc in range(NCORES):
        b, hf = jc // 2, jc % 2
        q_l, k_l, v_l, bm_l = [], [], [], []
        for gi, d in enumerate(DILS):
            for h in range(4 * hf, 4 * hf + 4):
                def rows(qkv):
                    r0 = gi * 1536 + qkv * 512 + h * 64
                    t = z1[b, r0:r0 + 64]
                    return t.reshape(64, S // d, d).transpose(0, 2, 1).reshape(64, S)
                q_l.append(rows(0)); k_l.append(rows(1))
                v = rows(2)
                v_l.append(v.reshape(64, NB, 128).transpose(2, 1, 0).reshape(128, NB * 64))
                bias_h = rel_bias[:, gi * 8 + h]
                same = np.where(qq >= kk, bias_h[t5_bucket_np(np.clip(qq - kk, 0, 128) * d)], -30000.0)
                prev = np.where(qq <= kk, bias_h[t5_bucket_np(np.clip(128 + qq - kk, 0, 128) * d)], -30000.0)
                bm_l.append(np.concatenate([np.tile(same, (1, 4)), np.tile(prev, (1, 4))], 1))
        in_maps.append({"q": np.ascontiguousarray(np.stack(q_l)), "k": np.ascontiguousarray(np.stack(k_l)),
                        "v": np.ascontiguousarray(np.stack(v_l)), "bm": np.ascontiguousarray(np.stack(bm_l).astype(np.float32))})
    res = run_bass_kernel_spmd(nc, in_maps, core_ids=list(range(NCORES)))
    att = np.zeros((4, 512, S), np.float32)
    for jc in range(NCORES):
        b, hf = jc // 2, jc % 2
        att[b, hf * 256:(hf + 1) * 256] = res.results[jc]["attT"]
    return att


CL = 64
RW_GN_EPS = 64e-5
WDEC = -0.6065306597126334


RW_STOP = None
RW_OFFSET = 0


def build_rwkv(S=SEQ, nh=12):
    kb = KB()
    NBT = S // 512
    FW = nh * 64
    zr_d = kb.din("zr", [FW, S]); zk_d = kb.din("zk", [FW, S])
    vt_d = kb.din("v_tok", [S, FW]); vp_d = kb.din("vprev_tok", [S, FW])
    zwd_d = kb.din("zwd", [64, S]); zad_d = kb.din("zad", [64, S]); zgd_d = kb.din("zgd", [224, S])
    cols_d = kb.din("cols", [64, 8 * nh])
    mul_d = kb.din("mu_lora", [128, 4])
    w2_d = kb.din("w2", [64, FW]); a2_d = kb.din("a2", [64, FW]); g2_d = kb.din("g2", [224, FW])
    rows_d = kb.din("rows", [64, 3 * FW])
    cst_d = kb.din("cst", [64, 2048])
    rmask_d = kb.din("rmask", [64, 512])
    out_d = kb.dout("tm_tok", [S, FW])

    cst = kb.sb([64, 2048]); cst_b = Buf()
    rmask = kb.sb([64, 512]); rmask_b = Buf()
    cols = kb.sb([64, 8 * nh]); cols_b = Buf()
    mul = kb.sb([128, 4]); mul_b = Buf()
    w2 = kb.sb([64, FW]); a2 = kb.sb([64, FW]); lw_b = Buf()
    g2s = kb.sb([128, FW]); g2a = kb.sb([128, FW], BF16); g2b = kb.sb([96, FW], BF16)
    rows = kb.sb([64, 3 * FW]); rows_b = Buf()
    onescol = kb.sb([64, 1]); onescol_b = Buf()
    tw = kb.sb([64, S]); xad = kb.sb([64, S]); sg0 = kb.sb([128, S], BF16); sg1 = kb.sb([96, S], BF16); lora_b = Buf()
    P_b = Buf()
    big = kb.sb([128, 4097]); big2 = kb.sb([128, 4096])

    kb.dma(cst[:], cst_d[:, :], w=[cst_b])
    kb.dma(rmask[:], rmask_d[:, :], w=[rmask_b])
    kb.dma(cols[:], cols_d[:, :], w=[cols_b])
    kb.dma(mul[:], mul_d[:, :], w=[mul_b])
    kb.dma(w2[:], w2_d[:, :], w=[lw_b]); kb.dma(a2[:], a2_d[:, :], w=[lw_b])
    kb.dma(g2s[:], g2_d[0:128, :], w=[P_b])
    kb.op("dve", lambda e: e.tensor_copy(out=g2a[:], in_=g2s[:]), r=[P_b], w=[lw_b])
    kb.dma(g2s[0:96, :], g2_d[128:224, :], r=[lw_b], w=[P_b])
    kb.op("dve", lambda e: e.tensor_copy(out=g2b[:], in_=g2s[0:96, :]), r=[P_b], w=[lw_b])
    kb.dma(rows[:], rows_d[:, :], w=[rows_b])
    kb.op("dve", lambda e: e.memset(onescol[:], 1.0), w=[onescol_b])
    ones64 = cst[:, 0:64]; ident = cst[:, 64:128]; maskG2 = cst[:, 128:384]
    identb = kb.sb([64, 64], BF16); onescolb = kb.sb([64, 1], BF16); cb_b = Buf()
    kb.op("dve", lambda e: e.tensor_copy(out=identb[:], in_=cst[:, 64:128]), r=[cst_b], w=[cb_b])
    kb.op("dve", lambda e: e.memset(onescolb[:], 1.0), r=[cb_b], w=[cb_b])
    maskU8 = cst[:, 384:896]; maskL8 = cst[:, 896:1408]; I8 = cst[:, 1408:1920]

    def shifted(src_ap, P, mucol, dst, func):
        kb.op("dve", lambda e: e.memset(big[0:P, 0:1], 0.0), r=[P_b], w=[P_b])
        kb.dma(big[0:P, 1:S + 1], src_ap, w=[P_b])
        kb.op("dve", lambda e: e.tensor_tensor(out=big2[0:P, 0:S], in0=big[0:P, 0:S], in1=big[0:P, 1:S + 1], op=ALU.subtract), r=[P_b], w=[P_b])
        kb.op("dve", lambda e: e.scalar_tensor_tensor(out=big2[0:P, 0:S], in0=big2[0:P, 0:S], scalar=mucol, in1=big[0:P, 1:S + 1], op0=ALU.mult, op1=ALU.add),
              r=[P_b, mul_b], w=[P_b])
        if func is None:
            kb.op("act", lambda e: e.copy(dst, big2[0:P, 0:S]), r=[P_b], w=[lora_b])
        else:
            kb.op("act", lambda e: e.activation(out=dst, in_=big2[0:P, 0:S], func=func), r=[P_b], w=[lora_b])
    shifted(zwd_d[:, :], 64, mul[0:64, 0:1], tw[:], AF.Tanh)
    shifted(zad_d[:, :], 64, mul[0:64, 1:2], xad[:], None)
    shifted(zgd_d[0:128, :], 128, mul[:, 2:3], sg0[:], AF.Sigmoid)
    shifted(zgd_d[128:224, :], 96, mul[0:96, 3:4], sg1[:], AF.Sigmoid)

    class _V:
        def __init__(self, t, i):
            self.t, self.i = t, i

        def __getitem__(self, idx):
            return self.t[0:64, self.i * 512:(self.i + 1) * 512][idx]

    ps = [kb.ps([128, 512]) for _ in range(8)]; pb = bufs(8)

    def make_set(si):
        T = {}
        if si == 0:
            scr = [_V(big, i) for i in range(8)] + [_V(big2, i) for i in range(5)]
        else:
            extra = kb.sb([64, 10 * 512])
            scr = [_V(big2, i) for i in range(5, 8)] + [_V(extra, i) for i in range(10)]
        T["scr"] = scr
        T["A_b"] = P_b if si == 0 else Buf()
        T["P_extra"] = [P_b]
        T["zrt"] = kb.sb([64, 513]); T["zkt"] = kb.sb([64, 513]); T["zin_b"] = Buf()
        T["AR"] = kb.sb([64, 8, 128], BF16); T["BK"] = kb.sb([64, 8, 128], BF16); T["rkr"] = kb.sb([64, 512], BF16); T["ARBK_b"] = Buf()
        T["St"] = kb.sb([64, 64]); T["St_b"] = Buf(); T["Stw"] = kb.sb([64, 64]); T["Stw_b"] = Buf()
        T["Vx"] = kb.sb([64, 8, 64]); T["Vx_b"] = Buf(); T["Vxb"] = kb.sb([64, 8, 64], BF16); T["Stb"] = kb.sb([64, 64], BF16); T["Tmb"] = kb.sb([64, 512], BF16)
        T["Pm"] = [kb.sb([64, 512], BF16) for _ in range(2)]; T["Qm"] = [kb.sb([64, 512], BF16) for _ in range(2)]; T["TTm"] = [kb.sb([64, 512], BF16) for _ in range(2)]
        T["Tm"] = kb.sb([64, 512]); T["C_b"] = Buf()
        T["Gm"] = kb.sb([64, 256], BF16); T["Gm_b"] = Buf()
        T["BKT"] = kb.sb([64, 128], BF16); T["BKT_b"] = Buf()
        T["Xs"] = kb.sb([64, 64], BF16); T["Xs_b"] = Buf(); T["Us"] = kb.sb([64, 64], BF16); T["Us_b"] = Buf()
        T["ep1"] = kb.sb([64, 8, 64]); T["ep2"] = kb.sb([64, 8, 64]); T["ep_b"] = Buf()
        T["st8"] = [kb.sb([64, 8]) for _ in range(3)]
        T["bank"] = [4 * si + k for k in range(4)]
        return T

    def head_gen(h, T):
        R, Kx, lwt, av, kk, kkn, Kp, cum, Ep, Em, Epr, t1, t2 = T["scr"]
        A_b = T["A_b"]; zrt, zkt, zin_b = T["zrt"], T["zkt"], T["zin_b"]
        AR, BK, rkr, ARBK_b = T["AR"], T["BK"], T["rkr"], T["ARBK_b"]
        St, St_b, Stw, Stw_b = T["St"], T["St_b"], T["Stw"], T["Stw_b"]
        Vx, Vx_b = T["Vx"], T["Vx_b"]
        Vxb, Stb, Tmb = T["Vxb"], T["Stb"], T["Tmb"]
        Pm, Qm, TTm, Tm, C_b = T["Pm"], T["Qm"], T["TTm"], T["Tm"], T["C_b"]
        Gm, Gm_b, BKT, BKT_b, Xs, Xs_b, Us, Us_b = T["Gm"], T["Gm_b"], T["BKT"], T["BKT_b"], T["Xs"], T["Xs_b"], T["Us"], T["Us_b"]
        ep1, ep2, ep_b, st8 = T["ep1"], T["ep2"], T["ep_b"], T["st8"]
        b0, b1, b2, b3 = T["bank"]
        AX_ = [A_b] + ([P_b] if A_b is not P_b else [])
        cc = lambda k: cols[:, k * nh + h:k * nh + h + 1]
        hc = slice(h * 64, (h + 1) * 64)
        kb.op("dve", lambda e: e.memset(St[:], 0.0), r=[St_b], w=[St_b])
        kb.op("dve", lambda e: e.memset(Stb[:], 0.0), r=[St_b], w=[St_b])
        for bi in range(NBT):
            t0 = bi * 512
            A_ = lambda eng, fn, extra_r=(): kb.op(eng, fn, r=AX_ + [zin_b] + list(extra_r), w=[A_b])
            for (zt, zd) in [(zrt, zr_d), (zkt, zk_d)]:
                if bi == 0:
                    kb.op("dve", lambda e: e.memset(zt[:, 0:1], 0.0), r=[zin_b, A_b], w=[zin_b])
                    kb.dma(zt[:, 1:513], zd[hc, 0:512], w=[zin_b])
                else:
                    kb.dma(zt[:], zd[hc, t0 - 1:t0 + 512], w=[zin_b])
            E = [ep_b]
            kb.dma(ep1[:], vt_d[t0:t0 + 512, hc].rearrange("(c t) i -> t c i", t=64), w=E, q="pool")
            kb.dma(ep2[:], vp_d[t0:t0 + 512, hc].rearrange("(c t) i -> t c i", t=64), w=E, q="pool")
            for (zt, dst, k) in [(zrt, R, 0), (zkt, Kx, 1)]:
                A_("dve", lambda e: e.tensor_tensor(out=t1[:], in0=zt[:, 0:512], in1=zt[:, 1:513], op=ALU.subtract))
                A_("dve", lambda e: e.scalar_tensor_tensor(out=dst[:], in0=t1[:], scalar=cc(k), in1=zt[:, 1:513], op0=ALU.mult, op1=ALU.add), [cols_b])
            mm(kb, ps[b0][0:64, :], w2[:, hc], tw[:, t0:t0 + 512], True, True, r=[lw_b, lora_b], w=[pb[b0]])
            A_("act", lambda e: e.activation(out=lwt[:], in_=ps[b0][0:64, :], func=AF.Sigmoid, bias=cc(2)), [pb[b0], cols_b])
            A_("act", lambda e: e.mul(lwt[:], lwt[:], WDEC))
            mm(kb, ps[b0][0:64, :], a2[:, hc], xad[:, t0:t0 + 512], True, True, r=[lw_b, lora_b], w=[pb[b0]])
            A_("act", lambda e: e.activation(out=av[:], in_=ps[b0][0:64, :], func=AF.Sigmoid, bias=cc(3)), [pb[b0], cols_b])
            A_("dve", lambda e: e.tensor_scalar(out=kk[:], in0=Kx[:], scalar1=cc(4), scalar2=None, op0=ALU.mult), [cols_b])
            A_("act", lambda e: e.activation(out=t1[:], in_=kk[:], func=AF.Square))
            mm(kb, ps[b0][0:64, :], ones64, t1[:], True, True, r=[cst_b, A_b], w=[pb[b0]])
            A_("act", lambda e: e.sqrt(t2[:], ps[b0][0:64, :]), [pb[b0]])
            yield
            A_("dve", lambda e: e.tensor_scalar_max(t2[:], t2[:], 1e-12))
            A_("dve", lambda e: e.reciprocal(t2[:], t2[:]))
            A_("dve", lambda e: e.tensor_tensor(out=kkn[:], in0=kk[:], in1=t2[:], op=ALU.mult))
            A_("dve", lambda e: e.tensor_scalar(out=t1[:], in0=av[:], scalar1=-1.0, scalar2=None, op0=ALU.add))
            A_("dve", lambda e: e.tensor_scalar(out=t1[:], in0=t1[:], scalar1=cc(5), scalar2=None, op0=ALU.mult), [cols_b])
            A_("dve", lambda e: e.scalar_tensor_tensor(out=Kp[:], in0=t1[:], scalar=1.0, in1=Kx[:], op0=ALU.add, op1=ALU.mult))
            A_("dve", lambda e: e.tensor_tensor_scan(out=cum[:], data0=rmask[:], data1=lwt[:], initial=0.0, op0=ALU.mult, op1=ALU.add), [rmask_b])
            A_("act", lambda e: e.activation(out=Ep[:], in_=cum[:], func=AF.Exp))
            A_("act", lambda e: e.activation(out=Em[:], in_=cum[:], func=AF.Exp, scale=-1.0))
            A_("dve", lambda e: e.tensor_tensor(out=t1[:], in0=cum[:], in1=lwt[:], op=ALU.subtract))
            A_("act", lambda e: e.activation(out=Epr[:], in_=t1[:], func=AF.Exp))
            yield
            v3 = lambda t: t[:].rearrange("p (c t) -> p c t", t=64)
            AB = AX_ + [ARBK_b]
            kb.op("dve", lambda e: e.scalar_tensor_tensor(out=AR[:, :, 0:64], in0=v3(kkn), scalar=-1.0, in1=v3(Epr), op0=ALU.mult, op1=ALU.mult), r=AB, w=[ARBK_b])
            kb.op("dve", lambda e: e.tensor_tensor(out=AR[:, :, 64:128], in0=v3(R), in1=v3(Ep), op=ALU.mult), r=AB, w=[ARBK_b])
            A_("dve", lambda e: e.tensor_tensor(out=t1[:], in0=kkn[:], in1=av[:], op=ALU.mult))
            kb.op("dve", lambda e: e.tensor_tensor(out=BK[:, :, 0:64], in0=v3(t1), in1=v3(Em), op=ALU.mult), r=AB, w=[ARBK_b])
            kb.op("dve", lambda e: e.tensor_tensor(out=BK[:, :, 64:128], in0=v3(Kp), in1=v3(Em), op=ALU.mult), r=AB, w=[ARBK_b])
            kb.op("dve", lambda e: e.scalar_tensor_tensor(out=rkr[:], in0=R[:], scalar=cc(6), in1=Kp[:], op0=ALU.mult, op1=ALU.mult), r=AB + [cols_b], w=[ARBK_b])
            Ep3 = v3(Ep)
            muv = rows[:, hc].rearrange("p (o i) -> p o i", o=1).to_broadcast([64, 8, 64])
            kb.op("dve", lambda e: e.tensor_tensor(out=ep2[:], in0=ep2[:], in1=ep1[:], op=ALU.subtract), r=E, w=E)
            kb.op("dve", lambda e: e.tensor_tensor(out=ep2[:], in0=ep2[:], in1=muv, op=ALU.mult), r=E + [rows_b], w=E)
            kb.op("dve", lambda e: e.tensor_tensor(out=Vx[:], in0=ep2[:], in1=ep1[:], op=ALU.add), r=E, w=[Vx_b])
            kb.op("act", lambda e: e.copy(Vxb[:], Vx[:]), r=[Vx_b], w=[Vx_b])
            yield
            CB = [C_b]
            for ch in range(8):
                mm(kb, ps[b0][0:64, ch * 64:(ch + 1) * 64], BK[:, ch, 0:64], AR[:, ch, 0:64], True, True, r=[ARBK_b], w=[pb[b0]])
                mm(kb, ps[b1][0:64, ch * 64:(ch + 1) * 64], AR[:, ch, 0:64], BK[:, ch, 0:64], True, True, r=[ARBK_b], w=[pb[b1]])
            kb.op("dve", lambda e: e.tensor_tensor(out=Tm[:], in0=ps[b0][0:64, :], in1=maskU8, op=ALU.mult), r=[pb[b0], cst_b] + CB, w=CB)
            kb.op("act", lambda e: e.copy(Pm[0][:], Tm[:]), r=CB, w=CB)
            kb.op("dve", lambda e: e.tensor_tensor(out=Qm[0][:], in0=ps[b1][0:64, :], in1=maskL8, op=ALU.mult), r=[pb[b1], cst_b] + CB, w=CB)
            kb.op("dve", lambda e: e.tensor_tensor(out=Tm[:], in0=Tm[:], in1=I8, op=ALU.add), r=[cst_b] + CB, w=CB)
            kb.op("dve", lambda e: e.tensor_tensor(out=TTm[0][:], in0=Qm[0][:], in1=I8, op=ALU.add), r=[cst_b] + CB, w=CB)
            yield
            NL = 5
            for lv in range(NL):
                a_, b_ = lv % 2, (lv + 1) % 2
                last = lv == NL - 1
                for ch in range(8):
                    sl = slice(ch * 64, (ch + 1) * 64)
                    mm(kb, ps[b0][0:64, sl], Qm[a_][:, sl], Pm[a_][:, sl], True, True, r=CB, w=[pb[b0]])
                    if not last:
                        mm(kb, ps[b1][0:64, sl], Pm[a_][:, sl], Qm[a_][:, sl], True, True, r=CB, w=[pb[b1]])
                kb.op("act", lambda e: e.copy(Pm[b_][:], ps[b0][0:64, :]), r=[pb[b0]] + CB, w=CB)
                if not last:
                    kb.op("dve", lambda e: e.tensor_copy(out=Qm[b_][:], in_=ps[b1][0:64, :]), r=[pb[b1]] + CB, w=CB)
                yield
                for ch in range(8):
                    sl = slice(ch * 64, (ch + 1) * 64)
                    mm(kb, ps[b2][0:64, sl], TTm[a_][:, sl], Pm[b_][:, sl], True, True, r=CB, w=[pb[b2]])
                    if not last:
                        mm(kb, ps[b3][0:64, sl], Pm[b_][:, sl], TTm[a_][:, sl], True, True, r=CB, w=[pb[b3]])
                kb.op("dve", lambda e: e.tensor_tensor(out=Tm[:], in0=ps[b2][0:64, :], in1=Tm[:], op=ALU.add), r=[pb[b2]] + CB, w=CB)
                if not last:
                    kb.op("dve", lambda e: e.tensor_tensor(out=TTm[b_][:], in0=ps[b3][0:64, :], in1=TTm[a_][:], op=ALU.add), r=[pb[b3]] + CB, w=CB)
                yield
            kb.op("act", lambda e: e.copy(Tmb[:], Tm[:]), r=CB, w=CB)
            for ch in range(8):
                mm(kb, ps[b0][0:64, 0:128], BK[:, ch, 0:64], AR[:, ch, :], True, True, r=[ARBK_b], w=[pb[b0]])
                mm(kb, ps[b0][0:64, 128:256], BK[:, ch, 64:128], AR[:, ch, :], True, True, r=[ARBK_b], w=[pb[b0]])
                kb.op("dve", lambda e: e.tensor_tensor(out=Gm[:], in0=ps[b0][0:64, 0:256], in1=maskG2, op=ALU.mult), r=[pb[b0], cst_b], w=[Gm_b])
                mm(kb, ps[b0][0:64, 256:320], BK[:, ch, 0:64], identb[:], True, True, r=[ARBK_b, cb_b], w=[pb[b0]])
                mm(kb, ps[b0][0:64, 320:384], BK[:, ch, 64:128], identb[:], True, True, r=[ARBK_b, cb_b], w=[pb[b0]])
                kb.op("act", lambda e: e.copy(BKT[:], ps[b0][0:64, 256:384]), r=[pb[b0]], w=[BKT_b])
                mm(kb, ps[b1][0:64, 0:64], AR[:, ch, 0:64], Stb[:], True, False, r=[ARBK_b, St_b], w=[pb[b1]])
                mm(kb, ps[b1][0:64, 0:64], Gm[:, 128:192], Vxb[:, ch, :], False, True, r=[Gm_b, Vx_b], w=[pb[b1]])
                kb.op("act", lambda e: e.copy(Xs[:], ps[b1][0:64, 0:64]), r=[pb[b1]], w=[Xs_b])
                yield
                mm(kb, ps[b1][0:64, 64:128], Tmb[:, ch * 64:(ch + 1) * 64], Xs[:], True, True, r=CB + [Xs_b], w=[pb[b1]])
                kb.op("act", lambda e: e.copy(Us[:], ps[b1][0:64, 64:128]), r=[pb[b1]], w=[Us_b])
                yield
                ysl = slice(ch * 64, (ch + 1) * 64)
                mm(kb, ps[b2][0:64, ysl], AR[:, ch, 64:128], Stb[:], True, False, r=[ARBK_b, St_b], w=[pb[b2]])
                mm(kb, ps[b2][0:64, ysl], Gm[:, 64:128], Us[:], False, False, r=[Gm_b, Us_b], w=[pb[b2]])
                mm(kb, ps[b2][0:64, ysl], Gm[:, 192:256], Vxb[:, ch, :], False, True, r=[Gm_b, Vx_b], w=[pb[b2]])
                kb.op("dve", lambda e: e.tensor_scalar(out=Stw[:], in0=St[:], scalar1=Ep3[:, ch, 63:64], scalar2=None, op0=ALU.mult), r=[St_b, A_b], w=[Stw_b])
                mm(kb, ps[b1][0:64, 128:192], BKT[:, 0:64], Us[:], True, False, r=[BKT_b, Us_b], w=[pb[b1]])
                mm(kb, ps[b1][0:64, 128:192], BKT[:, 64:128], Vxb[:, ch, :], False, True, r=[BKT_b, Vx_b], w=[pb[b1]])
                kb.op("dve", lambda e: e.scalar_tensor_tensor(out=St[:], in0=ps[b1][0:64, 128:192], scalar=Ep3[:, ch, 63:64], in1=Stw[:], op0=ALU.mult, op1=ALU.add),
                      r=[pb[b1], A_b, Stw_b], w=[St_b])
                kb.op("act", lambda e: e.copy(Stb[:], St[:]), r=[St_b], w=[St_b])
                mm(kb, ps[b1][0:64, 192 + ch:193 + ch], rkr[:, ch * 64:(ch + 1) * 64], onescolb[:], True, True, r=[ARBK_b, cb_b], w=[pb[b1]])
                mm(kb, ps[b3][0:64, ysl], sg0[:, t0 + ch * 64:t0 + (ch + 1) * 64], g2a[:, hc], True, False, r=[lora_b, lw_b], w=[pb[b3]])
                mm(kb, ps[b3][0:64, ysl], sg1[:, t0 + ch * 64:t0 + (ch + 1) * 64], g2b[:, hc], False, True, r=[lora_b, lw_b], w=[pb[b3]])
                yield
            Y3 = ps[b2][0:64, :].rearrange("p (c i) -> p c i", i=64)
            bc8 = lambda t: t[:, :].rearrange("p (c o) -> p c o", o=1).to_broadcast([64, 8, 64])
            rowb = lambda k: rows[:, k * FW + h * 64:k * FW + (h + 1) * 64].rearrange("p (o i) -> p o i", o=1).to_broadcast([64, 8, 64])
            kb.op("dve", lambda e: e.tensor_reduce(out=st8[0][:], in_=Y3, axis=AX.X, op=ALU.add), r=[pb[b2]] + E, w=E)
            kb.op("dve", lambda e: e.tensor_single_scalar(out=st8[0][:], in_=st8[0][:], scalar=1.0 / 64, op=ALU.mult), r=E, w=E)
            kb.op("dve", lambda e: e.tensor_tensor(out=ep1[:], in0=Y3, in1=bc8(st8[0]), op=ALU.subtract), r=[pb[b2]] + E, w=E)
            kb.op("dve", lambda e: e.tensor_tensor(out=ep2[:], in0=ep1[:], in1=ep1[:], op=ALU.mult), r=E, w=E)
            kb.op("dve", lambda e: e.tensor_reduce(out=st8[1][:], in_=ep2[:], axis=AX.X, op=ALU.add), r=E, w=E)
            kb.op("dve", lambda e: e.tensor_scalar(out=st8[1][:], in0=st8[1][:], scalar1=1.0 / 64, scalar2=RW_GN_EPS, op0=ALU.mult, op1=ALU.add), r=E, w=E)
            kb.op("act", lambda e: e.sqrt(st8[1][:], st8[1][:]), r=E, w=E)
            kb.op("dve", lambda e: e.reciprocal(st8[1][:], st8[1][:]), r=E, w=E)
            yield
            kb.op("dve", lambda e: e.tensor_tensor(out=ep1[:], in0=ep1[:], in1=bc8(st8[1]), op=ALU.mult), r=E, w=E)
            kb.op("dve", lambda e: e.tensor_tensor(out=ep1[:], in0=ep1[:], in1=rowb(1), op=ALU.mult), r=E + [rows_b], w=E)
            kb.op("dve", lambda e: e.tensor_tensor(out=ep1[:], in0=ep1[:], in1=rowb(2), op=ALU.add), r=E + [rows_b], w=E)
            kb.op("act", lambda e: e.copy(st8[2][:], ps[b1][0:64, 192:200]), r=[pb[b1]] + E, w=E)
            kb.op("dve", lambda e: e.tensor_tensor(out=ep2[:], in0=Vx[:], in1=bc8(st8[2]), op=ALU.mult), r=E + [Vx_b], w=E)
            kb.op("dve", lambda e: e.tensor_tensor(out=ep1[:], in0=ep1[:], in1=ep2[:], op=ALU.add), r=E, w=E)
            kb.op("dve", lambda e: e.tensor_tensor(out=ep2[:], in0=ep1[:], in1=ps[b3][0:64, :].rearrange("p (c i) -> p c i", i=64), op=ALU.mult), r=E + [pb[b3]], w=E)
            kb.dma(out_d[t0:t0 + 512, hc].rearrange("(c t) i -> t c i", t=64), ep2[:], r=E, final=True)
            yield

    sets = [make_set(0), make_set(1)]
    for h0 in range(0, nh, 2):
        gens = [head_gen(h0 + i, sets[i]) for i in range(min(2, nh - h0))]
        alive = list(gens)
        for _ in range(RW_OFFSET):
            try:
                next(gens[0])
            except StopIteration:
                alive.remove(gens[0])
                break
        while alive:
            for g in list(alive):
                try:
                    next(g)
                except StopIteration:
                    alive.remove(g)
    return kb.finish()


def run_rwkv(z1, rw, S=SEQ, nh=12, ncores=NCORES):
    nc = build_rwkv(S, nh)
    FW = nh * 64
    base = 4608
    mu = rw["mu"]
    s_ = np.arange(64)[:, None]; q_ = np.arange(128)[None, :]
    maskG = np.where(q_ < 64, s_ < q_, s_ <= (q_ - 64)).astype(np.float32)
    r64 = np.arange(64)[:, None]; c64 = np.arange(64)[None, :]
    U8 = np.tile((r64 < c64).astype(np.float32), (1, 8)); L8 = np.tile((r64 > c64).astype(np.float32), (1, 8)); I8 = np.tile(np.eye(64, dtype=np.float32), (1, 8))
    cst = np.zeros((64, 2048), np.float32)
    cst[:, 0:64] = 1.0; cst[:, 64:128] = np.eye(64); cst[:, 128:256] = maskG; cst[:, 256:384] = maskG
    cst[:, 384:896] = U8; cst[:, 896:1408] = L8; cst[:, 1408:1920] = I8
    rmask = np.ones((64, 512), np.float32); rmask[:, ::64] = 0
    in_maps = []
    for jc in range(ncores):
        b, hf = jc // 2, jc % 2
        fs = slice(hf * 768, hf * 768 + FW)
        def colv(v):
            return v[fs].reshape(nh, 64).T
        cols = np.zeros((64, 8 * nh), np.float32)
        for k, v in enumerate([mu[0:1536], mu[1536:3072], rw["w0"], rw["a0"], rw["k_k"], rw["k_a"], rw["r_k"].reshape(-1)]):
            cols[:, k * nh:(k + 1) * nh] = colv(v)
        mul = np.zeros((128, 4), np.float32)
        mul[0:64, 0] = mu[4608:4672]; mul[0:64, 1] = mu[4672:4736]; mul[:, 2] = mu[4736:4864]; mul[0:96, 3] = mu[4864:4960]
        rows = np.concatenate([np.broadcast_to(v[fs][None, :], (64, FW)) for v in [mu[3072:4608], rw["lnx_g"], rw["lnx_b"]]], 1)
        v_tok = np.ascontiguousarray(z1[b, base + 3072 + hf * 768: base + 3072 + hf * 768 + FW, :S].T)
        vprev = np.concatenate([np.zeros((1, FW), np.float32), v_tok[:-1]], 0)
        in_maps.append({
            "zr": np.ascontiguousarray(z1[b, base + hf * 768: base + hf * 768 + FW, :S]),
            "zk": np.ascontiguousarray(z1[b, base + 1536 + hf * 768: base + 1536 + hf * 768 + FW, :S]),
            "v_tok": v_tok, "vprev_tok": np.ascontiguousarray(vprev),
            "zwd": np.ascontiguousarray(z1[b, base + 4608:base + 4672, :S]), "zad": np.ascontiguousarray(z1[b, base + 4672:base + 4736, :S]),
            "zgd": np.ascontiguousarray(z1[b, base + 4736:base + 4960, :S]),
            "cols": cols, "mu_lora": mul, "w2": np.ascontiguousarray(rw["w2"][:, fs]), "a2": np.ascontiguousarray(rw["a2"][:, fs]),
            "g2": np.ascontiguousarray(rw["g2"][:, fs]), "rows": np.ascontiguousarray(rows), "cst": cst, "rmask": rmask})
    res = run_bass_kernel_spmd(nc, in_maps, core_ids=list(range(ncores)))
    tm = np.zeros((4, 1536, S), np.float32)
    for jc in range(ncores):
        b, hf = jc // 2, jc % 2
        tm[b, hf * 768:hf * 768 + FW] = res.results[jc]["tm_tok"].T
    return tm


def run_post(layer0, mixT, x_tok, mod_l, wout, wglu, ln, router_w, router_b, wgu, wd):
    nc = build_post(layer0)
    sel = np.zeros((16, 16, 128), np.float32)
    for e in range(16):
        sel[e, e, :] = 1.0
    sel = sel.reshape(16, 2048)
    ident = np.eye(128, dtype=np.float32)
    rb_bc = np.ascontiguousarray(np.broadcast_to(router_b[None, :], (128, 16)))
    in_maps = []
    for j in range(NCORES):
        b, hf = j // 2, j % 2
        ts = slice(hf * 2048, (hf + 1) * 2048)
        m = mod_l[b]
        vecs = [m[2 * D:3 * D], m[4 * D:5 * D], m[3 * D:4 * D], m[5 * D:6 * D], ln[0], ln[1], ln[2], ln[3]]
        pvec = np.ascontiguousarray(np.concatenate([fm16(v) for v in vecs], 1))
        im = {"mixT": np.ascontiguousarray(mixT[b][:, ts]), "xT": np.ascontiguousarray(x_tok[b, ts].T), "wout": wout, "pvec": pvec,
              "router_w": router_w, "router_b_bc": rb_bc, "wgu": wgu, "wd": wd, "ident": ident, "sel": sel}
        if layer0:
            im["wglu"] = wglu
        in_maps.append(im)
    res = run_bass_kernel_spmd(nc, in_maps, core_ids=list(range(NCORES)))
    out = np.zeros((4, SEQ, D), np.float32)
    for j in range(NCORES):
        b, hf = j // 2, j % 2
        out[b, hf * 2048:(hf + 1) * 2048] = res.results[j]["xoT"].T
    return out


def kernel(x, c, ada_w, ada_b, ln_mix_g, ln_mix_b, ln_ffn_g, ln_ffn_b, router_w, router_b,
           moe_w_gate_up, moe_w_down, rel_bias, ev_w_in, mla_q_norm, mla_w_uq, mla_kv_norm, mla_w_ukv,
           s5_lambda_re, s5_lambda_im, s5_b_re, s5_b_im, s5_c_re, s5_c_im, s5_d, s5_log_dt, s5_w_glu,
           ev_w_out, od_w_in, rw_mu, rw_w0, rw_w2, rw_a0, rw_a2, rw_g2, rw_k_k, rw_k_a, rw_r_k,
           rw_lnx_g, rw_lnx_b, od_w_out):
    f = lambda a: np.ascontiguousarray(np.asarray(a, dtype=np.float32))
    x = f(x)
    mod = run_ada(f(c), f(ada_w), f(ada_b))
    w = f(ev_w_in[0])
    wext = np.ascontiguousarray(np.concatenate([w, w[:, 800:832], w[:, 768:800]], 1))
    z0 = run_pre(x, mod[0], wext, 1, 0)
    att0 = run_mla(z0, f(mla_w_uq[0]), f(mla_w_ukv[0]), f(mla_q_norm[0]), f(mla_kv_norm[0]))
    ys5 = run_s5(z0, f(s5_lambda_re[0]), f(s5_lambda_im[0]), f(s5_b_re[0]), f(s5_b_im[0]), f(s5_c_re[0]), f(s5_c_im[0]), f(s5_d[0]), f(s5_log_dt[0]))
    del z0
    mix0 = np.concatenate([att0, ys5], 1)
    x1 = run_post(True, mix0, x, mod[0], f(ev_w_out[0]), f(s5_w_glu[0]), [f(ln_mix_g[0]), f(ln_mix_b[0]), f(ln_ffn_g[0]), f(ln_ffn_b[0])],
                  f(router_w), f(router_b), f(moe_w_gate_up[0]), f(moe_w_down[0]))
    del mix0, att0, ys5
    w = f(od_w_in[0])
    wext = np.ascontiguousarray(np.concatenate([w, np.zeros((D, 9600 - w.shape[1]), np.float32)], 1))
    z1 = run_pre(x1, mod[1], wext, 1, 0)
    att1 = run_dil(z1, f(rel_bias))
    rw = {"mu": f(rw_mu[0]), "w0": f(rw_w0[0]), "w2": f(rw_w2[0]), "a0": f(rw_a0[0]), "a2": f(rw_a2[0]), "g2": f(rw_g2[0]),
          "k_k": f(rw_k_k[0]), "k_a": f(rw_k_a[0]), "r_k": f(rw_r_k[0]), "lnx_g": f(rw_lnx_g[0]), "lnx_b": f(rw_lnx_b[0])}
    tm = run_rwkv(z1, rw)
    del z1
    mix1 = np.concatenate([att1, tm], 1)
    x2 = run_post(False, mix1, x1, mod[1], f(od_w_out[0]), None, [f(ln_mix_g[1]), f(ln_mix_b[1]), f(ln_ffn_g[1]), f(ln_ffn_b[1])],
                  f(router_w), f(router_b), f(moe_w_gate_up[1]), f(moe_w_down[1]))
    return x2.astype(np.float32)
```

```python
import contextlib
import numpy as np
import concourse.bass as bass
import concourse.mybir as mybir
from concourse.bass_utils import run_bass_kernel_spmd

F32 = mybir.dt.float32
BF16 = mybir.dt.bfloat16
AF = mybir.ActivationFunctionType
ALU = mybir.AluOpType
AX = mybir.AxisListType

D = 2048
NCORES = 8
DN_ALPHA = 4.0 ** 0.25
LN_EPS = 1e-5


SAME_ENGINE_WAIT = True


class Buf:
    __slots__ = ("w", "r", "dsem", "dval")

    def __init__(self):
        self.w = None
        self.r = {}
        self.dsem = None
        self.dval = 0


def bufs(n):
    return [Buf() for _ in range(n)]


class KB:
    def __init__(self):
        self.nc = bass.Bass("TRN2", target_bir_lowering=False)
        nc = self.nc
        self.eng = {"pe": nc.tensor, "act": nc.scalar, "dve": nc.vector, "pool": nc.gpsimd, "sp": nc.sync}
        self.stack = contextlib.ExitStack()
        self.sem, self.seq, self.seen = {}, {}, {}
        for e in self.eng:
            self.sem[e] = self.stack.enter_context(nc.semaphore("s_" + e))
            self.seq[e] = 0
            self.seen[e] = {}
        self.nsem = 0
        self.finals = []
        self.nm = 0

    def name(self, p):
        self.nm += 1
        return "%s%d" % (p, self.nm)

    def din(self, name, shape, dt=F32):
        return self.nc.dram_tensor(name, list(shape), dt, kind="ExternalInput").ap()

    def dout(self, name, shape, dt=F32):
        return self.nc.dram_tensor(name, list(shape), dt, kind="ExternalOutput").ap()

    def sb(self, shape, dt=F32, name=None):
        return self.stack.enter_context(self.nc.sbuf_tensor(name or self.name("sb"), list(shape), dt))

    def ps(self, shape, dt=F32, name=None):
        return self.stack.enter_context(self.nc.psum_tensor(name or self.name("ps"), list(shape), dt))

    def _wait(self, e, ev):
        sem, val, src = ev
        key = id(sem)
        if self.seen[e].get(key, 0) >= val:
            return
        if src == e and (e == "pe" or not SAME_ENGINE_WAIT):
            return
        self.eng[e].wait_ge(sem, val)
        self.seen[e][key] = val

    def _deps(self, e, r, w):
        for b in r:
            if b.w is not None:
                self._wait(e, b.w)
        for b in w:
            if b.w is not None:
                self._wait(e, b.w)
            for ev in b.r.values():
                self._wait(e, ev)

    def _record(self, ev, r, w):
        for b in r:
            b.r[id(ev[0])] = ev
        for b in w:
            b.w = ev
            b.r = {}

    def op(self, e, fn, r=(), w=()):
        self._deps(e, r, w)
        ins = fn(self.eng[e])
        self.seq[e] += 1
        ins.then_inc(self.sem[e], 1)
        self._record((self.sem[e], self.seq[e], e), r, w)
        return ins

    def dma(self, out, in_, r=(), w=(), q="sp", final=False):
        self._deps(q, r, w)
        ins = self.eng[q].dma_start(out=out, in_=in_)
        owner = w[0] if w else r[0]
        if owner.dsem is None:
            owner.dsem = self.stack.enter_context(self.nc.semaphore(self.name("sd")))
            self.nsem += 1
        owner.dval += 16
        ins.then_inc(owner.dsem, 16)
        ev = (owner.dsem, owner.dval, "dma")
        self._record(ev, r, w)
        if final:
            self.finals.append(ev)
        return ins

    def finish(self):
        for ev in self.finals:
            self._wait("sp", ev)
        self.stack.close()
        return self.nc


def mm(kb, out, lhsT, rhs, start, stop, r, w):
    return kb.op("pe", lambda e: e.matmul(out, lhsT=lhsT, rhs=rhs, start=start, stop=stop), r=r, w=w)


TT = 512


def build_post(layer0, ntok=2048):
    kb = KB()
    nc = kb.nc
    NT = ntok // TT
    mixT = kb.din("mixT", [D, ntok]).rearrange("(c p) t -> p c t", p=128)
    xT = kb.din("xT", [D, ntok]).rearrange("(c p) t -> p c t", p=128)
    wout = kb.din("wout", [D, D]).rearrange("(c p) n -> p c n", p=128)
    if layer0:
        wglu = kb.din("wglu", [1024, 1024]).rearrange("(c p) n -> p c n", p=128)
    pvec_d = kb.din("pvec", [128, 8 * 16])
    rw_d = kb.din("router_w", [D, 16]).rearrange("(c p) n -> p c n", p=128)
    rb_d = kb.din("router_b_bc", [128, 16])
    wgu = kb.din("wgu", [16, D, 1024])
    wd = kb.din("wd", [16, 512, D])
    ident_d = kb.din("ident", [128, 128])
    sel_d = kb.din("sel", [16, 16 * 128])
    xoT = kb.dout("xoT", [D, ntok]).rearrange("(c p) t -> p c t", p=128)

    xt = kb.sb([128, 16, TT]); xt_b = bufs(16)
    yacc = kb.sb([128, 16, TT]); yacc_b = bufs(16)
    mixb = kb.sb([128, 16, TT], BF16); mixb_b = bufs(16)
    hT = kb.sb([128, 16, TT], BF16); hT_b = bufs(16)
    NSTG = 2
    stg = [kb.sb([128, 2048]) for _ in range(NSTG)]; stg_b = bufs(NSTG)
    NWB = 6
    wb = [kb.sb([128, 4096], BF16) for _ in range(NWB)]; wb_b = bufs(NWB)
    aT = kb.sb([128, 4, TT], BF16); aT_b = bufs(4)
    tmp = [kb.sb([128, TT]) for _ in range(4)]; tmp_b = bufs(4)
    h32 = [kb.sb([128, TT]) for _ in range(2)]; h32_b = bufs(2)
    mean = kb.sb([128, TT]); mean_b = Buf()
    rstd = kb.sb([128, TT]); rstd_b = Buf()
    pvec = kb.sb([128, 8 * 16]); pvec_b = Buf()
    pv1 = kb.sb([128, 8 * 16]); pv1_b = Buf()
    rw = kb.sb([128, 16, 16]); rw_b = Buf()
    rb = kb.sb([128, 16]); rb_b = Buf()
    ident = kb.sb([128, 128]); ident_b = Buf()
    ones = kb.sb([128, 128]); ones_b = Buf()
    sel = kb.sb([16, 16 * 128]); sel_b = Buf()
    lgT = kb.sb([16, TT]); lgT_b = Buf()
    gatesT = kb.sb([16, TT]); gatesT_b = Buf()
    R = {n: kb.sb([128, 4, 16], name="r_" + n) for n in ["s", "sb", "masked", "m2", "sel1", "sel2", "ssel", "gates"]}
    R_b = {n: Buf() for n in R}
    S4 = {n: kb.sb([128, 16], name="q_" + n) for n in ["p0", "p1", "gscore", "gmask", "pen"]}
    S4_b = {n: Buf() for n in S4}
    S1 = {n: kb.sb([128, 4], name="o_" + n) for n in ["gmax", "m1", "m2", "den", "rden"]}
    S1_b = {n: Buf() for n in S1}

    pbank = [kb.ps([128, TT]) for _ in range(8)]; pb = bufs(8)

    stg_i = [0]; wb_i = [0]; tmp_i = [0]

    def nxt(ctr, n):
        i = ctr[0] % n
        ctr[0] += 1
        return i

    kb.dma(pvec[:], pvec_d[:, :], w=[pvec_b])
    kb.dma(rw[:], rw_d[:, :, :], w=[rw_b])
    kb.dma(rb[:], rb_d[:, :], w=[rb_b])
    kb.dma(ident[:], ident_d[:, :], w=[ident_b])
    kb.dma(sel[:], sel_d[:, :], w=[sel_b])
    kb.op("dve", lambda e: e.memset(ones[:], 1.0), w=[ones_b])
    kb.op("dve", lambda e: e.tensor_scalar_add(pv1[:], pvec[:], 1.0), r=[pvec_b], w=[pv1_b])

    def pcol(which, c, plus1=False):
        t = pv1 if plus1 else pvec
        return t[:, which * 16 + c: which * 16 + c + 1]

    def load_weight(src_ap_fn, ncols):
        wi = nxt(wb_i, NWB)
        src_ap_fn(wb[wi], wb_b[wi])
        return wb[wi], wb_b[wi]

    def layernorm(gi, bi, emit_h, out_dma_t0=None):
        ps_sum, ps_sq = pbank[6], pbank[7]
        for c in range(16):
            ti = nxt(tmp_i, 4)
            kb.op("act", lambda e: e.activation(out=tmp[ti][:], in_=xt[:, c, :], func=AF.Square), r=[xt_b[c]], w=[tmp_b[ti]])
            mm(kb, ps_sum[:], ones[:], xt[:, c, :], c == 0, c == 15, r=[ones_b, xt_b[c]], w=[pb[6]])
            mm(kb, ps_sq[:], ones[:], tmp[ti][:], c == 0, c == 15, r=[ones_b, tmp_b[ti]], w=[pb[7]])
        kb.op("act", lambda e: e.mul(mean[:], ps_sum[:], 1.0 / D), r=[pb[6]], w=[mean_b])
        ti = nxt(tmp_i, 4)
        kb.op("dve", lambda e: e.tensor_tensor(out=tmp[ti][:], in0=mean[:], in1=mean[:], op=ALU.mult), r=[mean_b], w=[tmp_b[ti]])
        kb.op("dve", lambda e: e.scalar_tensor_tensor(out=rstd[:], in0=ps_sq[:], scalar=1.0 / D, in1=tmp[ti][:], op0=ALU.mult, op1=ALU.subtract),
              r=[pb[7], tmp_b[ti]], w=[rstd_b])
        kb.op("dve", lambda e: e.tensor_scalar_add(rstd[:], rstd[:], LN_EPS), r=[rstd_b], w=[rstd_b])
        kb.op("act", lambda e: e.sqrt(rstd[:], rstd[:]), r=[rstd_b], w=[rstd_b])
        kb.op("dve", lambda e: e.reciprocal(rstd[:], rstd[:]), r=[rstd_b], w=[rstd_b])
        for c in range(16):
            kb.op("dve", lambda e: e.tensor_tensor(out=xt[:, c, :], in0=xt[:, c, :], in1=mean[:], op=ALU.subtract), r=[xt_b[c], mean_b], w=[xt_b[c]])
            kb.op("dve", lambda e: e.tensor_tensor(out=xt[:, c, :], in0=xt[:, c, :], in1=rstd[:], op=ALU.mult), r=[xt_b[c], rstd_b], w=[xt_b[c]])
            kb.op("act", lambda e: e.activation(out=xt[:, c, :], in_=xt[:, c, :], func=AF.Identity, scale=pcol(gi, c), bias=pcol(bi, c)),
                  r=[xt_b[c], pvec_b], w=[xt_b[c]])
            if emit_h:
                hi = c % 2
                kb.op("dve", lambda e: e.tensor_scalar(out=h32[hi][:], in0=xt[:, c, :], scalar1=pcol(1, c, True), scalar2=pcol(2, c), op0=ALU.mult, op1=ALU.add),
                      r=[xt_b[c], pvec_b, pv1_b], w=[h32_b[hi]])
                kb.op("act", lambda e: e.copy(hT[:, c, :], h32[hi][:]), r=[h32_b[hi]], w=[hT_b[c]])
                mm(kb, pbank[5][0:16, :], rw[:, c, :], h32[hi][:], c == 0, c == 15, r=[rw_b, h32_b[hi]], w=[pb[5]])
            if out_dma_t0 is not None:
                kb.dma(xoT[:, c, out_dma_t0:out_dma_t0 + TT], xt[:, c, :], r=[xt_b[c]], final=True)

    for tt in range(NT):
        t0 = tt * TT
        kb.dma(xt[:], xT[:, :, t0:t0 + TT], w=xt_b)
        for c in range(16):
            kb.op("act", lambda e: e.mul(xt[:, c, :], xt[:, c, :], DN_ALPHA), r=[xt_b[c]], w=[xt_b[c]])
        for q in range(4):
            si = nxt(stg_i, NSTG)
            sv = stg[si][:, :4 * TT].rearrange("p (c t) -> p c t", c=4)
            kb.dma(sv, mixT[:, 4 * q:4 * q + 4, t0:t0 + TT], w=[stg_b[si]])
            for cc in range(4):
                c = 4 * q + cc
                if layer0 and c >= 8:
                    ti = nxt(tmp_i, 4)
                    kb.op("dve", lambda e: e.tensor_tensor(out=tmp[ti][:], in0=sv[:, cc, :], in1=sv[:, cc, :], op=ALU.mult), r=[stg_b[si]], w=[tmp_b[ti]])
                    kb.op("dve", lambda e: e.tensor_scalar(out=tmp[ti][:], in0=tmp[ti][:], scalar1=0.044715, scalar2=1.0, op0=ALU.mult, op1=ALU.add), r=[tmp_b[ti]], w=[tmp_b[ti]])
                    kb.op("dve", lambda e: e.tensor_tensor(out=tmp[ti][:], in0=tmp[ti][:], in1=sv[:, cc, :], op=ALU.mult), r=[tmp_b[ti], stg_b[si]], w=[tmp_b[ti]])
                    kb.op("act", lambda e: e.activation(out=tmp[ti][:], in_=tmp[ti][:], func=AF.Sigmoid, scale=1.5957691216), r=[tmp_b[ti]], w=[tmp_b[ti]])
                    kb.op("dve", lambda e: e.tensor_tensor(out=hT[:, c, :], in0=tmp[ti][:], in1=sv[:, cc, :], op=ALU.mult), r=[tmp_b[ti], stg_b[si]], w=[hT_b[c]])
                else:
                    kb.op("act", lambda e: e.copy(mixb[:, c, :], sv[:, cc, :]), r=[stg_b[si]], w=[mixb_b[c]])
        if layer0:
            for m in range(8):
                def ld(st, sbuf_, m=m):
                    kb.dma(st[:, :8 * 128].rearrange("p (c n) -> p c n", c=8), wglu[:, :, m * 128:(m + 1) * 128], w=[sbuf_], q="pool")
                wt, wtb = load_weight(ld, 8 * 128)
                pi = m % 2
                for k in range(8):
                    mm(kb, pbank[pi][:], wt[:, k * 128:(k + 1) * 128], hT[:, 8 + k, :], k == 0, k == 7, r=[wtb, hT_b[8 + k]], w=[pb[pi]])
                ti = nxt(tmp_i, 4)
                kb.op("act", lambda e: e.activation(out=tmp[ti][:], in_=pbank[pi][:], func=AF.Sigmoid), r=[pb[pi]], w=[tmp_b[ti]])
                kb.op("dve", lambda e: e.tensor_tensor(out=mixb[:, 8 + m, :], in0=tmp[ti][:], in1=hT[:, 8 + m, :], op=ALU.mult), r=[tmp_b[ti], hT_b[8 + m]], w=[mixb_b[8 + m]])
        for m in range(16):
            def ld(st, sbuf_, m=m):
                kb.dma(st[:, :16 * 128].rearrange("p (c n) -> p c n", c=16), wout[:, :, m * 128:(m + 1) * 128], w=[sbuf_], q="pool")
            wt, wtb = load_weight(ld, 16 * 128)
            pi = m % 2
            for k in range(16):
                mm(kb, pbank[pi][:], wt[:, k * 128:(k + 1) * 128], mixb[:, k, :], k == 0, k == 15, r=[wtb, mixb_b[k]], w=[pb[pi]])
            kb.op("dve", lambda e: e.scalar_tensor_tensor(out=xt[:, m, :], in0=pbank[pi][:], scalar=pcol(0, m, True), in1=xt[:, m, :], op0=ALU.mult, op1=ALU.add),
                  r=[pb[pi], pv1_b, xt_b[m]], w=[xt_b[m]])
        layernorm(4, 5, True)
        kb.op("act", lambda e: e.copy(lgT[:], pbank[5][0:16, :]), r=[pb[5]], w=[lgT_b])
        for s in range(4):
            kb.op("pe", lambda e: e.transpose(pbank[4][:, s * 16:(s + 1) * 16], lgT[:, s * 128:(s + 1) * 128], ident[0:16, 0:16]), r=[lgT_b, ident_b], w=[pb[4]])
        lg = pbank[4][:, 0:64].rearrange("p (s e) -> p s e", s=4)
        kb.op("act", lambda e: e.activation(out=R["s"][:], in_=lg, func=AF.Sigmoid), r=[pb[4]], w=[R_b["s"]])
        rb_bc = rb[:].rearrange("p (o e) -> p o e", o=1).to_broadcast([128, 4, 16])
        kb.op("dve", lambda e: e.tensor_tensor(out=R["sb"][:], in0=R["s"][:], in1=rb_bc, op=ALU.add), r=[R_b["s"], rb_b], w=[R_b["sb"]])
        sbg = R["sb"][:].rearrange("p s (g k) -> p (s g) k", k=4)
        first = True
        for (i, j) in [(0, 1), (0, 2), (0, 3), (1, 2), (1, 3), (2, 3)]:
            if first:
                kb.op("dve", lambda e: e.tensor_tensor(out=S4["gscore"][:], in0=sbg[:, :, i], in1=sbg[:, :, j], op=ALU.add), r=[R_b["sb"]], w=[S4_b["gscore"]])
                first = False
            else:
                kb.op("dve", lambda e: e.tensor_tensor(out=S4["p0"][:], in0=sbg[:, :, i], in1=sbg[:, :, j], op=ALU.add), r=[R_b["sb"]], w=[S4_b["p0"]])
                kb.op("dve", lambda e: e.tensor_tensor(out=S4["gscore"][:], in0=S4["gscore"][:], in1=S4["p0"][:], op=ALU.max), r=[S4_b["p0"], S4_b["gscore"]], w=[S4_b["gscore"]])
        gs3 = S4["gscore"][:].rearrange("p (s g) -> p s g", g=4)
        kb.op("dve", lambda e: e.tensor_reduce(out=S1["gmax"][:], in_=gs3, axis=AX.X, op=ALU.max), r=[S4_b["gscore"]], w=[S1_b["gmax"]])
        gmax_bc = S1["gmax"][:].rearrange("p (s o) -> p s o", o=1).to_broadcast([128, 4, 4])
        gm3 = S4["gmask"][:].rearrange("p (s g) -> p s g", g=4)
        kb.op("dve", lambda e: e.tensor_tensor(out=gm3, in0=gs3, in1=gmax_bc, op=ALU.is_equal), r=[S4_b["gscore"], S1_b["gmax"]], w=[S4_b["gmask"]])
        kb.op("dve", lambda e: e.tensor_scalar(out=S4["pen"][:], in0=S4["gmask"][:], scalar1=1e30, scalar2=-1e30, op0=ALU.mult, op1=ALU.add), r=[S4_b["gmask"]], w=[S4_b["pen"]])
        gmask_bc = S4["gmask"][:].rearrange("p (q o) -> p q o", o=1).to_broadcast([128, 16, 4])
        pen_bc = S4["pen"][:].rearrange("p (q o) -> p q o", o=1).to_broadcast([128, 16, 4])
        msk = R["masked"][:].rearrange("p s (g k) -> p (s g) k", k=4)
        kb.op("dve", lambda e: e.tensor_tensor(out=msk, in0=sbg, in1=gmask_bc, op=ALU.mult), r=[R_b["sb"], S4_b["gmask"]], w=[R_b["masked"]])
        kb.op("dve", lambda e: e.tensor_tensor(out=msk, in0=msk, in1=pen_bc, op=ALU.add), r=[R_b["masked"], S4_b["pen"]], w=[R_b["masked"]])
        kb.op("dve", lambda e: e.tensor_reduce(out=S1["m1"][:], in_=R["masked"][:], axis=AX.X, op=ALU.max), r=[R_b["masked"]], w=[S1_b["m1"]])
        m1_bc = S1["m1"][:].rearrange("p (s o) -> p s o", o=1).to_broadcast([128, 4, 16])
        kb.op("dve", lambda e: e.tensor_tensor(out=R["sel1"][:], in0=R["masked"][:], in1=m1_bc, op=ALU.is_equal), r=[R_b["masked"], S1_b["m1"]], w=[R_b["sel1"]])
        kb.op("dve", lambda e: e.scalar_tensor_tensor(out=R["m2"][:], in0=R["sel1"][:], scalar=-1e30, in1=R["masked"][:], op0=ALU.mult, op1=ALU.add),
              r=[R_b["sel1"], R_b["masked"]], w=[R_b["m2"]])
        kb.op("dve", lambda e: e.tensor_reduce(out=S1["m2"][:], in_=R["m2"][:], axis=AX.X, op=ALU.max), r=[R_b["m2"]], w=[S1_b["m2"]])
        m2_bc = S1["m2"][:].rearrange("p (s o) -> p s o", o=1).to_broadcast([128, 4, 16])
        kb.op("dve", lambda e: e.tensor_tensor(out=R["sel2"][:], in0=R["m2"][:], in1=m2_bc, op=ALU.is_equal), r=[R_b["m2"], S1_b["m2"]], w=[R_b["sel2"]])
        kb.op("dve", lambda e: e.tensor_tensor(out=R["sel1"][:], in0=R["sel1"][:], in1=R["sel2"][:], op=ALU.add), r=[R_b["sel1"], R_b["sel2"]], w=[R_b["sel1"]])
        kb.op("dve", lambda e: e.tensor_tensor(out=R["ssel"][:], in0=R["sel1"][:], in1=R["s"][:], op=ALU.mult), r=[R_b["sel1"], R_b["s"]], w=[R_b["ssel"]])
        kb.op("dve", lambda e: e.tensor_reduce(out=S1["den"][:], in_=R["ssel"][:], axis=AX.X, op=ALU.add), r=[R_b["ssel"]], w=[S1_b["den"]])
        kb.op("dve", lambda e: e.reciprocal(S1["rden"][:], S1["den"][:]), r=[S1_b["den"]], w=[S1_b["rden"]])
        rden_bc = S1["rden"][:].rearrange("p (s o) -> p s o", o=1).to_broadcast([128, 4, 16])
        kb.op("dve", lambda e: e.tensor_tensor(out=R["gates"][:], in0=R["ssel"][:], in1=rden_bc, op=ALU.mult), r=[R_b["ssel"], S1_b["rden"]], w=[R_b["gates"]])
        for s in range(4):
            kb.op("pe", lambda e: e.transpose(pbank[5][0:16, s * 128:(s + 1) * 128], R["gates"][:, s, :], ident[:, :]), r=[R_b["gates"], ident_b], w=[pb[5]])
        kb.op("act", lambda e: e.copy(gatesT[:], pbank[5][0:16, :]), r=[pb[5]], w=[gatesT_b])
        for ex in range(16):
            mm(kb, pbank[4][:], sel[:, ex * 128:(ex + 1) * 128], gatesT[:], True, True, r=[sel_b, gatesT_b], w=[pb[4]])
            for j in range(4):
                def ld(st, sbuf_, ex=ex, j=j):
                    sv_ = st[:, :16 * 256].rearrange("p (c n) -> p c n", c=16)
                    src = wgu[ex].rearrange("(c p) n -> p c n", p=128)
                    kb.dma(sv_[:, :, 0:128], src[:, :, j * 128:(j + 1) * 128], w=[sbuf_], q="pool")
                    kb.dma(sv_[:, :, 128:256], src[:, :, 512 + j * 128:512 + (j + 1) * 128], w=[sbuf_], q="pool")
                wt, wtb = load_weight(ld, 16 * 256)
                pg, pu = (0, 1) if j % 2 == 0 else (2, 3)
                for k in range(16):
                    mm(kb, pbank[pg][:], wt[:, k * 256:k * 256 + 128], hT[:, k, :], k == 0, k == 15, r=[wtb, hT_b[k]], w=[pb[pg]])
                for k in range(16):
                    mm(kb, pbank[pu][:], wt[:, k * 256 + 128:k * 256 + 256], hT[:, k, :], k == 0, k == 15, r=[wtb, hT_b[k]], w=[pb[pu]])
                ti = nxt(tmp_i, 4)
                kb.op("act", lambda e: e.activation(out=tmp[ti][:], in_=pbank[pg][:], func=AF.Silu), r=[pb[pg]], w=[tmp_b[ti]])
                kb.op("dve", lambda e: e.tensor_tensor(out=tmp[ti][:], in0=tmp[ti][:], in1=pbank[pu][:], op=ALU.mult), r=[tmp_b[ti], pb[pu]], w=[tmp_b[ti]])
                kb.op("dve", lambda e: e.tensor_tensor(out=aT[:, j, :], in0=tmp[ti][:], in1=pbank[4][:], op=ALU.mult), r=[tmp_b[ti], pb[4]], w=[aT_b[j]])
            for mq in range(4):
                def ld(st, sbuf_, ex=ex, mq=mq):
                    kb.dma(st[:, :4 * 512].rearrange("p (c n) -> p c n", c=4), wd[ex].rearrange("(c p) n -> p c n", p=128)[:, :, mq * 512:(mq + 1) * 512], w=[sbuf_], q="pool")
                wt, wtb = load_weight(ld, 4 * 512)
                for mi in range(4):
                    m = mq * 4 + mi
                    pi = 6 + (m % 2)
                    for k in range(4):
                        mm(kb, pbank[pi][:], wt[:, k * 512 + mi * 128:k * 512 + (mi + 1) * 128], aT[:, k, :], k == 0, k == 3, r=[wtb, aT_b[k]], w=[pb[pi]])
                    if ex == 0:
                        kb.op("act", lambda e: e.copy(yacc[:, m, :], pbank[pi][:]), r=[pb[pi]], w=[yacc_b[m]])
                    else:
                        kb.op("dve", lambda e: e.tensor_tensor(out=yacc[:, m, :], in0=pbank[pi][:], in1=yacc[:, m, :], op=ALU.add), r=[pb[pi], yacc_b[m]], w=[yacc_b[m]])
        for c in range(16):
            kb.op("act", lambda e: e.activation(out=yacc[:, c, :], in_=yacc[:, c, :], func=AF.Identity, scale=pcol(3, c, True)), r=[yacc_b[c], pv1_b], w=[yacc_b[c]])
            kb.op("dve", lambda e: e.scalar_tensor_tensor(out=xt[:, c, :], in0=xt[:, c, :], scalar=DN_ALPHA, in1=yacc[:, c, :], op0=ALU.mult, op1=ALU.add),
                  r=[xt_b[c], yacc_b[c]], w=[xt_b[c]])
        layernorm(6, 7, False, out_dma_t0=t0)
    return kb.finish()


def build_ada():
    kb = KB()
    cT_d = kb.din("cT", [128, 16 * 4])
    w_d = kb.din("w", [2, D, 1536])
    b_d = kb.din("b", [128, 24])
    out_d = kb.dout("modT", [128, 24 * 4])
    cT = kb.sb([128, 64]); cT_b = Buf()
    bb = kb.sb([128, 24]); bb_b = Buf()
    ot = kb.sb([128, 96]); ot_b = Buf()
    stg = [kb.sb([128, 16, 128]) for _ in range(3)]; stg_b = bufs(3)
    ps = [kb.ps([128, 512]) for _ in range(2)]; ps_b = bufs(2)
    kb.dma(cT[:], cT_d[:, :], w=[cT_b])
    kb.dma(bb[:], b_d[:, :], w=[bb_b])
    kb.op("act", lambda e: e.activation(out=cT[:], in_=cT[:], func=AF.Silu), r=[cT_b], w=[cT_b])
    for l in range(2):
        for m in range(12):
            u = l * 12 + m
            si = u % 3
            kb.dma(stg[si][:], w_d[l].rearrange("(c p) n -> p c n", p=128)[:, :, m * 128:(m + 1) * 128], w=[stg_b[si]])
            pi = u % 2
            for k in range(16):
                mm(kb, ps[pi][:, 0:4], stg[si][:, k, :], cT[:, k * 4:(k + 1) * 4], k == 0, k == 15, r=[stg_b[si], cT_b], w=[ps_b[pi]])
            kb.op("dve", lambda e: e.tensor_scalar(out=ot[:, u * 4:(u + 1) * 4], in0=ps[pi][:, 0:4], scalar1=bb[:, u:u + 1], scalar2=None, op0=ALU.add),
                  r=[ps_b[pi], bb_b], w=[ot_b])
    kb.dma(out_d[:, :], ot[:], r=[ot_b], final=True)
    return kb.finish()


def run_ada(c, ada_w, ada_b):
    nc = build_ada()
    cT = np.ascontiguousarray(c.T.reshape(16, 128, 4).transpose(1, 0, 2).reshape(128, 64))
    in_maps = []
    for j in range(NCORES):
        w = np.ascontiguousarray(ada_w[:, :, j * 1536:(j + 1) * 1536])
        b = ada_b[:, j * 1536:(j + 1) * 1536].reshape(2, 12, 128).transpose(2, 0, 1).reshape(128, 24)
        in_maps.append({"cT": cT, "w": w, "b": np.ascontiguousarray(b)})
    res = run_bass_kernel_spmd(nc, in_maps, core_ids=list(range(NCORES)))
    mod = np.zeros((2, 4, 6 * D), np.float32)
    for j in range(NCORES):
        o = res.results[j]["modT"].reshape(128, 2, 12, 4)
        mod[:, :, j * 1536:(j + 1) * 1536] = o.transpose(1, 3, 2, 0).reshape(2, 4, 1536)
    return mod


def build_pre(ncol, ntok=2048):
    assert ncol % 128 == 0
    NM = ncol // 128
    kb = KB()
    NT = ntok // TT
    xT = kb.din("xT", [D, ntok]).rearrange("(c p) t -> p c t", p=128)
    w_d = kb.din("w", [D, ncol]).rearrange("(c p) n -> p c n", p=128)
    pv_d = kb.din("pvec", [128, 32])
    zT = kb.dout("zT", [ncol, ntok]).rearrange("(c p) t -> p c t", p=128)
    xt = [kb.sb([128, 16, TT]) for _ in range(2)]; xt_b = bufs(2)
    hT = [kb.sb([128, 16, TT], BF16) for _ in range(NT)]; hT_b = bufs(NT)
    stg = [kb.sb([128, 16, 128]) for _ in range(3)]; stg_b = bufs(3)
    wb = [kb.sb([128, 16, 128], BF16) for _ in range(3)]; wb_b = bufs(3)
    ot = [kb.sb([128, TT]) for _ in range(4)]; ot_b = bufs(4)
    pv = kb.sb([128, 32]); pv_b = Buf()
    pv1 = kb.sb([128, 32]); pv1_b = Buf()
    ps = [kb.ps([128, TT]) for _ in range(4)]; ps_b = bufs(4)
    kb.dma(pv[:], pv_d[:, :], w=[pv_b])
    kb.op("dve", lambda e: e.tensor_scalar_add(pv1[:], pv[:], 1.0), r=[pv_b], w=[pv1_b])
    for tt in range(NT):
        t0 = tt * TT
        xi = tt % 2
        kb.dma(xt[xi][:], xT[:, :, t0:t0 + TT], w=[xt_b[xi]])
        for c in range(16):
            kb.op("act", lambda e: e.activation(out=hT[tt][:, c, :], in_=xt[xi][:, c, :], func=AF.Identity, scale=pv1[:, c:c + 1], bias=pv[:, 16 + c:17 + c]),
                  r=[xt_b[xi], pv_b, pv1_b], w=[hT_b[tt]])
    u = 0
    for m in range(NM):
        si = m % 3
        kb.dma(wb[si][:], w_d[:, :, m * 128:(m + 1) * 128], w=[wb_b[si]], q="pool")
        for tt in range(NT):
            t0 = tt * TT
            pi = u % 4
            for k in range(16):
                mm(kb, ps[pi][:], wb[si][:, k, :], hT[tt][:, k, :], k == 0, k == 15, r=[wb_b[si], hT_b[tt]], w=[ps_b[pi]])
            if u % 2 == 0:
                kb.op("act", lambda e: e.copy(ot[pi][:], ps[pi][:]), r=[ps_b[pi]], w=[ot_b[pi]])
            else:
                kb.op("dve", lambda e: e.tensor_copy(out=ot[pi][:], in_=ps[pi][:]), r=[ps_b[pi]], w=[ot_b[pi]])
            kb.dma(zT[:, m, t0:t0 + TT], ot[pi][:], r=[ot_b[pi]], final=True, q="act" if u % 2 == 0 else "sp")
            u += 1
    return kb.finish()


def fm16(v):
    return np.ascontiguousarray(v.reshape(16, 128).T)


def run_pre(x_tok_major, mod_l, w, sc_idx, sh_idx):
    ncol = w.shape[1]
    nc = build_pre(ncol)
    in_maps = []
    for j in range(NCORES):
        b, hf = j // 2, j % 2
        xT = np.ascontiguousarray(x_tok_major[b, hf * 2048:(hf + 1) * 2048].T)
        sc = mod_l[b, sc_idx * D:(sc_idx + 1) * D]
        sh = mod_l[b, sh_idx * D:(sh_idx + 1) * D]
        in_maps.append({"xT": xT, "w": w, "pvec": np.ascontiguousarray(np.concatenate([fm16(sc), fm16(sh)], 1))})
    res = run_bass_kernel_spmd(nc, in_maps, core_ids=list(range(NCORES)))
    z = np.zeros((4, ncol, 4096), np.float32)
    for j in range(NCORES):
        b, hf = j // 2, j % 2
        z[b, :, hf * 2048:(hf + 1) * 2048] = res.results[j]["zT"]
    return z


SEQ = 4096
MLA_SCALE = 192.0 ** -0.5


def build_mla(S=SEQ):
    kb = KB()
    NT = S // TT
    zq = kb.din("zq", [512, S]).rearrange("(c p) t -> p c t", p=128)
    zkv = kb.din("zkv", [256, S]).rearrange("(c p) t -> p c t", p=128)
    zkr = kb.din("zkr", [128, S]).rearrange("(c p) t -> p c t", p=64)
    cs_d = kb.din("cs", [128, S]).rearrange("(c p) t -> p c t", p=64)
    wq_d = kb.din("wq", [512, 1024]).rearrange("(c p) n -> p c n", p=128)
    wk_d = kb.din("wk", [256, 512]).rearrange("(c p) n -> p c n", p=128)
    wv_d = kb.din("wv", [256, 512]).rearrange("(c p) n -> p c n", p=128)
    g_d = kb.din("g", [128, 6])
    mask_d = kb.din("mask", [128, 4 * TT])
    attT = kb.dout("attT", [512, S]).rearrange("(c p) t -> p c t", p=128)

    qnope = [kb.sb([128, S], BF16) for _ in range(4)]; qnope_b = [bufs(NT) for _ in range(4)]
    qrope = [kb.sb([128, S], BF16) for _ in range(2)]; qrope_b = [bufs(NT) for _ in range(2)]
    knope = [kb.sb([128, S], BF16) for _ in range(4)]; knope_b = [bufs(NT) for _ in range(4)]
    krope = kb.sb([128, S], BF16); krope_b = bufs(NT)
    V = kb.sb([128, S // 128, 512], BF16); V_b = bufs(NT)
    wq = kb.sb([128, 4, 1024], BF16); wk = kb.sb([128, 2, 512], BF16); wv = kb.sb([128, 2, 512], BF16); w_b = Buf()
    g = kb.sb([128, 6]); g_b = Buf()
    mask = kb.sb([128, 4 * TT], BF16); mask_b = Buf()
    ones = kb.sb([128, 128]); ones_b = Buf()
    onesb = kb.sb([128, 128], BF16); onesb_b = Buf()
    stg = kb.sb([128, 2048]); stg_b = Buf()
    zin = [kb.sb([128, 6, TT]) for _ in range(1)]; zin_b = bufs(1)
    zr = [kb.sb([128, 4, TT]) for _ in range(1)]; zr_b = bufs(1)
    qn = kb.sb([128, 6, TT], BF16); qn_b = bufs(6)
    tmp = [kb.sb([128, TT]) for _ in range(4)]; tmp_b = bufs(4)
    rs = [kb.sb([128, TT]) for _ in range(2)]; rs_b = bufs(2)
    pT = [kb.sb([128, TT], BF16) for _ in range(3)]; pT_b = bufs(3)
    ot = [kb.sb([128, TT]) for _ in range(2)]; ot_b = bufs(2)
    ps = [kb.ps([128, TT]) for _ in range(8)]; pb = bufs(8)
    tmp_i = [0]

    def nxt(ctr, n):
        i = ctr[0] % n
        ctr[0] += 1
        return i

    kb.dma(g[:], g_d[:, :], w=[g_b])
    kb.op("dve", lambda e: e.memset(ones[:], 1.0), w=[ones_b])
    kb.op("dve", lambda e: e.memset(onesb[:], 1.0), w=[onesb_b])
    kb.dma(stg[:, :2048], mask_d[:, :], w=[stg_b])
    kb.op("dve", lambda e: e.tensor_copy(out=mask[:], in_=stg[:, :2048]), r=[stg_b], w=[mask_b])
    for hh in range(2):
        kb.dma(stg[:].rearrange("p (c n) -> p c n", c=2), wq_d[:, 2 * hh:2 * hh + 2, :], w=[stg_b])
        kb.op("dve", lambda e: e.tensor_copy(out=wq[:, 2 * hh:2 * hh + 2, :].rearrange("p c n -> p (c n)"), in_=stg[:]), r=[stg_b, w_b], w=[w_b])
    kb.dma(stg[:, :1024].rearrange("p (c n) -> p c n", c=2), wk_d[:, :, :], w=[stg_b])
    kb.op("dve", lambda e: e.tensor_copy(out=wk[:].rearrange("p c n -> p (c n)"), in_=stg[:, :1024]), r=[stg_b, w_b], w=[w_b])
    kb.dma(stg[:, :1024].rearrange("p (c n) -> p c n", c=2), wv_d[:, :, :], w=[stg_b])
    kb.op("dve", lambda e: e.tensor_copy(out=wv[:].rearrange("p c n -> p (c n)"), in_=stg[:, :1024]), r=[stg_b, w_b], w=[w_b])

    for tt in range(NT):
        t0 = tt * TT
        zi = 0
        kb.dma(zin[zi][:, 0:4, :], zq[:, :, t0:t0 + TT], w=[zin_b[zi]])
        kb.dma(zin[zi][:, 4:6, :], zkv[:, :, t0:t0 + TT], w=[zin_b[zi]])
        for hp in range(2):
            kb.dma(zr[zi][hp * 64:(hp + 1) * 64, 0:2, :], zkr[:, :, t0:t0 + TT], w=[zr_b[zi]])
            kb.dma(zr[zi][hp * 64:(hp + 1) * 64, 2:4, :], cs_d[:, :, t0:t0 + TT], w=[zr_b[zi]])
        for (c0, c1, pi, dim, eps, ri) in [(0, 4, 6, 512, 1e-6, 0), (4, 6, 7, 256, 1e-6, 1)]:
            for c in range(c0, c1):
                ti = nxt(tmp_i, 4)
                kb.op("act", lambda e: e.activation(out=tmp[ti][:], in_=zin[zi][:, c, :], func=AF.Square), r=[zin_b[zi]], w=[tmp_b[ti]])
                mm(kb, ps[pi][:], ones[:], tmp[ti][:], c == c0, c == c1 - 1, r=[ones_b, tmp_b[ti]], w=[pb[pi]])
            kb.op("dve", lambda e: e.tensor_scalar(out=rs[ri][:], in0=ps[pi][:], scalar1=1.0 / dim, scalar2=eps, op0=ALU.mult, op1=ALU.add), r=[pb[pi]], w=[rs_b[ri]])
            kb.op("act", lambda e: e.sqrt(rs[ri][:], rs[ri][:]), r=[rs_b[ri]], w=[rs_b[ri]])
            kb.op("dve", lambda e: e.reciprocal(rs[ri][:], rs[ri][:]), r=[rs_b[ri]], w=[rs_b[ri]])
            for c in range(c0, c1):
                kb.op("dve", lambda e: e.scalar_tensor_tensor(out=qn[:, c, :], in0=zin[zi][:, c, :], scalar=g[:, c:c + 1], in1=rs[ri][:], op0=ALU.mult, op1=ALU.mult),
                      r=[zin_b[zi], g_b, rs_b[ri]], w=[qn_b[c]])
        for h in range(4):
            for k in range(4):
                mm(kb, ps[0][:], wq[:, k, h * 128:(h + 1) * 128], qn[:, k, :], k == 0, k == 3, r=[w_b, qn_b[k]], w=[pb[0]])
            kb.op("act", lambda e: e.copy(qnope[h][:, t0:t0 + TT], ps[0][:]), r=[pb[0]], w=[qnope_b[h][tt]])
            for k in range(2):
                mm(kb, ps[3][:], wk[:, k, h * 128:(h + 1) * 128], qn[:, 4 + k, :], k == 0, k == 1, r=[w_b, qn_b[4 + k]], w=[pb[3]])
            kb.op("act", lambda e: e.copy(knope[h][:, t0:t0 + TT], ps[3][:]), r=[pb[3]], w=[knope_b[h][tt]])
        for hp in range(2):
            for k in range(4):
                mm(kb, ps[1][:], wq[:, k, 512 + hp * 128:512 + (hp + 1) * 128], qn[:, k, :], k == 0, k == 3, r=[w_b, qn_b[k]], w=[pb[1]])
            for k in range(4):
                mm(kb, ps[2][:], wq[:, k, 768 + hp * 128:768 + (hp + 1) * 128], qn[:, k, :], k == 0, k == 3, r=[w_b, qn_b[k]], w=[pb[2]])
            t1 = nxt(tmp_i, 4)
            kb.op("dve", lambda e: e.tensor_tensor(out=tmp[t1][:], in0=ps[1][:], in1=zr[zi][:, 2, :], op=ALU.mult), r=[pb[1], zr_b[zi]], w=[tmp_b[t1]])
            t2 = nxt(tmp_i, 4)
            kb.op("dve", lambda e: e.tensor_tensor(out=tmp[t2][:], in0=ps[2][:], in1=zr[zi][:, 3, :], op=ALU.mult), r=[pb[2], zr_b[zi]], w=[tmp_b[t2]])
            kb.op("dve", lambda e: e.tensor_tensor(out=qrope[hp][:, t0:t0 + TT], in0=tmp[t1][:], in1=tmp[t2][:], op=ALU.add),
                  r=[tmp_b[t1], tmp_b[t2]], w=[qrope_b[hp][tt]])
        for blk in range(4):
            pi = 4 + blk % 2
            for k in range(2):
                mm(kb, ps[pi][:], qn[:, 4 + k, blk * 128:(blk + 1) * 128], wv[:, k, :], k == 0, k == 1, r=[w_b, qn_b[4 + k]], w=[pb[pi]])
            kb.op("act", lambda e: e.copy(V[:, tt * 4 + blk, :], ps[pi][:]), r=[pb[pi]], w=[V_b[tt]])
        t1 = nxt(tmp_i, 4)
        kb.op("dve", lambda e: e.tensor_tensor(out=tmp[t1][:], in0=zr[zi][:, 0, :], in1=zr[zi][:, 2, :], op=ALU.mult), r=[zr_b[zi]], w=[tmp_b[t1]])
        t2 = nxt(tmp_i, 4)
        kb.op("dve", lambda e: e.tensor_tensor(out=tmp[t2][:], in0=zr[zi][:, 1, :], in1=zr[zi][:, 3, :], op=ALU.mult), r=[zr_b[zi]], w=[tmp_b[t2]])
        kb.op("dve", lambda e: e.tensor_tensor(out=krope[:, t0:t0 + TT], in0=tmp[t1][:], in1=tmp[t2][:], op=ALU.add), r=[tmp_b[t1], tmp_b[t2]], w=[krope_b[tt]])

    sbanks = [0, 1, 2]
    cnt = [0]
    for h in range(4):
        for qb in range(NT):
            q0 = qb * TT
            nkb = 4 * qb + 4
            po, pd = (3, 4) if (h * NT + qb) % 2 == 0 else (5, 6)

            def qk(kk):
                sb_ = sbanks[kk % 3]
                kt = kk // 4
                mm(kb, ps[sb_][:], knope[h][:, kk * 128:(kk + 1) * 128], qnope[h][:, q0:q0 + TT], True, False,
                   r=[knope_b[h][kt], qnope_b[h][qb]], w=[pb[sb_]])
                ph = (h % 2) * 64
                mm(kb, ps[sb_][:], krope[ph:ph + 64, kk * 128:(kk + 1) * 128], qrope[h // 2][ph:ph + 64, q0:q0 + TT], False, True,
                   r=[krope_b[kt], qrope_b[h // 2][qb]], w=[pb[sb_]])
            qk(0)
            for kk in range(nkb):
                if kk + 1 < nkb:
                    qk(kk + 1)
                sb_ = sbanks[kk % 3]
                pi = cnt[0] % 3
                cnt[0] += 1
                kb.op("act", lambda e: e.activation(out=pT[pi][:], in_=ps[sb_][:], func=AF.Exp, scale=MLA_SCALE), r=[pb[sb_]], w=[pT_b[pi]])
                j = kk - 4 * qb
                if j >= 0:
                    kb.op("dve", lambda e: e.tensor_tensor(out=pT[pi][:], in0=pT[pi][:], in1=mask[:, j * TT:(j + 1) * TT], op=ALU.mult), r=[pT_b[pi], mask_b], w=[pT_b[pi]])
                mm(kb, ps[po][:], V[:, kk, h * 128:(h + 1) * 128], pT[pi][:], kk == 0, kk == nkb - 1, r=[V_b[kk // 4], pT_b[pi]], w=[pb[po]])
                mm(kb, ps[pd][:], onesb[:], pT[pi][:], kk == 0, kk == nkb - 1, r=[onesb_b, pT_b[pi]], w=[pb[pd]])
            oi = (h * NT + qb) % 2
            ti = nxt(tmp_i, 4)
            kb.op("dve", lambda e: e.reciprocal(tmp[ti][:], ps[pd][:]), r=[pb[pd]], w=[tmp_b[ti]])
            kb.op("dve", lambda e: e.tensor_tensor(out=ot[oi][:], in0=ps[po][:], in1=tmp[ti][:], op=ALU.mult), r=[pb[po], tmp_b[ti]], w=[ot_b[oi]])
            kb.dma(attT[:, h, q0:q0 + TT], ot[oi][:], r=[ot_b[oi]], final=True)
    return kb.finish()


def rope_tables(S=SEQ):
    inv = 10000.0 ** (-np.arange(0, 64, 2, dtype=np.float32) / 64)
    ang = np.arange(S, dtype=np.float32)[None, :] * inv[:, None]
    cos, sin = np.cos(ang).astype(np.float32), np.sin(ang).astype(np.float32)
    return np.ascontiguousarray(np.concatenate([cos, cos, -sin, sin], 0))


def causal_masks():
    m = np.zeros((4, 128, TT), np.float32)
    k = np.arange(128)[:, None]
    q = np.arange(TT)[None, :]
    for j in range(4):
        m[j] = (q >= k + 128 * j)
    return np.ascontiguousarray(m.transpose(1, 0, 2).reshape(128, 4 * TT))


def run_mla(z0, w_uq, w_ukv, q_norm, kv_norm):
    nc = build_mla()
    cs = rope_tables()
    mask = causal_masks()
    g = np.ascontiguousarray(np.concatenate([q_norm.reshape(4, 128).T, kv_norm.reshape(2, 128).T], 1))
    in_maps = []
    for j in range(NCORES):
        b, hf = j // 2, j % 2
        wq_cols, wk_cols, wv_cols = [], [], []
        hs = list(range(4 * hf, 4 * hf + 4))
        for h in hs:
            wq_cols.append(w_uq[:, h * 192:h * 192 + 128])
            wk_cols.append(w_ukv[:, h * 256:h * 256 + 128])
            wv_cols.append(w_ukv[:, h * 256 + 128:h * 256 + 256])
        for h in hs:
            wq_cols.append(w_uq[:, h * 192 + 128:h * 192 + 192])
        for h in hs:
            wq_cols += [w_uq[:, h * 192 + 160:h * 192 + 192], w_uq[:, h * 192 + 128:h * 192 + 160]]
        in_maps.append({
            "zq": np.ascontiguousarray(z0[b, 0:512]), "zkv": np.ascontiguousarray(z0[b, 512:768]),
            "zkr": np.ascontiguousarray(np.concatenate([z0[b, 768:832], z0[b, 1856:1920]], 0)),
            "cs": cs, "wq": np.ascontiguousarray(np.concatenate(wq_cols, 1)), "wk": np.ascontiguousarray(np.concatenate(wk_cols, 1)),
            "wv": np.ascontiguousarray(np.concatenate(wv_cols, 1)), "g": g, "mask": mask})
    res = run_bass_kernel_spmd(nc, in_maps, core_ids=list(range(NCORES)))
    att = np.zeros((4, 1024, SEQ), np.float32)
    for j in range(NCORES):
        b, hf = j // 2, j % 2
        att[b, hf * 512:(hf + 1) * 512] = res.results[j]["attT"]
    return att


TWO_PI = 6.283185307179586
NG = 32


def build_s5(S=SEQ):
    kb = KB()
    NT = S // TT
    uT = kb.din("uT", [NG * 16, S])
    lamre_d = kb.din("lamre", [128, NG]); lamim_d = kb.din("lamim", [128, NG]); logdt_d = kb.din("logdt", [128, NG])
    bt_d = kb.din("bt", [16, NG * 128]); btsw_d = kb.din("btsw", [16, NG * 128])
    ca_d = kb.din("ca", [128, NG * 16]); cb_d = kb.din("cb", [128, NG * 16])
    d_d = kb.din("dsk", [16, NG])
    iota_d = kb.din("iota", [128, S])
    yT = kb.dout("yT", [NG * 16, S])

    def t32(name=None):
        return kb.sb([128, NG], name=name)
    lamre, lamim, dt_, r_, th, cth, sth, nre, nim, den, fre, fim, tA, tB = [t32() for _ in range(14)]
    s1, s2, s3, s4 = [t32() for _ in range(4)]
    prm_b = Buf()
    sgn = kb.sb([128, 1]); negpi = kb.sb([128, 1]); ki = kb.sb([128, NG], mybir.dt.int32)
    KI = kb.sb([128, S], mybir.dt.int32); KI_b = Buf()
    bt = kb.sb([16, NG * 128], BF16); btsw = kb.sb([16, NG * 128], BF16); bstg = kb.sb([16, NG * 128]); bt_b = Buf(); bstg_b = Buf()
    ca = kb.sb([128, NG, 16]); cb = kb.sb([128, NG, 16]); cstage = kb.sb([128, NG, 16]); L1 = kb.sb([128, NG, 16], BF16); L2 = kb.sb([128, NG, 16], BF16); c_b = Buf()
    dsk = kb.sb([16, NG]); dsk_b = Buf()
    iota = kb.sb([128, S]); iota_b = Buf()
    u32 = kb.sb([16, S]); u32_b = Buf()
    ubf = kb.sb([16, S], BF16); ubf_b = Buf()
    A1 = kb.sb([128, S]); A1_b = Buf()
    A2 = kb.sb([128, S]); A2_b = Buf()
    T2 = kb.sb([128, S]); T2_b = Buf()
    bz = kb.sb([128, S]); bz_b = bufs(NT); z_b = Buf()
    Zc = kb.sb([128, S], BF16); Zc_b = Buf()
    Zs = kb.sb([128, S], BF16); Zs_b = Buf()
    ysb = kb.sb([16, S]); ysb_b = Buf()
    tmp = [kb.sb([128, TT]) for _ in range(4)]; tmp_b = bufs(4)
    ps = [kb.ps([128, TT]) for _ in range(8)]; pb = bufs(8)

    P = [prm_b]
    kb.dma(lamre[:], lamre_d[:, :], w=P)
    kb.dma(lamim[:], lamim_d[:, :], w=P)
    kb.dma(dt_[:], logdt_d[:, :], w=P)
    kb.dma(iota[:], iota_d[:, :], w=[iota_b])
    kb.dma(dsk[:], d_d[:, :], w=[dsk_b])
    kb.dma(bstg[:], bt_d[:, :], w=[bstg_b])
    kb.op("dve", lambda e: e.tensor_copy(out=bt[:], in_=bstg[:]), r=[bstg_b], w=[bt_b])
    kb.dma(bstg[:], btsw_d[:, :], w=[bstg_b])
    kb.op("dve", lambda e: e.tensor_copy(out=btsw[:], in_=bstg[:]), r=[bstg_b, bt_b], w=[bt_b])
    kb.dma(ca[:].rearrange("p g c -> p (g c)"), ca_d[:, :], w=[c_b])
    kb.dma(cb[:].rearrange("p g c -> p (g c)"), cb_d[:, :], w=[c_b])
    V = lambda fn: kb.op("dve", fn, r=P, w=P)
    A = lambda fn: kb.op("act", fn, r=P, w=P)
    V(lambda e: e.memset(sgn[0:64, :], 1.0))
    V(lambda e: e.memset(sgn[64:128, :], -1.0))
    V(lambda e: e.memset(negpi[:], -3.141592653589793))
    A(lambda e: e.activation(out=dt_[:], in_=dt_[:], func=AF.Exp))
    V(lambda e: e.tensor_tensor(out=r_[:], in0=lamre[:], in1=dt_[:], op=ALU.mult))
    A(lambda e: e.activation(out=r_[:], in_=r_[:], func=AF.Exp))
    V(lambda e: e.tensor_tensor(out=th[:], in0=lamim[:], in1=dt_[:], op=ALU.mult))
    V(lambda e: e.tensor_single_scalar(out=th[:], in_=th[:], scalar=1.0 / TWO_PI, op=ALU.mult))
    V(lambda e: e.tensor_copy(out=ki[:], in_=th[:]))
    V(lambda e: e.tensor_tensor(out=th[:], in0=th[:], in1=ki[:], op=ALU.subtract))
    A(lambda e: e.activation(out=sth[:], in_=th[:], func=AF.Sin, scale=TWO_PI))
    V(lambda e: e.tensor_single_scalar(out=tA[:], in_=th[:], scalar=0.25, op=ALU.add))
    V(lambda e: e.tensor_copy(out=ki[:], in_=tA[:]))
    V(lambda e: e.tensor_tensor(out=tA[:], in0=tA[:], in1=ki[:], op=ALU.subtract))
    A(lambda e: e.activation(out=cth[:], in_=tA[:], func=AF.Sin, scale=TWO_PI))
    V(lambda e: e.tensor_tensor(out=nre[:], in0=r_[:], in1=cth[:], op=ALU.mult))
    V(lambda e: e.tensor_single_scalar(out=nre[:], in_=nre[:], scalar=-1.0, op=ALU.add))
    V(lambda e: e.tensor_tensor(out=nim[:], in0=r_[:], in1=sth[:], op=ALU.mult))
    V(lambda e: e.tensor_tensor(out=den[:], in0=lamre[:], in1=lamre[:], op=ALU.mult))
    V(lambda e: e.tensor_tensor(out=tA[:], in0=lamim[:], in1=lamim[:], op=ALU.mult))
    V(lambda e: e.tensor_tensor(out=den[:], in0=den[:], in1=tA[:], op=ALU.add))
    V(lambda e: e.reciprocal(den[:], den[:]))
    V(lambda e: e.tensor_tensor(out=fre[:], in0=nre[:], in1=lamre[:], op=ALU.mult))
    V(lambda e: e.tensor_tensor(out=tA[:], in0=nim[:], in1=lamim[:], op=ALU.mult))
    V(lambda e: e.tensor_tensor(out=fre[:], in0=fre[:], in1=tA[:], op=ALU.add))
    V(lambda e: e.tensor_tensor(out=fre[:], in0=fre[:], in1=den[:], op=ALU.mult))
    V(lambda e: e.tensor_tensor(out=fim[:], in0=nim[:], in1=lamre[:], op=ALU.mult))
    V(lambda e: e.tensor_tensor(out=tA[:], in0=nre[:], in1=lamim[:], op=ALU.mult))
    V(lambda e: e.tensor_tensor(out=fim[:], in0=fim[:], in1=tA[:], op=ALU.subtract))
    V(lambda e: e.tensor_tensor(out=fim[:], in0=fim[:], in1=den[:], op=ALU.mult))
    V(lambda e: e.tensor_scalar(out=s1[:], in0=fre[:], scalar1=sgn[:, 0:1], scalar2=None, op0=ALU.mult))
    V(lambda e: e.tensor_single_scalar(out=s2[:], in_=fim[:], scalar=-1.0, op=ALU.mult))
    V(lambda e: e.tensor_scalar(out=s3[:], in0=s2[:], scalar1=sgn[:, 0:1], scalar2=None, op0=ALU.mult))
    V(lambda e: e.tensor_single_scalar(out=s4[:], in_=fre[:], scalar=-1.0, op=ALU.mult))

    def bc(t):
        return t[:].rearrange("p (g o) -> p g o", o=1).to_broadcast([128, NG, 16])
    PC = [prm_b, c_b]
    kb.op("dve", lambda e: e.tensor_tensor(out=cstage[:], in0=ca[:], in1=bc(s1), op=ALU.mult), r=PC, w=PC)
    kb.op("dve", lambda e: e.tensor_tensor(out=ca[:], in0=ca[:], in1=bc(s3), op=ALU.mult), r=PC, w=PC)
    kb.op("dve", lambda e: e.tensor_tensor(out=tmp[0][:, :NG * 16].rearrange("p (g c) -> p g c", c=16), in0=cb[:], in1=bc(s2), op=ALU.mult), r=PC, w=PC + [tmp_b[0]])
    kb.op("dve", lambda e: e.tensor_tensor(out=L1[:], in0=cstage[:], in1=tmp[0][:, :NG * 16].rearrange("p (g c) -> p g c", c=16), op=ALU.add), r=PC + [tmp_b[0]], w=PC)
    kb.op("dve", lambda e: e.tensor_tensor(out=cb[:], in0=cb[:], in1=bc(s4), op=ALU.mult), r=PC, w=PC)
    kb.op("dve", lambda e: e.tensor_tensor(out=L2[:], in0=ca[:], in1=cb[:], op=ALU.add), r=PC, w=PC)

    tmp_i = [1]
    for g in range(NG):
        kb.dma(u32[:], uT[g * 16:(g + 1) * 16, :], w=[u32_b])
        kb.op("act", lambda e: e.copy(ubf[:], u32[:]), r=[u32_b], w=[ubf_b])
        kb.op("dve", lambda e: e.tensor_scalar(out=KI[:], in0=iota[:], scalar1=th[:, g:g + 1], scalar2=None, op0=ALU.mult), r=[iota_b, prm_b], w=[KI_b])
        kb.op("dve", lambda e: e.scalar_tensor_tensor(out=A1[:], in0=iota[:], scalar=th[:, g:g + 1], in1=KI[:], op0=ALU.mult, op1=ALU.subtract), r=[iota_b, prm_b, KI_b], w=[A1_b])
        kb.op("dve", lambda e: e.tensor_single_scalar(out=A2[:], in_=A1[:], scalar=0.25, op=ALU.add), r=[A1_b], w=[A2_b])
        kb.op("dve", lambda e: e.tensor_copy(out=KI[:], in_=A2[:]), r=[A2_b], w=[KI_b])
        kb.op("dve", lambda e: e.tensor_tensor(out=A2[:], in0=A2[:], in1=KI[:], op=ALU.subtract), r=[A2_b, KI_b], w=[A2_b])
        kb.op("act", lambda e: e.activation(out=A1[:], in_=A1[:], func=AF.Sin, scale=TWO_PI), r=[A1_b], w=[A1_b])
        kb.op("act", lambda e: e.activation(out=A2[:], in_=A2[:], func=AF.Sin, scale=TWO_PI), r=[A2_b], w=[A2_b])
        kb.op("act", lambda e: e.activation(out=T2[:], in_=A1[:], func=AF.Identity, scale=sgn[:, 0:1]), r=[A1_b, prm_b], w=[T2_b])
        for tt in range(NT):
            t0 = tt * TT
            pa, pbk = (0, 1) if tt % 2 == 0 else (2, 3)
            mm(kb, ps[pa][:], bt[:, g * 128:(g + 1) * 128], ubf[:, t0:t0 + TT], True, True, r=[bt_b, ubf_b], w=[pb[pa]])
            mm(kb, ps[pbk][:], btsw[:, g * 128:(g + 1) * 128], ubf[:, t0:t0 + TT], True, True, r=[bt_b, ubf_b], w=[pb[pbk]])
            t1 = tmp_i[0] % 4; tmp_i[0] += 1
            kb.op("dve", lambda e: e.tensor_tensor(out=tmp[t1][:], in0=ps[pa][:], in1=A2[:, t0:t0 + TT], op=ALU.mult), r=[pb[pa], A2_b], w=[tmp_b[t1]])
            t2 = tmp_i[0] % 4; tmp_i[0] += 1
            kb.op("dve", lambda e: e.tensor_tensor(out=tmp[t2][:], in0=ps[pbk][:], in1=T2[:, t0:t0 + TT], op=ALU.mult), r=[pb[pbk], T2_b], w=[tmp_b[t2]])
            kb.op("dve", lambda e: e.tensor_tensor(out=bz[:, t0:t0 + TT], in0=tmp[t1][:], in1=tmp[t2][:], op=ALU.add), r=[tmp_b[t1], tmp_b[t2], z_b], w=[bz_b[tt]])
        kb.op("dve", lambda e: e.tensor_tensor_scan(out=bz[:], data0=r_[:, g:g + 1].to_broadcast([128, S]), data1=bz[:], initial=0.0, op0=ALU.mult, op1=ALU.add),
              r=bz_b + [prm_b], w=bz_b + [z_b])
        kb.op("dve", lambda e: e.tensor_tensor(out=Zc[:], in0=bz[:], in1=A2[:], op=ALU.mult), r=[z_b, A2_b], w=[Zc_b])
        kb.op("dve", lambda e: e.tensor_tensor(out=Zs[:], in0=bz[:], in1=A1[:], op=ALU.mult), r=[z_b, A1_b], w=[Zs_b])
        for tt in range(NT):
            t0 = tt * TT
            pi = 4 + tt % 4
            mm(kb, ps[pi][0:16, :], L1[:, g, :], Zc[:, t0:t0 + TT], True, False, r=[c_b, Zc_b], w=[pb[pi]])
            mm(kb, ps[pi][0:16, :], L2[:, g, :], Zs[:, t0:t0 + TT], False, True, r=[c_b, Zs_b], w=[pb[pi]])
            kb.op("dve", lambda e: e.scalar_tensor_tensor(out=ysb[:, t0:t0 + TT], in0=u32[:, t0:t0 + TT], scalar=dsk[:, g:g + 1], in1=ps[pi][0:16, :], op0=ALU.mult, op1=ALU.add),
                  r=[u32_b, dsk_b, pb[pi]], w=[ysb_b])
        kb.dma(yT[g * 16:(g + 1) * 16, :], ysb[:], r=[ysb_b], final=True)
    return kb.finish()


def run_s5(z0, lam_re, lam_im, b_re, b_im, c_re, c_im, d_skip, log_dt):
    nc = build_s5()
    iota = np.ascontiguousarray(np.broadcast_to(np.arange(SEQ, dtype=np.float32), (128, SEQ)))
    in_maps = []
    for j in range(NCORES):
        b, hf = j // 2, j % 2
        gs = slice(hf * NG, (hf + 1) * NG)
        lre = lam_re[gs].T; lim = lam_im[gs].T
        bre = b_re[gs].transpose(2, 0, 1); bim = b_im[gs].transpose(2, 0, 1)
        cre = c_re[gs].transpose(2, 0, 1); cim = c_im[gs].transpose(2, 0, 1)
        in_maps.append({
            "uT": np.ascontiguousarray(z0[b, 832 + hf * 512:832 + (hf + 1) * 512]),
            "lamre": np.ascontiguousarray(np.concatenate([lre, lre], 0)), "lamim": np.ascontiguousarray(np.concatenate([lim, lim], 0)),
            "logdt": np.ascontiguousarray(np.broadcast_to(log_dt[gs][None, :], (128, NG))),
            "bt": np.ascontiguousarray(np.concatenate([bre, bim], 2).reshape(16, NG * 128)),
            "btsw": np.ascontiguousarray(np.concatenate([bim, bre], 2).reshape(16, NG * 128)),
            "ca": np.ascontiguousarray(np.concatenate([cre, cim], 0).reshape(128, NG * 16)),
            "cb": np.ascontiguousarray(np.concatenate([cim, cre], 0).reshape(128, NG * 16)),
            "dsk": np.ascontiguousarray(d_skip[gs].T), "iota": iota})
    res = run_bass_kernel_spmd(nc, in_maps, core_ids=list(range(NCORES)))
    y = np.zeros((4, 1024, SEQ), np.float32)
    for j in range(NCORES):
        b, hf = j // 2, j % 2
        y[b, hf * 512:(hf + 1) * 512] = res.results[j]["yT"]
    return y


DILS = (1, 4, 16)


def build_dil(S=SEQ):
    kb = KB()
    NB = S // 128
    q_d = kb.din("q", [12, 64, S]); k_d = kb.din("k", [12, 64, S])
    v_d = kb.din("v", [12, 128, NB * 64])
    bm_d = kb.din("bm", [12, 128, 1024])
    attT = kb.dout("attT", [256, S])
    stg = [kb.sb([128, S]) for _ in range(2)]; stg_b = bufs(2)
    qs = [kb.sb([64, S], BF16) for _ in range(2)]; qs_b = bufs(2)
    ks = [kb.sb([64, S], BF16) for _ in range(2)]; ks_b = bufs(2)
    vs = [kb.sb([128, NB * 64], BF16) for _ in range(2)]; vs_b = bufs(2)
    eb = [kb.sb([128, 1024], BF16) for _ in range(2)]; eb_b = bufs(2)
    eb0 = [kb.sb([128, 512], BF16) for _ in range(2)]; eb0_b = bufs(2)
    num = kb.sb([64, S]); num_b = Buf()
    den = kb.sb([64, S]); den_b = Buf()
    onesb = kb.sb([128, 64], BF16); onesb_b = Buf()
    pT = [kb.sb([128, 512], BF16) for _ in range(4)]; pT_b = bufs(4)
    ps = [kb.ps([128, TT]) for _ in range(8)]; pb = bufs(8)
    kb.op("dve", lambda e: e.memset(onesb[:], 1.0), w=[onesb_b])
    u = 0
    pti = 0
    for h in range(4):
        for gi, d in enumerate(DILS):
            inst = gi * 4 + h
            bi = u % 2
            nb = NB // d
            kb.dma(stg[0][0:64, :], q_d[inst], w=[stg_b[0]])
            kb.op("pool", lambda e: e.tensor_copy(out=qs[bi][:], in_=stg[0][0:64, :]), r=[stg_b[0]], w=[qs_b[bi]])
            kb.dma(stg[1][0:64, :], k_d[inst], w=[stg_b[1]])
            kb.op("pool", lambda e: e.tensor_copy(out=ks[bi][:], in_=stg[1][0:64, :]), r=[stg_b[1]], w=[ks_b[bi]])
            kb.dma(stg[0][:, :NB * 64], v_d[inst], w=[stg_b[0]])
            kb.op("pool", lambda e: e.tensor_copy(out=vs[bi][:], in_=stg[0][:, :NB * 64]), r=[stg_b[0]], w=[vs_b[bi]])
            kb.dma(stg[1][:, :1024], bm_d[inst], w=[stg_b[1]])
            kb.op("act", lambda e: e.activation(out=eb[bi][:], in_=stg[1][:, :1024], func=AF.Exp), r=[stg_b[1]], w=[eb_b[bi]])
            kb.op("dve", lambda e: e.tensor_copy(out=eb0[bi][:], in_=eb[bi][:, 512:1024]), r=[eb_b[bi]], w=[eb0_b[bi]])
            kb.op("dve", lambda e: e.memset(eb0[bi][:, 0:128], 0.0), r=[eb0_b[bi]], w=[eb0_b[bi]])
            if d == 16:
                kb.op("dve", lambda e: e.memset(eb0[bi][:, 256:384], 0.0), r=[eb0_b[bi]], w=[eb0_b[bi]])
            for bt in range(NB // 4):
                B0 = bt * 4
                pss, psp, pso, psd = (0, 1, 2, 3) if bt % 2 == 0 else (4, 5, 6, 7)
                firsts = [(B0 + j) % nb == 0 for j in range(4)]
                for j in range(4):
                    B = B0 + j
                    mm(kb, ps[pss][:, j * 128:(j + 1) * 128], ks[bi][:, B * 128:(B + 1) * 128], qs[bi][:, B * 128:(B + 1) * 128], True, True,
                       r=[ks_b[bi], qs_b[bi]], w=[pb[pss]])
                    Bp = B if firsts[j] else B - 1
                    mm(kb, ps[psp][:, j * 128:(j + 1) * 128], ks[bi][:, Bp * 128:(Bp + 1) * 128], qs[bi][:, B * 128:(B + 1) * 128], True, True,
                       r=[ks_b[bi], qs_b[bi]], w=[pb[psp]])
                p1 = pti % 4; p2 = (pti + 1) % 4; pti += 2
                kb.op("act", lambda e: e.activation(out=pT[p1][:], in_=ps[pss][:], func=AF.Exp, scale=0.125), r=[pb[pss]], w=[pT_b[p1]])
                kb.op("act", lambda e: e.activation(out=pT[p2][:], in_=ps[psp][:], func=AF.Exp, scale=0.125), r=[pb[psp]], w=[pT_b[p2]])
                kb.op("dve", lambda e: e.tensor_tensor(out=pT[p1][:], in0=pT[p1][:], in1=eb[bi][:, 0:512], op=ALU.mult), r=[pT_b[p1], eb_b[bi]], w=[pT_b[p1]])
                ebp = eb0[bi][:] if any(firsts) else eb[bi][:, 512:1024]
                kb.op("dve", lambda e: e.tensor_tensor(out=pT[p2][:], in0=pT[p2][:], in1=ebp, op=ALU.mult), r=[pT_b[p2], eb_b[bi], eb0_b[bi]], w=[pT_b[p2]])
                for j in range(4):
                    B = B0 + j
                    Bp = B if firsts[j] else B - 1
                    cs_ = slice(j * 128, (j + 1) * 128)
                    mm(kb, ps[pso][0:64, cs_], vs[bi][:, B * 64:(B + 1) * 64], pT[p1][:, cs_], True, False, r=[vs_b[bi], pT_b[p1]], w=[pb[pso]])
                    mm(kb, ps[pso][0:64, cs_], vs[bi][:, Bp * 64:(Bp + 1) * 64], pT[p2][:, cs_], False, True, r=[vs_b[bi], pT_b[p2]], w=[pb[pso]])
                    mm(kb, ps[psd][0:64, cs_], onesb[:], pT[p1][:, cs_], True, False, r=[onesb_b, pT_b[p1]], w=[pb[psd]])
                    mm(kb, ps[psd][0:64, cs_], onesb[:], pT[p2][:, cs_], False, True, r=[onesb_b, pT_b[p2]], w=[pb[psd]])
                if d == 16:
                    r0 = B0 // nb
                    nv = num[:].rearrange("c (m r) -> c r m", r=16)[:, r0:r0 + 2, :]
                    dv_ = den[:].rearrange("c (m r) -> c r m", r=16)[:, r0:r0 + 2, :]
                    po = ps[pso][0:64, :].rearrange("c (r m) -> c r m", r=2)
                    pd = ps[psd][0:64, :].rearrange("c (r m) -> c r m", r=2)
                elif d == 4:
                    r0 = B0 // nb; m0 = (B0 % nb) * 128
                    nv = num[:].rearrange("c (m r) -> c r m", r=4)[:, r0, m0:m0 + 512]
                    dv_ = den[:].rearrange("c (m r) -> c r m", r=4)[:, r0, m0:m0 + 512]
                    po = ps[pso][0:64, :]; pd = ps[psd][0:64, :]
                else:
                    nv = num[:, B0 * 128:B0 * 128 + 512]; dv_ = den[:, B0 * 128:B0 * 128 + 512]
                    po = ps[pso][0:64, :]; pd = ps[psd][0:64, :]
                if gi == 0:
                    kb.op("act", lambda e: e.copy(nv, po), r=[pb[pso]], w=[num_b])
                    kb.op("act", lambda e: e.copy(dv_, pd), r=[pb[psd]], w=[den_b])
                else:
                    kb.op("dve", lambda e: e.tensor_tensor(out=nv, in0=po, in1=nv, op=ALU.add), r=[pb[pso], num_b], w=[num_b])
                    kb.op("dve", lambda e: e.tensor_tensor(out=dv_, in0=pd, in1=dv_, op=ALU.add), r=[pb[psd], den_b], w=[den_b])
            u += 1
        kb.op("dve", lambda e: e.reciprocal(den[:], den[:]), r=[den_b], w=[den_b])
        kb.op("dve", lambda e: e.tensor_tensor(out=num[:], in0=num[:], in1=den[:], op=ALU.mult), r=[num_b, den_b], w=[num_b])
        kb.dma(attT[h * 64:(h + 1) * 64, :], num[:], r=[num_b], final=True)
    return kb.finish()


def t5_bucket_np(dist):
    exact = 16
    logd = np.log(np.maximum(dist, 1).astype(np.float32) / exact) / np.float32(np.log(2048 / exact))
    large = np.minimum(exact + (logd * (32 - exact)).astype(np.int32), 31)
    return np.where(dist < exact, dist, large)


def run_dil(z1, rel_bias):
    nc = build_dil()
    S = SEQ
    NB = S // 128
    kk = np.arange(128)[:, None]; qq = np.arange(128)[None, :]
    in_maps = []
    for jc in range(NCORES):
        b, hf = jc // 2, jc % 2
        q_l, k_l, v_l, bm_l = [], [], [], []
        for gi, d in enumerate(DILS):
            for h in range(4 * hf, 4 * hf + 4):
                def rows(qkv):
                    r0 = gi * 1536 + qkv * 512 + h * 64
                    t = z1[b, r0:r0 + 64]
                    return t.reshape(64, S // d, d).transpose(0, 2, 1).reshape(64, S)
                q_l.append(rows(0)); k_l.append(rows(1))
                v = rows(2)
                v_l.append(v.reshape(64, NB, 128).transpose(2, 1, 0).reshape(128, NB * 64))
                bias_h = rel_bias[:, gi * 8 + h]
                same = np.where(qq >= kk, bias_h[t5_bucket_np(np.clip(qq - kk, 0, 128) * d)], -30000.0)
                prev = np.where(qq <= kk, bias_h[t5_bucket_np(np.clip(128 + qq - kk, 0, 128) * d)], -30000.0)
                bm_l.append(np.concatenate([np.tile(same, (1, 4)), np.tile(prev, (1, 4))], 1))
        in_maps.append({"q": np.ascontiguousarray(np.stack(q_l)), "k": np.ascontiguousarray(np.stack(k_l)),
                        "v": np.ascontiguousarray(np.stack(v_l)), "bm": np.ascontiguousarray(np.stack(bm_l).astype(np.float32))})
    res = run_bass_kernel_spmd(nc, in_maps, core_ids=list(range(NCORES)))
    att = np.zeros((4, 512, S), np.float32)
    for jc in range(NCORES):
        b, hf = jc // 2, jc % 2
        att[b, hf * 256:(hf + 1) * 256] = res.results[jc]["attT"]
    return att


CL = 64
RW_GN_EPS = 64e-5
WDEC = -0.6065306597126334


RW_STOP = None
RW_OFFSET = 0
RW_NHL = 2


def build_rwkv(S=SEQ, nh=12):
    kb = KB()
    NBT = S // 512
    FW = nh * 64
    zr_d = kb.din("zr", [FW, S]); zk_d = kb.din("zk", [FW, S])
    vt_d = kb.din("v_tok", [S, FW]); vp_d = kb.din("vprev_tok", [S, FW])
    zwd_d = kb.din("zwd", [64, S]); zad_d = kb.din("zad", [64, S]); zgd_d = kb.din("zgd", [224, S])
    npair = nh // 2
    cols_d = kb.din("cols", [128, 8 * npair])
    mul_d = kb.din("mu_lora", [128, 4])
    w2_d = kb.din("w2", [64, FW]); a2_d = kb.din("a2", [64, FW]); g2_d = kb.din("g2", [224, FW])
    rows_d = kb.din("rows", [64, 3 * FW])
    cst_d = kb.din("cst", [128, 2048])
    rmask_d = kb.din("rmask", [128, 512])
    bones_d = kb.din("blockones", [128, 128])
    out_d = kb.dout("tm_tok", [S, FW])

    cst = kb.sb([128, 2048]); cst_b = Buf()
    bones = kb.sb([128, 128])
    rmask = kb.sb([128, 512]); rmask_b = Buf()
    cols = kb.sb([128, 8 * npair]); cols_b = Buf()
    mul = kb.sb([128, 4]); mul_b = Buf()
    w2 = kb.sb([64, FW]); a2 = kb.sb([64, FW]); lw_b = Buf()
    g2s = kb.sb([128, FW]); g2a = kb.sb([128, FW], BF16); g2b = kb.sb([96, FW], BF16)
    rows = kb.sb([64, 3 * FW]); rows_b = Buf()
    onescol = kb.sb([64, 1]); onescol_b = Buf()
    tw = kb.sb([64, S]); xad = kb.sb([64, S]); sg0 = kb.sb([128, S], BF16); sg1 = kb.sb([96, S], BF16); lora_b = Buf()
    P_b = Buf()
    big = kb.sb([128, 4097]); big2 = kb.sb([128, 4096])

    kb.dma(cst[:], cst_d[:, :], w=[cst_b])
    kb.dma(rmask[:], rmask_d[:, :], w=[rmask_b])
    kb.dma(bones[:], bones_d[:, :], w=[cst_b])
    kb.dma(cols[:], cols_d[:, :], w=[cols_b])
    kb.dma(mul[:], mul_d[:, :], w=[mul_b])
    kb.dma(w2[:], w2_d[:, :], w=[lw_b]); kb.dma(a2[:], a2_d[:, :], w=[lw_b])
    kb.dma(g2s[:], g2_d[0:128, :], w=[P_b])
    kb.op("dve", lambda e: e.tensor_copy(out=g2a[:], in_=g2s[:]), r=[P_b], w=[lw_b])
    kb.dma(g2s[0:96, :], g2_d[128:224, :], r=[lw_b], w=[P_b])
    kb.op("dve", lambda e: e.tensor_copy(out=g2b[:], in_=g2s[0:96, :]), r=[P_b], w=[lw_b])
    kb.dma(rows[:], rows_d[:, :], w=[rows_b])
    kb.op("dve", lambda e: e.memset(onescol[:], 1.0), w=[onescol_b])
    maskG2 = cst[0:64, 128:384]
    identb = kb.sb([128, 64], BF16); onescolb = kb.sb([128, 1], BF16); cb_b = Buf()
    kb.op("dve", lambda e: e.tensor_copy(out=identb[:], in_=cst[:, 64:128]), r=[cst_b], w=[cb_b])
    kb.op("dve", lambda e: e.memset(onescolb[:], 1.0), r=[cb_b], w=[cb_b])
    maskU8 = cst[0:64, 384:896]; maskL8 = cst[0:64, 896:1408]; I8 = cst[0:64, 1408:1920]

    def shifted(src_ap, P, mucol, dst, func):
        kb.op("dve", lambda e: e.memset(big[0:P, 0:1], 0.0), r=[P_b], w=[P_b])
        kb.dma(big[0:P, 1:S + 1], src_ap, w=[P_b])
        kb.op("dve", lambda e: e.tensor_tensor(out=big2[0:P, 0:S], in0=big[0:P, 0:S], in1=big[0:P, 1:S + 1], op=ALU.subtract), r=[P_b], w=[P_b])
        kb.op("dve", lambda e: e.scalar_tensor_tensor(out=big2[0:P, 0:S], in0=big2[0:P, 0:S], scalar=mucol, in1=big[0:P, 1:S + 1], op0=ALU.mult, op1=ALU.add),
              r=[P_b, mul_b], w=[P_b])
        if func is None:
            kb.op("act", lambda e: e.copy(dst, big2[0:P, 0:S]), r=[P_b], w=[lora_b])
        else:
            kb.op("act", lambda e: e.activation(out=dst, in_=big2[0:P, 0:S], func=func), r=[P_b], w=[lora_b])
    shifted(zwd_d[:, :], 64, mul[0:64, 0:1], tw[:], AF.Tanh)
    shifted(zad_d[:, :], 64, mul[0:64, 1:2], xad[:], None)
    shifted(zgd_d[0:128, :], 128, mul[:, 2:3], sg0[:], AF.Sigmoid)
    shifted(zgd_d[128:224, :], 96, mul[0:96, 3:4], sg1[:], AF.Sigmoid)

    class _V:
        def __init__(self, t, i):
            self.t, self.i = t, i

        def __getitem__(self, idx):
            return self.t[:, self.i * 512:(self.i + 1) * 512][idx]

    ps = [kb.ps([128, 512]) for _ in range(8)]; pb = bufs(8)

    R, Kx, lwt, av, kk, kkn, Kp, cum = [_V(big, i) for i in range(8)]
    Ep, Em, Epr, t1, t2 = [_V(big2, i) for i in range(5)]
    A_b = P_b
    zrt = kb.sb([128, 513]); zkt = kb.sb([128, 513]); zin_b = Buf()
    AR = kb.sb([128, 8, 128], BF16); BK = kb.sb([128, 8, 128], BF16); rkr = kb.sb([128, 512], BF16); ARBK_b = Buf()
    AR_p, BK_p, rkr_p, ARBK_p, A_p = AR, BK, rkr, ARBK_b, A_b

    def make_set(si):
        T = {}
        T["St_b"] = Buf(); T["Stw_b"] = Buf()
        T["Vx"] = kb.sb([64, 8, 64]); T["Vx_b"] = Buf(); T["Vxb"] = kb.sb([64, 8, 64], BF16); T["Tmb"] = kb.sb([64, 512], BF16)
        T["Pm"] = [kb.sb([64, 512], BF16) for _ in range(2)]; T["Qm"] = [kb.sb([64, 512], BF16) for _ in range(2)]; T["TTm"] = [kb.sb([64, 512], BF16) for _ in range(2)]
        T["Tm"] = kb.sb([64, 512]); T["C_b"] = Buf()
        T["Gm"] = kb.sb([64, 256], BF16); T["Gm_b"] = Buf()
        T["BKT"] = kb.sb([64, 2, 128], BF16); T["BKT_b"] = Buf()
        T["Xs"] = kb.sb([64, 64], BF16); T["Xs_b"] = Buf(); T["Us"] = kb.sb([64, 64], BF16); T["Us_b"] = Buf()
        T["ep1"] = kb.sb([64, 8, 64]); T["ep2"] = kb.sb([64, 8, 64]); T["ep_b"] = Buf()
        T["st8"] = [kb.sb([64, 8]) for _ in range(3)]
        T["bank"] = [4 * si + k for k in range(4)]
        kb.op("dve", lambda e: e.memset(T["BKT"][:].rearrange("p a b -> p (a b)"), 0.0), w=[T["BKT_b"]])
        T["St"] = kb.sb([64, 64]); T["Stw"] = kb.sb([64, 64]); T["Stb"] = kb.sb([64, 64], BF16)
        if si == 0:
            T["AR"], T["BK"], T["rkr"], T["Ep"], T["OP_b"], T["EP_b"] = AR, BK, rkr, Ep, ARBK_b, A_b
        else:
            T["AR"] = kb.sb([64, 8, 128], BF16); T["BK"] = kb.sb([64, 8, 128], BF16); T["rkr"] = kb.sb([64, 512], BF16)
            T["Ep"] = kb.sb([64, 512]); T["OP_b"] = Buf(); T["EP_b"] = T["OP_b"]
        return T

    def phase_a(c, bi):
        t0 = bi * 512
        fc = slice(c * 128, (c + 1) * 128)
        cc = lambda k: cols[:, k * npair + c:k * npair + c + 1]
        A_ = lambda eng, fn, extra_r=(): kb.op(eng, fn, r=[A_b, zin_b] + list(extra_r), w=[A_b])
        for (zt, zd) in [(zrt, zr_d), (zkt, zk_d)]:
            if bi == 0:
                kb.op("dve", lambda e: e.memset(zt[:, 0:1], 0.0), r=[zin_b, A_b], w=[zin_b])
                kb.dma(zt[:, 1:513], zd[fc, 0:512], w=[zin_b])
            else:
                kb.dma(zt[:], zd[fc, t0 - 1:t0 + 512], w=[zin_b])
        for (zt, dst, k) in [(zrt, R, 0), (zkt, Kx, 1)]:
            A_("dve", lambda e: e.tensor_tensor(out=t1[:], in0=zt[:, 0:512], in1=zt[:, 1:513], op=ALU.subtract))
            A_("dve", lambda e: e.scalar_tensor_tensor(out=dst[:], in0=t1[:], scalar=cc(k), in1=zt[:, 1:513], op0=ALU.mult, op1=ALU.add), [cols_b])
        mm(kb, ps[0][:], w2[:, fc], tw[:, t0:t0 + 512], True, True, r=[lw_b, lora_b], w=[pb[0]])
        A_("act", lambda e: e.activation(out=lwt[:], in_=ps[0][:], func=AF.Sigmoid, bias=cc(2)), [pb[0], cols_b])
        A_("act", lambda e: e.mul(lwt[:], lwt[:], WDEC))
        mm(kb, ps[0][:], a2[:, fc], xad[:, t0:t0 + 512], True, True, r=[lw_b, lora_b], w=[pb[0]])
        A_("act", lambda e: e.activation(out=av[:], in_=ps[0][:], func=AF.Sigmoid, bias=cc(3)), [pb[0], cols_b])
        A_("dve", lambda e: e.tensor_scalar(out=kk[:], in0=Kx[:], scalar1=cc(4), scalar2=None, op0=ALU.mult), [cols_b])
        A_("act", lambda e: e.activation(out=t1[:], in_=kk[:], func=AF.Square))
        mm(kb, ps[0][:], bones[:], t1[:], True, True, r=[cst_b, A_b], w=[pb[0]])
        A_("act", lambda e: e.sqrt(t2[:], ps[0][:]), [pb[0]])
        A_("dve", lambda e: e.tensor_scalar_max(t2[:], t2[:], 1e-12))
        A_("dve", lambda e: e.reciprocal(t2[:], t2[:]))
        A_("dve", lambda e: e.tensor_tensor(out=kkn[:], in0=kk[:], in1=t2[:], op=ALU.mult))
        A_("dve", lambda e: e.tensor_scalar(out=t1[:], in0=av[:], scalar1=-1.0, scalar2=None, op0=ALU.add))
        A_("dve", lambda e: e.tensor_scalar(out=t1[:], in0=t1[:], scalar1=cc(5), scalar2=None, op0=ALU.mult), [cols_b])
        A_("dve", lambda e: e.scalar_tensor_tensor(out=Kp[:], in0=t1[:], scalar=1.0, in1=Kx[:], op0=ALU.add, op1=ALU.mult))
        A_("dve", lambda e: e.tensor_tensor_scan(out=cum[:], data0=rmask[:], data1=lwt[:], initial=0.0, op0=ALU.mult, op1=ALU.add), [rmask_b])
        A_("act", lambda e: e.activation(out=Ep[:], in_=cum[:], func=AF.Exp))
        A_("act", lambda e: e.activation(out=Em[:], in_=cum[:], func=AF.Exp, scale=-1.0))
        A_("dve", lambda e: e.tensor_tensor(out=t1[:], in0=cum[:], in1=lwt[:], op=ALU.subtract))
        A_("act", lambda e: e.activation(out=Epr[:], in_=t1[:], func=AF.Exp))
        v3 = lambda t: t[:].rearrange("p (c t) -> p c t", t=64)
        AB = [A_b, ARBK_b]
        kb.op("dve", lambda e: e.scalar_tensor_tensor(out=AR[:, :, 0:64], in0=v3(kkn), scalar=-1.0, in1=v3(Epr), op0=ALU.mult, op1=ALU.mult), r=AB, w=[ARBK_b])
        kb.op("dve", lambda e: e.tensor_tensor(out=AR[:, :, 64:128], in0=v3(R), in1=v3(Ep), op=ALU.mult), r=AB, w=[ARBK_b])
        A_("dve", lambda e: e.tensor_tensor(out=t1[:], in0=kkn[:], in1=av[:], op=ALU.mult))
        kb.op("dve", lambda e: e.tensor_tensor(out=BK[:, :, 0:64], in0=v3(t1), in1=v3(Em), op=ALU.mult), r=AB, w=[ARBK_b])
        kb.op("dve", lambda e: e.tensor_tensor(out=BK[:, :, 64:128], in0=v3(Kp), in1=v3(Em), op=ALU.mult), r=AB, w=[ARBK_b])
        kb.op("dve", lambda e: e.scalar_tensor_tensor(out=rkr[:], in0=R[:], scalar=cc(6), in1=Kp[:], op0=ALU.mult, op1=ALU.mult), r=AB + [cols_b], w=[ARBK_b])

    def head_batch_gen(h, T, bi):
        hl = h % 2
        PH = slice(0, 64)
        AR, BK, rkr, ARBK_b, A_b = T["AR"], T["BK"], T["rkr"], T["OP_b"], T["EP_b"]
        St, Stw, Stb = T["St"], T["Stw"], T["Stb"]
        if hl == 1:
            kb.dma(AR[:], AR_p[64:128], r=[ARBK_p], w=[ARBK_b])
            kb.dma(BK[:], BK_p[64:128], r=[ARBK_p], w=[ARBK_b])
            kb.dma(rkr[:], rkr_p[64:128], r=[ARBK_p], w=[ARBK_b])
            kb.dma(T["Ep"][:], Ep[64:128, :], r=[A_p], w=[ARBK_b])
        St_b, Stw_b = T["St_b"], T["Stw_b"]
        Vx, Vx_b, Vxb, Tmb = T["Vx"], T["Vx_b"], T["Vxb"], T["Tmb"]
        Pm, Qm, TTm, Tm, C_b = T["Pm"], T["Qm"], T["TTm"], T["Tm"], T["C_b"]
        Gm, Gm_b, BKT, BKT_b, Xs, Xs_b, Us, Us_b = T["Gm"], T["Gm_b"], T["BKT"], T["BKT_b"], T["Xs"], T["Xs_b"], T["Us"], T["Us_b"]
        ep1, ep2, ep_b, st8 = T["ep1"], T["ep2"], T["ep_b"], T["st8"]
        b0, b1, b2, b3 = T["bank"]
        hc = slice(h * 64, (h + 1) * 64)
        t0 = bi * 512
        E = [ep_b]
        Ep3 = T["Ep"][:].rearrange("p (c t) -> p c t", t=64)
        kb.dma(ep1[:], vt_d[t0:t0 + 512, hc].rearrange("(c t) i -> t c i", t=64), w=E, q="pool")
        kb.dma(ep2[:], vp_d[t0:t0 + 512, hc].rearrange("(c t) i -> t c i", t=64), w=E, q="pool")
        muv = rows[:, hc].rearrange("p (o i) -> p o i", o=1).to_broadcast([64, 8, 64])
        kb.op("dve", lambda e: e.tensor_tensor(out=ep2[:], in0=ep2[:], in1=ep1[:], op=ALU.subtract), r=E, w=E)
        kb.op("dve", lambda e: e.tensor_tensor(out=ep2[:], in0=ep2[:], in1=muv, op=ALU.mult), r=E + [rows_b], w=E)
        kb.op("dve", lambda e: e.tensor_tensor(out=Vx[:], in0=ep2[:], in1=ep1[:], op=ALU.add), r=E, w=[Vx_b])
        kb.op("act", lambda e: e.copy(Vxb[:], Vx[:]), r=[Vx_b], w=[Vx_b])
        yield
        CB = [C_b]
        for ch in range(8):
            mm(kb, ps[b0][0:64, ch * 64:(ch + 1) * 64], BK[PH, ch, 0:64], AR[PH, ch, 0:64], True, True, r=[ARBK_b], w=[pb[b0]])
            mm(kb, ps[b1][0:64, ch * 64:(ch + 1) * 64], AR[PH, ch, 0:64], BK[PH, ch, 0:64], True, True, r=[ARBK_b], w=[pb[b1]])
        kb.op("dve", lambda e: e.tensor_tensor(out=Tm[:], in0=ps[b0][0:64, :], in1=maskU8, op=ALU.mult), r=[pb[b0], cst_b] + CB, w=CB)
        kb.op("act", lambda e: e.copy(Pm[0][:], Tm[:]), r=CB, w=CB)
        kb.op("dve", lambda e: e.tensor_tensor(out=Qm[0][:], in0=ps[b1][0:64, :], in1=maskL8, op=ALU.mult), r=[pb[b1], cst_b] + CB, w=CB)
        kb.op("dve", lambda e: e.tensor_tensor(out=Tm[:], in0=Tm[:], in1=I8, op=ALU.add), r=[cst_b] + CB, w=CB)
        kb.op("dve", lambda e: e.tensor_tensor(out=TTm[0][:], in0=Qm[0][:], in1=I8, op=ALU.add), r=[cst_b] + CB, w=CB)
        yield
        NL = 5
        for lv in range(NL):
            a_, b_ = lv % 2, (lv + 1) % 2
            last = lv == NL - 1
            for ch in range(8):
                sl = slice(ch * 64, (ch + 1) * 64)
                mm(kb, ps[b0][0:64, sl], Qm[a_][:, sl], Pm[a_][:, sl], True, True, r=CB, w=[pb[b0]])
                if not last:
                    mm(kb, ps[b1][0:64, sl], Pm[a_][:, sl], Qm[a_][:, sl], True, True, r=CB, w=[pb[b1]])
            kb.op("act", lambda e: e.copy(Pm[b_][:], ps[b0][0:64, :]), r=[pb[b0]] + CB, w=CB)
            if not last:
                kb.op("dve", lambda e: e.tensor_copy(out=Qm[b_][:], in_=ps[b1][0:64, :]), r=[pb[b1]] + CB, w=CB)
            yield
            for ch in range(8):
                sl = slice(ch * 64, (ch + 1) * 64)
                mm(kb, ps[b2][0:64, sl], TTm[a_][:, sl], Pm[b_][:, sl], True, True, r=CB, w=[pb[b2]])
                if not last:
                    mm(kb, ps[b3][0:64, sl], Pm[b_][:, sl], TTm[a_][:, sl], True, True, r=CB, w=[pb[b3]])
            kb.op("dve", lambda e: e.tensor_tensor(out=Tm[:], in0=ps[b2][0:64, :], in1=Tm[:], op=ALU.add), r=[pb[b2]] + CB, w=CB)
            if not last:
                kb.op("dve", lambda e: e.tensor_tensor(out=TTm[b_][:], in0=ps[b3][0:64, :], in1=TTm[a_][:], op=ALU.add), r=[pb[b3]] + CB, w=CB)
            yield
        kb.op("act", lambda e: e.copy(Tmb[:], Tm[:]), r=CB, w=CB)
        for ch in range(8):
            mm(kb, ps[b0][0:64, 0:128], BK[PH, ch, 0:64], AR[PH, ch, :], True, True, r=[ARBK_b], w=[pb[b0]])
            mm(kb, ps[b0][0:64, 128:256], BK[PH, ch, 64:128], AR[PH, ch, :], True, True, r=[ARBK_b], w=[pb[b0]])
            kb.op("dve", lambda e: e.tensor_tensor(out=Gm[:], in0=ps[b0][0:64, 0:256], in1=maskG2, op=ALU.mult), r=[pb[b0], cst_b], w=[Gm_b])
            mm(kb, ps[b0][0:64, 256:320], BK[PH, ch, 0:64], identb[PH, :], True, True, r=[ARBK_b, cb_b], w=[pb[b0]])
            mm(kb, ps[b0][0:64, 320:384], BK[PH, ch, 64:128], identb[PH, :], True, True, r=[ARBK_b, cb_b], w=[pb[b0]])
            kb.op("act", lambda e: e.copy(BKT[:, :, PH], ps[b0][0:64, 256:384].rearrange("p (a j) -> p a j", a=2)), r=[pb[b0]], w=[BKT_b])
            mm(kb, ps[b1][0:64, 0:64], AR[PH, ch, 0:64], Stb[PH, :], True, False, r=[ARBK_b, St_b], w=[pb[b1]])
            mm(kb, ps[b1][0:64, 0:64], Gm[:, 128:192], Vxb[:, ch, :], False, True, r=[Gm_b, Vx_b], w=[pb[b1]])
            kb.op("act", lambda e: e.copy(Xs[:], ps[b1][0:64, 0:64]), r=[pb[b1]], w=[Xs_b])
            yield
            mm(kb, ps[b1][0:64, 64:128], Tmb[:, ch * 64:(ch + 1) * 64], Xs[:], True, True, r=CB + [Xs_b], w=[pb[b1]])
            kb.op("act", lambda e: e.copy(Us[:], ps[b1][0:64, 64:128]), r=[pb[b1]], w=[Us_b])
            yield
            ysl = slice(ch * 64, (ch + 1) * 64)
            mm(kb, ps[b2][0:64, ysl], AR[PH, ch, 64:128], Stb[PH, :], True, False, r=[ARBK_b, St_b], w=[pb[b2]])
            mm(kb, ps[b2][0:64, ysl], Gm[:, 64:128], Us[:], False, False, r=[Gm_b, Us_b], w=[pb[b2]])
            mm(kb, ps[b2][0:64, ysl], Gm[:, 192:256], Vxb[:, ch, :], False, True, r=[Gm_b, Vx_b], w=[pb[b2]])
            kb.op("dve", lambda e: e.tensor_scalar(out=Stw[PH, :], in0=St[PH, :], scalar1=Ep3[PH, ch, 63:64], scalar2=None, op0=ALU.mult), r=[St_b, A_b], w=[Stw_b])
            mm(kb, ps[b1][:, 128:192], BKT[:, 0, :], Us[:], True, False, r=[BKT_b, Us_b], w=[pb[b1]])
            mm(kb, ps[b1][:, 128:192], BKT[:, 1, :], Vxb[:, ch, :], False, True, r=[BKT_b, Vx_b], w=[pb[b1]])
            kb.op("dve", lambda e: e.scalar_tensor_tensor(out=St[PH, :], in0=ps[b1][PH, 128:192], scalar=Ep3[PH, ch, 63:64], in1=Stw[PH, :], op0=ALU.mult, op1=ALU.add),
                  r=[pb[b1], A_b, Stw_b], w=[St_b])
            kb.op("act", lambda e: e.copy(Stb[PH, :], St[PH, :]), r=[St_b], w=[St_b])
            mm(kb, ps[b1][0:64, 192 + ch:193 + ch], rkr[PH, ch * 64:(ch + 1) * 64], onescolb[PH, :], True, True, r=[ARBK_b, cb_b], w=[pb[b1]])
            mm(kb, ps[b3][0:64, ysl], sg0[:, t0 + ch * 64:t0 + (ch + 1) * 64], g2a[:, hc], True, False, r=[lora_b, lw_b], w=[pb[b3]])
            mm(kb, ps[b3][0:64, ysl], sg1[:, t0 + ch * 64:t0 + (ch + 1) * 64], g2b[:, hc], False, True, r=[lora_b, lw_b], w=[pb[b3]])
            yield
        Y3 = ps[b2][0:64, :].rearrange("p (c i) -> p c i", i=64)
        bc8 = lambda t: t[:, :].rearrange("p (c o) -> p c o", o=1).to_broadcast([64, 8, 64])
        rowb = lambda k: rows[:, k * FW + h * 64:k * FW + (h + 1) * 64].rearrange("p (o i) -> p o i", o=1).to_broadcast([64, 8, 64])
        kb.op("dve", lambda e: e.tensor_reduce(out=st8[0][:], in_=Y3, axis=AX.X, op=ALU.add), r=[pb[b2]] + E, w=E)
        kb.op("dve", lambda e: e.tensor_single_scalar(out=st8[0][:], in_=st8[0][:], scalar=1.0 / 64, op=ALU.mult), r=E, w=E)
        kb.op("dve", lambda e: e.tensor_tensor(out=ep1[:], in0=Y3, in1=bc8(st8[0]), op=ALU.subtract), r=[pb[b2]] + E, w=E)
        kb.op("dve", lambda e: e.tensor_tensor(out=ep2[:], in0=ep1[:], in1=ep1[:], op=ALU.mult), r=E, w=E)
        kb.op("dve", lambda e: e.tensor_reduce(out=st8[1][:], in_=ep2[:], axis=AX.X, op=ALU.add), r=E, w=E)
        kb.op("dve", lambda e: e.tensor_scalar(out=st8[1][:], in0=st8[1][:], scalar1=1.0 / 64, scalar2=RW_GN_EPS, op0=ALU.mult, op1=ALU.add), r=E, w=E)
        kb.op("act", lambda e: e.sqrt(st8[1][:], st8[1][:]), r=E, w=E)
        kb.op("dve", lambda e: e.reciprocal(st8[1][:], st8[1][:]), r=E, w=E)
        yield
        kb.op("dve", lambda e: e.tensor_tensor(out=ep1[:], in0=ep1[:], in1=bc8(st8[1]), op=ALU.mult), r=E, w=E)
        kb.op("dve", lambda e: e.tensor_tensor(out=ep1[:], in0=ep1[:], in1=rowb(1), op=ALU.mult), r=E + [rows_b], w=E)
        kb.op("dve", lambda e: e.tensor_tensor(out=ep1[:], in0=ep1[:], in1=rowb(2), op=ALU.add), r=E + [rows_b], w=E)
        kb.op("act", lambda e: e.copy(st8[2][:], ps[b1][0:64, 192:200]), r=[pb[b1]] + E, w=E)
        kb.op("dve", lambda e: e.tensor_tensor(out=ep2[:], in0=Vx[:], in1=bc8(st8[2]), op=ALU.mult), r=E + [Vx_b], w=E)
        kb.op("dve", lambda e: e.tensor_tensor(out=ep1[:], in0=ep1[:], in1=ep2[:], op=ALU.add), r=E, w=E)
        kb.op("dve", lambda e: e.tensor_tensor(out=ep2[:], in0=ep1[:], in1=ps[b3][0:64, :].rearrange("p (c i) -> p c i", i=64), op=ALU.mult), r=E + [pb[b3]], w=E)
        kb.dma(out_d[t0:t0 + 512, hc].rearrange("(c t) i -> t c i", t=64), ep2[:], r=E, final=True)
        yield

    sets = [make_set(0), make_set(1)]
    for c in range(npair):
        for hl in range(2):
            kb.op("dve", lambda e: e.memset(sets[hl]["St"][:], 0.0), r=[sets[hl]["St_b"]], w=[sets[hl]["St_b"]])
            kb.op("dve", lambda e: e.memset(sets[hl]["Stb"][:], 0.0), r=[sets[hl]["St_b"]], w=[sets[hl]["St_b"]])
        for bi in range(NBT):
            phase_a(c, bi)
            if RW_STOP == 'A':
                return kb.finish()
            alive = [head_batch_gen(2 * c + hl, sets[hl], bi) for hl in range(RW_NHL)]
            while alive:
                for g in list(alive):
                    try:
                        next(g)
                    except StopIteration:
                        alive.remove(g)
    return kb.finish()


def run_rwkv(z1, rw, S=SEQ, nh=12, ncores=NCORES):
    nc = build_rwkv(S, nh)
    FW = nh * 64
    base = 4608
    mu = rw["mu"]
    s_ = np.arange(64)[:, None]; q_ = np.arange(128)[None, :]
    maskG = np.where(q_ < 64, s_ < q_, s_ <= (q_ - 64)).astype(np.float32)
    r64 = np.arange(64)[:, None]; c64 = np.arange(64)[None, :]
    U8 = np.tile((r64 < c64).astype(np.float32), (1, 8)); L8 = np.tile((r64 > c64).astype(np.float32), (1, 8)); I8 = np.tile(np.eye(64, dtype=np.float32), (1, 8))
    cst = np.zeros((128, 2048), np.float32)
    cst[:, 64:128] = np.concatenate([np.eye(64), np.eye(64)], 0); cst[0:64, 128:256] = maskG; cst[0:64, 256:384] = maskG
    cst[0:64, 384:896] = U8; cst[0:64, 896:1408] = L8; cst[0:64, 1408:1920] = I8
    rmask = np.ones((128, 512), np.float32); rmask[:, ::64] = 0
    p64 = np.arange(128) // 64
    blockones = (p64[:, None] == p64[None, :]).astype(np.float32)
    in_maps = []
    for jc in range(ncores):
        b, hf = jc // 2, jc % 2
        fs = slice(hf * 768, hf * 768 + FW)
        npair = nh // 2
        def colv(v):
            return v[fs].reshape(npair, 128).T
        cols = np.zeros((128, 8 * npair), np.float32)
        for k, v in enumerate([mu[0:1536], mu[1536:3072], rw["w0"], rw["a0"], rw["k_k"], rw["k_a"], rw["r_k"].reshape(-1)]):
            cols[:, k * npair:(k + 1) * npair] = colv(v)
        mul = np.zeros((128, 4), np.float32)
        mul[0:64, 0] = mu[4608:4672]; mul[0:64, 1] = mu[4672:4736]; mul[:, 2] = mu[4736:4864]; mul[0:96, 3] = mu[4864:4960]
        rows = np.concatenate([np.broadcast_to(v[fs][None, :], (64, FW)) for v in [mu[3072:4608], rw["lnx_g"], rw["lnx_b"]]], 1)
        v_tok = np.ascontiguousarray(z1[b, base + 3072 + hf * 768: base + 3072 + hf * 768 + FW, :S].T)
        vprev = np.concatenate([np.zeros((1, FW), np.float32), v_tok[:-1]], 0)
        in_maps.append({
            "zr": np.ascontiguousarray(z1[b, base + hf * 768: base + hf * 768 + FW, :S]),
            "zk": np.ascontiguousarray(z1[b, base + 1536 + hf * 768: base + 1536 + hf * 768 + FW, :S]),
            "v_tok": v_tok, "vprev_tok": np.ascontiguousarray(vprev),
            "zwd": np.ascontiguousarray(z1[b, base + 4608:base + 4672, :S]), "zad": np.ascontiguousarray(z1[b, base + 4672:base + 4736, :S]),
            "zgd": np.ascontiguousarray(z1[b, base + 4736:base + 4960, :S]),
            "cols": cols, "mu_lora": mul, "w2": np.ascontiguousarray(rw["w2"][:, fs]), "a2": np.ascontiguousarray(rw["a2"][:, fs]),
            "g2": np.ascontiguousarray(rw["g2"][:, fs]), "rows": np.ascontiguousarray(rows), "cst": cst, "rmask": rmask, "blockones": blockones})
    res = run_bass_kernel_spmd(nc, in_maps, core_ids=list(range(ncores)))
    tm = np.zeros((4, 1536, S), np.float32)
    for jc in range(ncores):
        b, hf = jc // 2, jc % 2
        tm[b, hf * 768:hf * 768 + FW] = res.results[jc]["tm_tok"].T
    return tm


def run_post(layer0, mixT, x_tok, mod_l, wout, wglu, ln, router_w, router_b, wgu, wd):
    nc = build_post(layer0)
    sel = np.zeros((16, 16, 128), np.float32)
    for e in range(16):
        sel[e, e, :] = 1.0
    sel = sel.reshape(16, 2048)
    ident = np.eye(128, dtype=np.float32)
    rb_bc = np.ascontiguousarray(np.broadcast_to(router_b[None, :], (128, 16)))
    in_maps = []
    for j in range(NCORES):
        b, hf = j // 2, j % 2
        ts = slice(hf * 2048, (hf + 1) * 2048)
        m = mod_l[b]
        vecs = [m[2 * D:3 * D], m[4 * D:5 * D], m[3 * D:4 * D], m[5 * D:6 * D], ln[0], ln[1], ln[2], ln[3]]
        pvec = np.ascontiguousarray(np.concatenate([fm16(v) for v in vecs], 1))
        im = {"mixT": np.ascontiguousarray(mixT[b][:, ts]), "xT": np.ascontiguousarray(x_tok[b, ts].T), "wout": wout, "pvec": pvec,
              "router_w": router_w, "router_b_bc": rb_bc, "wgu": wgu, "wd": wd, "ident": ident, "sel": sel}
        if layer0:
            im["wglu"] = wglu
        in_maps.append(im)
    res = run_bass_kernel_spmd(nc, in_maps, core_ids=list(range(NCORES)))
    out = np.zeros((4, SEQ, D), np.float32)
    for j in range(NCORES):
        b, hf = j // 2, j % 2
        out[b, hf * 2048:(hf + 1) * 2048] = res.results[j]["xoT"].T
    return out


def kernel(x, c, ada_w, ada_b, ln_mix_g, ln_mix_b, ln_ffn_g, ln_ffn_b, router_w, router_b,
           moe_w_gate_up, moe_w_down, rel_bias, ev_w_in, mla_q_norm, mla_w_uq, mla_kv_norm, mla_w_ukv,
           s5_lambda_re, s5_lambda_im, s5_b_re, s5_b_im, s5_c_re, s5_c_im, s5_d, s5_log_dt, s5_w_glu,
           ev_w_out, od_w_in, rw_mu, rw_w0, rw_w2, rw_a0, rw_a2, rw_g2, rw_k_k, rw_k_a, rw_r_k,
           rw_lnx_g, rw_lnx_b, od_w_out):
    f = lambda a: np.ascontiguousarray(np.asarray(a, dtype=np.float32))
    x = f(x)
    mod = run_ada(f(c), f(ada_w), f(ada_b))
    w = f(ev_w_in[0])
    wext = np.ascontiguousarray(np.concatenate([w, w[:, 800:832], w[:, 768:800]], 1))
    z0 = run_pre(x, mod[0], wext, 1, 0)
    att0 = run_mla(z0, f(mla_w_uq[0]), f(mla_w_ukv[0]), f(mla_q_norm[0]), f(mla_kv_norm[0]))
    ys5 = run_s5(z0, f(s5_lambda_re[0]), f(s5_lambda_im[0]), f(s5_b_re[0]), f(s5_b_im[0]), f(s5_c_re[0]), f(s5_c_im[0]), f(s5_d[0]), f(s5_log_dt[0]))
    del z0
    mix0 = np.concatenate([att0, ys5], 1)
    x1 = run_post(True, mix0, x, mod[0], f(ev_w_out[0]), f(s5_w_glu[0]), [f(ln_mix_g[0]), f(ln_mix_b[0]), f(ln_ffn_g[0]), f(ln_ffn_b[0])],
                  f(router_w), f(router_b), f(moe_w_gate_up[0]), f(moe_w_down[0]))
    del mix0, att0, ys5
    w = f(od_w_in[0])
    wext = np.ascontiguousarray(np.concatenate([w, np.zeros((D, 9600 - w.shape[1]), np.float32)], 1))
    z1 = run_pre(x1, mod[1], wext, 1, 0)
    att1 = run_dil(z1, f(rel_bias))
    rw = {"mu": f(rw_mu[0]), "w0": f(rw_w0[0]), "w2": f(rw_w2[0]), "a0": f(rw_a0[0]), "a2": f(rw_a2[0]), "g2": f(rw_g2[0]),
          "k_k": f(rw_k_k[0]), "k_a": f(rw_k_a[0]), "r_k": f(rw_r_k[0]), "lnx_g": f(rw_lnx_g[0]), "lnx_b": f(rw_lnx_b[0])}
    tm = run_rwkv(z1, rw)
    del z1
    mix1 = np.concatenate([att1, tm], 1)
    x2 = run_post(False, mix1, x1, mod[1], f(od_w_out[0]), None, [f(ln_mix_g[1]), f(ln_mix_b[1]), f(ln_ffn_g[1]), f(ln_ffn_b[1])],
                  f(router_w), f(router_b), f(moe_w_gate_up[1]), f(moe_w_down[1]))
    return x2.astype(np.float32)
```
